# Optimizing a Trainium2 kernel written in Bass

```python
import math
import jax, jax.numpy as jnp
from jax import lax
import numpy as np

D_MODEL = 2048
BATCH = 4
SEQ = 4096
DEPTH = 4

GRID_W = 64
CTX_LEN = 256
EPS = 1e-6
N_MIXERS = 2
N_S5 = (DEPTH + 1) // 2
N_ML = DEPTH // 2

S5_GROUP = 16
S5_GROUPS = D_MODEL // S5_GROUP
S5_STATE = 64
S5_DT_MIN = 1e-3
S5_DT_MAX = 1e-1

ML_HEADS = 8
ML_DV = D_MODEL // ML_HEADS
ML_DQK = ML_DV // 2
ML_CHUNK = 64
GATE_CAP = 15.0
ML_QK_W = ML_HEADS * ML_DQK
ML_V_W = ML_HEADS * ML_DV
ML_IN = 2 * ML_QK_W + 2 * ML_V_W + 4 * ML_HEADS
ML_SPLITS = (ML_QK_W, 2 * ML_QK_W, 2 * ML_QK_W + ML_V_W, 2 * ML_QK_W + 2 * ML_V_W)

FFN_HIDDEN = -(-8 * D_MODEL // (3 * 256)) * 256

kernel_name = "hybrid_s5_mlstm_dit_trunk"


def rmsnorm(x, g):
    xf = x.astype(jnp.float32)
    y = xf * lax.rsqrt(jnp.mean(xf * xf, axis=-1, keepdims=True) + EPS)
    return (y * g.astype(jnp.float32)).astype(x.dtype)


def modulate(x, shift, scale):
    return x * (1 + scale) + shift


def swiglu(u, w_in, w_out):
    gate, up = jnp.split(u @ w_in, 2, axis=-1)
    return (jax.nn.silu(gate) * up) @ w_out


def s5_discretize(lam_re, lam_im, log_dt, b_re, b_im):
    lam_re = lam_re.astype(jnp.float32)
    lam_im = lam_im.astype(jnp.float32)
    dt = jnp.exp(log_dt.astype(jnp.float32))[:, None]
    mag = jnp.exp(lam_re * dt)
    a_re = mag * jnp.cos(lam_im * dt)
    a_im = mag * jnp.sin(lam_im * dt)
    den = lam_re * lam_re + lam_im * lam_im
    f_re = ((a_re - 1.0) * lam_re + a_im * lam_im) / den
    f_im = (a_im * lam_re - (a_re - 1.0) * lam_im) / den
    b_re = b_re.astype(jnp.float32)
    b_im = b_im.astype(jnp.float32)
    bb_re = f_re[..., None] * b_re - f_im[..., None] * b_im
    bb_im = f_re[..., None] * b_im + f_im[..., None] * b_re
    return a_re, a_im, bb_re, bb_im


def _complex_affine_combine(e1, e2):
    a1r, a1i, b1r, b1i = e1
    a2r, a2i, b2r, b2i = e2
    return (a2r * a1r - a2i * a1i,
            a2r * a1i + a2i * a1r,
            a2r * b1r - a2i * b1i + b2r,
            a2r * b1i + a2i * b1r + b2i)


def s5_scan(a_re, a_im, bu_re, bu_im, h0, reverse):
    seq = bu_re.shape[0]
    if h0 is not None:
        h0_re, h0_im = h0
        first = seq - 1 if reverse else 0
        bu_re = bu_re.at[first].add(a_re * h0_re - a_im * h0_im)
        bu_im = bu_im.at[first].add(a_re * h0_im + a_im * h0_re)
    ar = jnp.broadcast_to(a_re, (seq, 1) + a_re.shape)
    ai = jnp.broadcast_to(a_im, (seq, 1) + a_im.shape)
    _, _, h_re, h_im = lax.associative_scan(_complex_affine_combine, (ar, ai, bu_re, bu_im),
                                            reverse=reverse, axis=0)
    return h_re, h_im


def s5_readout(h_re, h_im, c_re, c_im):
    return (jnp.einsum('lbgp,gcp->blgc', h_re, c_re) - jnp.einsum('lbgp,gcp->blgc', h_im, c_im))


def s5_mixer(u_lat, u_ctx, lam_re, lam_im, log_dt, b_re, b_im, c_re, c_im, d_skip, w_glu, with_ctx_out):
    bsz, seq, dm = u_lat.shape
    ctx_len = u_ctx.shape[1]
    ul = u_lat.astype(jnp.float32)
    uc = u_ctx.astype(jnp.float32)
    ul_g = ul.reshape(bsz, seq, S5_GROUPS, S5_GROUP)
    uc_g = uc.reshape(bsz, ctx_len, S5_GROUPS, S5_GROUP)
    dsk = d_skip.astype(jnp.float32)
    y_lat = ul * dsk
    y_ctx = uc * dsk if with_ctx_out else None
    for d in range(2):
        rev = d == 1
        a_re, a_im, bb_re, bb_im = s5_discretize(lam_re[d], lam_im[d], log_dt[d], b_re[d], b_im[d])
        cr = c_re[d].astype(jnp.float32)
        ci = c_im[d].astype(jnp.float32)
        hc_re, hc_im = s5_scan(a_re, a_im,
                               jnp.einsum('blgc,gpc->lbgp', uc_g, bb_re),
                               jnp.einsum('blgc,gpc->lbgp', uc_g, bb_im), None, rev)
        end = 0 if rev else -1
        h0 = (hc_re[end], hc_im[end])
        hl_re, hl_im = s5_scan(a_re, a_im,
                               jnp.einsum('blgc,gpc->lbgp', ul_g, bb_re),
                               jnp.einsum('blgc,gpc->lbgp', ul_g, bb_im), h0, rev)
        y_lat = y_lat + s5_readout(hl_re, hl_im, cr, ci).reshape(bsz, seq, dm)
        if with_ctx_out:
            y_ctx = y_ctx + s5_readout(hc_re, hc_im, cr, ci).reshape(bsz, ctx_len, dm)

    def glu(y):
        a, b = jnp.split(jax.nn.gelu(y.astype(u_lat.dtype)) @ w_glu, 2, axis=-1)
        return a * jax.nn.sigmoid(b)

    if with_ctx_out:
        out = glu(jnp.concatenate([y_ctx, y_lat], axis=1))
        return out[:, ctx_len:], out[:, :ctx_len]
    return glu(y_lat), None


def mlstm_chunk_scan(q, k, v, log_i, log_f, state):
    bsz, nh, seq, dk = q.shape
    dv = v.shape[-1]
    nc = seq // ML_CHUNK

    def chunks(t):
        return jnp.moveaxis(t.reshape((bsz, nh, nc, ML_CHUNK) + t.shape[3:]), 2, 0)

    lower = jnp.tril(jnp.ones((ML_CHUNK, ML_CHUNK), dtype=bool))

    def step(carry, xs):
        c_mat, n_vec, m = carry
        qb, kb, vb, li, lf = xs
        b = jnp.cumsum(lf, axis=-1)
        dmat = jnp.where(lower, b[..., :, None] - b[..., None, :] + li[..., None, :], -jnp.inf)
        inter = b + m[..., None]
        m_t = jnp.maximum(inter, jnp.max(dmat, axis=-1))
        w_intra = jnp.exp(dmat - m_t[..., None])
        w_inter = jnp.exp(inter - m_t)
        s = jnp.einsum('bhtd,bhsd->bhts', qb, kb) * w_intra
        num = (w_inter[..., None] * jnp.einsum('bhtd,bhde->bhte', qb, c_mat)
               + jnp.einsum('bhts,bhse->bhte', s, vb))
        den = w_inter * jnp.einsum('bhtd,bhd->bht', qb, n_vec) + jnp.sum(s, axis=-1)
        h = num / jnp.maximum(jnp.abs(den), jnp.exp(-m_t))[..., None]
        b_last = b[..., -1]
        g = b_last[..., None] - b + li
        m_new = jnp.maximum(b_last + m, jnp.max(g, axis=-1))
        wg = jnp.exp(g - m_new[..., None])
        decay = jnp.exp(b_last + m - m_new)
        c_new = decay[..., None, None] * c_mat + jnp.einsum('bhs,bhsd,bhse->bhde', wg, kb, vb)
        n_new = decay[..., None] * n_vec + jnp.einsum('bhs,bhsd->bhd', wg, kb)
        return (c_new, n_new, m_new), h

    state, h = lax.scan(step, state, tuple(chunks(t) for t in (q, k, v, log_i, log_f)))
    h = jnp.moveaxis(h, 0, 2).reshape(bsz, nh, seq, dv)
    return h, state


def mlstm_mixer(u_lat, u_ctx, w_in, b_gates, norm_w, w_out, with_ctx_out):
    bsz, seq, dm = u_lat.shape
    ctx_len = u_ctx.shape[1]
    rows = seq // GRID_W
    u_lat_cm = u_lat.reshape(bsz, rows, GRID_W, dm).transpose(0, 2, 1, 3).reshape(bsz, seq, dm)
    z = jnp.concatenate([u_ctx, u_lat_cm], axis=1) @ w_in
    q, k, v, o, g = jnp.split(z, ML_SPLITS, axis=-1)

    def heads(t, dh):
        return t.reshape(bsz, -1, ML_HEADS, dh).transpose(0, 2, 1, 3).astype(jnp.float32)

    q = heads(q, ML_DQK) * (ML_DQK ** -0.5)
    k = heads(k, ML_DQK)
    v = heads(v, ML_DV)
    g = g.astype(jnp.float32) + b_gates.astype(jnp.float32)
    g = GATE_CAP * jnp.tanh(g / GATE_CAP)
    g = g.reshape(bsz, -1, 4, ML_HEADS).transpose(2, 0, 3, 1)
    zero_state = (jnp.zeros((bsz, ML_HEADS, ML_DQK, ML_DV), jnp.float32),
                  jnp.zeros((bsz, ML_HEADS, ML_DQK), jnp.float32),
                  jnp.zeros((bsz, ML_HEADS), jnp.float32))
    h_lat = []
    h_ctx = []
    for d in range(2):
        flip = d == 1
        log_i = g[2 * d]
        log_f = jax.nn.log_sigmoid(g[2 * d + 1])
        seqs = (q, k, v, log_i, log_f)
        args_c = [t[:, :, :ctx_len] for t in seqs]
        args_l = [t[:, :, ctx_len:] for t in seqs]
        if flip:
            args_c = [jnp.flip(t, 2) for t in args_c]
            args_l = [jnp.flip(t, 2) for t in args_l]
        hc, st = mlstm_chunk_scan(*args_c, zero_state)
        hl, _ = mlstm_chunk_scan(*args_l, st)
        h_lat.append(jnp.flip(hl, 2) if flip else hl)
        if with_ctx_out:
            h_ctx.append(jnp.flip(hc, 2) if flip else hc)

    def readout(h, o_part):
        hn = h * lax.rsqrt(jnp.mean(h * h, axis=-1, keepdims=True) + EPS)
        hn = hn.transpose(0, 2, 1, 3).reshape(bsz, -1, dm) * norm_w.astype(jnp.float32)
        return (hn * jax.nn.sigmoid(o_part.astype(jnp.float32))).astype(u_lat.dtype) @ w_out

    y_lat = readout(h_lat[0] + h_lat[1], o[:, ctx_len:])
    y_lat = y_lat.reshape(bsz, GRID_W, rows, dm).transpose(0, 2, 1, 3).reshape(bsz, seq, dm)
    y_ctx = readout(h_ctx[0] + h_ctx[1], o[:, :ctx_len]) if with_ctx_out else None
    return y_lat, y_ctx


def setup_inputs(seed: int = 0) -> dict:
    key = jax.random.key(seed)
    ks = jax.random.split(key, 24)
    f32 = jnp.float32
    nrm = lambda k, shape, s: (jax.random.normal(k, shape, f32) * s)
    G, P, H = S5_GROUPS, S5_STATE, ML_HEADS
    lam_im_base = jnp.pi * jnp.arange(P, dtype=f32)
    f_bias = jnp.linspace(3.0, 6.0, H, dtype=f32)
    z_h = jnp.zeros((H,), f32)
    gate_base = jnp.concatenate([z_h, f_bias, z_h, f_bias])
    return {
        "x": nrm(ks[0], (BATCH, SEQ, D_MODEL), 1.0),
        "c": nrm(ks[1], (BATCH, D_MODEL), 1.0),
        "ctx": nrm(ks[2], (BATCH, CTX_LEN, D_MODEL), 1.0),
        "c_ctx": nrm(ks[3], (D_MODEL,), 1.0),
        "ada_w": nrm(ks[4], (DEPTH, D_MODEL, 6 * D_MODEL), 0.5 * D_MODEL ** -0.5),
        "ada_b": nrm(ks[5], (DEPTH, 6 * D_MODEL), 0.01),
        "norm1": 1.0 + nrm(ks[6], (DEPTH, D_MODEL), 0.02),
        "norm2": 1.0 + nrm(ks[7], (DEPTH, D_MODEL), 0.02),
        "norm_f": 1.0 + nrm(ks[8], (D_MODEL,), 0.02),
        "s5_lam_re": -0.5 + nrm(ks[9], (N_S5, 2, G, P), 0.01),
        "s5_lam_im": lam_im_base + nrm(ks[10], (N_S5, 2, G, P), 0.01),
        "s5_log_dt": jax.random.uniform(ks[11], (N_S5, 2, G), f32,
                                        minval=math.log(S5_DT_MIN), maxval=math.log(S5_DT_MAX)),
        "s5_b_re": nrm(ks[12], (N_S5, 2, G, P, S5_GROUP), (2 * S5_GROUP) ** -0.5),
        "s5_b_im": nrm(ks[13], (N_S5, 2, G, P, S5_GROUP), (2 * S5_GROUP) ** -0.5),
        "s5_c_re": nrm(ks[14], (N_S5, 2, G, S5_GROUP, P), (2 * P) ** -0.5),
        "s5_c_im": nrm(ks[15], (N_S5, 2, G, S5_GROUP, P), (2 * P) ** -0.5),
        "s5_d": nrm(ks[16], (N_S5, D_MODEL), 1.0),
        "s5_w_glu": nrm(ks[17], (N_S5, D_MODEL, 2 * D_MODEL), D_MODEL ** -0.5),
        "ml_w_in": nrm(ks[18], (N_ML, D_MODEL, ML_IN), D_MODEL ** -0.5),
        "ml_b_gates": gate_base + nrm(ks[19], (N_ML, 4 * H), 0.01),
        "ml_norm": 1.0 + nrm(ks[20], (N_ML, D_MODEL), 0.02),
        "ml_w_out": nrm(ks[21], (N_ML, D_MODEL, D_MODEL), D_MODEL ** -0.5),
        "ffn_w_in": nrm(ks[22], (DEPTH, D_MODEL, 2 * FFN_HIDDEN), D_MODEL ** -0.5),
        "ffn_w_out": nrm(ks[23], (DEPTH, FFN_HIDDEN, D_MODEL), FFN_HIDDEN ** -0.5),
    }


def reference(x, c, ctx, c_ctx, ada_w, ada_b, norm1, norm2, norm_f,
              s5_lam_re, s5_lam_im, s5_log_dt, s5_b_re, s5_b_im, s5_c_re, s5_c_im, s5_d, s5_w_glu,
              ml_w_in, ml_b_gates, ml_norm, ml_w_out, ffn_w_in, ffn_w_out):
    ctx_len = ctx.shape[1]
    cond_lat = jax.nn.silu(c)
    cond_ctx = jax.nn.silu(c_ctx)
    for i in range(DEPTH):
        last = i == DEPTH - 1
        j = i // N_MIXERS
        mod_l = jnp.split((cond_lat @ ada_w[i] + ada_b[i])[:, None, :], 6, axis=-1)
        mod_c = jnp.split(cond_ctx @ ada_w[i] + ada_b[i], 6, axis=-1)
        u_l = modulate(rmsnorm(x, norm1[i]), mod_l[0], mod_l[1])
        u_c = modulate(rmsnorm(ctx, norm1[i]), mod_c[0], mod_c[1])
        if i % N_MIXERS == 0:
            y_l, y_c = s5_mixer(u_l, u_c, s5_lam_re[j], s5_lam_im[j], s5_log_dt[j], s5_b_re[j], s5_b_im[j],
                                s5_c_re[j], s5_c_im[j], s5_d[j], s5_w_glu[j], not last)
        else:
            y_l, y_c = mlstm_mixer(u_l, u_c, ml_w_in[j], ml_b_gates[j], ml_norm[j], ml_w_out[j], not last)
        x = x + mod_l[2] * y_l
        if last:
            f_l = swiglu(modulate(rmsnorm(x, norm2[i]), mod_l[3], mod_l[4]), ffn_w_in[i], ffn_w_out[i])
            x = x + mod_l[5] * f_l
        else:
            ctx = ctx + mod_c[2] * y_c
            u = jnp.concatenate([modulate(rmsnorm(ctx, norm2[i]), mod_c[3], mod_c[4]),
                                 modulate(rmsnorm(x, norm2[i]), mod_l[3], mod_l[4])], axis=1)
            f = swiglu(u, ffn_w_in[i], ffn_w_out[i])
            ctx = ctx + mod_c[5] * f[:, :ctx_len]
            x = x + mod_l[5] * f[:, ctx_len:]
    return rmsnorm(x, norm_f)
```

```python
import numpy as np
import concourse.bass as bass
import concourse.mybir as mybir
from concourse.bass_utils import run_bass_kernel_spmd

F32 = mybir.dt.float32
BF16 = mybir.dt.bfloat16
AF = mybir.ActivationFunctionType
ALU = mybir.AluOpType
AX = mybir.AxisListType

ENGS = ("pe", "act", "dve", "pool", "sp")

D = 2048
KD = D // 128
FH = 5632
NHC = FH // 128
S_CTX = 256
EPS = 1e-6
T0 = 8
G = 128
PST = 64
ML_H = 8
ML_IN = 6176
GATE_CAP = 15.0


class T:
    def __init__(self, h, name):
        self.h = h
        self.name = name
        self.last_w = None
        self.readers = []
        self.ld_sem = None
        self.ld_cnt = 0
        self.st_sem = None
        self.st_cnt = 0

    def __getitem__(self, k):
        return self.h[k]


class Prog:
    def __init__(self, nc, same_engine_sync=True):
        self.nc = nc
        self.ops = {e: [] for e in ENGS}
        self.cnt = {e: 0 for e in ENGS}
        self.waited = {}
        self.esem = {}
        self.same_engine_sync = same_engine_sync
        self.all_st = []
        self._stack = []
        self.dma_sems = []
        self.dma_sem_i = 0
        self.n_instr = 0
        for e in ("pe", "act", "dve", "pool"):
            self.esem[e] = nc.alloc_semaphore("prog_" + e)
        self.free_sems = [nc.alloc_semaphore(f"dsem{i}") for i in range(90)]
        self.sem_users = {}

    def sbuf(self, name, shape, dt):
        self.uid = getattr(self, "uid", 0) + 1
        name = f"{name}_{self.uid}"
        cm = self.nc.sbuf_tensor(name, list(shape), dt)
        h = cm.__enter__()
        self._stack.append(cm)
        return T(h, name)

    def psum(self, name, shape, dt=F32):
        self.uid = getattr(self, "uid", 0) + 1
        name = f"{name}_{self.uid}"
        cm = self.nc.psum_tensor(name, list(shape), dt)
        h = cm.__enter__()
        self._stack.append(cm)
        return T(h, name)

    def mark(self):
        return len(self._stack)

    def release(self, mark):
        while len(self._stack) > mark:
            cm = self._stack.pop()
            cm.__exit__(None, None, None)

    def _get_sem(self, t, kind):
        if not self.free_sems:
            raise RuntimeError("out of DMA semaphores")
        return self.free_sems.pop()

    def _need(self, eng, dep, waits):
        if dep is None:
            return
        if dep[0] == "eng":
            _, e2, seq = dep
            if e2 == eng and (eng == "pe" or not self.same_engine_sync):
                return
            key = (eng, "eng", e2)
            if self.waited.get(key, 0) >= seq:
                return
            self.waited[key] = seq
            waits.append((self.esem[e2], seq))
        else:
            _, sem, val = dep
            key = (eng, "sem", sem.num)
            if self.waited.get(key, 0) >= val:
                return
            self.waited[key] = val
            waits.append((sem, val))

    def op(self, eng, fn, reads=(), writes=()):
        waits = []
        for t in reads:
            self._need(eng, t.last_w, waits)
        for t in writes:
            self._need(eng, t.last_w, waits)
            for r in t.readers:
                self._need(eng, r, waits)
        self.cnt[eng] += 1
        seq = self.cnt[eng]
        me = ("eng", eng, seq)
        for t in writes:
            t.last_w = me
            t.readers = []
        for t in reads:
            if t.last_w is not me:
                t.readers.append(me)
                if len(t.readers) > 48:
                    best = {}
                    for r in t.readers:
                        k = (r[0], r[1] if r[0] == "eng" else r[1].num)
                        if k not in best or best[k][2] < r[2]:
                            best[k] = r
                    t.readers = list(best.values())
        self.ops[eng].append((waits, fn, (self.esem[eng], 1)))
        self.n_instr += 1

    def dma(self, q, out, in_, out_t=None, in_t=None, **kw):
        waits = []
        if in_t is not None:
            self._need(q, in_t.last_w, waits)
        if out_t is not None:
            self._need(q, out_t.last_w, waits)
            for r in out_t.readers:
                self._need(q, r, waits)
        if out_t is not None:
            if out_t.ld_sem is None:
                out_t.ld_sem = self._get_sem(out_t, "ld")
                out_t.ld_cnt = self.sem_users.get(out_t.ld_sem.num, 0)
            out_t.ld_cnt += 16
            self.sem_users[out_t.ld_sem.num] = out_t.ld_cnt
            sem, val = out_t.ld_sem, out_t.ld_cnt
            out_t.last_w = ("dma", sem, val)
            out_t.readers = []
            if in_t is not None:
                in_t.readers.append(("dma", sem, val))
        else:
            if in_t.st_sem is None:
                in_t.st_sem = self._get_sem(in_t, "st")
                in_t.st_cnt = self.sem_users.get(in_t.st_sem.num, 0)
                self.all_st.append(in_t)
            in_t.st_cnt += 16
            self.sem_users[in_t.st_sem.num] = in_t.st_cnt
            sem, val = in_t.st_sem, in_t.st_cnt
            in_t.readers.append(("dma", sem, val))

        def fn(e, out=out, in_=in_, kw=kw):
            return e.dma_start(out=out, in_=in_, **kw)

        self.ops[q].append((waits, fn, (sem, 16)))
        self.n_instr += 1

    def barrier_all(self):
        for e in ENGS:
            waits = []
            for e2 in ("pe", "act", "dve", "pool"):
                if self.cnt[e2] > 0:
                    self._need(e, ("eng", e2, self.cnt[e2]), waits)
            for t in self.all_st:
                self._need(e, ("dma", t.st_sem, t.st_cnt), waits)
            if waits:
                self.ops[e].append((waits, None, None))

    def end_phase(self, tiles):
        for t in tiles:
            if t.ld_sem is not None and t.last_w is not None and t.last_w[0] == "dma":
                w = []
                self._need("sp", t.last_w, w)
                if w:
                    self.ops["sp"].append((w, None, None))
        self.barrier_all()
        for t in tiles:
            for s in (t.ld_sem, t.st_sem):
                if s is not None:
                    self.free_sems.append(s)
            if t in self.all_st:
                self.all_st.remove(t)
            t.ld_sem = None
            t.st_sem = None

    def emit(self):
        nc = self.nc
        ops = self.ops

        def run(eng_obj, lst):
            for waits, fn, inc in lst:
                for sem, val in waits:
                    eng_obj.wait_ge(sem, val)
                if fn is not None:
                    ins = fn(eng_obj)
                    ins.then_inc(inc[0], inc[1])

        with nc.Block() as block:
            @block.tensor
            def _(e):
                run(e, ops["pe"])

            @block.scalar
            def _(e):
                run(e, ops["act"])

            @block.vector
            def _(e):
                run(e, ops["dve"])

            @block.gpsimd
            def _(e):
                run(e, ops["pool"])

            @block.sync
            def _(e):
                run(e, ops["sp"])
        self.ops = {e: [] for e in ENGS}


class Ring:
    def __init__(self, tiles):
        self.tiles = tiles
        self.i = 0

    def next(self):
        t = self.tiles[self.i % len(self.tiles)]
        self.i += 1
        return t


def col_blocks(lo, hi, step=512):
    out = []
    a = lo
    while a < hi:
        b = min(hi, (a // step + 1) * step)
        out.append((a, b))
        a = b
    return out


class Ctx:
    pass


def make_consts(P, K):
    K.ident_b = P.sbuf("ident_b", [128, 128], BF16)
    K.ident_f = P.sbuf("ident_f", [128, 128], F32)
    for t in (K.ident_b, K.ident_f):
        P.op("pool", lambda e, t=t: e.memset(t[:], 0.0), writes=[t])
        P.op("pool", lambda e, t=t: e.affine_select(out=t[:], in_=t[:], compare_op=ALU.not_equal, fill=1.0,
                                                    base=0, pattern=[[-1, 128]], channel_multiplier=1),
             reads=[t], writes=[t])
    K.eps_t = P.sbuf("eps_t", [128, 1], F32)
    P.op("pool", lambda e: e.memset(K.eps_t[:], EPS), writes=[K.eps_t])
    K.cbias = P.sbuf("cbias", [128, 8], F32)
    for k in range(8):
        P.op("pool", lambda e, k=k: e.memset(K.cbias[:, k:k + 1], -(2 * k - 1) * float(np.pi)), writes=[K.cbias])


def phase_precast(P, K, jobs, shape, tag):
    m = P.mark()
    f32r = Ring([P.sbuf(f"pc_f{i}_{tag}", shape, F32) for i in range(2)])
    b16r = Ring([P.sbuf(f"pc_b{i}_{tag}", shape, BF16) for i in range(2)])
    tiles = f32r.tiles + b16r.tiles
    for i, (parts, d) in enumerate(jobs):
        tf = f32r.next()
        tb = b16r.next()
        for pi_, (sl, s_) in enumerate(parts):
            P.dma("sp" if (i + pi_) % 2 == 0 else "act", sl(tf), s_, out_t=tf)
        P.op("pool", lambda e, tb=tb, tf=tf: e.tensor_copy(out=tb[:], in_=tf[:]), reads=[tf], writes=[tb])
        P.dma("sp" if i % 2 == 1 else "act", d, tb[:], in_t=tb)
    P.end_phase(tiles)
    P.emit()
    P.release(m)


def load_rep(P, q, tile, dram_row_ap):
    P.dma(q, tile[:], dram_row_ap.partition_broadcast(128), out_t=tile)


def rmsnorm_mod_T(P, K, xt, A_rep, sh_rep, uT, col0, scr):
    junk = scr["junk"]
    ssq = scr["stat"].next()
    P.op("act", lambda e: e.activation(out=junk[:], in_=xt[:], func=AF.Square, accum_out=ssq[:, 0:1]),
         reads=[xt], writes=[junk, ssq])
    P.op("act", lambda e: e.activation(out=ssq[:, 1:2], in_=ssq[:, 0:1], func=AF.Sqrt, bias=K.eps_t[:, 0:1], scale=1.0 / D),
         reads=[ssq, K.eps_t], writes=[ssq])
    P.op("dve", lambda e: e.reciprocal(out=ssq[:, 2:3], in_=ssq[:, 1:2]), reads=[ssq], writes=[ssq])
    t1 = scr["t1"]
    P.op("dve", lambda e: e.scalar_tensor_tensor(out=t1[:], in0=xt[:], scalar=ssq[:, 2:3], in1=A_rep[:],
                                                  op0=ALU.mult, op1=ALU.mult), reads=[xt, ssq, A_rep], writes=[t1])
    ub = scr["ub"].next()
    P.op("pool", lambda e: e.tensor_tensor(out=ub[:], in0=t1[:], in1=sh_rep[:], op=ALU.add), reads=[t1, sh_rep], writes=[ub])
    transpose_to(P, K, ub, uT, col0, scr)
    return ssq


def transpose_to(P, K, ub, uT, col0, scr):
    for g in range(KD // 8):
        pt = scr["pt"].next()
        for j in range(8):
            k = g * 8 + j
            P.op("pe", lambda e, pt=pt, j=j, k=k: e.transpose(out=pt[:, j, :], in_=ub[:, k * 128:(k + 1) * 128], identity=K.ident_b[:]),
                 reads=[ub, K.ident_b], writes=[pt])
        eng = "act" if g % 2 == 0 else "dve"
        if eng == "act":
            P.op("act", lambda e, pt=pt, g=g: e.activation(out=uT[:, g * 8:(g + 1) * 8, col0:col0 + 128], in_=pt[:], func=AF.Copy),
                 reads=[pt], writes=[uT])
        else:
            P.op("dve", lambda e, pt=pt, g=g: e.tensor_copy(out=uT[:, g * 8:(g + 1) * 8, col0:col0 + 128], in_=pt[:]),
                 reads=[pt], writes=[uT])


def phase_cond(P, K, io):
    K.condT = P.sbuf("condT", [128, KD, 2], F32)
    m = P.mark()
    craw = P.sbuf("craw", [KD, 2, 128], F32)
    csil = P.sbuf("csil", [KD, 2, 128], F32)
    pt = P.psum("cond_pt", [128, 2, KD], F32)
    P.dma("sp", craw[:, 0, :], io["c"].rearrange("o (k p) -> (o k) p", p=128), out_t=craw)
    P.dma("sp", craw[:, 1, :], io["c_ctx"].rearrange("o (k p) -> (o k) p", p=128), out_t=craw)
    P.op("act", lambda e: e.activation(out=csil[:], in_=craw[:], func=AF.Silu), reads=[craw], writes=[csil])
    for r in range(2):
        P.op("pe", lambda e, r=r: e.transpose(out=pt[:, r, :], in_=csil[:, r, :], identity=K.ident_f[0:KD, 0:KD]),
             reads=[csil, K.ident_f], writes=[pt])
    P.op("dve", lambda e: e.tensor_copy(out=K.condT[:].rearrange("p k r -> p r k"), in_=pt[:]), reads=[pt], writes=[K.condT])
    P.end_phase([craw, csil, pt])
    P.emit()
    P.release(m)


def phase_ada(P, K, io, layer, mod_out):
    m = P.mark()
    NB = 6 * D // 512
    wr = Ring([P.sbuf(f"ada_w{i}", [128, KD, 512], F32) for i in range(2)])
    br = Ring([P.sbuf(f"ada_b{i}", [2, 512], F32) for i in range(2)])
    orr = Ring([P.sbuf(f"ada_o{i}", [2, 512], F32) for i in range(2)])
    pr = Ring([P.psum(f"ada_p{i}", [2, 512], F32) for i in range(2)])
    aw = io["ada_w"][layer].rearrange("(k p) n -> p k n", p=128)
    ab = io["ada_b"][layer:layer + 1, :]
    for nb in range(NB):
        wt = wr.next(); bt = br.next(); ot = orr.next(); ps = pr.next()
        cs = slice(nb * 512, (nb + 1) * 512)
        P.dma("sp" if nb % 2 == 0 else "act", wt[:], aw[:, :, cs], out_t=wt)
        P.dma("sp", bt[:], ab[:, cs].partition_broadcast(2), out_t=bt)
        for k in range(KD):
            P.op("pe", lambda e, ps=ps, wt=wt, k=k: e.matmul(ps[:], lhsT=K.condT[:, k, :], rhs=wt[:, k, :], start=(k == 0), stop=(k == KD - 1)),
                 reads=[K.condT, wt], writes=[ps])
        P.op("dve", lambda e, ot=ot, ps=ps, bt=bt: e.tensor_tensor(out=ot[:], in0=ps[:], in1=bt[:], op=ALU.add), reads=[ps, bt], writes=[ot])
        P.dma("sp", mod_out[:, cs], ot[:], in_t=ot)
    P.end_phase(wr.tiles + br.tiles + orr.tiles + pr.tiles)
    P.emit()
    P.release(m)


def load_mod_tiles(P, K, io, layer, mod_d, row, which, names, normkey):
    out = {}
    gt = None
    for name, ci, kind in which:
        t = P.sbuf(f"mod_{name}", [128, D], F32)
        load_rep(P, "sp", t, mod_d[row:row + 1, ci * D:(ci + 1) * D])
        if kind == "A":
            if gt is None:
                gt = P.sbuf("mod_g", [128, D], F32)
                load_rep(P, "act", gt, io[normkey][layer:layer + 1, :])
            P.op("dve", lambda e, t=t, gt=gt: e.scalar_tensor_tensor(out=t[:], in0=t[:], scalar=1.0, in1=gt[:], op0=ALU.add, op1=ALU.mult),
                 reads=[t, gt], writes=[t])
        out[name] = t
    if gt is not None:
        out["_g"] = gt
    return out


def lat_rows(ap_lat, p0, S_LAT, n=128):
    ROWS = S_LAT // 64
    w0, nw = p0 // ROWS, n // ROWS
    return ap_lat.rearrange("(r w) d -> w r d", w=64)[w0:w0 + nw]


def token_tiles(S_LAT):
    tl = [(1, 0, S_CTX // 128)]
    for t0 in range(0, S_LAT, 512):
        tl.append((0, S_CTX + t0, min(4, (S_LAT - t0) // 128)))
    return tl


def phase_b_s5(P, K, io, layer, mod_d, xs, u_tok, S_LAT):
    m = P.mark()
    xr = Ring([P.sbuf(f"b_x{i}", [128, D], F32) for i in range(3)])
    scr = dict(junk=P.sbuf("b_junk", [128, D], BF16), t1=P.sbuf("b_t1", [128, D], F32),
               stat=Ring([P.sbuf(f"b_st{i}", [128, 4], F32) for i in range(4)]),
               ub=Ring([P.sbuf(f"b_ub{i}", [128, D], BF16) for i in range(3)]))
    tiles = xr.tiles + [scr["junk"], scr["t1"]] + scr["stat"].tiles + scr["ub"].tiles
    for row in (1, 0):
        mm = P.mark()
        md = load_mod_tiles(P, K, io, layer, mod_d, row, [("sh", 0, "raw"), ("A", 1, "A")], None, "norm1")
        ntok = S_CTX if row == 1 else S_LAT
        base = 0 if row == 1 else S_CTX
        for t in range(ntok // 128):
            xt = xr.next()
            P.dma("sp", xt[:], xs[base + t * 128: base + (t + 1) * 128, :], out_t=xt)
            ssq = scr["stat"].next()
            junk = scr["junk"]
            P.op("act", lambda e, xt=xt, ssq=ssq: e.activation(out=junk[:], in_=xt[:], func=AF.Square, accum_out=ssq[:, 0:1]),
                 reads=[xt], writes=[junk, ssq])
            P.op("act", lambda e, ssq=ssq: e.activation(out=ssq[:, 1:2], in_=ssq[:, 0:1], func=AF.Sqrt, bias=K.eps_t[:, 0:1], scale=1.0 / D),
                 reads=[ssq, K.eps_t], writes=[ssq])
            P.op("dve", lambda e, ssq=ssq: e.reciprocal(out=ssq[:, 2:3], in_=ssq[:, 1:2]), reads=[ssq], writes=[ssq])
            t1 = scr["t1"]
            P.op("dve", lambda e, xt=xt, ssq=ssq: e.scalar_tensor_tensor(out=t1[:], in0=xt[:], scalar=ssq[:, 2:3], in1=md["A"][:],
                                                                          op0=ALU.mult, op1=ALU.mult), reads=[xt, ssq, md["A"]], writes=[t1])
            ub = scr["ub"].next()
            P.op("pool", lambda e, ub=ub: e.tensor_tensor(out=ub[:], in0=t1[:], in1=md["sh"][:], op=ALU.add), reads=[t1, md["sh"]], writes=[ub])
            P.dma("act", u_tok[base + t * 128: base + (t + 1) * 128, :], ub[:], in_t=ub)
            if row == 1:
                P.dma("act", u_tok[S_CTX + S_LAT + t * 128: S_CTX + S_LAT + (t + 1) * 128, :], ub[:], in_t=ub)
        P.end_phase(list(md.values()))
        P.emit()
        P.release(mm)
    P.end_phase(tiles)
    P.emit()
    P.release(m)


def phase_d(P, K, io, layer, mod_d, xs_in, xs_out, mix_tok, wproj, wgu_t, wo_t, S_LAT, mixer, last, out_final, tok_rows=None):
    m = P.mark()
    xt = [P.sbuf(f"d_x{i}", [128, D], F32) for i in range(4)]
    bfr = Ring([P.sbuf(f"d_bf{i}", [128, D], BF16) for i in range(2)])
    u2T = P.sbuf("d_u2T", [128, KD, 512], BF16)
    hT = P.sbuf("d_hT", [128, NHC, 512], BF16)
    wpr = Ring([P.sbuf(f"d_wp{i}", [128, KD, 256], BF16) for i in range(2)])
    wgr = Ring([P.sbuf(f"d_wgu{i}", [128, KD, 256], BF16) for i in range(2)])
    wor = Ring([P.sbuf(f"d_wo{i}", [128, 4, 512], BF16) for i in range(3)])
    sgr = Ring([P.sbuf(f"d_sg{i}", [128, 512], BF16) for i in range(2)])
    f1r = Ring([P.sbuf(f"d_f1{i}", [128, 512], F32) for i in range(2)])
    f2r = Ring([P.sbuf(f"d_f2{i}", [128, 512], F32) for i in range(2)])
    t1 = P.sbuf("d_t1", [128, D], F32)
    gcur = P.sbuf("d_gcur", [128, D], F32)
    statr = Ring([P.sbuf(f"d_st{i}", [128, 4], F32) for i in range(4)])
    ptr = Ring([P.psum(f"d_pt{i}", [128, 8, 128], BF16) for i in range(2)])
    mmr = Ring([P.psum(f"d_mm{i}", [128, 512], F32) for i in range(6)])
    tiles = (xt + bfr.tiles + [u2T, hT, t1, gcur] + wpr.tiles + wgr.tiles + wor.tiles + sgr.tiles + f1r.tiles + f2r.tiles
             + statr.tiles + ptr.tiles + mmr.tiles)
    scr = dict(pt=ptr)
    qi = [0]

    def q():
        qi[0] += 1
        return "sp" if qi[0] % 2 == 0 else "act"

    ROWS = S_LAT // 64

    def xdma(tile, ap, tok0, s, load):
        has_ctx = ap.shape[0] == S_CTX + S_LAT
        if tok_rows is None or tok0 < S_CTX:
            off = 0 if has_ctx else -S_CTX
            d_ap = ap[tok0 + off + s * 128: tok0 + off + (s + 1) * 128, :]
            s_ap = tile[:]
        else:
            lat = ap[S_CTX:S_CTX + S_LAT, :] if has_ctx else ap
            lr = lat_rows(lat, tok0 - S_CTX + s * 128, S_LAT)
            for wi in range(128 // ROWS):
                if load:
                    P.dma(q(), tile[wi * ROWS:(wi + 1) * ROWS, :], lr[wi], out_t=tile)
                else:
                    P.dma(q(), lr[wi], tile[wi * ROWS:(wi + 1) * ROWS, :], in_t=tile)
            return
        if load:
            P.dma(q(), s_ap, d_ap, out_t=tile)
        else:
            P.dma(q(), d_ap, s_ap, in_t=tile)

    def norm_stats(x_t):
        ssq = statr.next()
        junk = bfr.next()
        P.op("act", lambda e: e.activation(out=junk[:], in_=x_t[:], func=AF.Square, accum_out=ssq[:, 0:1]), reads=[x_t], writes=[junk, ssq])
        P.op("act", lambda e: e.activation(out=ssq[:, 1:2], in_=ssq[:, 0:1], func=AF.Sqrt, bias=K.eps_t[:, 0:1], scale=1.0 / D),
             reads=[ssq, K.eps_t], writes=[ssq])
        P.op("dve", lambda e: e.reciprocal(out=ssq[:, 2:3], in_=ssq[:, 1:2]), reads=[ssq], writes=[ssq])
        return ssq

    cur_row = None
    md = None
    mm_mark = None
    for (row, tok0, nsub) in token_tiles(S_LAT):
        if last and row == 1:
            continue
        if row != cur_row:
            if md is not None:
                P.end_phase(list(md.values()))
                P.emit()
                P.release(mm_mark)
            mm_mark = P.mark()
            md = {}
            if last:
                md["nf"] = P.sbuf("mod_nf", [128, D], F32)
                load_rep(P, "sp", md["nf"], io["norm_f"])
            md.update(load_mod_tiles(P, K, io, layer, mod_d, row, [("sh2", 3, "raw"), ("A2", 4, "A")], None, "norm2"))
            cur_row = row
        TT = nsub * 128
        load_rep(P, "sp", gcur, mod_d[row:row + 1, 2 * D:3 * D])
        for s in range(nsub):
            xdma(xt[s], xs_in, tok0, s, True)
            mb = bfr.next()
            P.dma(q(), mb[:], mix_tok[tok0 + s * 128: tok0 + (s + 1) * 128, :], out_t=mb)
            transpose_to(P, K, mb, hT, s * 128, scr)
        for nb in range(8):
            wa = wpr.next()
            P.dma(q(), wa[:], wproj[nb], out_t=wa)
            if mixer == "s5":
                wb = wpr.next()
                P.dma(q(), wb[:], wproj[8 + nb], out_t=wb)
            cs = slice(nb * 256, (nb + 1) * 256)
            for s in range(nsub):
                pa = mmr.next()
                for k in range(KD):
                    P.op("pe", lambda e, pa=pa, wa=wa, k=k, s=s: e.matmul(pa[:, 0:256], lhsT=hT[:, k, s * 128:(s + 1) * 128], rhs=wa[:, k, :],
                                                                         start=(k == 0), stop=(k == KD - 1)), reads=[hT, wa], writes=[pa])
                f1 = f1r.next()
                if mixer == "s5":
                    pb = mmr.next()
                    for k in range(KD):
                        P.op("pe", lambda e, pb=pb, wb=wb, k=k, s=s: e.matmul(pb[:, 0:256], lhsT=hT[:, k, s * 128:(s + 1) * 128], rhs=wb[:, k, :],
                                                                             start=(k == 0), stop=(k == KD - 1)), reads=[hT, wb], writes=[pb])
                    f2 = f2r.next()
                    P.op("act", lambda e, f2=f2, pb=pb: e.activation(out=f2[:, 0:256], in_=pb[:, 0:256], func=AF.Sigmoid), reads=[pb], writes=[f2])
                    P.op("dve", lambda e, f1=f1, pa=pa, f2=f2: e.tensor_tensor(out=f1[:, 0:256], in0=pa[:, 0:256], in1=f2[:, 0:256], op=ALU.mult),
                         reads=[pa, f2], writes=[f1])
                    P.op("pool", lambda e, f1=f1, cs=cs: e.tensor_tensor(out=f1[:, 0:256], in0=f1[:, 0:256], in1=gcur[:, cs], op=ALU.mult),
                         reads=[f1, gcur], writes=[f1])
                else:
                    P.op("dve", lambda e, f1=f1, pa=pa, cs=cs: e.tensor_tensor(out=f1[:, 0:256], in0=pa[:, 0:256], in1=gcur[:, cs], op=ALU.mult),
                         reads=[pa, gcur], writes=[f1])
                P.op("pool", lambda e, f1=f1, s=s, cs=cs: e.tensor_tensor(out=xt[s][:, cs], in0=xt[s][:, cs], in1=f1[:, 0:256], op=ALU.add),
                     reads=[f1, xt[s]], writes=[xt[s]])
        load_rep(P, "sp", gcur, mod_d[row:row + 1, 5 * D:6 * D])
        for s in range(nsub):
            ssq = norm_stats(xt[s])
            P.op("dve", lambda e, s=s, ssq=ssq: e.scalar_tensor_tensor(out=t1[:], in0=xt[s][:], scalar=ssq[:, 2:3], in1=md["A2"][:],
                                                                        op0=ALU.mult, op1=ALU.mult), reads=[xt[s], ssq, md["A2"]], writes=[t1])
            ub = bfr.next()
            P.op("pool", lambda e, ub=ub: e.tensor_tensor(out=ub[:], in0=t1[:], in1=md["sh2"][:], op=ALU.add), reads=[t1, md["sh2"]], writes=[ub])
            transpose_to(P, K, ub, u2T, s * 128, scr)
        for c in range(NHC):
            wgu = wgr.next()
            P.dma(q(), wgu[:], wgu_t[c], out_t=wgu)
            pg = mmr.next(); pu = mmr.next()
            for k in range(KD):
                P.op("pe", lambda e, pg=pg, wgu=wgu, k=k: e.matmul(pg[:, :TT], lhsT=wgu[:, k, 0:128], rhs=u2T[:, k, :TT],
                                                                  start=(k == 0), stop=(k == KD - 1)), reads=[wgu, u2T], writes=[pg])
            for k in range(KD):
                P.op("pe", lambda e, pu=pu, wgu=wgu, k=k: e.matmul(pu[:, :TT], lhsT=wgu[:, k, 128:256], rhs=u2T[:, k, :TT],
                                                                  start=(k == 0), stop=(k == KD - 1)), reads=[wgu, u2T], writes=[pu])
            sg = sgr.next()
            P.op("act", lambda e, sg=sg, pg=pg: e.activation(out=sg[:, :TT], in_=pg[:, :TT], func=AF.Silu), reads=[pg], writes=[sg])
            P.op("dve", lambda e, sg=sg, pu=pu, c=c: e.tensor_tensor(out=hT[:, c, :TT], in0=pu[:, :TT], in1=sg[:, :TT], op=ALU.mult),
                 reads=[pu, sg], writes=[hT])
        for nt in range(4):
            cs = slice(nt * 512, (nt + 1) * 512)
            pf = [mmr.next() for _ in range(nsub)]
            for w in range(11):
                wo = wor.next()
                P.dma(q(), wo[:], wo_t[nt * 11 + w], out_t=wo)
                for s in range(nsub):
                    for cc in range(4):
                        c = w * 4 + cc
                        P.op("pe", lambda e, p_=pf[s], wo=wo, cc=cc, c=c, s=s: e.matmul(p_[:], lhsT=hT[:, c, s * 128:(s + 1) * 128], rhs=wo[:, cc, :],
                                                                                      start=(c == 0), stop=(c == NHC - 1)), reads=[hT, wo], writes=[pf[s]])
            for s in range(nsub):
                f1 = f1r.next()
                P.op("dve", lambda e, f1=f1, p_=pf[s], cs=cs: e.tensor_tensor(out=f1[:], in0=p_[:], in1=gcur[:, cs], op=ALU.mult), reads=[pf[s], gcur], writes=[f1])
                P.op("pool", lambda e, f1=f1, s=s, cs=cs: e.tensor_tensor(out=xt[s][:, cs], in0=xt[s][:, cs], in1=f1[:], op=ALU.add), reads=[f1, xt[s]], writes=[xt[s]])
        for s in range(nsub):
            if not last:
                xdma(xt[s], xs_out, tok0, s, False)
            else:
                ssq = norm_stats(xt[s])
                P.op("dve", lambda e, s=s, ssq=ssq: e.scalar_tensor_tensor(out=t1[:], in0=xt[s][:], scalar=ssq[:, 2:3], in1=md["nf"][:],
                                                                            op0=ALU.mult, op1=ALU.mult), reads=[xt[s], ssq, md["nf"]], writes=[t1])
                xdma(t1, out_final, tok0, s, False)
    if md is not None:
        P.end_phase(list(md.values()))
        P.emit()
        P.release(mm_mark)
    P.end_phase(tiles)
    P.emit()
    P.release(m)


def bc(ap, axis, n):
    a = ap.unsqueeze(axis)
    shp = list(a.shape)
    shp[axis] = n
    return a.broadcast_to(shp)


def phase_c_s5(P, K, io, jl, u_tok, gy_tok, S_LAT, dve2="pool"):
    NTOK3 = 2 * S_CTX + S_LAT
    NCH = NTOK3 // T0
    NF = (S_CTX + S_LAT) // T0
    CTXC = S_CTX // T0
    NBK = 32
    BL = NF // NBK
    assert NF == NBK * BL
    NBLK = (NCH + 127) // 128
    NFB = (NF + 127) // 128
    PI = float(np.pi)
    m = P.mark()
    V = lambda e: e

    def ew(eng, fn, reads, writes):
        P.op(eng, fn, reads=reads, writes=writes)

    Pw = P.sbuf("c_Pw", [128, 2, 2, 64, T0 + 1], F32)
    PWB = P.sbuf("c_PWB", [128, 2, 2, 64, BL + 1], F32)
    Bn = P.sbuf("c_Bn", [128, 2, 2, 64, 16], F32)
    Bb = P.sbuf("c_Bb", [128, 2, 2, 64, 16], F32)
    Dp = P.sbuf("c_Dp", [128, 128], F32)
    cz = [P.sbuf(f"c_cz{i}", [128, 16, 128], F32) for i in range(2)]
    m_tmp = P.mark()
    lam = P.sbuf("c_lam", [128, 2, 2, 64], F32)
    dtt = P.sbuf("c_dt", [128, 2, 64], F32)
    for d in range(2):
        for jj in range(2):
            ps_ = slice(jj * 64, (jj + 1) * 64)
            P.dma("sp", lam[ps_, 0, d, :], io["s5_lam_re"][jl, d].rearrange("(i j) p -> j p i", j=2)[jj], out_t=lam, allow_slow_non_contiguous=True)
            P.dma("act", lam[ps_, 1, d, :], io["s5_lam_im"][jl, d].rearrange("(i j) p -> j p i", j=2)[jj], out_t=lam, allow_slow_non_contiguous=True)
            P.dma("sp", dtt[ps_, d, :], io["s5_log_dt"][jl, d:d + 1, :].rearrange("o (i j) -> o j i", j=2)[:, jj, :].partition_broadcast(64),
                  out_t=dtt, allow_slow_non_contiguous=True)
    w = [P.sbuf(f"c_w{i}", [128, 2, 64], F32) for i in range(10)]
    A1 = P.sbuf("c_A1", [128, 2, 2, 64], F32)
    Fc = P.sbuf("c_F", [128, 2, 2, 64], F32)
    A8 = P.sbuf("c_A8", [128, 2, 2, 64], F32)
    ptc = Ring([P.psum(f"c_ptc{i}", [128, 4, 128], F32) for i in range(2)])
    lre, lim = lam[:, 0], lam[:, 1]
    ew("act", lambda e: e.activation(out=dtt[:], in_=dtt[:], func=AF.Exp), [dtt], [dtt])
    ew("dve", lambda e: e.tensor_tensor(out=w[0][:], in0=lre, in1=dtt[:], op=ALU.mult), [lam, dtt], [w[0]])
    ew("act", lambda e: e.activation(out=w[0][:], in_=w[0][:], func=AF.Exp), [w[0]], [w[0]])
    ew("dve", lambda e: e.tensor_tensor(out=w[1][:], in0=lim, in1=dtt[:], op=ALU.mult), [lam, dtt], [w[1]])
    for (dst, shift) in ((w[2], 0.0), (w[3], PI / 2)):
        ew("dve", lambda e, dst=dst, shift=shift: e.tensor_scalar(out=dst[:], in0=w[1][:], scalar1=float(shift), scalar2=None, op0=ALU.add), [w[1]], [dst])
        ew("dve", lambda e, dst=dst: e.tensor_copy(out=w[4][:], in_=dst[:]), [dst], [w[4]])
        for k in range(1, 7):
            ew("act", lambda e, k=k: e.activation(out=w[5][:], in_=w[4][:], func=AF.Sign, bias=K.cbias[:, k:k + 1], scale=1.0), [w[4], K.cbias], [w[5]])
            ew("dve", lambda e, dst=dst: e.scalar_tensor_tensor(out=dst[:], in0=w[5][:], scalar=-PI, in1=dst[:], op0=ALU.mult, op1=ALU.add), [w[5], dst], [dst])
        ew("dve", lambda e, dst=dst: e.tensor_scalar(out=dst[:], in0=dst[:], scalar1=-6.0 * PI, scalar2=None, op0=ALU.add), [dst], [dst])
        ew("act", lambda e, dst=dst: e.activation(out=dst[:], in_=dst[:], func=AF.Sin), [dst], [dst])
    ew("dve", lambda e: e.tensor_tensor(out=A1[:, 0], in0=w[0][:], in1=w[3][:], op=ALU.mult), [w[0], w[3]], [A1])
    ew("dve", lambda e: e.tensor_tensor(out=A1[:, 1], in0=w[0][:], in1=w[2][:], op=ALU.mult), [w[0], w[2]], [A1])
    ew("dve", lambda e: e.tensor_tensor(out=w[4][:], in0=lre, in1=lre, op=ALU.mult), [lam], [w[4]])
    ew("dve", lambda e: e.tensor_tensor(out=w[5][:], in0=lim, in1=lim, op=ALU.mult), [lam], [w[5]])
    ew("dve", lambda e: e.tensor_tensor(out=w[4][:], in0=w[4][:], in1=w[5][:], op=ALU.add), [w[4], w[5]], [w[4]])
    ew("dve", lambda e: e.reciprocal(out=w[4][:], in_=w[4][:]), [w[4]], [w[4]])
    ew("dve", lambda e: e.tensor_scalar(out=w[5][:], in0=A1[:, 0], scalar1=-1.0, scalar2=None, op0=ALU.add), [A1], [w[5]])
    ew("dve", lambda e: e.tensor_tensor(out=w[6][:], in0=w[5][:], in1=lre, op=ALU.mult), [w[5], lam], [w[6]])
    ew("dve", lambda e: e.tensor_tensor(out=w[7][:], in0=A1[:, 1], in1=lim, op=ALU.mult), [A1, lam], [w[7]])
    ew("dve", lambda e: e.tensor_tensor(out=w[6][:], in0=w[6][:], in1=w[7][:], op=ALU.add), [w[6], w[7]], [w[6]])
    ew("dve", lambda e: e.tensor_tensor(out=Fc[:, 0], in0=w[6][:], in1=w[4][:], op=ALU.mult), [w[6], w[4]], [Fc])
    ew("dve", lambda e: e.tensor_tensor(out=w[6][:], in0=A1[:, 1], in1=lre, op=ALU.mult), [A1, lam], [w[6]])
    ew("dve", lambda e: e.tensor_tensor(out=w[7][:], in0=w[5][:], in1=lim, op=ALU.mult), [w[5], lam], [w[7]])
    ew("dve", lambda e: e.tensor_tensor(out=w[6][:], in0=w[6][:], in1=w[7][:], op=ALU.subtract), [w[6], w[7]], [w[6]])
    ew("dve", lambda e: e.tensor_tensor(out=Fc[:, 1], in0=w[6][:], in1=w[4][:], op=ALU.mult), [w[6], w[4]], [Fc])

    def cmul_pow(dst, n, base_re, base_im, tag):
        ew("dve", lambda e: e.memset(dst[:, 0, :, :, 0:1], 1.0), [], [dst])
        ew("dve", lambda e: e.memset(dst[:, 1, :, :, 0:1], 0.0), [], [dst])
        for k in range(1, n):
            pr, pi_ = dst[:, 0, :, :, k - 1], dst[:, 1, :, :, k - 1]
            ew("dve", lambda e, pr=pr: e.tensor_tensor(out=w[6][:], in0=pr, in1=base_re, op=ALU.mult), [dst, A1], [w[6]])
            ew("dve", lambda e, pi_=pi_: e.tensor_tensor(out=w[7][:], in0=pi_, in1=base_im, op=ALU.mult), [dst, A1], [w[7]])
            ew("dve", lambda e, k=k: e.tensor_tensor(out=dst[:, 0, :, :, k], in0=w[6][:], in1=w[7][:], op=ALU.subtract), [w[6], w[7]], [dst])
            ew("dve", lambda e, pr=pr: e.tensor_tensor(out=w[6][:], in0=pr, in1=base_im, op=ALU.mult), [dst, A1], [w[6]])
            ew("dve", lambda e, pi_=pi_: e.tensor_tensor(out=w[7][:], in0=pi_, in1=base_re, op=ALU.mult), [dst, A1], [w[7]])
            ew("dve", lambda e, k=k: e.tensor_tensor(out=dst[:, 1, :, :, k], in0=w[6][:], in1=w[7][:], op=ALU.add), [w[6], w[7]], [dst])

    cmul_pow(Pw, T0 + 1, A1[:, 0], A1[:, 1], "pw")
    ew("dve", lambda e: e.tensor_copy(out=A8[:], in_=Pw[:, :, :, :, T0]), [Pw], [A8])
    cmul_pow(PWB, BL + 1, A8[:, 0], A8[:, 1], "pwb")

    for d in range(2):
        for jj in range(2):
            ps_ = slice(jj * 64, (jj + 1) * 64)
            P.dma("sp", Bn[ps_, 0, d], io["s5_b_re"][jl, d].rearrange("(i j) p c -> j p i c", j=2)[jj], out_t=Bn)
            P.dma("act", Bn[ps_, 1, d], io["s5_b_im"][jl, d].rearrange("(i j) p c -> j p i c", j=2)[jj], out_t=Bn)
    tbT = cz[0]
    tbv = cz[0][:].rearrange("p (d a) (b c) -> p d (a b) c", d=2, c=16)
    fre = bc(Fc[:, 0], 3, 16)
    fim = bc(Fc[:, 1], 3, 16)
    ew("dve", lambda e: e.tensor_tensor(out=Bb[:, 0], in0=Bn[:, 0], in1=fre, op=ALU.mult), [Bn, Fc], [Bb])
    ew("dve", lambda e: e.tensor_tensor(out=tbv, in0=Bn[:, 1], in1=fim, op=ALU.mult), [Bn, Fc], [tbT])
    ew("dve", lambda e: e.tensor_tensor(out=Bb[:, 0], in0=Bb[:, 0], in1=tbv, op=ALU.subtract), [Bb, tbT], [Bb])
    ew("dve", lambda e: e.tensor_tensor(out=Bb[:, 1], in0=Bn[:, 1], in1=fre, op=ALU.mult), [Bn, Fc], [Bb])
    ew("dve", lambda e: e.tensor_tensor(out=tbv, in0=Bn[:, 0], in1=fim, op=ALU.mult), [Bn, Fc], [tbT])
    ew("dve", lambda e: e.tensor_tensor(out=Bb[:, 1], in0=Bb[:, 1], in1=tbv, op=ALU.add), [Bb, tbT], [Bb])

    Cn = Bn
    for t in cz:
        ew("pool", lambda e, t=t: e.memset(t[:], 0.0), [], [t])
    ci = 0
    for d in range(2):
        for part, key in ((0, "s5_c_re"), (1, "s5_c_im")):
            t = cz[ci % 2]; ci += 1
            src = io[key][jl, d].rearrange("(kc q j) c p -> q j c kc p", q=4, j=2)
            for q_ in range(4):
                for j_ in range(2):
                    r0 = 32 * q_ + 16 * j_
                    P.dma("sp" if (q_ + j_) % 2 == 0 else "act", t[r0:r0 + 16, :, 64 * j_:64 * j_ + 64], src[q_, j_], out_t=t)
            for k4 in range(4):
                pt = ptc.next()
                for kk in range(4):
                    kc = k4 * 4 + kk
                    ew("pe", lambda e, pt=pt, kk=kk, kc=kc, t=t: e.transpose(out=pt[:, kk, :], in_=t[:, kc, :], identity=K.ident_f[:]), [t, K.ident_f], [pt])
                for jj in range(2):
                    ps_ = slice(jj * 64, (jj + 1) * 64)
                    src_ap = pt[ps_].rearrange("p k (q j c) -> p k q j c", q=4, j=2)[:, :, :, jj, :]
                    dst_ap = Cn[ps_, part, d, 16 * k4:16 * k4 + 16, :].rearrange("p (k q) c -> p k q c", q=4)
                    if part == 0:
                        ew("dve", lambda e, dst_ap=dst_ap, src_ap=src_ap: e.tensor_copy(out=dst_ap, in_=src_ap), [pt], [Cn])
                    else:
                        ew("dve", lambda e, dst_ap=dst_ap, src_ap=src_ap: e.tensor_scalar(out=dst_ap, in0=src_ap, scalar1=-1.0, scalar2=None, op0=ALU.mult), [pt], [Cn])
    for t_ in range(T0):
        P.dma("sp", Dp[16 * t_:16 * t_ + 16, :], io["s5_d"][jl:jl + 1, :].rearrange("o (g c) -> (o c) g", c=16), out_t=Dp, allow_slow_non_contiguous=True)

    P.end_phase([lam, dtt, A1, Fc, A8] + w + ptc.tiles)
    P.emit()
    P.release(m_tmp)

    VF, VB = cz[0], cz[1]
    VFv = cz[0][:].rearrange("p (a b x) (y d) -> p a b (x y) d", a=2, b=4, d=16)
    VBv = cz[1][:].rearrange("p (a b x) (y d) -> p a b (x y) d", a=2, b=4, d=16)
    ew("pool", lambda e: e.memset(cz[0][:], 0.0), [], [VF])
    ew("pool", lambda e: e.memset(cz[1][:], 0.0), [], [VB])
    vt = P.sbuf("c_vt", [128, 4, 8, 16], F32)
    vu = P.sbuf("c_vu", [128, 4, 8, 16], F32)
    Ro = P.sbuf("c_Ro", [128, 2, 2, 4, 8, 16], BF16)
    TS = P.sbuf("c_TS", [128, 2, 2, 4, 128], BF16)
    IT = P.sbuf("c_IT", [128, 2, 8, 128], BF16)
    U = P.sbuf("c_U", [128, 8, NBLK * 128], BF16)
    Zr = Ring([P.sbuf(f"c_Z{i}", [128, 8, 128], BF16) for i in range(2)])
    Zpr = Ring([P.sbuf(f"c_Zp{i}", [128, 8, 8, 16], BF16) for i in range(2)])
    Hs = P.sbuf("c_Hs", [128, 2, 8, NBK, BL], F32)
    Hr = P.sbuf("c_Hr", [128, 2, 2, 4, NCH], BF16)
    ew("pool", lambda e: e.memset(Hr[:], 0.0), [], [Hr])
    A8l = P.sbuf("c_A8l", [128, 2, 8], F32)
    ABl = P.sbuf("c_ABl", [128, 2, 8], F32)
    PWl = P.sbuf("c_PWl", [128, 2, 8, BL], F32)
    s1 = [P.sbuf(f"c_s1{i}", [128, 8, NBK], F32) for i in range(2)]
    s3 = P.sbuf("c_s3", [128, 4, NBK - 1, max(BL - 1, 1)], F32)
    Ysb = Ring([P.sbuf(f"c_Y{i}", [128, NFB * 128], F32) for i in range(2)])
    Zo = P.sbuf("c_Zo", [128, NFB, 8, 128], BF16)
    gl_ = [P.sbuf(f"c_g{i}", [128, NFB * 128], F32) for i in range(2)]
    Ybr = Ring([P.sbuf(f"c_Yb{i}", [128, NFB * 128], BF16) for i in range(2)])
    ptb = Ring([P.psum(f"c_ptb{i}", [128, 8, 128], BF16) for i in range(2)])
    psS = Ring([P.psum(f"c_psS{i}", [128, 1024], F32) for i in range(1)])
    psY = Ring([P.psum(f"c_psY{i}", [128, 1024], F32) for i in range(1)])
    psT = Ring([P.psum(f"c_psT{i}", [128, 4, 128], F32) for i in range(1)])
    alt = ["dve", dve2]

    for b in range(16):
        i0 = 4 * b
        f0 = 128 * b
        for d in range(2):
            for part in range(2):
                pre = bc(Pw[:, 0, d, i0:i0 + 4, 0:T0], 3, 16)
                pim = bc(Pw[:, 1, d, i0:i0 + 4, 0:T0], 3, 16)
                bre = bc(Bb[:, 0, d, i0:i0 + 4, :], 2, T0)
                bim = bc(Bb[:, 1, d, i0:i0 + 4, :], 2, T0)
                dstv = VFv[:, part, :, 7::-1, :] if d == 0 else VBv[:, part, :, 8:16, :]
                dt_ = VF if d == 0 else VB
                if part == 0:
                    ew("dve", lambda e, pre=pre, bre=bre: e.tensor_tensor(out=vt[:], in0=pre, in1=bre, op=ALU.mult), [Pw, Bb], [vt])
                    ew("dve", lambda e, pim=pim, bim=bim, dstv=dstv: e.tensor_tensor(out=dstv, in0=pim, in1=bim, op=ALU.mult), [Pw, Bb], [dt_])
                    ew("dve", lambda e, dstv=dstv: e.tensor_tensor(out=dstv, in0=vt[:], in1=dstv, op=ALU.subtract), [vt, dt_], [dt_])
                else:
                    ew("dve", lambda e, pre=pre, bim=bim: e.tensor_tensor(out=vt[:], in0=pre, in1=bim, op=ALU.mult), [Pw, Bb], [vt])
                    ew("dve", lambda e, pim=pim, bre=bre, dstv=dstv: e.tensor_tensor(out=dstv, in0=pim, in1=bre, op=ALU.mult), [Pw, Bb], [dt_])
                    ew("dve", lambda e, dstv=dstv: e.tensor_tensor(out=dstv, in0=vt[:], in1=dstv, op=ALU.add), [vt, dt_], [dt_])
            ks = slice(1, T0 + 1) if d == 0 else slice(T0, 0, -1)
            pre = bc(Pw[:, 0, d, i0:i0 + 4, ks], 3, 16)
            pim = bc(Pw[:, 1, d, i0:i0 + 4, ks], 3, 16)
            cre = bc(Cn[:, 0, d, i0:i0 + 4, :], 2, T0)
            cimn = bc(Cn[:, 1, d, i0:i0 + 4, :], 2, T0)
            ew("dve", lambda e, pre=pre, cre=cre: e.tensor_tensor(out=vt[:], in0=pre, in1=cre, op=ALU.mult), [Pw, Cn], [vt])
            ew(dve2, lambda e, pim=pim, cimn=cimn: e.tensor_tensor(out=vu[:], in0=pim, in1=cimn, op=ALU.mult), [Pw, Cn], [vu])
            ew("dve", lambda e, d=d: e.tensor_tensor(out=Ro[:, d, 0], in0=vt[:], in1=vu[:], op=ALU.add), [vt, vu], [Ro])
            ew("dve", lambda e, pim=pim, cre=cre: e.tensor_tensor(out=vt[:], in0=pim, in1=cre, op=ALU.mult), [Pw, Cn], [vt])
            ew(dve2, lambda e, pre=pre, cimn=cimn: e.tensor_tensor(out=vu[:], in0=pre, in1=cimn, op=ALU.mult), [Pw, Cn], [vu])
            ew("dve", lambda e, d=d: e.tensor_tensor(out=Ro[:, d, 1], in0=vu[:], in1=vt[:], op=ALU.subtract), [vt, vu], [Ro])
        for d in range(2):
            for part in range(2):
                pt = psT.next()
                for il in range(4):
                    src = (VFv[:, part, il, 0:8, :] if d == 0 else VBv[:, part, il, 8:16, :]).rearrange("p b c -> p (b c)")
                    ew("pe", lambda e, pt=pt, il=il, src=src: e.transpose(out=pt[:, il, :], in_=src, identity=K.ident_f[:]), [VF, VB, K.ident_f], [pt])
                ew("act", lambda e, pt=pt, d=d, part=part: e.activation(out=TS[:, d, part], in_=pt[:], func=AF.Copy), [pt], [TS])
        for d in range(2):
            for g4 in range(2):
                pt = psT.next()
                for gg in range(4):
                    g_ = g4 * 4 + gg
                    il, jj = g_ // 2, g_ % 2
                    ps_ = slice(jj * 64, (jj + 1) * 64)
                    for t_ in range(T0):
                        for part in range(2):
                            VX = VFv if d == 0 else VBv
                            w0 = (7 - t_) if d == 0 else (8 - t_)
                            lhsT = VX[ps_, part, il, w0:w0 + 8, :].rearrange("p b c -> p (b c)")
                            rhs = Cn[ps_, part, d, i0 + il, :]
                            ew("pe", lambda e, pt=pt, gg=gg, t_=t_, part=part, lhsT=lhsT, rhs=rhs: e.matmul(
                                pt[:, gg, 16 * t_:16 * t_ + 16], lhsT=lhsT, rhs=rhs, start=(part == 0), stop=(part == 1), skip_group_check=True),
                               [VF, VB, Cn], [pt])
                ew("act", lambda e, pt=pt, d=d, g4=g4: e.activation(out=IT[:, d, g4 * 4:(g4 + 1) * 4], in_=pt[:], func=AF.Copy), [pt], [IT])
        for part in range(2):
            for d in range(2):
                ew("dve", lambda e, part=part, d=d: e.tensor_copy(out=A8l[:, part, d * 4:(d + 1) * 4], in_=PWB[:, part, d, i0:i0 + 4, 1]), [PWB], [A8l])
                ew("dve", lambda e, part=part, d=d: e.tensor_copy(out=ABl[:, part, d * 4:(d + 1) * 4], in_=PWB[:, part, d, i0:i0 + 4, BL]), [PWB], [ABl])
                ew("dve", lambda e, part=part, d=d: e.tensor_copy(out=PWl[:, part, d * 4:(d + 1) * 4, :], in_=PWB[:, part, d, i0:i0 + 4, 1:BL + 1]), [PWB], [PWl])
        for blk in range(NBLK):
            nn = min(128, NCH - blk * 128)
            Z = Zr.next()
            P.dma("sp" if blk % 2 == 0 else "act", Z[0:nn],
                  u_tok[blk * 1024: blk * 1024 + nn * 8, f0:f0 + 128].rearrange("(n s) f -> n s f", s=8), out_t=Z)
            pt = ptb.next()
            Zp = Zpr.next()
            ew("pool", lambda e, Z=Z, Zp=Zp, nn=nn: e.tensor_copy(out=Zp[0:nn], in_=Z[0:nn].rearrange("n s (g c) -> n g s c", c=16)), [Z], [Zp])
            for g_ in range(8):
                ew("pe", lambda e, pt=pt, g_=g_, Zp=Zp, nn=nn: e.transpose(out=pt[:, g_, 0:nn], in_=Zp[0:nn, g_].rearrange("n s c -> n (s c)"), identity=K.ident_b[0:nn, 0:nn]),
                   [Zp, K.ident_b], [pt])
            ew("act" if blk % 2 == 0 else "dve",
               (lambda e, pt=pt, blk=blk, nn=nn: e.activation(out=U[:, :, blk * 128: blk * 128 + nn], in_=pt[:, :, 0:nn], func=AF.Copy)) if blk % 2 == 0 else
               (lambda e, pt=pt, blk=blk, nn=nn: e.tensor_copy(out=U[:, :, blk * 128: blk * 128 + nn], in_=pt[:, :, 0:nn])), [pt], [U])
        for il in range(4):
            for d in range(2):
                lane = d * 4 + il
                lo, hi = (0, NF) if d == 0 else (CTXC, NCH)
                for part in range(2):
                    ps = psS.next()
                    for (a, b_) in col_blocks(0, NF):
                        for jj in range(2):
                            ps_ = slice(jj * 64, (jj + 1) * 64)
                            ew("pe", lambda e, ps=ps, ps_=ps_, a=a, b_=b_, d=d, part=part, il=il, jj=jj, lo=lo: e.matmul(
                                ps[ps_, a:b_], lhsT=TS[:, d, part, il, ps_], rhs=U[:, 2 * il + jj, lo + a: lo + b_], start=True, stop=True, skip_group_check=True),
                               [TS, U], [ps])
                    dst = Hs[:, part, lane].rearrange("p b l -> p (b l)")
                    if d == 0:
                        ew("act", lambda e, dst=dst, ps=ps: e.activation(out=dst, in_=ps[:, 0:NF], func=AF.Copy), [ps], [Hs])
                    else:
                        ew("dve", lambda e, dst=dst, ps=ps: e.tensor_copy(out=dst[:, ::-1], in_=ps[:, 0:NF]), [ps], [Hs])
        are = bc(A8l[:, 0, :], 2, NBK)
        aim = bc(A8l[:, 1, :], 2, NBK)
        for l in range(1, BL):
            cr, ci_ = Hs[:, 0, :, :, l], Hs[:, 1, :, :, l]
            pr, pi_ = Hs[:, 0, :, :, l - 1], Hs[:, 1, :, :, l - 1]
            ew("dve", lambda e, pr=pr: e.tensor_tensor(out=s1[0][:], in0=pr, in1=are, op=ALU.mult), [Hs, A8l], [s1[0]])
            ew("dve", lambda e, cr=cr: e.tensor_tensor(out=cr, in0=cr, in1=s1[0][:], op=ALU.add), [Hs, s1[0]], [Hs])
            ew("dve", lambda e, pi_=pi_: e.tensor_tensor(out=s1[0][:], in0=pi_, in1=aim, op=ALU.mult), [Hs, A8l], [s1[0]])
            ew("dve", lambda e, cr=cr: e.tensor_tensor(out=cr, in0=cr, in1=s1[0][:], op=ALU.subtract), [Hs, s1[0]], [Hs])
            ew("dve", lambda e, pi_=pi_: e.tensor_tensor(out=s1[1][:], in0=pi_, in1=are, op=ALU.mult), [Hs, A8l], [s1[1]])
            ew("dve", lambda e, ci_=ci_: e.tensor_tensor(out=ci_, in0=ci_, in1=s1[1][:], op=ALU.add), [Hs, s1[1]], [Hs])
            ew("dve", lambda e, pr=pr: e.tensor_tensor(out=s1[1][:], in0=pr, in1=aim, op=ALU.mult), [Hs, A8l], [s1[1]])
            ew("dve", lambda e, ci_=ci_: e.tensor_tensor(out=ci_, in0=ci_, in1=s1[1][:], op=ALU.add), [Hs, s1[1]], [Hs])
        for bk in range(1, NBK):
            cr, ci_ = Hs[:, 0, :, bk, BL - 1], Hs[:, 1, :, bk, BL - 1]
            pr, pi_ = Hs[:, 0, :, bk - 1, BL - 1], Hs[:, 1, :, bk - 1, BL - 1]
            t0_, t1_ = s1[0][:, :, 0], s1[1][:, :, 0]
            ew("dve", lambda e, pr=pr, t0_=t0_: e.tensor_tensor(out=t0_, in0=pr, in1=ABl[:, 0, :], op=ALU.mult), [Hs, ABl], [s1[0]])
            ew("dve", lambda e, cr=cr, t0_=t0_: e.tensor_tensor(out=cr, in0=cr, in1=t0_, op=ALU.add), [Hs, s1[0]], [Hs])
            ew("dve", lambda e, pi_=pi_, t0_=t0_: e.tensor_tensor(out=t0_, in0=pi_, in1=ABl[:, 1, :], op=ALU.mult), [Hs, ABl], [s1[0]])
            ew("dve", lambda e, cr=cr, t0_=t0_: e.tensor_tensor(out=cr, in0=cr, in1=t0_, op=ALU.subtract), [Hs, s1[0]], [Hs])
            ew("dve", lambda e, pi_=pi_, t1_=t1_: e.tensor_tensor(out=t1_, in0=pi_, in1=ABl[:, 0, :], op=ALU.mult), [Hs, ABl], [s1[1]])
            ew("dve", lambda e, ci_=ci_, t1_=t1_: e.tensor_tensor(out=ci_, in0=ci_, in1=t1_, op=ALU.add), [Hs, s1[1]], [Hs])
            ew("dve", lambda e, pr=pr, t1_=t1_: e.tensor_tensor(out=t1_, in0=pr, in1=ABl[:, 1, :], op=ALU.mult), [Hs, ABl], [s1[1]])
            ew("dve", lambda e, ci_=ci_, t1_=t1_: e.tensor_tensor(out=ci_, in0=ci_, in1=t1_, op=ALU.add), [Hs, s1[1]], [Hs])
        if BL > 1:
            for lh in range(2):
                ls = slice(lh * 4, (lh + 1) * 4)
                ere = bc(Hs[:, 0, ls, 0:NBK - 1, BL - 1], 3, BL - 1)
                eim = bc(Hs[:, 1, ls, 0:NBK - 1, BL - 1], 3, BL - 1)
                pwr = bc(PWl[:, 0, ls, 0:BL - 1], 2, NBK - 1)
                pwi = bc(PWl[:, 1, ls, 0:BL - 1], 2, NBK - 1)
                hre = Hs[:, 0, ls, 1:NBK, 0:BL - 1]
                him = Hs[:, 1, ls, 1:NBK, 0:BL - 1]
                for (x_, y_, dst, op_) in ((ere, pwr, hre, ALU.add), (eim, pwi, hre, ALU.subtract), (eim, pwr, him, ALU.add), (ere, pwi, him, ALU.add)):
                    ew("dve", lambda e, x_=x_, y_=y_: e.tensor_tensor(out=s3[:], in0=x_, in1=y_, op=ALU.mult), [Hs, PWl], [s3])
                    ew("dve", lambda e, dst=dst, op_=op_: e.tensor_tensor(out=dst, in0=dst, in1=s3[:], op=op_), [Hs, s3], [Hs])
        for part in range(2):
            hf = Hs[:, part, 0:4].rearrange("p a b l -> p a (b l)")
            hb = Hs[:, part, 4:8].rearrange("p a b l -> p a (b l)")
            ew("act", lambda e, part=part, hf=hf: e.activation(out=Hr[:, 0, part, :, 1:NF], in_=hf[:, :, 0:NF - 1], func=AF.Copy), [Hs], [Hr])
            ew(dve2, lambda e, part=part, hb=hb: e.tensor_copy(out=Hr[:, 1, part, :, CTXC:NCH - 1], in_=hb[:, :, NF - 2::-1]), [Hs], [Hr])
        for g_ in range(8):
            il, jj = g_ // 2, g_ % 2
            ps_ = slice(jj * 64, (jj + 1) * 64)
            py = psY.next()
            first = {}
            mm = []
            for d in range(2):
                lo, hi = (0, NF) if d == 0 else (CTXC, NCH)
                for (a, b_) in col_blocks(lo, hi):
                    mm.append((a, b_, IT[:, d, g_, :], U[:, g_, a:b_]))
                    mm.append((a, b_, Ro[ps_, d, 0, il].rearrange("p t c -> p (t c)"), Hr[ps_, d, 0, il, a:b_]))
                    mm.append((a, b_, Ro[ps_, d, 1, il].rearrange("p t c -> p (t c)"), Hr[ps_, d, 1, il, a:b_]))
            nlast = {}
            for idx, (a, b_, _, _) in enumerate(mm):
                nlast[a // 512] = idx
            for idx, (a, b_, lhsT, rhs) in enumerate(mm):
                bank = a // 512
                st = bank not in first
                first[bank] = True
                ew("pe", lambda e, py=py, a=a, b_=b_, lhsT=lhsT, rhs=rhs, st=st, sp=(nlast[bank] == idx): e.matmul(
                    py[:, a:b_], lhsT=lhsT, rhs=rhs, start=st, stop=sp, skip_group_check=True), [IT, U, Ro, Hr], [py])
            Y = Ysb.next()
            ew("dve", lambda e, Y=Y, py=py, g_=g_: e.scalar_tensor_tensor(out=Y[:, 0:NF], in0=U[:, g_, 0:NF], scalar=Dp[:, 8 * b + g_: 8 * b + g_ + 1],
                                                                          in1=py[:, 0:NF], op0=ALU.mult, op1=ALU.add), [U, Dp, py], [Y])
            ew("dve", lambda e, Y=Y, py=py: e.tensor_tensor(out=Y[:, 0:CTXC], in0=Y[:, 0:CTXC], in1=py[:, NF:NCH], op=ALU.add), [Y, py], [Y])
            ga, gb = gl_[0], gl_[1]
            ew(dve2, lambda e, Y=Y: e.tensor_tensor(out=ga[:, 0:NF], in0=Y[:, 0:NF], in1=Y[:, 0:NF], op=ALU.mult), [Y], [ga])
            ew("dve", lambda e: e.tensor_scalar(out=ga[:, 0:NF], in0=ga[:, 0:NF], scalar1=0.044715, scalar2=1.0, op0=ALU.mult, op1=ALU.add), [ga], [ga])
            ew(dve2, lambda e, Y=Y: e.tensor_tensor(out=ga[:, 0:NF], in0=ga[:, 0:NF], in1=Y[:, 0:NF], op=ALU.mult), [ga, Y], [ga])
            ew("act", lambda e: e.activation(out=gb[:, 0:NF], in_=ga[:, 0:NF], func=AF.Sigmoid, scale=1.5957691216057308), [ga], [gb])
            Yb = Ybr.next()
            ew("dve", lambda e, Y=Y, Yb=Yb: e.tensor_tensor(out=Yb[:, 0:NF], in0=gb[:, 0:NF], in1=Y[:, 0:NF], op=ALU.mult), [gb, Y], [Yb])
            pt = ptb.next()
            for blk in range(NFB):
                nn = min(128, NF - blk * 128)
                ew("pe", lambda e, pt=pt, blk=blk, nn=nn, Yb=Yb: e.transpose(out=pt[0:nn, blk, :], in_=Yb[:, blk * 128: blk * 128 + nn], identity=K.ident_b[:]),
                   [Yb, K.ident_b], [pt])
            nfull = NF // 128
            if nfull > 0:
                ew("act", lambda e, pt=pt, g_=g_: e.activation(out=Zo[:, 0:nfull, :, 16 * g_:16 * g_ + 16],
                                                               in_=pt[:, 0:nfull, :].rearrange("p k (t c) -> p k t c", c=16), func=AF.Copy), [pt], [Zo])
            if NF % 128:
                nn = NF % 128
                ew("act", lambda e, pt=pt, g_=g_, nn=nn: e.activation(out=Zo[0:nn, nfull, :, 16 * g_:16 * g_ + 16],
                                                                      in_=pt[0:nn, nfull, :].rearrange("p (t c) -> p t c", c=16), func=AF.Copy), [pt], [Zo])
        for blk in range(NFB):
            nn = min(128, NF - blk * 128)
            P.dma("sp" if blk % 2 == 0 else "act",
                  gy_tok[blk * 1024: blk * 1024 + nn * 8, f0:f0 + 128].rearrange("(n s) f -> n s f", s=8), Zo[0:nn, blk], in_t=Zo)
        P.emit()
    allt = ([Pw, PWB, Bn, Bb, Dp, vt, vu, Ro, TS, IT, U, Hs, Hr, A8l, ABl, PWl, s3, Zo] + cz + Zr.tiles + Zpr.tiles + s1
            + Ysb.tiles + gl_ + Ybr.tiles + ptb.tiles + psS.tiles + psY.tiles + psT.tiles)
    P.end_phase(allt)
    P.emit()
    P.release(m)


W_SHAPES = {
    "ada_w": [4, D, 6 * D], "ada_b": [4, 6 * D], "norm1": [4, D], "norm2": [4, D], "norm_f": [1, D],
    "s5_lam_re": [2, 2, G, PST], "s5_lam_im": [2, 2, G, PST], "s5_log_dt": [2, 2, G],
    "s5_b_re": [2, 2, G, PST, 16], "s5_b_im": [2, 2, G, PST, 16], "s5_c_re": [2, 2, G, 16, PST], "s5_c_im": [2, 2, G, 16, PST],
    "s5_d": [2, D], "s5_w_glu": [2, D, 2 * D], "ml_w_in": [2, D, ML_IN], "ml_b_gates": [2, 32], "ml_norm": [2, D],
    "ml_w_out": [2, D, D], "ffn_w_in": [4, D, 2 * FH], "ffn_w_out": [4, FH, D],
}


def build_program(S_LAT, layers=(0, 1, 2, 3), debug_x=False, depth_total=4):
    nc = bass.Bass("TRN2", target_bir_lowering=False)
    io = {}
    io["x"] = nc.dram_tensor("x", [S_LAT, D], F32, kind="ExternalInput").ap()
    io["ctx"] = nc.dram_tensor("ctx", [S_CTX, D], F32, kind="ExternalInput").ap()
    io["c"] = nc.dram_tensor("c", [1, D], F32, kind="ExternalInput").ap()
    io["c_ctx"] = nc.dram_tensor("c_ctx", [1, D], F32, kind="ExternalInput").ap()
    for k, shp in W_SHAPES.items():
        io[k] = nc.dram_tensor(k, shp, F32, kind="ExternalInput").ap()
    out = nc.dram_tensor("out", [S_LAT, D], F32, kind="ExternalOutput").ap()
    NTOK = S_CTX + S_LAT

    def scratch(name, shape, dt):
        return nc.dram_tensor(name, shape, dt, kind="Internal").ap()

    P = Prog(nc)
    K = Ctx()
    make_consts(P, K)
    phase_cond(P, K, io)
    xs = [scratch(f"xs{i}", [NTOK, D], F32) if not (debug_x and i == len(layers)) else
          nc.dram_tensor("xdbg", [NTOK, D], F32, kind="ExternalOutput").ap() for i in range(len(layers) + 1)]
    m0 = P.mark()
    cp = Ring([P.sbuf(f"cp{i}", [128, D], F32) for i in range(3)])
    for t in range(NTOK // 128):
        tl = cp.next()
        src = io["ctx"][t * 128:(t + 1) * 128, :] if t < S_CTX // 128 else io["x"][t * 128 - S_CTX:(t + 1) * 128 - S_CTX, :]
        P.dma("sp", tl[:], src, out_t=tl)
        P.dma("act", xs[0][t * 128:(t + 1) * 128, :], tl[:], in_t=tl)
    P.end_phase(cp.tiles)
    P.emit()
    P.release(m0)
    mods = [scratch(f"mod{i}", [2, 6 * D], F32) for i in layers]
    u_tok = scratch("u_tok", [2 * S_CTX + S_LAT, D], BF16)
    mix_tok = scratch("mix_tok", [NTOK, D], BF16)
    wgu_t = scratch("wgu_t", [NHC, 128, KD, 256], BF16)
    wo_t = scratch("wo_t", [NHC, 128, 4, 512], BF16)
    wglu_t = scratch("wglu_t", [16, 128, KD, 256], BF16)
    NT3_ = 2 * S_CTX + S_LAT
    wq_t = scratch("wq_t", [ML_H, 128, KD, 128], BF16)
    wk_t = scratch("wk_t", [ML_H, 128, KD, 128], BF16)
    wtok_t = scratch("wtok_t", [8, 128, KD, 512], BF16)
    wgate_t = scratch("wgate_t", [4, 128, KD, 8], BF16)
    wout_t = scratch("wout_t", [8, 128, KD, 256], BF16)
    qT_d = scratch("qT_d", [1024, NT3_], BF16)
    kT_d = scratch("kT_d", [1024, NT3_], BF16)
    v_d = scratch("v_d", [NT3_, D], BF16)
    o_d = scratch("o_d", [NT3_, D], BF16)
    gT_d = scratch("gT_d", [4, 8, NT3_], F32)
    hf_d = scratch("hf_d", [NTOK, D], F32)
    for li, layer in enumerate(layers):
        last = layer == depth_total - 1
        jl = layer // 2
        phase_ada(P, K, io, layer, mods[li])
        wi = io["ffn_w_in"][layer].rearrange("(k p) n -> p k n", p=128)
        jobs = []
        for c in range(NHC):
            jobs.append(([(lambda t: t[:, :, 0:128], wi[:, :, c * 128:(c + 1) * 128]),
                          (lambda t: t[:, :, 128:256], wi[:, :, FH + c * 128: FH + (c + 1) * 128])], wgu_t[c]))
        phase_precast(P, K, jobs, [128, KD, 256], "a")
        wo_src = io["ffn_w_out"][layer].rearrange("(w cc p) (nt n) -> nt w p cc n", cc=4, p=128, n=512)
        jobs = [([(lambda t: t[:], wo_src[nt, w])], wo_t[nt * 11 + w]) for nt in range(4) for w in range(11)]
        phase_precast(P, K, jobs, [128, 4, 512], "b")
        if layer % 2 == 0:
            wsrc = io["s5_w_glu"][jl].rearrange("(k p) (n c) -> n p k c", p=128, c=256)
            jobs = [([(lambda t: t[:], wsrc[n])], wglu_t[n]) for n in range(16)]
            phase_precast(P, K, jobs, [128, KD, 256], "c")
            phase_b_s5(P, K, io, layer, mods[li], xs[li], u_tok, S_LAT)
            phase_c_s5(P, K, io, jl, u_tok, mix_tok, S_LAT)
            phase_d(P, K, io, layer, mods[li], xs[li], xs[li + 1], mix_tok, [wglu_t[n] for n in range(16)],
                    [wgu_t[c] for c in range(NHC)], [wo_t[c] for c in range(NHC)], S_LAT, "s5", last, out)
        else:
            NT3 = 2 * S_CTX + S_LAT
            win = io["ml_w_in"][jl].rearrange("(k p) n -> p k n", p=128)
            jobs = [([(lambda t: t[:], win[:, :, h * 128:(h + 1) * 128])], wq_t[h]) for h in range(ML_H)]
            jobs += [([(lambda t: t[:], win[:, :, 1024 + h * 128:1024 + (h + 1) * 128])], wk_t[h]) for h in range(ML_H)]
            phase_precast(P, K, jobs, [128, KD, 128], "d")
            jobs = [([(lambda t: t[:], win[:, :, 2048 + j * 512:2048 + (j + 1) * 512])], wtok_t[j]) for j in range(8)]
            phase_precast(P, K, jobs, [128, KD, 512], "e")
            jobs = [([(lambda t: t[:], win[:, :, 6144 + ty * 8:6144 + (ty + 1) * 8])], wgate_t[ty]) for ty in range(4)]
            phase_precast(P, K, jobs, [128, KD, 8], "f")
            wsrc = io["ml_w_out"][jl].rearrange("(k p) (n c) -> n p k c", p=128, c=256)
            jobs = [([(lambda t: t[:], wsrc[n])], wout_t[n]) for n in range(8)]
            phase_precast(P, K, jobs, [128, KD, 256], "g")
            phase_b_ml(P, K, io, layer, mods[li], xs[li], S_LAT, [wq_t[h] for h in range(ML_H)], [wk_t[h] for h in range(ML_H)],
                       [wtok_t[j] for j in range(8)], [wgate_t[ty] for ty in range(4)], qT_d, kT_d, v_d, o_d, gT_d)
            phase_e_ml(P, K, io, jl, S_LAT, qT_d, kT_d, v_d, o_d, gT_d, hf_d, mix_tok)
            phase_d(P, K, io, layer, mods[li], xs[li], xs[li + 1], mix_tok, [wout_t[n] for n in range(8)],
                    [wgu_t[c] for c in range(NHC)], [wo_t[c] for c in range(NHC)], S_LAT, "ml", last, out, tok_rows=True)
    P.barrier_all()
    P.emit()
    print("instructions:", P.n_instr)
    return nc


def phase_b_ml(P, K, io, layer, mod_d, xs, S_LAT, wq_t, wk_t, wtok_t, wgate_t, qT_d, kT_d, v_d, o_d, gT_d):
    m = P.mark()
    ROWS = S_LAT // 64
    S = S_CTX + S_LAT
    xr = Ring([P.sbuf(f"e_x{i}", [128, D], F32) for i in range(2)])
    bfr = Ring([P.sbuf(f"e_bf{i}", [128, D], BF16) for i in range(2)])
    t1 = P.sbuf("e_t1", [128, D], F32)
    uT = P.sbuf("e_uT", [128, KD, 512], BF16)
    wqr = Ring([P.sbuf(f"e_wq{i}", [128, KD, 128], BF16) for i in range(3)])
    wtr = Ring([P.sbuf(f"e_wt{i}", [128, KD, 512], BF16) for i in range(2)])
    wg = P.sbuf("e_wg", [128, 4, KD, 8], BF16)
    obr = Ring([P.sbuf(f"e_ob{i}", [128, 512], BF16) for i in range(4)])
    ogr = Ring([P.sbuf(f"e_og{i}", [8, 512], F32) for i in range(2)])
    statr = Ring([P.sbuf(f"e_st{i}", [128, 4], F32) for i in range(4)])
    ptr = Ring([P.psum(f"e_pt{i}", [128, 8, 128], BF16) for i in range(2)])
    mmr = Ring([P.psum(f"e_mm{i}", [128, 512], F32) for i in range(5)])
    pg = P.psum("e_pg", [8, 512], F32)
    scr = dict(pt=ptr)
    tiles = xr.tiles + bfr.tiles + [t1, uT, wg, pg] + wqr.tiles + wtr.tiles + obr.tiles + ogr.tiles + statr.tiles + ptr.tiles + mmr.tiles
    for ty in range(4):
        P.dma("sp", wg[:, ty], wgate_t[ty], out_t=wg)
    qi = [0]

    def q():
        qi[0] += 1
        return "sp" if qi[0] % 2 == 0 else "act"

    xs_lat = xs[S_CTX:S_CTX + S_LAT, :]
    for row in (1, 0):
        mm_mark = P.mark()
        md = load_mod_tiles(P, K, io, layer, mod_d, row, [("sh", 0, "raw"), ("A", 1, "A")], None, "norm1")
        ntok = S_CTX if row == 1 else S_LAT
        for t0 in range(0, ntok, 512):
            nsub = min(4, (ntok - t0) // 128)
            TT = nsub * 128
            dsts = [t0, S + t0] if row == 1 else [S_CTX + t0]
            for s in range(nsub):
                xt = xr.next()
                if row == 1:
                    P.dma(q(), xt[:], xs[t0 + s * 128: t0 + (s + 1) * 128, :], out_t=xt)
                else:
                    lr = lat_rows(xs_lat, t0 + s * 128, S_LAT)
                    for wi in range(128 // ROWS):
                        P.dma(q(), xt[wi * ROWS:(wi + 1) * ROWS, :], lr[wi], out_t=xt)
                ssq = statr.next()
                junk = bfr.next()
                P.op("act", lambda e, xt=xt, ssq=ssq, junk=junk: e.activation(out=junk[:], in_=xt[:], func=AF.Square, accum_out=ssq[:, 0:1]), reads=[xt], writes=[junk, ssq])
                P.op("act", lambda e, ssq=ssq: e.activation(out=ssq[:, 1:2], in_=ssq[:, 0:1], func=AF.Sqrt, bias=K.eps_t[:, 0:1], scale=1.0 / D), reads=[ssq, K.eps_t], writes=[ssq])
                P.op("dve", lambda e, ssq=ssq: e.reciprocal(out=ssq[:, 2:3], in_=ssq[:, 1:2]), reads=[ssq], writes=[ssq])
                P.op("dve", lambda e, xt=xt, ssq=ssq: e.scalar_tensor_tensor(out=t1[:], in0=xt[:], scalar=ssq[:, 2:3], in1=md["A"][:], op0=ALU.mult, op1=ALU.mult),
                     reads=[xt, ssq, md["A"]], writes=[t1])
                ub = bfr.next()
                P.op("pool", lambda e, ub=ub: e.tensor_tensor(out=ub[:], in0=t1[:], in1=md["sh"][:], op=ALU.add), reads=[t1, md["sh"]], writes=[ub])
                transpose_to(P, K, ub, uT, s * 128, scr)
            for (wt_, dd) in ((wq_t, qT_d), (wk_t, kT_d)):
                for h in range(ML_H):
                    wq = wqr.next()
                    P.dma(q(), wq[:], wt_[h], out_t=wq)
                    ps = mmr.next()
                    for k in range(KD):
                        P.op("pe", lambda e, ps=ps, wq=wq, k=k: e.matmul(ps[:, :TT], lhsT=wq[:, k, :], rhs=uT[:, k, :TT], start=(k == 0), stop=(k == KD - 1)),
                             reads=[wq, uT], writes=[ps])
                    ob = obr.next()
                    P.op("act", lambda e, ob=ob, ps=ps: e.activation(out=ob[:, :TT], in_=ps[:, :TT], func=AF.Copy), reads=[ps], writes=[ob])
                    for p0 in dsts:
                        P.dma(q(), dd[h * 128:(h + 1) * 128, p0:p0 + TT], ob[:, :TT], in_t=ob)
            for j in range(8):
                wt = wtr.next()
                P.dma(q(), wt[:], wtok_t[j], out_t=wt)
                dd, c0 = (v_d, j * 512) if j < 4 else (o_d, (j - 4) * 512)
                for s in range(nsub):
                    ps = mmr.next()
                    for k in range(KD):
                        P.op("pe", lambda e, ps=ps, wt=wt, k=k, s=s: e.matmul(ps[:], lhsT=uT[:, k, s * 128:(s + 1) * 128], rhs=wt[:, k, :], start=(k == 0), stop=(k == KD - 1)),
                             reads=[wt, uT], writes=[ps])
                    ob = obr.next()
                    if (j + s) % 2 == 0:
                        P.op("act", lambda e, ob=ob, ps=ps: e.activation(out=ob[:], in_=ps[:], func=AF.Copy), reads=[ps], writes=[ob])
                    else:
                        P.op("dve", lambda e, ob=ob, ps=ps: e.tensor_copy(out=ob[:], in_=ps[:]), reads=[ps], writes=[ob])
                    for p0 in dsts:
                        P.dma(q(), dd[p0 + s * 128: p0 + (s + 1) * 128, c0:c0 + 512], ob[:], in_t=ob)
            for ty in range(4):
                for k in range(KD):
                    P.op("pe", lambda e, k=k, ty=ty: e.matmul(pg[:, :TT], lhsT=wg[:, ty, k, :], rhs=uT[:, k, :TT], start=(k == 0), stop=(k == KD - 1)),
                         reads=[wg, uT], writes=[pg])
                og = ogr.next()
                P.op("dve", lambda e, og=og: e.tensor_copy(out=og[:, :TT], in_=pg[:, :TT]), reads=[pg], writes=[og])
                for p0 in dsts:
                    P.dma(q(), gT_d[ty, :, p0:p0 + TT], og[:, :TT], in_t=og)
        P.end_phase(list(md.values()))
        P.emit()
        P.release(mm_mark)
    P.end_phase(tiles)
    P.emit()
    P.release(m)


def phase_e_ml(P, K, io, jl, S_LAT, qT_d, kT_d, v_d, o_d, gT_d, hf_d, mix_tok):
    S = S_CTX + S_LAT
    NTOK3 = S + S_CTX
    NC = S // 64
    NC3 = NTOK3 // 64
    CC = S_CTX // 64
    SCALE = float(128 ** -0.5)
    m = P.mark()
    omT = P.sbuf("f_omT", [64, NC, 40], F32)
    clT = P.sbuf("f_clT", [64, NC, 40], F32)
    lamR = P.sbuf("f_lamR", [128, 16, NC], F32)
    lamSR = P.sbuf("f_lamSR", [128, 16, NC], F32)
    m1 = P.mark()
    X = [P.sbuf(f"f_X{i}", [40, S], F32) for i in range(5)]
    Mr = P.sbuf("f_Mr", [40, NC], F32)
    lam = P.sbuf("f_lam", [40, NC], F32)
    bia = P.sbuf("f_bias", [40, 4], F32)
    one = P.sbuf("f_one", [40, 2], F32)
    sel = P.sbuf("f_sel", [40, 16, 128], F32)
    pto = Ring([P.psum(f"f_pto{i}", [64, 12, 40], F32) for i in range(2)])
    pl = P.psum("f_pl", [128, NC], F32)
    ew = lambda eng, fn, r, w: P.op(eng, fn, reads=r, writes=w)
    for t in X:
        ew("pool", lambda e, t=t: e.memset(t[:], 0.0), [], [t])
    ew("pool", lambda e: e.memset(bia[:], 0.0), [], [bia])
    ew("pool", lambda e: e.memset(one[:, 0:1], 1.0), [], [one])
    ew("pool", lambda e: e.memset(one[:, 1:2], 0.0), [], [one])
    bg = io["ml_b_gates"][jl:jl + 1, :]
    for (col, lo, p0) in ((0, 0, 0), (1, 8, 0), (0, 16, 32), (1, 24, 32)):
        P.dma("sp", bia[p0:p0 + 8, col:col + 1], bg[:, lo:lo + 8].rearrange("o h -> h o"), out_t=bia, allow_slow_non_contiguous=True)
    P.dma("sp", X[3][0:8, :], gT_d[0, :, 0:S], out_t=X[3])
    P.dma("act", X[3][32:40, :], gT_d[2, :, S_CTX:NTOK3], out_t=X[3])
    P.dma("sp", X[4][0:8, :], gT_d[1, :, 0:S], out_t=X[4])
    P.dma("act", X[4][32:40, :], gT_d[3, :, S_CTX:NTOK3], out_t=X[4])
    ew("dve", lambda e: e.tensor_scalar(out=bia[:, 2:4], in0=bia[:, 0:2], scalar1=1.0 / GATE_CAP, scalar2=None, op0=ALU.mult), [bia], [bia])
    for (src, dst) in ((X[3], X[0]), (X[4], X[1])):
        ew("dve", lambda e, src=src, dst=dst: e.tensor_copy(out=dst[0:8, :], in_=src[0:8, :]), [src], [dst])
        ew("pool", lambda e, src=src, dst=dst: e.tensor_copy(out=dst[32:40, :], in_=src[32:40, ::-1]), [src], [dst])
    for (t, c) in ((X[0], 2), (X[1], 3)):
        ew("act", lambda e, t=t, c=c: e.activation(out=t[:], in_=t[:], func=AF.Tanh, bias=bia[:, c:c + 1], scale=1.0 / GATE_CAP), [t, bia], [t])
        ew("dve", lambda e, t=t: e.tensor_scalar(out=t[:], in0=t[:], scalar1=GATE_CAP, scalar2=None, op0=ALU.mult), [t], [t])
    ew("act", lambda e: e.activation(out=X[3][:], in_=X[1][:], func=AF.Exp, scale=-1.0), [X[1]], [X[3]])
    ew("dve", lambda e: e.tensor_scalar(out=X[4][:], in0=X[3][:], scalar1=2.0, scalar2=None, op0=ALU.add), [X[3]], [X[4]])
    ew("dve", lambda e: e.reciprocal(out=X[4][:], in_=X[4][:]), [X[4]], [X[4]])
    ew("dve", lambda e: e.tensor_tensor(out=X[4][:], in0=X[4][:], in1=X[3][:], op=ALU.mult), [X[4], X[3]], [X[4]])
    ew("pool", lambda e: e.tensor_tensor(out=X[2][:], in0=X[4][:], in1=X[4][:], op=ALU.mult), [X[4]], [X[2]])
    ew("dve", lambda e: e.tensor_scalar(out=X[3][:], in0=X[2][:], scalar1=1.0 / 15, scalar2=1.0 / 13, op0=ALU.mult, op1=ALU.add), [X[2]], [X[3]])
    for cst in (1.0 / 11, 1.0 / 9, 1.0 / 7, 1.0 / 5, 1.0 / 3, 1.0):
        ew("dve", lambda e: e.tensor_tensor(out=X[3][:], in0=X[3][:], in1=X[2][:], op=ALU.mult), [X[3], X[2]], [X[3]])
        ew("dve", lambda e, cst=cst: e.tensor_scalar(out=X[3][:], in0=X[3][:], scalar1=float(cst), scalar2=None, op0=ALU.add), [X[3]], [X[3]])
    ew("dve", lambda e: e.scalar_tensor_tensor(out=X[1][:], in0=X[4][:], scalar=-2.0, in1=X[3][:], op0=ALU.mult, op1=ALU.mult), [X[4], X[3]], [X[1]])
    ew("dve", lambda e: e.tensor_tensor_scan(out=X[2][:], data0=one[:, 0:1].broadcast_to([40, S]), data1=X[1][:], initial=0.0, op0=ALU.mult, op1=ALU.add),
       [one, X[1]], [X[2]])
    ew("dve", lambda e: e.tensor_tensor(out=X[0][:], in0=X[0][:], in1=X[2][:], op=ALU.subtract), [X[0], X[2]], [X[0]])
    ew("dve", lambda e: e.tensor_tensor_scan(out=X[3][:], data0=one[:, 1:2].broadcast_to([40, S]), data1=X[0][:], initial=0.0, op0=ALU.add, op1=ALU.max),
       [one, X[0]], [X[3]])
    ew("dve", lambda e: e.tensor_copy(out=Mr[:], in_=X[3][:, 63::64]), [X[3]], [Mr])
    mrb = bc(Mr[:], 2, 64)
    ew("dve", lambda e: e.tensor_tensor(out=X[0][:].rearrange("p (n t) -> p n t", t=64), in0=X[0][:].rearrange("p (n t) -> p n t", t=64), in1=mrb, op=ALU.subtract), [X[0], Mr], [X[0]])
    ew("act", lambda e: e.activation(out=X[0][:], in_=X[0][:], func=AF.Exp), [X[0]], [X[0]])
    ew("dve", lambda e: e.tensor_tensor(out=X[2][:].rearrange("p (n t) -> p n t", t=64), in0=X[2][:].rearrange("p (n t) -> p n t", t=64), in1=mrb, op=ALU.add), [X[2], Mr], [X[2]])
    ew("act", lambda e: e.activation(out=X[2][:], in_=X[2][:], func=AF.Exp, scale=-1.0), [X[2]], [X[2]])
    ew("dve", lambda e: e.tensor_scalar(out=lam[:, 0:1], in0=Mr[:, 0:1], scalar1=-1.0, scalar2=None, op0=ALU.mult), [Mr], [lam])
    ew("dve", lambda e: e.tensor_tensor(out=lam[:, 1:NC], in0=Mr[:, 0:NC - 1], in1=Mr[:, 1:NC], op=ALU.subtract), [Mr], [lam])
    ew("act", lambda e: e.activation(out=lam[:], in_=lam[:], func=AF.Exp), [lam], [lam])
    for (src, dst) in ((X[0], X[3]), (X[2], X[4])):
        ew("dve", lambda e, src=src, dst=dst: e.tensor_copy(out=dst[0:8, :], in_=src[0:8, :]), [src], [dst])
        ew("pool", lambda e, src=src, dst=dst: e.tensor_copy(out=dst[32:40, :], in_=src[32:40, ::-1]), [src], [dst])
    for (src, dstT) in ((X[3], omT), (X[4], clT)):
        for k0 in range(0, NC, 12):
            nk = min(12, NC - k0)
            pt = pto.next()
            for kk in range(nk):
                k = k0 + kk
                ew("pe", lambda e, pt=pt, kk=kk, k=k, src=src: e.transpose(out=pt[:, kk, :], in_=src[:, 64 * k:64 * k + 64], identity=K.ident_f[0:40, 0:40]),
                   [src, K.ident_f], [pt])
            ew("act", lambda e, pt=pt, k0=k0, nk=nk, dstT=dstT: e.activation(out=dstT[:, k0:k0 + nk, :], in_=pt[:, 0:nk, :], func=AF.Copy), [pt], [dstT])
    for r in range(16):
        row = (r // 8) * 32 + (r % 8)
        ew("dve", lambda e, r=r, row=row: e.tensor_copy(out=sel[:, r, :], in_=K.ident_f[0:40, row:row + 1].broadcast_to([40, 128])), [K.ident_f], [sel])
    for r in range(16):
        ew("pe", lambda e, r=r: e.matmul(pl[:], lhsT=sel[:, r, :], rhs=lam[:], start=True, stop=True), [sel, lam], [pl])
        ew("act", lambda e, r=r: e.activation(out=lamR[:, r, :], in_=pl[:], func=AF.Copy), [pl], [lamR])
    ew("dve", lambda e: e.tensor_scalar(out=lamSR[:], in0=lamR[:], scalar1=SCALE, scalar2=None, op0=ALU.mult), [lamR], [lamSR])
    P.end_phase(X + [Mr, lam, bia, one, sel, pl] + pto.tiles)
    P.emit()
    P.release(m1)

    qT = P.sbuf("f_qT", [128, NTOK3], BF16)
    kT = P.sbuf("f_kT", [128, NTOK3], BF16)
    vv = P.sbuf("f_vv", [64, NC3, 257], BF16)
    Cf = P.sbuf("f_Cf", [128, 257], F32)
    Csr = Ring([P.sbuf(f"f_Cs{i}", [128, 257], BF16) for i in range(2)])
    Spr = Ring([P.sbuf(f"f_Sp{i}", [64, 64], BF16) for i in range(3)])
    kwr = Ring([P.sbuf(f"f_kw{i}", [64, 128], BF16) for i in range(3)])
    mask = [P.sbuf(f"f_mask{i}", [64, 64], F32) for i in range(2)]
    dnr = Ring([P.sbuf(f"f_dn{i}", [64, 6], F32) for i in range(4)])
    hfr = Ring([P.sbuf(f"f_hf{i}", [64, 256], F32) for i in range(3)])
    hsr = Ring([P.sbuf(f"f_hs{i}", [64, 256], F32) for i in range(3)])
    oor = Ring([P.sbuf(f"f_oo{i}", [64, 256], BF16) for i in range(3)])
    sgr = Ring([P.sbuf(f"f_sg{i}", [64, 256], F32) for i in range(2)])
    hor = Ring([P.sbuf(f"f_ho{i}", [64, 256], BF16) for i in range(3)])
    junk = P.sbuf("f_junk", [64, 256], BF16)
    nw = P.sbuf("f_nw", [64, D], F32)
    eps64 = P.sbuf("f_eps", [64, 1], F32)
    pS = Ring([P.psum(f"f_pS{i}", [64, 64], F32) for i in range(2)])
    pK = Ring([P.psum(f"f_pK{i}", [64, 128], BF16) for i in range(2)])
    pH = Ring([P.psum(f"f_pH{i}", [64, 257], F32) for i in range(2)])
    pC = Ring([P.psum(f"f_pC{i}", [128, 257], F32) for i in range(2)])
    tiles = ([qT, kT, vv, Cf, junk, nw, eps64, omT, clT, lamR, lamSR] + Csr.tiles + Spr.tiles + kwr.tiles + mask + dnr.tiles + hfr.tiles + hsr.tiles + oor.tiles
             + sgr.tiles + hor.tiles + pS.tiles + pK.tiles + pH.tiles + pC.tiles)
    ew("pool", lambda e: e.memset(eps64[:], EPS), [], [eps64])
    for i, (cm, pat) in enumerate(((-1, 1), (1, -1))):
        ew("pool", lambda e, i=i: e.memset(mask[i][:], SCALE), [], [mask[i]])
        ew("pool", lambda e, i=i, cm=cm, pat=pat: e.affine_select(out=mask[i][:], in_=mask[i][:], compare_op=ALU.is_ge, fill=0.0, base=0,
                                                                  pattern=[[pat, 64]], channel_multiplier=cm), [mask[i]], [mask[i]])
    ew("pool", lambda e: e.memset(vv[:, :, 256:257], 1.0), [], [vv])
    load_rep_n = lambda: P.dma("sp", nw[:], io["ml_norm"][jl:jl + 1, :].partition_broadcast(64), out_t=nw)
    load_rep_n()

    for h in range(ML_H):
        P.dma("sp", qT[:], qT_d[h * 128:(h + 1) * 128, :], out_t=qT)
        P.dma("act", kT[:], kT_d[h * 128:(h + 1) * 128, :], out_t=kT)
        half = NC3 // 2
        P.dma("sp", vv[:, 0:half, 0:256], v_d[0:half * 64, h * 256:(h + 1) * 256].rearrange("(n t) e -> t n e", t=64), out_t=vv)
        P.dma("act", vv[:, half:NC3, 0:256], v_d[half * 64:NTOK3, h * 256:(h + 1) * 256].rearrange("(n t) e -> t n e", t=64), out_t=vv)
        for d in range(2):
            r = d * 8 + h
            ew("dve", lambda e: e.memset(Cf[:], 0.0), [], [Cf])
            Cs = Csr.next()
            ew("pool", lambda e, Cs=Cs: e.memset(Cs[:], 0.0), [], [Cs])

            def front(mi):
                c = mi if d == 0 else NC3 - 1 - mi
                k = c if d == 0 else c - CC
                cols = slice(64 * c, 64 * c + 64)
                ps = pS.next()
                ew("pe", lambda e, ps=ps, cols=cols: e.matmul(ps[:], lhsT=kT[:, cols], rhs=qT[:, cols], start=True, stop=True), [kT, qT], [ps])
                pk = pK.next()
                ew("pe", lambda e, pk=pk, cols=cols: e.transpose(out=pk[:], in_=kT[:, cols], identity=K.ident_b[:]), [kT, K.ident_b], [pk])
                Sp = Spr.next()
                om = omT[:, k, 32 * d + h: 32 * d + h + 1]
                ew("dve", lambda e, Sp=Sp, ps=ps, om=om: e.scalar_tensor_tensor(out=Sp[:], in0=ps[:], scalar=om, in1=mask[d][:], op0=ALU.mult, op1=ALU.mult),
                   [ps, omT, mask[d]], [Sp])
                kw = kwr.next()
                ew("act", lambda e, kw=kw, pk=pk, om=om: e.activation(out=kw[:], in_=pk[:], func=AF.Copy, scale=om), [pk, omT], [kw])
                return (c, k, cols, Sp, kw)

            nxt = front(0)
            for mi in range(NC):
                c, k, cols, Sp, kw = nxt
                if mi + 1 < NC:
                    nxt = front(mi + 1)
                tc = c if c < NC else c - NC
                ph = pH.next()
                ew("pe", lambda e, ph=ph, Sp=Sp, c=c: e.matmul(ph[:], lhsT=Sp[:], rhs=vv[:, c, :], start=True, stop=False), [Sp, vv], [ph])
                ew("pe", lambda e, ph=ph, Cs=Cs, cols=cols: e.matmul(ph[:], lhsT=qT[:, cols], rhs=Cs[:], start=False, stop=True), [qT, Cs], [ph])
                pc = pC.next()
                ew("pe", lambda e, pc=pc, kw=kw, c=c: e.matmul(pc[:], lhsT=kw[:], rhs=vv[:, c, :], start=True, stop=True), [kw, vv], [pc])
                ew("dve", lambda e, pc=pc, mi=mi, r=r: e.scalar_tensor_tensor(out=Cf[:], in0=Cf[:], scalar=lamR[:, r, mi:mi + 1], in1=pc[:], op0=ALU.mult, op1=ALU.add),
                   [Cf, lamR, pc], [Cf])
                if mi + 1 < NC:
                    Cs = Csr.next()
                    ew("act", lambda e, Cs=Cs, mi=mi, r=r: e.activation(out=Cs[:], in_=Cf[:], func=AF.Copy, scale=lamSR[:, r, mi + 1:mi + 2]), [Cf, lamSR], [Cs])
                dn = dnr.next()
                ew("act", lambda e, dn=dn, ph=ph: e.activation(out=dn[:, 4:5], in_=ph[:, 256:257], func=AF.Copy), [ph], [dn])
                ew("dve", lambda e, dn=dn: e.scalar_tensor_tensor(out=dn[:, 0:1], in0=dn[:, 4:5], scalar=-1.0, in1=dn[:, 4:5], op0=ALU.mult, op1=ALU.max),
                   [dn], [dn])
                ew("dve", lambda e, dn=dn, k=k: e.tensor_tensor(out=dn[:, 1:2], in0=dn[:, 0:1], in1=clT[:, k, 32 * d + h: 32 * d + h + 1], op=ALU.max), [dn, clT], [dn])
                ew("dve", lambda e, dn=dn: e.reciprocal(out=dn[:, 2:3], in_=dn[:, 1:2]), [dn], [dn])
                rows = slice(64 * tc, 64 * tc + 64)
                hcols = slice(h * 256, (h + 1) * 256)
                if d == 0:
                    hf = hfr.next()
                    ew("act", lambda e, hf=hf, ph=ph, dn=dn: e.activation(out=hf[:], in_=ph[:, 0:256], func=AF.Copy, scale=dn[:, 2:3]), [ph, dn], [hf])
                    P.dma("sp", hf_d[rows, hcols], hf[:], in_t=hf)
                else:
                    hf = hfr.next()
                    P.dma("sp", hf[:], hf_d[rows, hcols], out_t=hf)
                    oo = oor.next()
                    P.dma("act", oo[:], o_d[rows, hcols], out_t=oo)
                    hs = hsr.next()
                    ew("dve", lambda e, hs=hs, ph=ph, dn=dn, hf=hf: e.scalar_tensor_tensor(out=hs[:], in0=ph[:, 0:256], scalar=dn[:, 2:3], in1=hf[:], op0=ALU.mult, op1=ALU.add),
                       [ph, dn, hf], [hs])
                    ew("act", lambda e, hs=hs, dn=dn: e.activation(out=junk[:], in_=hs[:], func=AF.Square, accum_out=dn[:, 3:4]), [hs], [junk, dn])
                    ew("act", lambda e, dn=dn: e.activation(out=dn[:, 3:4], in_=dn[:, 3:4], func=AF.Sqrt, bias=eps64[:, 0:1], scale=1.0 / 256), [dn, eps64], [dn])
                    ew("dve", lambda e, dn=dn: e.reciprocal(out=dn[:, 3:4], in_=dn[:, 3:4]), [dn], [dn])
                    sg = sgr.next()
                    ew("act", lambda e, sg=sg, oo=oo: e.activation(out=sg[:], in_=oo[:], func=AF.Sigmoid), [oo], [sg])
                    ew("dve", lambda e, hs=hs, dn=dn, hcols=hcols: e.scalar_tensor_tensor(out=hs[:], in0=hs[:], scalar=dn[:, 3:4], in1=nw[:, hcols], op0=ALU.mult, op1=ALU.mult),
                       [hs, dn, nw], [hs])
                    ho = hor.next()
                    ew("pool", lambda e, ho=ho, hs=hs, sg=sg: e.tensor_tensor(out=ho[:], in0=hs[:], in1=sg[:], op=ALU.mult), [hs, sg], [ho])
                    P.dma("act", mix_tok[rows, hcols], ho[:], in_t=ho)
            if d == 0:
                P.barrier_all()
            P.emit()
    P.end_phase(tiles)
    P.emit()
    P.release(m)


_NC_CACHE = {}


def kernel(**inputs):
    S_LAT = inputs["x"].shape[1]
    B = inputs["x"].shape[0]
    if S_LAT not in _NC_CACHE:
        _NC_CACHE[S_LAT] = build_program(S_LAT)
    nc = _NC_CACHE[S_LAT]
    shared = {}
    for k in W_SHAPES:
        a = np.ascontiguousarray(np.asarray(inputs[k], dtype=np.float32))
        if k == "norm_f":
            a = a.reshape(1, D)
        shared[k] = a
    shared["c_ctx"] = np.ascontiguousarray(np.asarray(inputs["c_ctx"], dtype=np.float32)).reshape(1, D)
    n_cores = 8
    in_maps = []
    for core in range(n_cores):
        b = core % B
        mp = dict(shared)
        mp["x"] = np.ascontiguousarray(np.asarray(inputs["x"][b], dtype=np.float32))
        mp["ctx"] = np.ascontiguousarray(np.asarray(inputs["ctx"][b], dtype=np.float32))
        mp["c"] = np.ascontiguousarray(np.asarray(inputs["c"][b:b + 1], dtype=np.float32))
        in_maps.append(mp)
    res = run_bass_kernel_spmd(nc, in_maps, core_ids=list(range(n_cores)))
    out = np.stack([np.asarray(res.results[b]["out"]) for b in range(B)], axis=0)
    return out.astype(np.float32)
```

```python
import numpy as np
import concourse.bass as bass
import concourse.mybir as mybir
from concourse.bass_utils import run_bass_kernel_spmd

F32 = mybir.dt.float32
BF16 = mybir.dt.bfloat16
AF = mybir.ActivationFunctionType
ALU = mybir.AluOpType
AX = mybir.AxisListType

ENGS = ("pe", "act", "dve", "pool", "sp")

D = 2048
KD = D // 128
FH = 5632
NHC = FH // 128
S_CTX = 256
EPS = 1e-6
T0 = 8
G = 128
PST = 64
ML_H = 8
ML_IN = 6176
GATE_CAP = 15.0


class T:
    def __init__(self, h, name):
        self.h = h
        self.name = name
        self.last_w = None
        self.readers = []
        self.ld_sem = None
        self.ld_cnt = 0
        self.st_sem = None
        self.st_cnt = 0

    def __getitem__(self, k):
        return self.h[k]


class Prog:
    def __init__(self, nc, same_engine_sync=True):
        self.nc = nc
        self.ops = {e: [] for e in ENGS}
        self.cnt = {e: 0 for e in ENGS}
        self.waited = {}
        self.esem = {}
        self.same_engine_sync = same_engine_sync
        self.all_st = []
        self._stack = []
        self.dma_sems = []
        self.dma_sem_i = 0
        self.n_instr = 0
        for e in ("pe", "act", "dve", "pool"):
            self.esem[e] = nc.alloc_semaphore("prog_" + e)
        self.free_sems = [nc.alloc_semaphore(f"dsem{i}") for i in range(90)]
        self.sem_users = {}

    def sbuf(self, name, shape, dt):
        self.uid = getattr(self, "uid", 0) + 1
        name = f"{name}_{self.uid}"
        cm = self.nc.sbuf_tensor(name, list(shape), dt)
        h = cm.__enter__()
        self._stack.append(cm)
        return T(h, name)

    def psum(self, name, shape, dt=F32):
        self.uid = getattr(self, "uid", 0) + 1
        name = f"{name}_{self.uid}"
        cm = self.nc.psum_tensor(name, list(shape), dt)
        h = cm.__enter__()
        self._stack.append(cm)
        return T(h, name)

    def mark(self):
        return len(self._stack)

    def release(self, mark):
        while len(self._stack) > mark:
            cm = self._stack.pop()
            cm.__exit__(None, None, None)

    def _get_sem(self, t, kind):
        if not self.free_sems:
            raise RuntimeError("out of DMA semaphores")
        return self.free_sems.pop()

    def _need(self, eng, dep, waits):
        if dep is None:
            return
        if dep[0] == "eng":
            _, e2, seq = dep
            if e2 == eng and (eng == "pe" or not self.same_engine_sync):
                return
            key = (eng, "eng", e2)
            if self.waited.get(key, 0) >= seq:
                return
            self.waited[key] = seq
            waits.append((self.esem[e2], seq))
        else:
            _, sem, val = dep
            key = (eng, "sem", sem.num)
            if self.waited.get(key, 0) >= val:
                return
            self.waited[key] = val
            waits.append((sem, val))

    def op(self, eng, fn, reads=(), writes=()):
        waits = []
        for t in reads:
            self._need(eng, t.last_w, waits)
        for t in writes:
            self._need(eng, t.last_w, waits)
            for r in t.readers:
                self._need(eng, r, waits)
        self.cnt[eng] += 1
        seq = self.cnt[eng]
        me = ("eng", eng, seq)
        for t in writes:
            t.last_w = me
            t.readers = []
        for t in reads:
            if t.last_w is not me:
                t.readers.append(me)
                if len(t.readers) > 48:
                    best = {}
                    for r in t.readers:
                        k = (r[0], r[1] if r[0] == "eng" else r[1].num)
                        if k not in best or best[k][2] < r[2]:
                            best[k] = r
                    t.readers = list(best.values())
        self.ops[eng].append((waits, fn, (self.esem[eng], 1)))
        self.n_instr += 1

    def dma(self, q, out, in_, out_t=None, in_t=None, **kw):
        waits = []
        if in_t is not None:
            self._need(q, in_t.last_w, waits)
        if out_t is not None:
            self._need(q, out_t.last_w, waits)
            for r in out_t.readers:
                self._need(q, r, waits)
        if out_t is not None:
            if out_t.ld_sem is None:
                out_t.ld_sem = self._get_sem(out_t, "ld")
                out_t.ld_cnt = self.sem_users.get(out_t.ld_sem.num, 0)
            out_t.ld_cnt += 16
            self.sem_users[out_t.ld_sem.num] = out_t.ld_cnt
            sem, val = out_t.ld_sem, out_t.ld_cnt
            out_t.last_w = ("dma", sem, val)
            out_t.readers = []
            if in_t is not None:
                in_t.readers.append(("dma", sem, val))
        else:
            if in_t.st_sem is None:
                in_t.st_sem = self._get_sem(in_t, "st")
                in_t.st_cnt = self.sem_users.get(in_t.st_sem.num, 0)
                self.all_st.append(in_t)
            in_t.st_cnt += 16
            self.sem_users[in_t.st_sem.num] = in_t.st_cnt
            sem, val = in_t.st_sem, in_t.st_cnt
            in_t.readers.append(("dma", sem, val))

        def fn(e, out=out, in_=in_, kw=kw):
            return e.dma_start(out=out, in_=in_, **kw)

        self.ops[q].append((waits, fn, (sem, 16)))
        self.n_instr += 1

    def barrier_all(self):
        for e in ENGS:
            waits = []
            for e2 in ("pe", "act", "dve", "pool"):
                if self.cnt[e2] > 0:
                    self._need(e, ("eng", e2, self.cnt[e2]), waits)
            for t in self.all_st:
                self._need(e, ("dma", t.st_sem, t.st_cnt), waits)
            if waits:
                self.ops[e].append((waits, None, None))

    def end_phase(self, tiles):
        for t in tiles:
            if t.ld_sem is not None and t.last_w is not None and t.last_w[0] == "dma":
                w = []
                self._need("sp", t.last_w, w)
                if w:
                    self.ops["sp"].append((w, None, None))
        self.barrier_all()
        for t in tiles:
            for s in (t.ld_sem, t.st_sem):
                if s is not None:
                    self.free_sems.append(s)
            if t in self.all_st:
                self.all_st.remove(t)
            t.ld_sem = None
            t.st_sem = None

    def emit(self):
        nc = self.nc
        ops = self.ops

        def run(eng_obj, lst):
            for waits, fn, inc in lst:
                for sem, val in waits:
                    eng_obj.wait_ge(sem, val)
                if fn is not None:
                    ins = fn(eng_obj)
                    ins.then_inc(inc[0], inc[1])

        with nc.Block() as block:
            @block.tensor
            def _(e):
                run(e, ops["pe"])

            @block.scalar
            def _(e):
                run(e, ops["act"])

            @block.vector
            def _(e):
                run(e, ops["dve"])

            @block.gpsimd
            def _(e):
                run(e, ops["pool"])

            @block.sync
            def _(e):
                run(e, ops["sp"])
        self.ops = {e: [] for e in ENGS}


class Ring:
    def __init__(self, tiles):
        self.tiles = tiles
        self.i = 0

    def next(self):
        t = self.tiles[self.i % len(self.tiles)]
        self.i += 1
        return t


def col_blocks(lo, hi, step=512):
    out = []
    a = lo
    while a < hi:
        b = min(hi, (a // step + 1) * step)
        out.append((a, b))
        a = b
    return out


class Ctx:
    pass


def make_consts(P, K):
    K.ident_b = P.sbuf("ident_b", [128, 128], BF16)
    K.ident_f = P.sbuf("ident_f", [128, 128], F32)
    for t in (K.ident_b, K.ident_f):
        P.op("pool", lambda e, t=t: e.memset(t[:], 0.0), writes=[t])
        P.op("pool", lambda e, t=t: e.affine_select(out=t[:], in_=t[:], compare_op=ALU.not_equal, fill=1.0,
                                                    base=0, pattern=[[-1, 128]], channel_multiplier=1),
             reads=[t], writes=[t])
    K.eps_t = P.sbuf("eps_t", [128, 1], F32)
    P.op("pool", lambda e: e.memset(K.eps_t[:], EPS), writes=[K.eps_t])
    K.cbias = P.sbuf("cbias", [128, 8], F32)
    for k in range(8):
        P.op("pool", lambda e, k=k: e.memset(K.cbias[:, k:k + 1], -(2 * k - 1) * float(np.pi)), writes=[K.cbias])


def phase_precast(P, K, jobs, shape, tag):
    m = P.mark()
    f32r = Ring([P.sbuf(f"pc_f{i}_{tag}", shape, F32) for i in range(4)])
    b16r = Ring([P.sbuf(f"pc_b{i}_{tag}", shape, BF16) for i in range(4)])
    tiles = f32r.tiles + b16r.tiles
    for i, (parts, d) in enumerate(jobs):
        tf = f32r.next()
        tb = b16r.next()
        for pi_, (sl, s_) in enumerate(parts):
            P.dma("sp" if (i + pi_) % 2 == 0 else "act", sl(tf), s_, out_t=tf)
        P.op("pool", lambda e, tb=tb, tf=tf: e.tensor_copy(out=tb[:], in_=tf[:]), reads=[tf], writes=[tb])
        P.dma("sp" if i % 2 == 1 else "act", d, tb[:], in_t=tb)
    P.end_phase(tiles)
    P.emit()
    P.release(m)


def load_rep(P, q, tile, dram_row_ap):
    P.dma(q, tile[:], dram_row_ap.partition_broadcast(128), out_t=tile)


def rmsnorm_mod_T(P, K, xt, A_rep, sh_rep, uT, col0, scr):
    junk = scr["junk"]
    ssq = scr["stat"].next()
    P.op("act", lambda e: e.activation(out=junk[:], in_=xt[:], func=AF.Square, accum_out=ssq[:, 0:1]),
         reads=[xt], writes=[junk, ssq])
    P.op("act", lambda e: e.activation(out=ssq[:, 1:2], in_=ssq[:, 0:1], func=AF.Sqrt, bias=K.eps_t[:, 0:1], scale=1.0 / D),
         reads=[ssq, K.eps_t], writes=[ssq])
    P.op("dve", lambda e: e.reciprocal(out=ssq[:, 2:3], in_=ssq[:, 1:2]), reads=[ssq], writes=[ssq])
    t1 = scr["t1"]
    P.op("dve", lambda e: e.scalar_tensor_tensor(out=t1[:], in0=xt[:], scalar=ssq[:, 2:3], in1=A_rep[:],
                                                  op0=ALU.mult, op1=ALU.mult), reads=[xt, ssq, A_rep], writes=[t1])
    ub = scr["ub"].next()
    P.op("pool", lambda e: e.tensor_tensor(out=ub[:], in0=t1[:], in1=sh_rep[:], op=ALU.add), reads=[t1, sh_rep], writes=[ub])
    transpose_to(P, K, ub, uT, col0, scr)
    return ssq


def transpose_to(P, K, ub, uT, col0, scr):
    for g in range(KD // 8):
        pt = scr["pt"].next()
        for j in range(8):
            k = g * 8 + j
            P.op("pe", lambda e, pt=pt, j=j, k=k: e.transpose(out=pt[:, j, :], in_=ub[:, k * 128:(k + 1) * 128], identity=K.ident_b[:]),
                 reads=[ub, K.ident_b], writes=[pt])
        eng = "act" if g % 2 == 0 else "dve"
        if eng == "act":
            P.op("act", lambda e, pt=pt, g=g: e.activation(out=uT[:, g * 8:(g + 1) * 8, col0:col0 + 128], in_=pt[:], func=AF.Copy),
                 reads=[pt], writes=[uT])
        else:
            P.op("dve", lambda e, pt=pt, g=g: e.tensor_copy(out=uT[:, g * 8:(g + 1) * 8, col0:col0 + 128], in_=pt[:]),
                 reads=[pt], writes=[uT])


def phase_cond(P, K, io):
    K.condT = P.sbuf("condT", [128, KD, 2], F32)
    m = P.mark()
    craw = P.sbuf("craw", [KD, 2, 128], F32)
    csil = P.sbuf("csil", [KD, 2, 128], F32)
    pt = P.psum("cond_pt", [128, 2, KD], F32)
    P.dma("sp", craw[:, 0, :], io["c"].rearrange("o (k p) -> (o k) p", p=128), out_t=craw)
    P.dma("sp", craw[:, 1, :], io["c_ctx"].rearrange("o (k p) -> (o k) p", p=128), out_t=craw)
    P.op("act", lambda e: e.activation(out=csil[:], in_=craw[:], func=AF.Silu), reads=[craw], writes=[csil])
    for r in range(2):
        P.op("pe", lambda e, r=r: e.transpose(out=pt[:, r, :], in_=csil[:, r, :], identity=K.ident_f[0:KD, 0:KD]),
             reads=[csil, K.ident_f], writes=[pt])
    P.op("dve", lambda e: e.tensor_copy(out=K.condT[:].rearrange("p k r -> p r k"), in_=pt[:]), reads=[pt], writes=[K.condT])
    P.end_phase([craw, csil, pt])
    P.emit()
    P.release(m)


def phase_ada(P, K, io, layer, mod_out):
    m = P.mark()
    NB = 6 * D // 512
    wr = Ring([P.sbuf(f"ada_w{i}", [128, KD, 512], F32) for i in range(4)])
    br = Ring([P.sbuf(f"ada_b{i}", [2, 512], F32) for i in range(2)])
    orr = Ring([P.sbuf(f"ada_o{i}", [2, 512], F32) for i in range(2)])
    pr = Ring([P.psum(f"ada_p{i}", [2, 512], F32) for i in range(2)])
    aw = io["ada_w"][layer].rearrange("(k p) n -> p k n", p=128)
    ab = io["ada_b"][layer:layer + 1, :]
    for nb in range(NB):
        wt = wr.next(); bt = br.next(); ot = orr.next(); ps = pr.next()
        cs = slice(nb * 512, (nb + 1) * 512)
        P.dma("sp" if nb % 2 == 0 else "act", wt[:], aw[:, :, cs], out_t=wt)
        P.dma("sp", bt[:], ab[:, cs].partition_broadcast(2), out_t=bt)
        for k in range(KD):
            P.op("pe", lambda e, ps=ps, wt=wt, k=k: e.matmul(ps[:], lhsT=K.condT[:, k, :], rhs=wt[:, k, :], start=(k == 0), stop=(k == KD - 1)),
                 reads=[K.condT, wt], writes=[ps])
        P.op("dve", lambda e, ot=ot, ps=ps, bt=bt: e.tensor_tensor(out=ot[:], in0=ps[:], in1=bt[:], op=ALU.add), reads=[ps, bt], writes=[ot])
        P.dma("sp", mod_out[:, cs], ot[:], in_t=ot)
    P.end_phase(wr.tiles + br.tiles + orr.tiles + pr.tiles)
    P.emit()
    P.release(m)


def load_mod_tiles(P, K, io, layer, mod_d, row, which, names, normkey):
    out = {}
    gt = None
    for name, ci, kind in which:
        t = P.sbuf(f"mod_{name}", [128, D], F32)
        load_rep(P, "sp", t, mod_d[row:row + 1, ci * D:(ci + 1) * D])
        if kind == "A":
            if gt is None:
                gt = P.sbuf("mod_g", [128, D], F32)
                load_rep(P, "act", gt, io[normkey][layer:layer + 1, :])
            P.op("dve", lambda e, t=t, gt=gt: e.scalar_tensor_tensor(out=t[:], in0=t[:], scalar=1.0, in1=gt[:], op0=ALU.add, op1=ALU.mult),
                 reads=[t, gt], writes=[t])
        out[name] = t
    if gt is not None:
        out["_g"] = gt
    return out


def lat_rows(ap_lat, p0, S_LAT, n=128):
    ROWS = S_LAT // 64
    w0, nw = p0 // ROWS, n // ROWS
    return ap_lat.rearrange("(r w) d -> w r d", w=64)[w0:w0 + nw]


def token_tiles(S_LAT):
    tl = [(1, 0, S_CTX // 128)]
    for t0 in range(0, S_LAT, 512):
        tl.append((0, S_CTX + t0, min(4, (S_LAT - t0) // 128)))
    return tl


def phase_b_s5(P, K, io, layer, mod_d, xs, u_tok, S_LAT):
    m = P.mark()
    xr = Ring([P.sbuf(f"b_x{i}", [128, D], F32) for i in range(3)])
    scr = dict(junk=P.sbuf("b_junk", [128, D], BF16), t1=P.sbuf("b_t1", [128, D], F32),
               stat=Ring([P.sbuf(f"b_st{i}", [128, 4], F32) for i in range(4)]),
               ub=Ring([P.sbuf(f"b_ub{i}", [128, D], BF16) for i in range(3)]))
    tiles = xr.tiles + [scr["junk"], scr["t1"]] + scr["stat"].tiles + scr["ub"].tiles
    for row in (1, 0):
        mm = P.mark()
        md = load_mod_tiles(P, K, io, layer, mod_d, row, [("sh", 0, "raw"), ("A", 1, "A")], None, "norm1")
        ntok = S_CTX if row == 1 else S_LAT
        base = 0 if row == 1 else S_CTX
        for t in range(ntok // 128):
            xt = xr.next()
            P.dma("sp", xt[:], xs[base + t * 128: base + (t + 1) * 128, :], out_t=xt)
            ssq = scr["stat"].next()
            junk = scr["junk"]
            P.op("act", lambda e, xt=xt, ssq=ssq: e.activation(out=junk[:], in_=xt[:], func=AF.Square, accum_out=ssq[:, 0:1]),
                 reads=[xt], writes=[junk, ssq])
            P.op("act", lambda e, ssq=ssq: e.activation(out=ssq[:, 1:2], in_=ssq[:, 0:1], func=AF.Sqrt, bias=K.eps_t[:, 0:1], scale=1.0 / D),
                 reads=[ssq, K.eps_t], writes=[ssq])
            P.op("dve", lambda e, ssq=ssq: e.reciprocal(out=ssq[:, 2:3], in_=ssq[:, 1:2]), reads=[ssq], writes=[ssq])
            t1 = scr["t1"]
            P.op("dve", lambda e, xt=xt, ssq=ssq: e.scalar_tensor_tensor(out=t1[:], in0=xt[:], scalar=ssq[:, 2:3], in1=md["A"][:],
                                                                          op0=ALU.mult, op1=ALU.mult), reads=[xt, ssq, md["A"]], writes=[t1])
            ub = scr["ub"].next()
            P.op("pool", lambda e, ub=ub: e.tensor_tensor(out=ub[:], in0=t1[:], in1=md["sh"][:], op=ALU.add), reads=[t1, md["sh"]], writes=[ub])
            P.dma("act", u_tok[base + t * 128: base + (t + 1) * 128, :], ub[:], in_t=ub)
            if row == 1:
                P.dma("act", u_tok[S_CTX + S_LAT + t * 128: S_CTX + S_LAT + (t + 1) * 128, :], ub[:], in_t=ub)
        P.end_phase(list(md.values()))
        P.emit()
        P.release(mm)
    P.end_phase(tiles)
    P.emit()
    P.release(m)


def phase_d(P, K, io, layer, mod_d, xs_in, xs_out, mix_tok, wproj, wgu_t, wo_t, S_LAT, mixer, last, out_final, tok_rows=None):
    m = P.mark()
    xt = [P.sbuf(f"d_x{i}", [128, D], F32) for i in range(4)]
    bfr = Ring([P.sbuf(f"d_bf{i}", [128, D], BF16) for i in range(2)])
    u2T = P.sbuf("d_u2T", [128, KD, 512], BF16)
    hT = P.sbuf("d_hT", [128, NHC, 512], BF16)
    wpr = Ring([P.sbuf(f"d_wp{i}", [128, KD, 256], BF16) for i in range(2)])
    wgr = Ring([P.sbuf(f"d_wgu{i}", [128, KD, 256], BF16) for i in range(2)])
    wor = Ring([P.sbuf(f"d_wo{i}", [128, 4, 512], BF16) for i in range(3)])
    sgr = Ring([P.sbuf(f"d_sg{i}", [128, 512], BF16) for i in range(2)])
    f1r = Ring([P.sbuf(f"d_f1{i}", [128, 512], F32) for i in range(2)])
    f2r = Ring([P.sbuf(f"d_f2{i}", [128, 512], F32) for i in range(2)])
    t1 = P.sbuf("d_t1", [128, D], F32)
    gcur = P.sbuf("d_gcur", [128, D], F32)
    statr = Ring([P.sbuf(f"d_st{i}", [128, 4], F32) for i in range(4)])
    ptr = Ring([P.psum(f"d_pt{i}", [128, 8, 128], BF16) for i in range(2)])
    mmr = Ring([P.psum(f"d_mm{i}", [128, 512], F32) for i in range(6)])
    tiles = (xt + bfr.tiles + [u2T, hT, t1, gcur] + wpr.tiles + wgr.tiles + wor.tiles + sgr.tiles + f1r.tiles + f2r.tiles
             + statr.tiles + ptr.tiles + mmr.tiles)
    scr = dict(pt=ptr)
    qi = [0]

    def q():
        qi[0] += 1
        return "sp" if qi[0] % 2 == 0 else "act"

    ROWS = S_LAT // 64

    def xdma(tile, ap, tok0, s, load):
        has_ctx = ap.shape[0] == S_CTX + S_LAT
        if tok_rows is None or tok0 < S_CTX:
            off = 0 if has_ctx else -S_CTX
            d_ap = ap[tok0 + off + s * 128: tok0 + off + (s + 1) * 128, :]
            s_ap = tile[:]
        else:
            lat = ap[S_CTX:S_CTX + S_LAT, :] if has_ctx else ap
            lr = lat_rows(lat, tok0 - S_CTX + s * 128, S_LAT)
            for wi in range(128 // ROWS):
                if load:
                    P.dma(q(), tile[wi * ROWS:(wi + 1) * ROWS, :], lr[wi], out_t=tile)
                else:
                    P.dma(q(), lr[wi], tile[wi * ROWS:(wi + 1) * ROWS, :], in_t=tile)
            return
        if load:
            P.dma(q(), s_ap, d_ap, out_t=tile)
        else:
            P.dma(q(), d_ap, s_ap, in_t=tile)

    def norm_stats(x_t):
        ssq = statr.next()
        junk = bfr.next()
        P.op("act", lambda e: e.activation(out=junk[:], in_=x_t[:], func=AF.Square, accum_out=ssq[:, 0:1]), reads=[x_t], writes=[junk, ssq])
        P.op("act", lambda e: e.activation(out=ssq[:, 1:2], in_=ssq[:, 0:1], func=AF.Sqrt, bias=K.eps_t[:, 0:1], scale=1.0 / D),
             reads=[ssq, K.eps_t], writes=[ssq])
        P.op("dve", lambda e: e.reciprocal(out=ssq[:, 2:3], in_=ssq[:, 1:2]), reads=[ssq], writes=[ssq])
        return ssq

    cur_row = None
    md = None
    mm_mark = None
    for (row, tok0, nsub) in token_tiles(S_LAT):
        if last and row == 1:
            continue
        if row != cur_row:
            if md is not None:
                P.end_phase(list(md.values()))
                P.emit()
                P.release(mm_mark)
            mm_mark = P.mark()
            md = {}
            if last:
                md["nf"] = P.sbuf("mod_nf", [128, D], F32)
                load_rep(P, "sp", md["nf"], io["norm_f"])
            md.update(load_mod_tiles(P, K, io, layer, mod_d, row, [("sh2", 3, "raw"), ("A2", 4, "A")], None, "norm2"))
            cur_row = row
        TT = nsub * 128
        load_rep(P, "sp", gcur, mod_d[row:row + 1, 2 * D:3 * D])
        for s in range(nsub):
            xdma(xt[s], xs_in, tok0, s, True)
            mb = bfr.next()
            P.dma(q(), mb[:], mix_tok[tok0 + s * 128: tok0 + (s + 1) * 128, :], out_t=mb)
            transpose_to(P, K, mb, hT, s * 128, scr)
        for nb in range(8):
            wa = wpr.next()
            P.dma(q(), wa[:], wproj[nb], out_t=wa)
            if mixer == "s5":
                wb = wpr.next()
                P.dma(q(), wb[:], wproj[8 + nb], out_t=wb)
            cs = slice(nb * 256, (nb + 1) * 256)
            for s in range(nsub):
                pa = mmr.next()
                for k in range(KD):
                    P.op("pe", lambda e, pa=pa, wa=wa, k=k, s=s: e.matmul(pa[:, 0:256], lhsT=hT[:, k, s * 128:(s + 1) * 128], rhs=wa[:, k, :],
                                                                         start=(k == 0), stop=(k == KD - 1)), reads=[hT, wa], writes=[pa])
                f1 = f1r.next()
                if mixer == "s5":
                    pb = mmr.next()
                    for k in range(KD):
                        P.op("pe", lambda e, pb=pb, wb=wb, k=k, s=s: e.matmul(pb[:, 0:256], lhsT=hT[:, k, s * 128:(s + 1) * 128], rhs=wb[:, k, :],
                                                                             start=(k == 0), stop=(k == KD - 1)), reads=[hT, wb], writes=[pb])
                    f2 = f2r.next()
                    P.op("act", lambda e, f2=f2, pb=pb: e.activation(out=f2[:, 0:256], in_=pb[:, 0:256], func=AF.Sigmoid), reads=[pb], writes=[f2])
                    P.op("dve", lambda e, f1=f1, pa=pa, f2=f2: e.tensor_tensor(out=f1[:, 0:256], in0=pa[:, 0:256], in1=f2[:, 0:256], op=ALU.mult),
                         reads=[pa, f2], writes=[f1])
                    P.op("pool", lambda e, f1=f1, cs=cs: e.tensor_tensor(out=f1[:, 0:256], in0=f1[:, 0:256], in1=gcur[:, cs], op=ALU.mult),
                         reads=[f1, gcur], writes=[f1])
                else:
                    P.op("dve", lambda e, f1=f1, pa=pa, cs=cs: e.tensor_tensor(out=f1[:, 0:256], in0=pa[:, 0:256], in1=gcur[:, cs], op=ALU.mult),
                         reads=[pa, gcur], writes=[f1])
                P.op("pool", lambda e, f1=f1, s=s, cs=cs: e.tensor_tensor(out=xt[s][:, cs], in0=xt[s][:, cs], in1=f1[:, 0:256], op=ALU.add),
                     reads=[f1, xt[s]], writes=[xt[s]])
        load_rep(P, "sp", gcur, mod_d[row:row + 1, 5 * D:6 * D])
        for s in range(nsub):
            ssq = norm_stats(xt[s])
            P.op("dve", lambda e, s=s, ssq=ssq: e.scalar_tensor_tensor(out=t1[:], in0=xt[s][:], scalar=ssq[:, 2:3], in1=md["A2"][:],
                                                                        op0=ALU.mult, op1=ALU.mult), reads=[xt[s], ssq, md["A2"]], writes=[t1])
            ub = bfr.next()
            P.op("pool", lambda e, ub=ub: e.tensor_tensor(out=ub[:], in0=t1[:], in1=md["sh2"][:], op=ALU.add), reads=[t1, md["sh2"]], writes=[ub])
            transpose_to(P, K, ub, u2T, s * 128, scr)
        for c in range(NHC):
            wgu = wgr.next()
            P.dma(q(), wgu[:], wgu_t[c], out_t=wgu)
            pg = mmr.next(); pu = mmr.next()
            for k in range(KD):
                P.op("pe", lambda e, pg=pg, wgu=wgu, k=k: e.matmul(pg[:, :TT], lhsT=wgu[:, k, 0:128], rhs=u2T[:, k, :TT],
                                                                  start=(k == 0), stop=(k == KD - 1)), reads=[wgu, u2T], writes=[pg])
            for k in range(KD):
                P.op("pe", lambda e, pu=pu, wgu=wgu, k=k: e.matmul(pu[:, :TT], lhsT=wgu[:, k, 128:256], rhs=u2T[:, k, :TT],
                                                                  start=(k == 0), stop=(k == KD - 1)), reads=[wgu, u2T], writes=[pu])
            sg = sgr.next()
            P.op("act", lambda e, sg=sg, pg=pg: e.activation(out=sg[:, :TT], in_=pg[:, :TT], func=AF.Silu), reads=[pg], writes=[sg])
            P.op("dve", lambda e, sg=sg, pu=pu, c=c: e.tensor_tensor(out=hT[:, c, :TT], in0=pu[:, :TT], in1=sg[:, :TT], op=ALU.mult),
                 reads=[pu, sg], writes=[hT])
        for nt in range(4):
            cs = slice(nt * 512, (nt + 1) * 512)
            pf = [mmr.next() for _ in range(nsub)]
            for w in range(11):
                wo = wor.next()
                P.dma(q(), wo[:], wo_t[nt * 11 + w], out_t=wo)
                for s in range(nsub):
                    for cc in range(4):
                        c = w * 4 + cc
                        P.op("pe", lambda e, p_=pf[s], wo=wo, cc=cc, c=c, s=s: e.matmul(p_[:], lhsT=hT[:, c, s * 128:(s + 1) * 128], rhs=wo[:, cc, :],
                                                                                      start=(c == 0), stop=(c == NHC - 1)), reads=[hT, wo], writes=[pf[s]])
            for s in range(nsub):
                f1 = f1r.next()
                P.op("dve", lambda e, f1=f1, p_=pf[s], cs=cs: e.tensor_tensor(out=f1[:], in0=p_[:], in1=gcur[:, cs], op=ALU.mult), reads=[pf[s], gcur], writes=[f1])
                P.op("pool", lambda e, f1=f1, s=s, cs=cs: e.tensor_tensor(out=xt[s][:, cs], in0=xt[s][:, cs], in1=f1[:], op=ALU.add), reads=[f1, xt[s]], writes=[xt[s]])
        for s in range(nsub):
            if not last:
                xdma(xt[s], xs_out, tok0, s, False)
            else:
                ssq = norm_stats(xt[s])
                P.op("dve", lambda e, s=s, ssq=ssq: e.scalar_tensor_tensor(out=t1[:], in0=xt[s][:], scalar=ssq[:, 2:3], in1=md["nf"][:],
                                                                            op0=ALU.mult, op1=ALU.mult), reads=[xt[s], ssq, md["nf"]], writes=[t1])
                xdma(t1, out_final, tok0, s, False)
    if md is not None:
        P.end_phase(list(md.values()))
        P.emit()
        P.release(mm_mark)
    P.end_phase(tiles)
    P.emit()
    P.release(m)


def bc(ap, axis, n):
    a = ap.unsqueeze(axis)
    shp = list(a.shape)
    shp[axis] = n
    return a.broadcast_to(shp)


def phase_c_s5(P, K, io, jl, u_tok, gy_tok, S_LAT, dve2="pool"):
    NTOK3 = 2 * S_CTX + S_LAT
    NCH = NTOK3 // T0
    NF = (S_CTX + S_LAT) // T0
    CTXC = S_CTX // T0
    NBK = 32
    BL = NF // NBK
    assert NF == NBK * BL
    NBLK = (NCH + 127) // 128
    NFB = (NF + 127) // 128
    PI = float(np.pi)
    m = P.mark()
    V = lambda e: e

    def ew(eng, fn, reads, writes):
        P.op(eng, fn, reads=reads, writes=writes)

    Pw = P.sbuf("c_Pw", [128, 2, 2, 64, T0 + 1], F32)
    PWB = P.sbuf("c_PWB", [128, 2, 2, 64, BL + 1], F32)
    Bn = P.sbuf("c_Bn", [128, 2, 2, 64, 16], F32)
    Bb = P.sbuf("c_Bb", [128, 2, 2, 64, 16], F32)
    Dp = P.sbuf("c_Dp", [128, 128], F32)
    cz = [P.sbuf(f"c_cz{i}", [128, 16, 128], F32) for i in range(2)]
    m_tmp = P.mark()
    lam = P.sbuf("c_lam", [128, 2, 2, 64], F32)
    dtt = P.sbuf("c_dt", [128, 2, 64], F32)
    for d in range(2):
        for jj in range(2):
            ps_ = slice(jj * 64, (jj + 1) * 64)
            P.dma("sp", lam[ps_, 0, d, :], io["s5_lam_re"][jl, d].rearrange("(i j) p -> j p i", j=2)[jj], out_t=lam, allow_slow_non_contiguous=True)
            P.dma("act", lam[ps_, 1, d, :], io["s5_lam_im"][jl, d].rearrange("(i j) p -> j p i", j=2)[jj], out_t=lam, allow_slow_non_contiguous=True)
            P.dma("sp", dtt[ps_, d, :], io["s5_log_dt"][jl, d:d + 1, :].rearrange("o (i j) -> o j i", j=2)[:, jj, :].partition_broadcast(64),
                  out_t=dtt, allow_slow_non_contiguous=True)
    w = [P.sbuf(f"c_w{i}", [128, 2, 64], F32) for i in range(10)]
    A1 = P.sbuf("c_A1", [128, 2, 2, 64], F32)
    Fc = P.sbuf("c_F", [128, 2, 2, 64], F32)
    A8 = P.sbuf("c_A8", [128, 2, 2, 64], F32)
    ptc = Ring([P.psum(f"c_ptc{i}", [128, 4, 128], F32) for i in range(2)])
    lre, lim = lam[:, 0], lam[:, 1]
    ew("act", lambda e: e.activation(out=dtt[:], in_=dtt[:], func=AF.Exp), [dtt], [dtt])
    ew("dve", lambda e: e.tensor_tensor(out=w[0][:], in0=lre, in1=dtt[:], op=ALU.mult), [lam, dtt], [w[0]])
    ew("act", lambda e: e.activation(out=w[0][:], in_=w[0][:], func=AF.Exp), [w[0]], [w[0]])
    ew("dve", lambda e: e.tensor_tensor(out=w[1][:], in0=lim, in1=dtt[:], op=ALU.mult), [lam, dtt], [w[1]])
    for (dst, shift) in ((w[2], 0.0), (w[3], PI / 2)):
        ew("dve", lambda e, dst=dst, shift=shift: e.tensor_scalar(out=dst[:], in0=w[1][:], scalar1=float(shift), scalar2=None, op0=ALU.add), [w[1]], [dst])
        ew("dve", lambda e, dst=dst: e.tensor_copy(out=w[4][:], in_=dst[:]), [dst], [w[4]])
        for k in range(1, 7):
            ew("act", lambda e, k=k: e.activation(out=w[5][:], in_=w[4][:], func=AF.Sign, bias=K.cbias[:, k:k + 1], scale=1.0), [w[4], K.cbias], [w[5]])
            ew("dve", lambda e, dst=dst: e.scalar_tensor_tensor(out=dst[:], in0=w[5][:], scalar=-PI, in1=dst[:], op0=ALU.mult, op1=ALU.add), [w[5], dst], [dst])
        ew("dve", lambda e, dst=dst: e.tensor_scalar(out=dst[:], in0=dst[:], scalar1=-6.0 * PI, scalar2=None, op0=ALU.add), [dst], [dst])
        ew("act", lambda e, dst=dst: e.activation(out=dst[:], in_=dst[:], func=AF.Sin), [dst], [dst])
    ew("dve", lambda e: e.tensor_tensor(out=A1[:, 0], in0=w[0][:], in1=w[3][:], op=ALU.mult), [w[0], w[3]], [A1])
    ew("dve", lambda e: e.tensor_tensor(out=A1[:, 1], in0=w[0][:], in1=w[2][:], op=ALU.mult), [w[0], w[2]], [A1])
    ew("dve", lambda e: e.tensor_tensor(out=w[4][:], in0=lre, in1=lre, op=ALU.mult), [lam], [w[4]])
    ew("dve", lambda e: e.tensor_tensor(out=w[5][:], in0=lim, in1=lim, op=ALU.mult), [lam], [w[5]])
    ew("dve", lambda e: e.tensor_tensor(out=w[4][:], in0=w[4][:], in1=w[5][:], op=ALU.add), [w[4], w[5]], [w[4]])
    ew("dve", lambda e: e.reciprocal(out=w[4][:], in_=w[4][:]), [w[4]], [w[4]])
    ew("dve", lambda e: e.tensor_scalar(out=w[5][:], in0=A1[:, 0], scalar1=-1.0, scalar2=None, op0=ALU.add), [A1], [w[5]])
    ew("dve", lambda e: e.tensor_tensor(out=w[6][:], in0=w[5][:], in1=lre, op=ALU.mult), [w[5], lam], [w[6]])
    ew("dve", lambda e: e.tensor_tensor(out=w[7][:], in0=A1[:, 1], in1=lim, op=ALU.mult), [A1, lam], [w[7]])
    ew("dve", lambda e: e.tensor_tensor(out=w[6][:], in0=w[6][:], in1=w[7][:], op=ALU.add), [w[6], w[7]], [w[6]])
    ew("dve", lambda e: e.tensor_tensor(out=Fc[:, 0], in0=w[6][:], in1=w[4][:], op=ALU.mult), [w[6], w[4]], [Fc])
    ew("dve", lambda e: e.tensor_tensor(out=w[6][:], in0=A1[:, 1], in1=lre, op=ALU.mult), [A1, lam], [w[6]])
    ew("dve", lambda e: e.tensor_tensor(out=w[7][:], in0=w[5][:], in1=lim, op=ALU.mult), [w[5], lam], [w[7]])
    ew("dve", lambda e: e.tensor_tensor(out=w[6][:], in0=w[6][:], in1=w[7][:], op=ALU.subtract), [w[6], w[7]], [w[6]])
    ew("dve", lambda e: e.tensor_tensor(out=Fc[:, 1], in0=w[6][:], in1=w[4][:], op=ALU.mult), [w[6], w[4]], [Fc])

    def cmul_pow(dst, n, base_re, base_im, tag):
        ew("dve", lambda e: e.memset(dst[:, 0, :, :, 0:1], 1.0), [], [dst])
        ew("dve", lambda e: e.memset(dst[:, 1, :, :, 0:1], 0.0), [], [dst])
        for k in range(1, n):
            pr, pi_ = dst[:, 0, :, :, k - 1], dst[:, 1, :, :, k - 1]
            ew("dve", lambda e, pr=pr: e.tensor_tensor(out=w[6][:], in0=pr, in1=base_re, op=ALU.mult), [dst, A1], [w[6]])
            ew("dve", lambda e, pi_=pi_: e.tensor_tensor(out=w[7][:], in0=pi_, in1=base_im, op=ALU.mult), [dst, A1], [w[7]])
            ew("dve", lambda e, k=k: e.tensor_tensor(out=dst[:, 0, :, :, k], in0=w[6][:], in1=w[7][:], op=ALU.subtract), [w[6], w[7]], [dst])
            ew("dve", lambda e, pr=pr: e.tensor_tensor(out=w[6][:], in0=pr, in1=base_im, op=ALU.mult), [dst, A1], [w[6]])
            ew("dve", lambda e, pi_=pi_: e.tensor_tensor(out=w[7][:], in0=pi_, in1=base_re, op=ALU.mult), [dst, A1], [w[7]])
            ew("dve", lambda e, k=k: e.tensor_tensor(out=dst[:, 1, :, :, k], in0=w[6][:], in1=w[7][:], op=ALU.add), [w[6], w[7]], [dst])

    cmul_pow(Pw, T0 + 1, A1[:, 0], A1[:, 1], "pw")
    ew("dve", lambda e: e.tensor_copy(out=A8[:], in_=Pw[:, :, :, :, T0]), [Pw], [A8])
    cmul_pow(PWB, BL + 1, A8[:, 0], A8[:, 1], "pwb")

    for d in range(2):
        for jj in range(2):
            ps_ = slice(jj * 64, (jj + 1) * 64)
            P.dma("sp", Bn[ps_, 0, d], io["s5_b_re"][jl, d].rearrange("(i j) p c -> j p i c", j=2)[jj], out_t=Bn)
            P.dma("act", Bn[ps_, 1, d], io["s5_b_im"][jl, d].rearrange("(i j) p c -> j p i c", j=2)[jj], out_t=Bn)
    tbT = cz[0]
    tbv = cz[0][:].rearrange("p (d a) (b c) -> p d (a b) c", d=2, c=16)
    fre = bc(Fc[:, 0], 3, 16)
    fim = bc(Fc[:, 1], 3, 16)
    ew("dve", lambda e: e.tensor_tensor(out=Bb[:, 0], in0=Bn[:, 0], in1=fre, op=ALU.mult), [Bn, Fc], [Bb])
    ew("dve", lambda e: e.tensor_tensor(out=tbv, in0=Bn[:, 1], in1=fim, op=ALU.mult), [Bn, Fc], [tbT])
    ew("dve", lambda e: e.tensor_tensor(out=Bb[:, 0], in0=Bb[:, 0], in1=tbv, op=ALU.subtract), [Bb, tbT], [Bb])
    ew("dve", lambda e: e.tensor_tensor(out=Bb[:, 1], in0=Bn[:, 1], in1=fre, op=ALU.mult), [Bn, Fc], [Bb])
    ew("dve", lambda e: e.tensor_tensor(out=tbv, in0=Bn[:, 0], in1=fim, op=ALU.mult), [Bn, Fc], [tbT])
    ew("dve", lambda e: e.tensor_tensor(out=Bb[:, 1], in0=Bb[:, 1], in1=tbv, op=ALU.add), [Bb, tbT], [Bb])

    Cn = Bn
    for t in cz:
        ew("pool", lambda e, t=t: e.memset(t[:], 0.0), [], [t])
    ci = 0
    for d in range(2):
        for part, key in ((0, "s5_c_re"), (1, "s5_c_im")):
            t = cz[ci % 2]; ci += 1
            src = io[key][jl, d].rearrange("(kc q j) c p -> q j c kc p", q=4, j=2)
            for q_ in range(4):
                for j_ in range(2):
                    r0 = 32 * q_ + 16 * j_
                    P.dma("sp" if (q_ + j_) % 2 == 0 else "act", t[r0:r0 + 16, :, 64 * j_:64 * j_ + 64], src[q_, j_], out_t=t)
            for k4 in range(4):
                pt = ptc.next()
                for kk in range(4):
                    kc = k4 * 4 + kk
                    ew("pe", lambda e, pt=pt, kk=kk, kc=kc, t=t: e.transpose(out=pt[:, kk, :], in_=t[:, kc, :], identity=K.ident_f[:]), [t, K.ident_f], [pt])
                for jj in range(2):
                    ps_ = slice(jj * 64, (jj + 1) * 64)
                    src_ap = pt[ps_].rearrange("p k (q j c) -> p k q j c", q=4, j=2)[:, :, :, jj, :]
                    dst_ap = Cn[ps_, part, d, 16 * k4:16 * k4 + 16, :].rearrange("p (k q) c -> p k q c", q=4)
                    if part == 0:
                        ew("dve", lambda e, dst_ap=dst_ap, src_ap=src_ap: e.tensor_copy(out=dst_ap, in_=src_ap), [pt], [Cn])
                    else:
                        ew("dve", lambda e, dst_ap=dst_ap, src_ap=src_ap: e.tensor_scalar(out=dst_ap, in0=src_ap, scalar1=-1.0, scalar2=None, op0=ALU.mult), [pt], [Cn])
    for t_ in range(T0):
        P.dma("sp", Dp[16 * t_:16 * t_ + 16, :], io["s5_d"][jl:jl + 1, :].rearrange("o (g c) -> (o c) g", c=16), out_t=Dp, allow_slow_non_contiguous=True)

    P.end_phase([lam, dtt, A1, Fc, A8] + w + ptc.tiles)
    P.emit()
    P.release(m_tmp)

    VF, VB = cz[0], cz[1]
    VFv = cz[0][:].rearrange("p (a b x) (y d) -> p a b (x y) d", a=2, b=4, d=16)
    VBv = cz[1][:].rearrange("p (a b x) (y d) -> p a b (x y) d", a=2, b=4, d=16)
    ew("pool", lambda e: e.memset(cz[0][:], 0.0), [], [VF])
    ew("pool", lambda e: e.memset(cz[1][:], 0.0), [], [VB])
    vt = P.sbuf("c_vt", [128, 4, 8, 16], F32)
    vu = P.sbuf("c_vu", [128, 4, 8, 16], F32)
    Ro = P.sbuf("c_Ro", [128, 2, 2, 4, 8, 16], BF16)
    TS = P.sbuf("c_TS", [128, 2, 2, 4, 128], BF16)
    IT = P.sbuf("c_IT", [128, 2, 8, 128], BF16)
    U = P.sbuf("c_U", [128, 8, NBLK * 128], BF16)
    Zr = Ring([P.sbuf(f"c_Z{i}", [128, 8, 128], BF16) for i in range(2)])
    Zpr = Ring([P.sbuf(f"c_Zp{i}", [128, 8, 8, 16], BF16) for i in range(2)])
    Hs = P.sbuf("c_Hs", [128, 2, 8, NBK, BL], F32)
    Hr = P.sbuf("c_Hr", [128, 2, 2, 4, NCH], BF16)
    ew("pool", lambda e: e.memset(Hr[:], 0.0), [], [Hr])
    A8l = P.sbuf("c_A8l", [128, 2, 8], F32)
    ABl = P.sbuf("c_ABl", [128, 2, 8], F32)
    PWl = P.sbuf("c_PWl", [128, 2, 8, BL], F32)
    s1 = [P.sbuf(f"c_s1{i}", [128, 8, NBK], F32) for i in range(2)]
    s3 = P.sbuf("c_s3", [128, 4, NBK - 1, max(BL - 1, 1)], F32)
    Ysb = Ring([P.sbuf(f"c_Y{i}", [128, NFB * 128], F32) for i in range(2)])
    Zo = P.sbuf("c_Zo", [128, NFB, 8, 128], BF16)
    gl_ = [P.sbuf(f"c_g{i}", [128, NFB * 128], F32) for i in range(2)]
    Ybr = Ring([P.sbuf(f"c_Yb{i}", [128, NFB * 128], BF16) for i in range(2)])
    ptb = Ring([P.psum(f"c_ptb{i}", [128, 8, 128], BF16) for i in range(2)])
    psS = Ring([P.psum(f"c_psS{i}", [128, 1024], F32) for i in range(1)])
    psY = Ring([P.psum(f"c_psY{i}", [128, 1024], F32) for i in range(1)])
    psT = Ring([P.psum(f"c_psT{i}", [128, 4, 128], F32) for i in range(1)])
    alt = ["dve", dve2]

    for b in range(16):
        i0 = 4 * b
        f0 = 128 * b
        for d in range(2):
            for part in range(2):
                pre = bc(Pw[:, 0, d, i0:i0 + 4, 0:T0], 3, 16)
                pim = bc(Pw[:, 1, d, i0:i0 + 4, 0:T0], 3, 16)
                bre = bc(Bb[:, 0, d, i0:i0 + 4, :], 2, T0)
                bim = bc(Bb[:, 1, d, i0:i0 + 4, :], 2, T0)
                dstv = VFv[:, part, :, 7::-1, :] if d == 0 else VBv[:, part, :, 8:16, :]
                dt_ = VF if d == 0 else VB
                if part == 0:
                    ew("dve", lambda e, pre=pre, bre=bre: e.tensor_tensor(out=vt[:], in0=pre, in1=bre, op=ALU.mult), [Pw, Bb], [vt])
                    ew("dve", lambda e, pim=pim, bim=bim, dstv=dstv: e.tensor_tensor(out=dstv, in0=pim, in1=bim, op=ALU.mult), [Pw, Bb], [dt_])
                    ew("dve", lambda e, dstv=dstv: e.tensor_tensor(out=dstv, in0=vt[:], in1=dstv, op=ALU.subtract), [vt, dt_], [dt_])
                else:
                    ew("dve", lambda e, pre=pre, bim=bim: e.tensor_tensor(out=vt[:], in0=pre, in1=bim, op=ALU.mult), [Pw, Bb], [vt])
                    ew("dve", lambda e, pim=pim, bre=bre, dstv=dstv: e.tensor_tensor(out=dstv, in0=pim, in1=bre, op=ALU.mult), [Pw, Bb], [dt_])
                    ew("dve", lambda e, dstv=dstv: e.tensor_tensor(out=dstv, in0=vt[:], in1=dstv, op=ALU.add), [vt, dt_], [dt_])
            ks = slice(1, T0 + 1) if d == 0 else slice(T0, 0, -1)
            pre = bc(Pw[:, 0, d, i0:i0 + 4, ks], 3, 16)
            pim = bc(Pw[:, 1, d, i0:i0 + 4, ks], 3, 16)
            cre = bc(Cn[:, 0, d, i0:i0 + 4, :], 2, T0)
            cimn = bc(Cn[:, 1, d, i0:i0 + 4, :], 2, T0)
            ew("dve", lambda e, pre=pre, cre=cre: e.tensor_tensor(out=vt[:], in0=pre, in1=cre, op=ALU.mult), [Pw, Cn], [vt])
            ew(dve2, lambda e, pim=pim, cimn=cimn: e.tensor_tensor(out=vu[:], in0=pim, in1=cimn, op=ALU.mult), [Pw, Cn], [vu])
            ew("dve", lambda e, d=d: e.tensor_tensor(out=Ro[:, d, 0], in0=vt[:], in1=vu[:], op=ALU.add), [vt, vu], [Ro])
            ew("dve", lambda e, pim=pim, cre=cre: e.tensor_tensor(out=vt[:], in0=pim, in1=cre, op=ALU.mult), [Pw, Cn], [vt])
            ew(dve2, lambda e, pre=pre, cimn=cimn: e.tensor_tensor(out=vu[:], in0=pre, in1=cimn, op=ALU.mult), [Pw, Cn], [vu])
            ew("dve", lambda e, d=d: e.tensor_tensor(out=Ro[:, d, 1], in0=vu[:], in1=vt[:], op=ALU.subtract), [vt, vu], [Ro])
        for d in range(2):
            for part in range(2):
                pt = psT.next()
                for il in range(4):
                    src = (VFv[:, part, il, 0:8, :] if d == 0 else VBv[:, part, il, 8:16, :]).rearrange("p b c -> p (b c)")
                    ew("pe", lambda e, pt=pt, il=il, src=src: e.transpose(out=pt[:, il, :], in_=src, identity=K.ident_f[:]), [VF, VB, K.ident_f], [pt])
                ew("act", lambda e, pt=pt, d=d, part=part: e.activation(out=TS[:, d, part], in_=pt[:], func=AF.Copy), [pt], [TS])
        for d in range(2):
            for g4 in range(2):
                pt = psT.next()
                for gg in range(4):
                    g_ = g4 * 4 + gg
                    il, jj = g_ // 2, g_ % 2
                    ps_ = slice(jj * 64, (jj + 1) * 64)
                    for t_ in range(T0):
                        for part in range(2):
                            VX = VFv if d == 0 else VBv
                            w0 = (7 - t_) if d == 0 else (8 - t_)
                            lhsT = VX[ps_, part, il, w0:w0 + 8, :].rearrange("p b c -> p (b c)")
                            rhs = Cn[ps_, part, d, i0 + il, :]
                            ew("pe", lambda e, pt=pt, gg=gg, t_=t_, part=part, lhsT=lhsT, rhs=rhs: e.matmul(
                                pt[:, gg, 16 * t_:16 * t_ + 16], lhsT=lhsT, rhs=rhs, start=(part == 0), stop=(part == 1), skip_group_check=True),
                               [VF, VB, Cn], [pt])
                ew("act", lambda e, pt=pt, d=d, g4=g4: e.activation(out=IT[:, d, g4 * 4:(g4 + 1) * 4], in_=pt[:], func=AF.Copy), [pt], [IT])
        for part in range(2):
            for d in range(2):
                ew("dve", lambda e, part=part, d=d: e.tensor_copy(out=A8l[:, part, d * 4:(d + 1) * 4], in_=PWB[:, part, d, i0:i0 + 4, 1]), [PWB], [A8l])
                ew("dve", lambda e, part=part, d=d: e.tensor_copy(out=ABl[:, part, d * 4:(d + 1) * 4], in_=PWB[:, part, d, i0:i0 + 4, BL]), [PWB], [ABl])
                ew("dve", lambda e, part=part, d=d: e.tensor_copy(out=PWl[:, part, d * 4:(d + 1) * 4, :], in_=PWB[:, part, d, i0:i0 + 4, 1:BL + 1]), [PWB], [PWl])
        for blk in range(NBLK):
            nn = min(128, NCH - blk * 128)
            Z = Zr.next()
            P.dma("sp" if blk % 2 == 0 else "act", Z[0:nn],
                  u_tok[blk * 1024: blk * 1024 + nn * 8, f0:f0 + 128].rearrange("(n s) f -> n s f", s=8), out_t=Z)
            pt = ptb.next()
            Zp = Zpr.next()
            ew("pool", lambda e, Z=Z, Zp=Zp, nn=nn: e.tensor_copy(out=Zp[0:nn], in_=Z[0:nn].rearrange("n s (g c) -> n g s c", c=16)), [Z], [Zp])
            for g_ in range(8):
                ew("pe", lambda e, pt=pt, g_=g_, Zp=Zp, nn=nn: e.transpose(out=pt[:, g_, 0:nn], in_=Zp[0:nn, g_].rearrange("n s c -> n (s c)"), identity=K.ident_b[0:nn, 0:nn]),
                   [Zp, K.ident_b], [pt])
            ew("act" if blk % 2 == 0 else "dve",
               (lambda e, pt=pt, blk=blk, nn=nn: e.activation(out=U[:, :, blk * 128: blk * 128 + nn], in_=pt[:, :, 0:nn], func=AF.Copy)) if blk % 2 == 0 else
               (lambda e, pt=pt, blk=blk, nn=nn: e.tensor_copy(out=U[:, :, blk * 128: blk * 128 + nn], in_=pt[:, :, 0:nn])), [pt], [U])
        for il in range(4):
            for d in range(2):
                lane = d * 4 + il
                lo, hi = (0, NF) if d == 0 else (CTXC, NCH)
                for part in range(2):
                    ps = psS.next()
                    for (a, b_) in col_blocks(0, NF):
                        for jj in range(2):
                            ps_ = slice(jj * 64, (jj + 1) * 64)
                            ew("pe", lambda e, ps=ps, ps_=ps_, a=a, b_=b_, d=d, part=part, il=il, jj=jj, lo=lo: e.matmul(
                                ps[ps_, a:b_], lhsT=TS[:, d, part, il, ps_], rhs=U[:, 2 * il + jj, lo + a: lo + b_], start=True, stop=True, skip_group_check=True),
                               [TS, U], [ps])
                    dst = Hs[:, part, lane].rearrange("p b l -> p (b l)")
                    if d == 0:
                        ew("act", lambda e, dst=dst, ps=ps: e.activation(out=dst, in_=ps[:, 0:NF], func=AF.Copy), [ps], [Hs])
                    else:
                        ew("dve", lambda e, dst=dst, ps=ps: e.tensor_copy(out=dst[:, ::-1], in_=ps[:, 0:NF]), [ps], [Hs])
        are = bc(A8l[:, 0, :], 2, NBK)
        aim = bc(A8l[:, 1, :], 2, NBK)
        for l in range(1, BL):
            cr, ci_ = Hs[:, 0, :, :, l], Hs[:, 1, :, :, l]
            pr, pi_ = Hs[:, 0, :, :, l - 1], Hs[:, 1, :, :, l - 1]
            ew("dve", lambda e, pr=pr: e.tensor_tensor(out=s1[0][:], in0=pr, in1=are, op=ALU.mult), [Hs, A8l], [s1[0]])
            ew("dve", lambda e, cr=cr: e.tensor_tensor(out=cr, in0=cr, in1=s1[0][:], op=ALU.add), [Hs, s1[0]], [Hs])
            ew("dve", lambda e, pi_=pi_: e.tensor_tensor(out=s1[0][:], in0=pi_, in1=aim, op=ALU.mult), [Hs, A8l], [s1[0]])
            ew("dve", lambda e, cr=cr: e.tensor_tensor(out=cr, in0=cr, in1=s1[0][:], op=ALU.subtract), [Hs, s1[0]], [Hs])
            ew("dve", lambda e, pi_=pi_: e.tensor_tensor(out=s1[1][:], in0=pi_, in1=are, op=ALU.mult), [Hs, A8l], [s1[1]])
            ew("dve", lambda e, ci_=ci_: e.tensor_tensor(out=ci_, in0=ci_, in1=s1[1][:], op=ALU.add), [Hs, s1[1]], [Hs])
            ew("dve", lambda e, pr=pr: e.tensor_tensor(out=s1[1][:], in0=pr, in1=aim, op=ALU.mult), [Hs, A8l], [s1[1]])
            ew("dve", lambda e, ci_=ci_: e.tensor_tensor(out=ci_, in0=ci_, in1=s1[1][:], op=ALU.add), [Hs, s1[1]], [Hs])
        for bk in range(1, NBK):
            cr, ci_ = Hs[:, 0, :, bk, BL - 1], Hs[:, 1, :, bk, BL - 1]
            pr, pi_ = Hs[:, 0, :, bk - 1, BL - 1], Hs[:, 1, :, bk - 1, BL - 1]
            t0_, t1_ = s1[0][:, :, 0], s1[1][:, :, 0]
            ew("dve", lambda e, pr=pr, t0_=t0_: e.tensor_tensor(out=t0_, in0=pr, in1=ABl[:, 0, :], op=ALU.mult), [Hs, ABl], [s1[0]])
            ew("dve", lambda e, cr=cr, t0_=t0_: e.tensor_tensor(out=cr, in0=cr, in1=t0_, op=ALU.add), [Hs, s1[0]], [Hs])
            ew("dve", lambda e, pi_=pi_, t0_=t0_: e.tensor_tensor(out=t0_, in0=pi_, in1=ABl[:, 1, :], op=ALU.mult), [Hs, ABl], [s1[0]])
            ew("dve", lambda e, cr=cr, t0_=t0_: e.tensor_tensor(out=cr, in0=cr, in1=t0_, op=ALU.subtract), [Hs, s1[0]], [Hs])
            ew("dve", lambda e, pi_=pi_, t1_=t1_: e.tensor_tensor(out=t1_, in0=pi_, in1=ABl[:, 0, :], op=ALU.mult), [Hs, ABl], [s1[1]])
            ew("dve", lambda e, ci_=ci_, t1_=t1_: e.tensor_tensor(out=ci_, in0=ci_, in1=t1_, op=ALU.add), [Hs, s1[1]], [Hs])
            ew("dve", lambda e, pr=pr, t1_=t1_: e.tensor_tensor(out=t1_, in0=pr, in1=ABl[:, 1, :], op=ALU.mult), [Hs, ABl], [s1[1]])
            ew("dve", lambda e, ci_=ci_, t1_=t1_: e.tensor_tensor(out=ci_, in0=ci_, in1=t1_, op=ALU.add), [Hs, s1[1]], [Hs])
        if BL > 1:
            for lh in range(2):
                ls = slice(lh * 4, (lh + 1) * 4)
                ere = bc(Hs[:, 0, ls, 0:NBK - 1, BL - 1], 3, BL - 1)
                eim = bc(Hs[:, 1, ls, 0:NBK - 1, BL - 1], 3, BL - 1)
                pwr = bc(PWl[:, 0, ls, 0:BL - 1], 2, NBK - 1)
                pwi = bc(PWl[:, 1, ls, 0:BL - 1], 2, NBK - 1)
                hre = Hs[:, 0, ls, 1:NBK, 0:BL - 1]
                him = Hs[:, 1, ls, 1:NBK, 0:BL - 1]
                for (x_, y_, dst, op_) in ((ere, pwr, hre, ALU.add), (eim, pwi, hre, ALU.subtract), (eim, pwr, him, ALU.add), (ere, pwi, him, ALU.add)):
                    ew("dve", lambda e, x_=x_, y_=y_: e.tensor_tensor(out=s3[:], in0=x_, in1=y_, op=ALU.mult), [Hs, PWl], [s3])
                    ew("dve", lambda e, dst=dst, op_=op_: e.tensor_tensor(out=dst, in0=dst, in1=s3[:], op=op_), [Hs, s3], [Hs])
        for part in range(2):
            hf = Hs[:, part, 0:4].rearrange("p a b l -> p a (b l)")
            hb = Hs[:, part, 4:8].rearrange("p a b l -> p a (b l)")
            ew("act", lambda e, part=part, hf=hf: e.activation(out=Hr[:, 0, part, :, 1:NF], in_=hf[:, :, 0:NF - 1], func=AF.Copy), [Hs], [Hr])
            ew(dve2, lambda e, part=part, hb=hb: e.tensor_copy(out=Hr[:, 1, part, :, CTXC:NCH - 1], in_=hb[:, :, NF - 2::-1]), [Hs], [Hr])
        for g_ in range(8):
            il, jj = g_ // 2, g_ % 2
            ps_ = slice(jj * 64, (jj + 1) * 64)
            py = psY.next()
            first = {}
            mm = []
            for d in range(2):
                lo, hi = (0, NF) if d == 0 else (CTXC, NCH)
                for (a, b_) in col_blocks(lo, hi):
                    mm.append((a, b_, IT[:, d, g_, :], U[:, g_, a:b_]))
                    mm.append((a, b_, Ro[ps_, d, 0, il].rearrange("p t c -> p (t c)"), Hr[ps_, d, 0, il, a:b_]))
                    mm.append((a, b_, Ro[ps_, d, 1, il].rearrange("p t c -> p (t c)"), Hr[ps_, d, 1, il, a:b_]))
            nlast = {}
            for idx, (a, b_, _, _) in enumerate(mm):
                nlast[a // 512] = idx
            for idx, (a, b_, lhsT, rhs) in enumerate(mm):
                bank = a // 512
                st = bank not in first
                first[bank] = True
                ew("pe", lambda e, py=py, a=a, b_=b_, lhsT=lhsT, rhs=rhs, st=st, sp=(nlast[bank] == idx): e.matmul(
                    py[:, a:b_], lhsT=lhsT, rhs=rhs, start=st, stop=sp, skip_group_check=True), [IT, U, Ro, Hr], [py])
            Y = Ysb.next()
            ew("dve", lambda e, Y=Y, py=py, g_=g_: e.scalar_tensor_tensor(out=Y[:, 0:NF], in0=U[:, g_, 0:NF], scalar=Dp[:, 8 * b + g_: 8 * b + g_ + 1],
                                                                          in1=py[:, 0:NF], op0=ALU.mult, op1=ALU.add), [U, Dp, py], [Y])
            ew("dve", lambda e, Y=Y, py=py: e.tensor_tensor(out=Y[:, 0:CTXC], in0=Y[:, 0:CTXC], in1=py[:, NF:NCH], op=ALU.add), [Y, py], [Y])
            ga, gb = gl_[0], gl_[1]
            ew(dve2, lambda e, Y=Y: e.tensor_tensor(out=ga[:, 0:NF], in0=Y[:, 0:NF], in1=Y[:, 0:NF], op=ALU.mult), [Y], [ga])
            ew("dve", lambda e: e.tensor_scalar(out=ga[:, 0:NF], in0=ga[:, 0:NF], scalar1=0.044715, scalar2=1.0, op0=ALU.mult, op1=ALU.add), [ga], [ga])
            ew(dve2, lambda e, Y=Y: e.tensor_tensor(out=ga[:, 0:NF], in0=ga[:, 0:NF], in1=Y[:, 0:NF], op=ALU.mult), [ga, Y], [ga])
            ew("act", lambda e: e.activation(out=gb[:, 0:NF], in_=ga[:, 0:NF], func=AF.Sigmoid, scale=1.5957691216057308), [ga], [gb])
            Yb = Ybr.next()
            ew("dve", lambda e, Y=Y, Yb=Yb: e.tensor_tensor(out=Yb[:, 0:NF], in0=gb[:, 0:NF], in1=Y[:, 0:NF], op=ALU.mult), [gb, Y], [Yb])
            pt = ptb.next()
            for blk in range(NFB):
                nn = min(128, NF - blk * 128)
                ew("pe", lambda e, pt=pt, blk=blk, nn=nn, Yb=Yb: e.transpose(out=pt[0:nn, blk, :], in_=Yb[:, blk * 128: blk * 128 + nn], identity=K.ident_b[:]),
                   [Yb, K.ident_b], [pt])
            nfull = NF // 128
            if nfull > 0:
                ew("act", lambda e, pt=pt, g_=g_: e.activation(out=Zo[:, 0:nfull, :, 16 * g_:16 * g_ + 16],
                                                               in_=pt[:, 0:nfull, :].rearrange("p k (t c) -> p k t c", c=16), func=AF.Copy), [pt], [Zo])
            if NF % 128:
                nn = NF % 128
                ew("act", lambda e, pt=pt, g_=g_, nn=nn: e.activation(out=Zo[0:nn, nfull, :, 16 * g_:16 * g_ + 16],
                                                                      in_=pt[0:nn, nfull, :].rearrange("p (t c) -> p t c", c=16), func=AF.Copy), [pt], [Zo])
        for blk in range(NFB):
            nn = min(128, NF - blk * 128)
            P.dma("sp" if blk % 2 == 0 else "act",
                  gy_tok[blk * 1024: blk * 1024 + nn * 8, f0:f0 + 128].rearrange("(n s) f -> n s f", s=8), Zo[0:nn, blk], in_t=Zo)
        P.emit()
    allt = ([Pw, PWB, Bn, Bb, Dp, vt, vu, Ro, TS, IT, U, Hs, Hr, A8l, ABl, PWl, s3, Zo] + cz + Zr.tiles + Zpr.tiles + s1
            + Ysb.tiles + gl_ + Ybr.tiles + ptb.tiles + psS.tiles + psY.tiles + psT.tiles)
    P.end_phase(allt)
    P.emit()
    P.release(m)


W_SHAPES = {
    "ada_w": [4, D, 6 * D], "ada_b": [4, 6 * D], "norm1": [4, D], "norm2": [4, D], "norm_f": [1, D],
    "s5_lam_re": [2, 2, G, PST], "s5_lam_im": [2, 2, G, PST], "s5_log_dt": [2, 2, G],
    "s5_b_re": [2, 2, G, PST, 16], "s5_b_im": [2, 2, G, PST, 16], "s5_c_re": [2, 2, G, 16, PST], "s5_c_im": [2, 2, G, 16, PST],
    "s5_d": [2, D], "s5_w_glu": [2, D, 2 * D], "ml_w_in": [2, D, ML_IN], "ml_b_gates": [2, 32], "ml_norm": [2, D],
    "ml_w_out": [2, D, D], "ffn_w_in": [4, D, 2 * FH], "ffn_w_out": [4, FH, D],
}


def build_program(S_LAT, layers=(0, 1, 2, 3), debug_x=False, depth_total=4):
    nc = bass.Bass("TRN2", target_bir_lowering=False)
    io = {}
    io["x"] = nc.dram_tensor("x", [S_LAT, D], F32, kind="ExternalInput").ap()
    io["ctx"] = nc.dram_tensor("ctx", [S_CTX, D], F32, kind="ExternalInput").ap()
    io["c"] = nc.dram_tensor("c", [1, D], F32, kind="ExternalInput").ap()
    io["c_ctx"] = nc.dram_tensor("c_ctx", [1, D], F32, kind="ExternalInput").ap()
    for k, shp in W_SHAPES.items():
        io[k] = nc.dram_tensor(k, shp, F32, kind="ExternalInput").ap()
    out = nc.dram_tensor("out", [S_LAT, D], F32, kind="ExternalOutput").ap()
    NTOK = S_CTX + S_LAT

    def scratch(name, shape, dt):
        return nc.dram_tensor(name, shape, dt, kind="Internal").ap()

    P = Prog(nc)
    K = Ctx()
    make_consts(P, K)
    phase_cond(P, K, io)
    xs = [scratch(f"xs{i}", [NTOK, D], F32) if not (debug_x and i == len(layers)) else
          nc.dram_tensor("xdbg", [NTOK, D], F32, kind="ExternalOutput").ap() for i in range(len(layers) + 1)]
    m0 = P.mark()
    cp = Ring([P.sbuf(f"cp{i}", [128, D], F32) for i in range(3)])
    for t in range(NTOK // 128):
        tl = cp.next()
        src = io["ctx"][t * 128:(t + 1) * 128, :] if t < S_CTX // 128 else io["x"][t * 128 - S_CTX:(t + 1) * 128 - S_CTX, :]
        P.dma("sp", tl[:], src, out_t=tl)
        P.dma("act", xs[0][t * 128:(t + 1) * 128, :], tl[:], in_t=tl)
    P.end_phase(cp.tiles)
    P.emit()
    P.release(m0)
    mods = [scratch(f"mod{i}", [2, 6 * D], F32) for i in layers]
    u_tok = scratch("u_tok", [2 * S_CTX + S_LAT, D], BF16)
    mix_tok = scratch("mix_tok", [NTOK, D], BF16)
    wgu_t = scratch("wgu_t", [NHC, 128, KD, 256], BF16)
    wo_t = scratch("wo_t", [NHC, 128, 4, 512], BF16)
    wglu_t = scratch("wglu_t", [16, 128, KD, 256], BF16)
    NT3_ = 2 * S_CTX + S_LAT
    wq_t = scratch("wq_t", [ML_H, 128, KD, 128], BF16)
    wk_t = scratch("wk_t", [ML_H, 128, KD, 128], BF16)
    wtok_t = scratch("wtok_t", [8, 128, KD, 512], BF16)
    wgate_t = scratch("wgate_t", [4, 128, KD, 8], BF16)
    wout_t = scratch("wout_t", [8, 128, KD, 256], BF16)
    qT_d = scratch("qT_d", [1024, NT3_], BF16)
    kT_d = scratch("kT_d", [1024, NT3_], BF16)
    v_d = scratch("v_d", [NT3_, D], BF16)
    o_d = scratch("o_d", [NT3_, D], BF16)
    gT_d = scratch("gT_d", [4, 8, NT3_], F32)
    hf_d = scratch("hf_d", [NTOK, D], F32)
    for li, layer in enumerate(layers):
        last = layer == depth_total - 1
        jl = layer // 2
        phase_ada(P, K, io, layer, mods[li])
        wi = io["ffn_w_in"][layer].rearrange("(k p) n -> p k n", p=128)
        jobs = []
        for c in range(NHC):
            jobs.append(([(lambda t: t[:, :, 0:128], wi[:, :, c * 128:(c + 1) * 128]),
                          (lambda t: t[:, :, 128:256], wi[:, :, FH + c * 128: FH + (c + 1) * 128])], wgu_t[c]))
        phase_precast(P, K, jobs, [128, KD, 256], "a")
        wo_src = io["ffn_w_out"][layer].rearrange("(w cc p) (nt n) -> nt w p cc n", cc=4, p=128, n=512)
        jobs = [([(lambda t: t[:], wo_src[nt, w])], wo_t[nt * 11 + w]) for nt in range(4) for w in range(11)]
        phase_precast(P, K, jobs, [128, 4, 512], "b")
        if layer % 2 == 0:
            wsrc = io["s5_w_glu"][jl].rearrange("(k p) (n c) -> n p k c", p=128, c=256)
            jobs = [([(lambda t: t[:], wsrc[n])], wglu_t[n]) for n in range(16)]
            phase_precast(P, K, jobs, [128, KD, 256], "c")
            phase_b_s5(P, K, io, layer, mods[li], xs[li], u_tok, S_LAT)
            phase_c_s5(P, K, io, jl, u_tok, mix_tok, S_LAT)
            phase_d(P, K, io, layer, mods[li], xs[li], xs[li + 1], mix_tok, [wglu_t[n] for n in range(16)],
                    [wgu_t[c] for c in range(NHC)], [wo_t[c] for c in range(NHC)], S_LAT, "s5", last, out)
        else:
            NT3 = 2 * S_CTX + S_LAT
            win = io["ml_w_in"][jl].rearrange("(k p) n -> p k n", p=128)
            jobs = [([(lambda t: t[:], win[:, :, h * 128:(h + 1) * 128])], wq_t[h]) for h in range(ML_H)]
            jobs += [([(lambda t: t[:], win[:, :, 1024 + h * 128:1024 + (h + 1) * 128])], wk_t[h]) for h in range(ML_H)]
            phase_precast(P, K, jobs, [128, KD, 128], "d")
            jobs = [([(lambda t: t[:], win[:, :, 2048 + j * 512:2048 + (j + 1) * 512])], wtok_t[j]) for j in range(8)]
            phase_precast(P, K, jobs, [128, KD, 512], "e")
            jobs = [([(lambda t: t[:], win[:, :, 6144 + ty * 8:6144 + (ty + 1) * 8])], wgate_t[ty]) for ty in range(4)]
            phase_precast(P, K, jobs, [128, KD, 8], "f")
            wsrc = io["ml_w_out"][jl].rearrange("(k p) (n c) -> n p k c", p=128, c=256)
            jobs = [([(lambda t: t[:], wsrc[n])], wout_t[n]) for n in range(8)]
            phase_precast(P, K, jobs, [128, KD, 256], "g")
            phase_b_ml(P, K, io, layer, mods[li], xs[li], S_LAT, [wq_t[h] for h in range(ML_H)], [wk_t[h] for h in range(ML_H)],
                       [wtok_t[j] for j in range(8)], [wgate_t[ty] for ty in range(4)], qT_d, kT_d, v_d, o_d, gT_d)
            phase_e_ml(P, K, io, jl, S_LAT, qT_d, kT_d, v_d, o_d, gT_d, hf_d, mix_tok)
            phase_d(P, K, io, layer, mods[li], xs[li], xs[li + 1], mix_tok, [wout_t[n] for n in range(8)],
                    [wgu_t[c] for c in range(NHC)], [wo_t[c] for c in range(NHC)], S_LAT, "ml", last, out, tok_rows=True)
    P.barrier_all()
    P.emit()
    print("instructions:", P.n_instr)
    return nc


def phase_b_ml(P, K, io, layer, mod_d, xs, S_LAT, wq_t, wk_t, wtok_t, wgate_t, qT_d, kT_d, v_d, o_d, gT_d):
    m = P.mark()
    ROWS = S_LAT // 64
    S = S_CTX + S_LAT
    xr = Ring([P.sbuf(f"e_x{i}", [128, D], F32) for i in range(2)])
    bfr = Ring([P.sbuf(f"e_bf{i}", [128, D], BF16) for i in range(2)])
    t1 = P.sbuf("e_t1", [128, D], F32)
    uT = P.sbuf("e_uT", [128, KD, 512], BF16)
    wqr = Ring([P.sbuf(f"e_wq{i}", [128, KD, 128], BF16) for i in range(3)])
    wtr = Ring([P.sbuf(f"e_wt{i}", [128, KD, 512], BF16) for i in range(2)])
    wg = P.sbuf("e_wg", [128, 4, KD, 8], BF16)
    obr = Ring([P.sbuf(f"e_ob{i}", [128, 512], BF16) for i in range(4)])
    ogr = Ring([P.sbuf(f"e_og{i}", [8, 512], F32) for i in range(2)])
    statr = Ring([P.sbuf(f"e_st{i}", [128, 4], F32) for i in range(4)])
    ptr = Ring([P.psum(f"e_pt{i}", [128, 8, 128], BF16) for i in range(2)])
    mmr = Ring([P.psum(f"e_mm{i}", [128, 512], F32) for i in range(5)])
    pg = P.psum("e_pg", [8, 512], F32)
    scr = dict(pt=ptr)
    tiles = xr.tiles + bfr.tiles + [t1, uT, wg, pg] + wqr.tiles + wtr.tiles + obr.tiles + ogr.tiles + statr.tiles + ptr.tiles + mmr.tiles
    for ty in range(4):
        P.dma("sp", wg[:, ty], wgate_t[ty], out_t=wg)
    qi = [0]

    def q():
        qi[0] += 1
        return "sp" if qi[0] % 2 == 0 else "act"

    xs_lat = xs[S_CTX:S_CTX + S_LAT, :]
    for row in (1, 0):
        mm_mark = P.mark()
        md = load_mod_tiles(P, K, io, layer, mod_d, row, [("sh", 0, "raw"), ("A", 1, "A")], None, "norm1")
        ntok = S_CTX if row == 1 else S_LAT
        for t0 in range(0, ntok, 512):
            nsub = min(4, (ntok - t0) // 128)
            TT = nsub * 128
            dsts = [t0, S + t0] if row == 1 else [S_CTX + t0]
            for s in range(nsub):
                xt = xr.next()
                if row == 1:
                    P.dma(q(), xt[:], xs[t0 + s * 128: t0 + (s + 1) * 128, :], out_t=xt)
                else:
                    lr = lat_rows(xs_lat, t0 + s * 128, S_LAT)
                    for wi in range(128 // ROWS):
                        P.dma(q(), xt[wi * ROWS:(wi + 1) * ROWS, :], lr[wi], out_t=xt)
                ssq = statr.next()
                junk = bfr.next()
                P.op("act", lambda e, xt=xt, ssq=ssq, junk=junk: e.activation(out=junk[:], in_=xt[:], func=AF.Square, accum_out=ssq[:, 0:1]), reads=[xt], writes=[junk, ssq])
                P.op("act", lambda e, ssq=ssq: e.activation(out=ssq[:, 1:2], in_=ssq[:, 0:1], func=AF.Sqrt, bias=K.eps_t[:, 0:1], scale=1.0 / D), reads=[ssq, K.eps_t], writes=[ssq])
                P.op("dve", lambda e, ssq=ssq: e.reciprocal(out=ssq[:, 2:3], in_=ssq[:, 1:2]), reads=[ssq], writes=[ssq])
                P.op("dve", lambda e, xt=xt, ssq=ssq: e.scalar_tensor_tensor(out=t1[:], in0=xt[:], scalar=ssq[:, 2:3], in1=md["A"][:], op0=ALU.mult, op1=ALU.mult),
                     reads=[xt, ssq, md["A"]], writes=[t1])
                ub = bfr.next()
                P.op("pool", lambda e, ub=ub: e.tensor_tensor(out=ub[:], in0=t1[:], in1=md["sh"][:], op=ALU.add), reads=[t1, md["sh"]], writes=[ub])
                transpose_to(P, K, ub, uT, s * 128, scr)
            for (wt_, dd) in ((wq_t, qT_d), (wk_t, kT_d)):
                for h in range(ML_H):
                    wq = wqr.next()
                    P.dma(q(), wq[:], wt_[h], out_t=wq)
                    ps = mmr.next()
                    for k in range(KD):
                        P.op("pe", lambda e, ps=ps, wq=wq, k=k: e.matmul(ps[:, :TT], lhsT=wq[:, k, :], rhs=uT[:, k, :TT], start=(k == 0), stop=(k == KD - 1)),
                             reads=[wq, uT], writes=[ps])
                    ob = obr.next()
                    P.op("act", lambda e, ob=ob, ps=ps: e.activation(out=ob[:, :TT], in_=ps[:, :TT], func=AF.Copy), reads=[ps], writes=[ob])
                    for p0 in dsts:
                        P.dma(q(), dd[h * 128:(h + 1) * 128, p0:p0 + TT], ob[:, :TT], in_t=ob)
            for j in range(8):
                wt = wtr.next()
                P.dma(q(), wt[:], wtok_t[j], out_t=wt)
                dd, c0 = (v_d, j * 512) if j < 4 else (o_d, (j - 4) * 512)
                for s in range(nsub):
                    ps = mmr.next()
                    for k in range(KD):
                        P.op("pe", lambda e, ps=ps, wt=wt, k=k, s=s: e.matmul(ps[:], lhsT=uT[:, k, s * 128:(s + 1) * 128], rhs=wt[:, k, :], start=(k == 0), stop=(k == KD - 1)),
                             reads=[wt, uT], writes=[ps])
                    ob = obr.next()
                    if (j + s) % 2 == 0:
                        P.op("act", lambda e, ob=ob, ps=ps: e.activation(out=ob[:], in_=ps[:], func=AF.Copy), reads=[ps], writes=[ob])
                    else:
                        P.op("dve", lambda e, ob=ob, ps=ps: e.tensor_copy(out=ob[:], in_=ps[:]), reads=[ps], writes=[ob])
                    for p0 in dsts:
                        P.dma(q(), dd[p0 + s * 128: p0 + (s + 1) * 128, c0:c0 + 512], ob[:], in_t=ob)
            for ty in range(4):
                for k in range(KD):
                    P.op("pe", lambda e, k=k, ty=ty: e.matmul(pg[:, :TT], lhsT=wg[:, ty, k, :], rhs=uT[:, k, :TT], start=(k == 0), stop=(k == KD - 1)),
                         reads=[wg, uT], writes=[pg])
                og = ogr.next()
                P.op("dve", lambda e, og=og: e.tensor_copy(out=og[:, :TT], in_=pg[:, :TT]), reads=[pg], writes=[og])
                for p0 in dsts:
                    P.dma(q(), gT_d[ty, :, p0:p0 + TT], og[:, :TT], in_t=og)
        P.end_phase(list(md.values()))
        P.emit()
        P.release(mm_mark)
    P.end_phase(tiles)
    P.emit()
    P.release(m)


def phase_e_ml(P, K, io, jl, S_LAT, qT_d, kT_d, v_d, o_d, gT_d, hf_d, mix_tok):
    S = S_CTX + S_LAT
    NTOK3 = S + S_CTX
    NC = S // 64
    NC3 = NTOK3 // 64
    CC = S_CTX // 64
    SCALE = float(128 ** -0.5)
    m = P.mark()
    omT = P.sbuf("f_omT", [64, NC, 40], F32)
    clT = P.sbuf("f_clT", [64, NC, 40], F32)
    lamR = P.sbuf("f_lamR", [128, 16, NC], F32)
    lamSR = P.sbuf("f_lamSR", [128, 16, NC], F32)
    m1 = P.mark()
    X = [P.sbuf(f"f_X{i}", [40, S], F32) for i in range(5)]
    Mr = P.sbuf("f_Mr", [40, NC], F32)
    lam = P.sbuf("f_lam", [40, NC], F32)
    bia = P.sbuf("f_bias", [40, 4], F32)
    one = P.sbuf("f_one", [40, 2], F32)
    sel = P.sbuf("f_sel", [40, 16, 128], F32)
    pto = Ring([P.psum(f"f_pto{i}", [64, 12, 40], F32) for i in range(2)])
    pl = P.psum("f_pl", [128, NC], F32)
    ew = lambda eng, fn, r, w: P.op(eng, fn, reads=r, writes=w)
    for t in X:
        ew("pool", lambda e, t=t: e.memset(t[:], 0.0), [], [t])
    ew("pool", lambda e: e.memset(bia[:], 0.0), [], [bia])
    ew("pool", lambda e: e.memset(one[:, 0:1], 1.0), [], [one])
    ew("pool", lambda e: e.memset(one[:, 1:2], 0.0), [], [one])
    bg = io["ml_b_gates"][jl:jl + 1, :]
    for (col, lo, p0) in ((0, 0, 0), (1, 8, 0), (0, 16, 32), (1, 24, 32)):
        P.dma("sp", bia[p0:p0 + 8, col:col + 1], bg[:, lo:lo + 8].rearrange("o h -> h o"), out_t=bia, allow_slow_non_contiguous=True)
    P.dma("sp", X[3][0:8, :], gT_d[0, :, 0:S], out_t=X[3])
    P.dma("act", X[3][32:40, :], gT_d[2, :, S_CTX:NTOK3], out_t=X[3])
    P.dma("sp", X[4][0:8, :], gT_d[1, :, 0:S], out_t=X[4])
    P.dma("act", X[4][32:40, :], gT_d[3, :, S_CTX:NTOK3], out_t=X[4])
    ew("dve", lambda e: e.tensor_scalar(out=bia[:, 2:4], in0=bia[:, 0:2], scalar1=1.0 / GATE_CAP, scalar2=None, op0=ALU.mult), [bia], [bia])
    for (src, dst) in ((X[3], X[0]), (X[4], X[1])):
        ew("dve", lambda e, src=src, dst=dst: e.tensor_copy(out=dst[0:8, :], in_=src[0:8, :]), [src], [dst])
        ew("pool", lambda e, src=src, dst=dst: e.tensor_copy(out=dst[32:40, :], in_=src[32:40, ::-1]), [src], [dst])
    for (t, c) in ((X[0], 2), (X[1], 3)):
        ew("act", lambda e, t=t, c=c: e.activation(out=t[:], in_=t[:], func=AF.Tanh, bias=bia[:, c:c + 1], scale=1.0 / GATE_CAP), [t, bia], [t])
        ew("dve", lambda e, t=t: e.tensor_scalar(out=t[:], in0=t[:], scalar1=GATE_CAP, scalar2=None, op0=ALU.mult), [t], [t])
    ew("act", lambda e: e.activation(out=X[3][:], in_=X[1][:], func=AF.Exp, scale=-1.0), [X[1]], [X[3]])
    ew("dve", lambda e: e.tensor_scalar(out=X[4][:], in0=X[3][:], scalar1=2.0, scalar2=None, op0=ALU.add), [X[3]], [X[4]])
    ew("dve", lambda e: e.reciprocal(out=X[4][:], in_=X[4][:]), [X[4]], [X[4]])
    ew("dve", lambda e: e.tensor_tensor(out=X[4][:], in0=X[4][:], in1=X[3][:], op=ALU.mult), [X[4], X[3]], [X[4]])
    ew("pool", lambda e: e.tensor_tensor(out=X[2][:], in0=X[4][:], in1=X[4][:], op=ALU.mult), [X[4]], [X[2]])
    ew("dve", lambda e: e.tensor_scalar(out=X[3][:], in0=X[2][:], scalar1=1.0 / 15, scalar2=1.0 / 13, op0=ALU.mult, op1=ALU.add), [X[2]], [X[3]])
    for cst in (1.0 / 11, 1.0 / 9, 1.0 / 7, 1.0 / 5, 1.0 / 3, 1.0):
        ew("dve", lambda e: e.tensor_tensor(out=X[3][:], in0=X[3][:], in1=X[2][:], op=ALU.mult), [X[3], X[2]], [X[3]])
        ew("dve", lambda e, cst=cst: e.tensor_scalar(out=X[3][:], in0=X[3][:], scalar1=float(cst), scalar2=None, op0=ALU.add), [X[3]], [X[3]])
    ew("dve", lambda e: e.scalar_tensor_tensor(out=X[1][:], in0=X[4][:], scalar=-2.0, in1=X[3][:], op0=ALU.mult, op1=ALU.mult), [X[4], X[3]], [X[1]])
    ew("dve", lambda e: e.tensor_tensor_scan(out=X[2][:], data0=one[:, 0:1].broadcast_to([40, S]), data1=X[1][:], initial=0.0, op0=ALU.mult, op1=ALU.add),
       [one, X[1]], [X[2]])
    ew("dve", lambda e: e.tensor_tensor(out=X[0][:], in0=X[0][:], in1=X[2][:], op=ALU.subtract), [X[0], X[2]], [X[0]])
    ew("dve", lambda e: e.tensor_tensor_scan(out=X[3][:], data0=one[:, 1:2].broadcast_to([40, S]), data1=X[0][:], initial=0.0, op0=ALU.add, op1=ALU.max),
       [one, X[0]], [X[3]])
    ew("dve", lambda e: e.tensor_copy(out=Mr[:], in_=X[3][:, 63::64]), [X[3]], [Mr])
    mrb = bc(Mr[:], 2, 64)
    ew("dve", lambda e: e.tensor_tensor(out=X[0][:].rearrange("p (n t) -> p n t", t=64), in0=X[0][:].rearrange("p (n t) -> p n t", t=64), in1=mrb, op=ALU.subtract), [X[0], Mr], [X[0]])
    ew("act", lambda e: e.activation(out=X[0][:], in_=X[0][:], func=AF.Exp), [X[0]], [X[0]])
    ew("dve", lambda e: e.tensor_tensor(out=X[2][:].rearrange("p (n t) -> p n t", t=64), in0=X[2][:].rearrange("p (n t) -> p n t", t=64), in1=mrb, op=ALU.add), [X[2], Mr], [X[2]])
    ew("act", lambda e: e.activation(out=X[2][:], in_=X[2][:], func=AF.Exp, scale=-1.0), [X[2]], [X[2]])
    ew("dve", lambda e: e.tensor_scalar(out=lam[:, 0:1], in0=Mr[:, 0:1], scalar1=-1.0, scalar2=None, op0=ALU.mult), [Mr], [lam])
    ew("dve", lambda e: e.tensor_tensor(out=lam[:, 1:NC], in0=Mr[:, 0:NC - 1], in1=Mr[:, 1:NC], op=ALU.subtract), [Mr], [lam])
    ew("act", lambda e: e.activation(out=lam[:], in_=lam[:], func=AF.Exp), [lam], [lam])
    for (src, dst) in ((X[0], X[3]), (X[2], X[4])):
        ew("dve", lambda e, src=src, dst=dst: e.tensor_copy(out=dst[0:8, :], in_=src[0:8, :]), [src], [dst])
        ew("pool", lambda e, src=src, dst=dst: e.tensor_copy(out=dst[32:40, :], in_=src[32:40, ::-1]), [src], [dst])
    for (src, dstT) in ((X[3], omT), (X[4], clT)):
        for k0 in range(0, NC, 12):
            nk = min(12, NC - k0)
            pt = pto.next()
            for kk in range(nk):
                k = k0 + kk
                ew("pe", lambda e, pt=pt, kk=kk, k=k, src=src: e.transpose(out=pt[:, kk, :], in_=src[:, 64 * k:64 * k + 64], identity=K.ident_f[0:40, 0:40]),
                   [src, K.ident_f], [pt])
            ew("act", lambda e, pt=pt, k0=k0, nk=nk, dstT=dstT: e.activation(out=dstT[:, k0:k0 + nk, :], in_=pt[:, 0:nk, :], func=AF.Copy), [pt], [dstT])
    for r in range(16):
        row = (r // 8) * 32 + (r % 8)
        ew("dve", lambda e, r=r, row=row: e.tensor_copy(out=sel[:, r, :], in_=K.ident_f[0:40, row:row + 1].broadcast_to([40, 128])), [K.ident_f], [sel])
    for r in range(16):
        ew("pe", lambda e, r=r: e.matmul(pl[:], lhsT=sel[:, r, :], rhs=lam[:], start=True, stop=True), [sel, lam], [pl])
        ew("act", lambda e, r=r: e.activation(out=lamR[:, r, :], in_=pl[:], func=AF.Copy), [pl], [lamR])
    ew("dve", lambda e: e.tensor_scalar(out=lamSR[:], in0=lamR[:], scalar1=SCALE, scalar2=None, op0=ALU.mult), [lamR], [lamSR])
    P.end_phase(X + [Mr, lam, bia, one, sel, pl] + pto.tiles)
    P.emit()
    P.release(m1)

    qT = P.sbuf("f_qT", [128, NTOK3], BF16)
    kT = P.sbuf("f_kT", [128, NTOK3], BF16)
    vv = P.sbuf("f_vv", [64, NC3, 257], BF16)
    Cf = P.sbuf("f_Cf", [128, 257], F32)
    Csr = Ring([P.sbuf(f"f_Cs{i}", [128, 257], BF16) for i in range(2)])
    Spr = Ring([P.sbuf(f"f_Sp{i}", [64, 64], BF16) for i in range(3)])
    kwr = Ring([P.sbuf(f"f_kw{i}", [64, 128], BF16) for i in range(3)])
    mask = [P.sbuf(f"f_mask{i}", [64, 64], F32) for i in range(2)]
    dnr = Ring([P.sbuf(f"f_dn{i}", [64, 6], F32) for i in range(4)])
    hfr = Ring([P.sbuf(f"f_hf{i}", [64, 256], F32) for i in range(3)])
    hsr = Ring([P.sbuf(f"f_hs{i}", [64, 256], F32) for i in range(3)])
    oor = Ring([P.sbuf(f"f_oo{i}", [64, 256], BF16) for i in range(3)])
    sgr = Ring([P.sbuf(f"f_sg{i}", [64, 256], F32) for i in range(2)])
    hor = Ring([P.sbuf(f"f_ho{i}", [64, 256], BF16) for i in range(3)])
    junk = P.sbuf("f_junk", [64, 256], BF16)
    nw = P.sbuf("f_nw", [64, D], F32)
    eps64 = P.sbuf("f_eps", [64, 1], F32)
    pS = Ring([P.psum(f"f_pS{i}", [64, 64], F32) for i in range(2)])
    pK = Ring([P.psum(f"f_pK{i}", [64, 128], BF16) for i in range(2)])
    pH = Ring([P.psum(f"f_pH{i}", [64, 257], F32) for i in range(2)])
    pC = Ring([P.psum(f"f_pC{i}", [128, 257], F32) for i in range(2)])
    tiles = ([qT, kT, vv, Cf, junk, nw, eps64, omT, clT, lamR, lamSR] + Csr.tiles + Spr.tiles + kwr.tiles + mask + dnr.tiles + hfr.tiles + hsr.tiles + oor.tiles
             + sgr.tiles + hor.tiles + pS.tiles + pK.tiles + pH.tiles + pC.tiles)
    ew("pool", lambda e: e.memset(eps64[:], EPS), [], [eps64])
    for i, (cm, pat) in enumerate(((-1, 1), (1, -1))):
        ew("pool", lambda e, i=i: e.memset(mask[i][:], SCALE), [], [mask[i]])
        ew("pool", lambda e, i=i, cm=cm, pat=pat: e.affine_select(out=mask[i][:], in_=mask[i][:], compare_op=ALU.is_ge, fill=0.0, base=0,
                                                                  pattern=[[pat, 64]], channel_multiplier=cm), [mask[i]], [mask[i]])
    ew("pool", lambda e: e.memset(vv[:, :, 256:257], 1.0), [], [vv])
    load_rep_n = lambda: P.dma("sp", nw[:], io["ml_norm"][jl:jl + 1, :].partition_broadcast(64), out_t=nw)
    load_rep_n()

    for h in range(ML_H):
        P.dma("sp", qT[:], qT_d[h * 128:(h + 1) * 128, :], out_t=qT)
        P.dma("act", kT[:], kT_d[h * 128:(h + 1) * 128, :], out_t=kT)
        half = NC3 // 2
        P.dma("sp", vv[:, 0:half, 0:256], v_d[0:half * 64, h * 256:(h + 1) * 256].rearrange("(n t) e -> t n e", t=64), out_t=vv)
        P.dma("act", vv[:, half:NC3, 0:256], v_d[half * 64:NTOK3, h * 256:(h + 1) * 256].rearrange("(n t) e -> t n e", t=64), out_t=vv)
        for d in range(2):
            r = d * 8 + h
            ew("dve", lambda e: e.memset(Cf[:], 0.0), [], [Cf])
            Cs = Csr.next()
            ew("pool", lambda e, Cs=Cs: e.memset(Cs[:], 0.0), [], [Cs])

            def front(mi):
                c = mi if d == 0 else NC3 - 1 - mi
                k = c if d == 0 else c - CC
                cols = slice(64 * c, 64 * c + 64)
                ps = pS.next()
                ew("pe", lambda e, ps=ps, cols=cols: e.matmul(ps[:], lhsT=kT[:, cols], rhs=qT[:, cols], start=True, stop=True), [kT, qT], [ps])
                pk = pK.next()
                ew("pe", lambda e, pk=pk, cols=cols: e.transpose(out=pk[:], in_=kT[:, cols], identity=K.ident_b[:]), [kT, K.ident_b], [pk])
                Sp = Spr.next()
                om = omT[:, k, 32 * d + h: 32 * d + h + 1]
                ew("dve", lambda e, Sp=Sp, ps=ps, om=om: e.scalar_tensor_tensor(out=Sp[:], in0=ps[:], scalar=om, in1=mask[d][:], op0=ALU.mult, op1=ALU.mult),
                   [ps, omT, mask[d]], [Sp])
                kw = kwr.next()
                ew("act", lambda e, kw=kw, pk=pk, om=om: e.activation(out=kw[:], in_=pk[:], func=AF.Copy, scale=om), [pk, omT], [kw])
                return (c, k, cols, Sp, kw)

            nxt = front(0)
            for mi in range(NC):
                c, k, cols, Sp, kw = nxt
                if mi + 1 < NC:
                    nxt = front(mi + 1)
                tc = c if c < NC else c - NC
                ph = pH.next()
                ew("pe", lambda e, ph=ph, Sp=Sp, c=c: e.matmul(ph[:], lhsT=Sp[:], rhs=vv[:, c, :], start=True, stop=False), [Sp, vv], [ph])
                ew("pe", lambda e, ph=ph, Cs=Cs, cols=cols: e.matmul(ph[:], lhsT=qT[:, cols], rhs=Cs[:], start=False, stop=True), [qT, Cs], [ph])
                pc = pC.next()
                ew("pe", lambda e, pc=pc, kw=kw, c=c: e.matmul(pc[:], lhsT=kw[:], rhs=vv[:, c, :], start=True, stop=True), [kw, vv], [pc])
                ew("dve", lambda e, pc=pc, mi=mi, r=r: e.scalar_tensor_tensor(out=Cf[:], in0=Cf[:], scalar=lamR[:, r, mi:mi + 1], in1=pc[:], op0=ALU.mult, op1=ALU.add),
                   [Cf, lamR, pc], [Cf])
                if mi + 1 < NC:
                    Cs = Csr.next()
                    ew("act", lambda e, Cs=Cs, mi=mi, r=r: e.activation(out=Cs[:], in_=Cf[:], func=AF.Copy, scale=lamSR[:, r, mi + 1:mi + 2]), [Cf, lamSR], [Cs])
                dn = dnr.next()
                ew("act", lambda e, dn=dn, ph=ph: e.activation(out=dn[:, 4:5], in_=ph[:, 256:257], func=AF.Copy), [ph], [dn])
                ew("dve", lambda e, dn=dn: e.scalar_tensor_tensor(out=dn[:, 0:1], in0=dn[:, 4:5], scalar=-1.0, in1=dn[:, 4:5], op0=ALU.mult, op1=ALU.max),
                   [dn], [dn])
                ew("dve", lambda e, dn=dn, k=k: e.tensor_tensor(out=dn[:, 1:2], in0=dn[:, 0:1], in1=clT[:, k, 32 * d + h: 32 * d + h + 1], op=ALU.max), [dn, clT], [dn])
                ew("dve", lambda e, dn=dn: e.reciprocal(out=dn[:, 2:3], in_=dn[:, 1:2]), [dn], [dn])
                rows = slice(64 * tc, 64 * tc + 64)
                hcols = slice(h * 256, (h + 1) * 256)
                if d == 0:
                    hf = hfr.next()
                    ew("act", lambda e, hf=hf, ph=ph, dn=dn: e.activation(out=hf[:], in_=ph[:, 0:256], func=AF.Copy, scale=dn[:, 2:3]), [ph, dn], [hf])
                    P.dma("sp", hf_d[rows, hcols], hf[:], in_t=hf)
                else:
                    hf = hfr.next()
                    P.dma("sp", hf[:], hf_d[rows, hcols], out_t=hf)
                    oo = oor.next()
                    P.dma("act", oo[:], o_d[rows, hcols], out_t=oo)
                    hs = hsr.next()
                    ew("dve", lambda e, hs=hs, ph=ph, dn=dn, hf=hf: e.scalar_tensor_tensor(out=hs[:], in0=ph[:, 0:256], scalar=dn[:, 2:3], in1=hf[:], op0=ALU.mult, op1=ALU.add),
                       [ph, dn, hf], [hs])
                    ew("act", lambda e, hs=hs, dn=dn: e.activation(out=junk[:], in_=hs[:], func=AF.Square, accum_out=dn[:, 3:4]), [hs], [junk, dn])
                    ew("act", lambda e, dn=dn: e.activation(out=dn[:, 3:4], in_=dn[:, 3:4], func=AF.Sqrt, bias=eps64[:, 0:1], scale=1.0 / 256), [dn, eps64], [dn])
                    ew("dve", lambda e, dn=dn: e.reciprocal(out=dn[:, 3:4], in_=dn[:, 3:4]), [dn], [dn])
                    sg = sgr.next()
                    ew("act", lambda e, sg=sg, oo=oo: e.activation(out=sg[:], in_=oo[:], func=AF.Sigmoid), [oo], [sg])
                    ew("dve", lambda e, hs=hs, dn=dn, hcols=hcols: e.scalar_tensor_tensor(out=hs[:], in0=hs[:], scalar=dn[:, 3:4], in1=nw[:, hcols], op0=ALU.mult, op1=ALU.mult),
                       [hs, dn, nw], [hs])
                    ho = hor.next()
                    ew("pool", lambda e, ho=ho, hs=hs, sg=sg: e.tensor_tensor(out=ho[:], in0=hs[:], in1=sg[:], op=ALU.mult), [hs, sg], [ho])
                    P.dma("act", mix_tok[rows, hcols], ho[:], in_t=ho)
            if d == 0:
                P.barrier_all()
            P.emit()
    P.end_phase(tiles)
    P.emit()
    P.release(m)


_NC_CACHE = {}


def kernel(**inputs):
    S_LAT = inputs["x"].shape[1]
    B = inputs["x"].shape[0]
    if S_LAT not in _NC_CACHE:
        _NC_CACHE[S_LAT] = build_program(S_LAT)
    nc = _NC_CACHE[S_LAT]
    shared = {}
    for k in W_SHAPES:
        a = np.ascontiguousarray(np.asarray(inputs[k], dtype=np.float32))
        if k == "norm_f":
            a = a.reshape(1, D)
        shared[k] = a
    shared["c_ctx"] = np.ascontiguousarray(np.asarray(inputs["c_ctx"], dtype=np.float32)).reshape(1, D)
    n_cores = 8
    in_maps = []
    for core in range(n_cores):
        b = core % B
        mp = dict(shared)
        mp["x"] = np.ascontiguousarray(np.asarray(inputs["x"][b], dtype=np.float32))
        mp["ctx"] = np.ascontiguousarray(np.asarray(inputs["ctx"][b], dtype=np.float32))
        mp["c"] = np.ascontiguousarray(np.asarray(inputs["c"][b:b + 1], dtype=np.float32))
        in_maps.append(mp)
    res = run_bass_kernel_spmd(nc, in_maps, core_ids=list(range(n_cores)))
    out = np.stack([np.asarray(res.results[b]["out"]) for b in range(B)], axis=0)
    return out.astype(np.float32)
```

```python
import numpy as np
import concourse.bass as bass
import concourse.mybir as mybir
from concourse.bass_utils import run_bass_kernel_spmd

F32 = mybir.dt.float32
BF16 = mybir.dt.bfloat16
AF = mybir.ActivationFunctionType
ALU = mybir.AluOpType
AX = mybir.AxisListType

ENGS = ("pe", "act", "dve", "pool", "sp")

D = 2048
KD = D // 128
FH = 5632
NHC = FH // 128
S_CTX = 256
EPS = 1e-6
T0 = 8
G = 128
PST = 64
ML_H = 8
ML_IN = 6176
GATE_CAP = 15.0


class T:
    def __init__(self, h, name):
        self.h = h
        self.name = name
        self.last_w = None
        self.readers = []
        self.ld_sem = None
        self.ld_cnt = 0
        self.st_sem = None
        self.st_cnt = 0

    def __getitem__(self, k):
        return self.h[k]


class Prog:
    def __init__(self, nc, same_engine_sync=True):
        self.nc = nc
        self.ops = {e: [] for e in ENGS}
        self.cnt = {e: 0 for e in ENGS}
        self.waited = {}
        self.esem = {}
        self.same_engine_sync = same_engine_sync
        self.all_st = []
        self._stack = []
        self.dma_sems = []
        self.dma_sem_i = 0
        self.n_instr = 0
        for e in ("pe", "act", "dve", "pool"):
            self.esem[e] = nc.alloc_semaphore("prog_" + e)
        self.free_sems = [nc.alloc_semaphore(f"dsem{i}") for i in range(90)]
        self.sem_users = {}

    def sbuf(self, name, shape, dt):
        self.uid = getattr(self, "uid", 0) + 1
        name = f"{name}_{self.uid}"
        cm = self.nc.sbuf_tensor(name, list(shape), dt)
        h = cm.__enter__()
        self._stack.append(cm)
        return T(h, name)

    def psum(self, name, shape, dt=F32):
        self.uid = getattr(self, "uid", 0) + 1
        name = f"{name}_{self.uid}"
        cm = self.nc.psum_tensor(name, list(shape), dt)
        h = cm.__enter__()
        self._stack.append(cm)
        return T(h, name)

    def mark(self):
        return len(self._stack)

    def release(self, mark):
        while len(self._stack) > mark:
            cm = self._stack.pop()
            cm.__exit__(None, None, None)

    def _get_sem(self, t, kind):
        if not self.free_sems:
            raise RuntimeError("out of DMA semaphores")
        return self.free_sems.pop()

    def _need(self, eng, dep, waits):
        if dep is None:
            return
        if dep[0] == "eng":
            _, e2, seq = dep
            if e2 == eng and (eng == "pe" or not self.same_engine_sync):
                return
            key = (eng, "eng", e2)
            if self.waited.get(key, 0) >= seq:
                return
            self.waited[key] = seq
            waits.append((self.esem[e2], seq))
        else:
            _, sem, val = dep
            key = (eng, "sem", sem.num)
            if self.waited.get(key, 0) >= val:
                return
            self.waited[key] = val
            waits.append((sem, val))

    def op(self, eng, fn, reads=(), writes=()):
        waits = []
        for t in reads:
            self._need(eng, t.last_w, waits)
        for t in writes:
            self._need(eng, t.last_w, waits)
            for r in t.readers:
                self._need(eng, r, waits)
        self.cnt[eng] += 1
        seq = self.cnt[eng]
        me = ("eng", eng, seq)
        for t in writes:
            t.last_w = me
            t.readers = []
        for t in reads:
            if t.last_w is not me:
                t.readers.append(me)
                if len(t.readers) > 48:
                    best = {}
                    for r in t.readers:
                        k = (r[0], r[1] if r[0] == "eng" else r[1].num)
                        if k not in best or best[k][2] < r[2]:
                            best[k] = r
                    t.readers = list(best.values())
        self.ops[eng].append((waits, fn, (self.esem[eng], 1)))
        self.n_instr += 1

    def dma(self, q, out, in_, out_t=None, in_t=None, **kw):
        waits = []
        if in_t is not None:
            self._need(q, in_t.last_w, waits)
        if out_t is not None:
            self._need(q, out_t.last_w, waits)
            for r in out_t.readers:
                self._need(q, r, waits)
        if out_t is not None:
            if out_t.ld_sem is None:
                out_t.ld_sem = self._get_sem(out_t, "ld")
                out_t.ld_cnt = self.sem_users.get(out_t.ld_sem.num, 0)
            out_t.ld_cnt += 16
            self.sem_users[out_t.ld_sem.num] = out_t.ld_cnt
            sem, val = out_t.ld_sem, out_t.ld_cnt
            out_t.last_w = ("dma", sem, val)
            out_t.readers = []
            if in_t is not None:
                in_t.readers.append(("dma", sem, val))
        else:
            if in_t.st_sem is None:
                in_t.st_sem = self._get_sem(in_t, "st")
                in_t.st_cnt = self.sem_users.get(in_t.st_sem.num, 0)
                self.all_st.append(in_t)
            in_t.st_cnt += 16
            self.sem_users[in_t.st_sem.num] = in_t.st_cnt
            sem, val = in_t.st_sem, in_t.st_cnt
            in_t.readers.append(("dma", sem, val))

        def fn(e, out=out, in_=in_, kw=kw):
            return e.dma_start(out=out, in_=in_, **kw)

        self.ops[q].append((waits, fn, (sem, 16)))
        self.n_instr += 1

    def barrier_all(self):
        for e in ENGS:
            waits = []
            for e2 in ("pe", "act", "dve", "pool"):
                if self.cnt[e2] > 0:
                    self._need(e, ("eng", e2, self.cnt[e2]), waits)
            for t in self.all_st:
                self._need(e, ("dma", t.st_sem, t.st_cnt), waits)
            if waits:
                self.ops[e].append((waits, None, None))

    def end_phase(self, tiles):
        for t in tiles:
            if t.ld_sem is not None and t.last_w is not None and t.last_w[0] == "dma":
                w = []
                self._need("sp", t.last_w, w)
                if w:
                    self.ops["sp"].append((w, None, None))
        self.barrier_all()
        for t in tiles:
            for s in (t.ld_sem, t.st_sem):
                if s is not None:
                    self.free_sems.append(s)
            if t in self.all_st:
                self.all_st.remove(t)
            t.ld_sem = None
            t.st_sem = None

    def emit(self):
        nc = self.nc
        ops = self.ops

        def run(eng_obj, lst):
            for waits, fn, inc in lst:
                for sem, val in waits:
                    eng_obj.wait_ge(sem, val)
                if fn is not None:
                    ins = fn(eng_obj)
                    ins.then_inc(inc[0], inc[1])

        with nc.Block() as block:
            @block.tensor
            def _(e):
                run(e, ops["pe"])

            @block.scalar
            def _(e):
                run(e, ops["act"])

            @block.vector
            def _(e):
                run(e, ops["dve"])

            @block.gpsimd
            def _(e):
                run(e, ops["pool"])

            @block.sync
            def _(e):
                run(e, ops["sp"])
        self.ops = {e: [] for e in ENGS}


class Ring:
    def __init__(self, tiles):
        self.tiles = tiles
        self.i = 0

    def next(self):
        t = self.tiles[self.i % len(self.tiles)]
        self.i += 1
        return t


def col_blocks(lo, hi, step=512):
    out = []
    a = lo
    while a < hi:
        b = min(hi, (a // step + 1) * step)
        out.append((a, b))
        a = b
    return out


class Ctx:
    pass


def make_consts(P, K):
    K.ident_b = P.sbuf("ident_b", [128, 128], BF16)
    K.ident_f = P.sbuf("ident_f", [128, 128], F32)
    for t in (K.ident_b, K.ident_f):
        P.op("pool", lambda e, t=t: e.memset(t[:], 0.0), writes=[t])
        P.op("pool", lambda e, t=t: e.affine_select(out=t[:], in_=t[:], compare_op=ALU.not_equal, fill=1.0,
                                                    base=0, pattern=[[-1, 128]], channel_multiplier=1),
             reads=[t], writes=[t])
    K.eps_t = P.sbuf("eps_t", [128, 1], F32)
    P.op("pool", lambda e: e.memset(K.eps_t[:], EPS), writes=[K.eps_t])
    K.cbias = P.sbuf("cbias", [128, 8], F32)
    for k in range(8):
        P.op("pool", lambda e, k=k: e.memset(K.cbias[:, k:k + 1], -(2 * k - 1) * float(np.pi)), writes=[K.cbias])


def phase_precast(P, K, jobs, shape, tag):
    m = P.mark()
    f32r = Ring([P.sbuf(f"pc_f{i}_{tag}", shape, F32) for i in range(4)])
    b16r = Ring([P.sbuf(f"pc_b{i}_{tag}", shape, BF16) for i in range(4)])
    tiles = f32r.tiles + b16r.tiles
    for i, (parts, d) in enumerate(jobs):
        tf = f32r.next()
        tb = b16r.next()
        for pi_, (sl, s_) in enumerate(parts):
            P.dma("sp" if (i + pi_) % 2 == 0 else "act", sl(tf), s_, out_t=tf)
        P.op("pool", lambda e, tb=tb, tf=tf: e.tensor_copy(out=tb[:], in_=tf[:]), reads=[tf], writes=[tb])
        P.dma("sp" if i % 2 == 1 else "act", d, tb[:], in_t=tb)
    P.end_phase(tiles)
    P.emit()
    P.release(m)


def load_rep(P, q, tile, dram_row_ap):
    P.dma(q, tile[:], dram_row_ap.partition_broadcast(128), out_t=tile)


def rmsnorm_mod_T(P, K, xt, A_rep, sh_rep, uT, col0, scr):
    junk = scr["junk"]
    ssq = scr["stat"].next()
    P.op("act", lambda e: e.activation(out=junk[:], in_=xt[:], func=AF.Square, accum_out=ssq[:, 0:1]),
         reads=[xt], writes=[junk, ssq])
    P.op("act", lambda e: e.activation(out=ssq[:, 1:2], in_=ssq[:, 0:1], func=AF.Sqrt, bias=K.eps_t[:, 0:1], scale=1.0 / D),
         reads=[ssq, K.eps_t], writes=[ssq])
    P.op("dve", lambda e: e.reciprocal(out=ssq[:, 2:3], in_=ssq[:, 1:2]), reads=[ssq], writes=[ssq])
    t1 = scr["t1"]
    P.op("dve", lambda e: e.scalar_tensor_tensor(out=t1[:], in0=xt[:], scalar=ssq[:, 2:3], in1=A_rep[:],
                                                  op0=ALU.mult, op1=ALU.mult), reads=[xt, ssq, A_rep], writes=[t1])
    ub = scr["ub"].next()
    P.op("pool", lambda e: e.tensor_tensor(out=ub[:], in0=t1[:], in1=sh_rep[:], op=ALU.add), reads=[t1, sh_rep], writes=[ub])
    transpose_to(P, K, ub, uT, col0, scr)
    return ssq


def transpose_to(P, K, ub, uT, col0, scr):
    for g in range(KD // 8):
        pt = scr["pt"].next()
        for j in range(8):
            k = g * 8 + j
            P.op("pe", lambda e, pt=pt, j=j, k=k: e.transpose(out=pt[:, j, :], in_=ub[:, k * 128:(k + 1) * 128], identity=K.ident_b[:]),
                 reads=[ub, K.ident_b], writes=[pt])
        eng = "act" if g % 2 == 0 else "dve"
        if eng == "act":
            P.op("act", lambda e, pt=pt, g=g: e.activation(out=uT[:, g * 8:(g + 1) * 8, col0:col0 + 128], in_=pt[:], func=AF.Copy),
                 reads=[pt], writes=[uT])
        else:
            P.op("dve", lambda e, pt=pt, g=g: e.tensor_copy(out=uT[:, g * 8:(g + 1) * 8, col0:col0 + 128], in_=pt[:]),
                 reads=[pt], writes=[uT])


def phase_cond(P, K, io):
    K.condT = P.sbuf("condT", [128, KD, 2], F32)
    m = P.mark()
    craw = P.sbuf("craw", [KD, 2, 128], F32)
    csil = P.sbuf("csil", [KD, 2, 128], F32)
    pt = P.psum("cond_pt", [128, 2, KD], F32)
    P.dma("sp", craw[:, 0, :], io["c"].rearrange("o (k p) -> (o k) p", p=128), out_t=craw)
    P.dma("sp", craw[:, 1, :], io["c_ctx"].rearrange("o (k p) -> (o k) p", p=128), out_t=craw)
    P.op("act", lambda e: e.activation(out=csil[:], in_=craw[:], func=AF.Silu), reads=[craw], writes=[csil])
    for r in range(2):
        P.op("pe", lambda e, r=r: e.transpose(out=pt[:, r, :], in_=csil[:, r, :], identity=K.ident_f[0:KD, 0:KD]),
             reads=[csil, K.ident_f], writes=[pt])
    P.op("dve", lambda e: e.tensor_copy(out=K.condT[:].rearrange("p k r -> p r k"), in_=pt[:]), reads=[pt], writes=[K.condT])
    P.end_phase([craw, csil, pt])
    P.emit()
    P.release(m)


def phase_ada(P, K, io, layer, mod_out):
    m = P.mark()
    NB = 6 * D // 512
    wr = Ring([P.sbuf(f"ada_w{i}", [128, KD, 512], F32) for i in range(4)])
    br = Ring([P.sbuf(f"ada_b{i}", [2, 512], F32) for i in range(2)])
    orr = Ring([P.sbuf(f"ada_o{i}", [2, 512], F32) for i in range(2)])
    pr = Ring([P.psum(f"ada_p{i}", [2, 512], F32) for i in range(2)])
    aw = io["ada_w"][layer].rearrange("(k p) n -> p k n", p=128)
    ab = io["ada_b"][layer:layer + 1, :]
    for nb in range(NB):
        wt = wr.next(); bt = br.next(); ot = orr.next(); ps = pr.next()
        cs = slice(nb * 512, (nb + 1) * 512)
        P.dma("sp" if nb % 2 == 0 else "act", wt[:], aw[:, :, cs], out_t=wt)
        P.dma("sp", bt[:], ab[:, cs].partition_broadcast(2), out_t=bt)
        for k in range(KD):
            P.op("pe", lambda e, ps=ps, wt=wt, k=k: e.matmul(ps[:], lhsT=K.condT[:, k, :], rhs=wt[:, k, :], start=(k == 0), stop=(k == KD - 1)),
                 reads=[K.condT, wt], writes=[ps])
        P.op("dve", lambda e, ot=ot, ps=ps, bt=bt: e.tensor_tensor(out=ot[:], in0=ps[:], in1=bt[:], op=ALU.add), reads=[ps, bt], writes=[ot])
        P.dma("sp", mod_out[:, cs], ot[:], in_t=ot)
    P.end_phase(wr.tiles + br.tiles + orr.tiles + pr.tiles)
    P.emit()
    P.release(m)


def load_mod_tiles(P, K, io, layer, mod_d, row, which, names, normkey):
    out = {}
    gt = None
    for name, ci, kind in which:
        t = P.sbuf(f"mod_{name}", [128, D], F32)
        load_rep(P, "sp", t, mod_d[row:row + 1, ci * D:(ci + 1) * D])
        if kind == "A":
            if gt is None:
                gt = P.sbuf("mod_g", [128, D], F32)
                load_rep(P, "act", gt, io[normkey][layer:layer + 1, :])
            P.op("dve", lambda e, t=t, gt=gt: e.scalar_tensor_tensor(out=t[:], in0=t[:], scalar=1.0, in1=gt[:], op0=ALU.add, op1=ALU.mult),
                 reads=[t, gt], writes=[t])
        out[name] = t
    if gt is not None:
        out["_g"] = gt
    return out


def lat_rows(ap_lat, p0, S_LAT, n=128):
    ROWS = S_LAT // 64
    w0, nw = p0 // ROWS, n // ROWS
    return ap_lat.rearrange("(r w) d -> w r d", w=64)[w0:w0 + nw]


def token_tiles(S_LAT):
    tl = [(1, 0, S_CTX // 128)]
    for t0 in range(0, S_LAT, 512):
        tl.append((0, S_CTX + t0, min(4, (S_LAT - t0) // 128)))
    return tl


def phase_b_s5(P, K, io, layer, mod_d, xs, u_tok, S_LAT):
    m = P.mark()
    xr = Ring([P.sbuf(f"b_x{i}", [128, D], F32) for i in range(3)])
    scr = dict(junk=P.sbuf("b_junk", [128, D], BF16), t1=P.sbuf("b_t1", [128, D], F32),
               stat=Ring([P.sbuf(f"b_st{i}", [128, 4], F32) for i in range(4)]),
               ub=Ring([P.sbuf(f"b_ub{i}", [128, D], BF16) for i in range(3)]))
    tiles = xr.tiles + [scr["junk"], scr["t1"]] + scr["stat"].tiles + scr["ub"].tiles
    for row in (1, 0):
        mm = P.mark()
        md = load_mod_tiles(P, K, io, layer, mod_d, row, [("sh", 0, "raw"), ("A", 1, "A")], None, "norm1")
        ntok = S_CTX if row == 1 else S_LAT
        base = 0 if row == 1 else S_CTX
        for t in range(ntok // 128):
            xt = xr.next()
            P.dma("sp", xt[:], xs[base + t * 128: base + (t + 1) * 128, :], out_t=xt)
            ssq = scr["stat"].next()
            junk = scr["junk"]
            P.op("act", lambda e, xt=xt, ssq=ssq: e.activation(out=junk[:], in_=xt[:], func=AF.Square, accum_out=ssq[:, 0:1]),
                 reads=[xt], writes=[junk, ssq])
            P.op("act", lambda e, ssq=ssq: e.activation(out=ssq[:, 1:2], in_=ssq[:, 0:1], func=AF.Sqrt, bias=K.eps_t[:, 0:1], scale=1.0 / D),
                 reads=[ssq, K.eps_t], writes=[ssq])
            P.op("dve", lambda e, ssq=ssq: e.reciprocal(out=ssq[:, 2:3], in_=ssq[:, 1:2]), reads=[ssq], writes=[ssq])
            t1 = scr["t1"]
            P.op("dve", lambda e, xt=xt, ssq=ssq: e.scalar_tensor_tensor(out=t1[:], in0=xt[:], scalar=ssq[:, 2:3], in1=md["A"][:],
                                                                          op0=ALU.mult, op1=ALU.mult), reads=[xt, ssq, md["A"]], writes=[t1])
            ub = scr["ub"].next()
            P.op("pool", lambda e, ub=ub: e.tensor_tensor(out=ub[:], in0=t1[:], in1=md["sh"][:], op=ALU.add), reads=[t1, md["sh"]], writes=[ub])
            P.dma("act", u_tok[base + t * 128: base + (t + 1) * 128, :], ub[:], in_t=ub)
            if row == 1:
                P.dma("act", u_tok[S_CTX + S_LAT + t * 128: S_CTX + S_LAT + (t + 1) * 128, :], ub[:], in_t=ub)
        P.end_phase(list(md.values()))
        P.emit()
        P.release(mm)
    P.end_phase(tiles)
    P.emit()
    P.release(m)


def phase_d(P, K, io, layer, mod_d, xs_in, xs_out, mix_tok, wproj, wgu_t, wo_t, S_LAT, mixer, last, out_final, tok_rows=None):
    m = P.mark()
    xt = [P.sbuf(f"d_x{i}", [128, D], F32) for i in range(4)]
    bfr = Ring([P.sbuf(f"d_bf{i}", [128, D], BF16) for i in range(2)])
    u2T = P.sbuf("d_u2T", [128, KD, 512], BF16)
    hT = P.sbuf("d_hT", [128, NHC, 512], BF16)
    wpr = Ring([P.sbuf(f"d_wp{i}", [128, KD, 256], BF16) for i in range(2)])
    wgr = Ring([P.sbuf(f"d_wgu{i}", [128, KD, 256], BF16) for i in range(2)])
    wor = Ring([P.sbuf(f"d_wo{i}", [128, 4, 512], BF16) for i in range(3)])
    sgr = Ring([P.sbuf(f"d_sg{i}", [128, 512], BF16) for i in range(2)])
    f1r = Ring([P.sbuf(f"d_f1{i}", [128, 512], F32) for i in range(2)])
    f2r = Ring([P.sbuf(f"d_f2{i}", [128, 512], F32) for i in range(2)])
    t1 = P.sbuf("d_t1", [128, D], F32)
    gcur = P.sbuf("d_gcur", [128, D], F32)
    statr = Ring([P.sbuf(f"d_st{i}", [128, 4], F32) for i in range(4)])
    ptr = Ring([P.psum(f"d_pt{i}", [128, 8, 128], BF16) for i in range(2)])
    mmr = Ring([P.psum(f"d_mm{i}", [128, 512], F32) for i in range(6)])
    tiles = (xt + bfr.tiles + [u2T, hT, t1, gcur] + wpr.tiles + wgr.tiles + wor.tiles + sgr.tiles + f1r.tiles + f2r.tiles
             + statr.tiles + ptr.tiles + mmr.tiles)
    scr = dict(pt=ptr)
    qi = [0]

    def q():
        qi[0] += 1
        return "sp" if qi[0] % 2 == 0 else "act"

    ROWS = S_LAT // 64

    def xdma(tile, ap, tok0, s, load):
        has_ctx = ap.shape[0] == S_CTX + S_LAT
        if tok_rows is None or tok0 < S_CTX:
            off = 0 if has_ctx else -S_CTX
            d_ap = ap[tok0 + off + s * 128: tok0 + off + (s + 1) * 128, :]
            s_ap = tile[:]
        else:
            lat = ap[S_CTX:S_CTX + S_LAT, :] if has_ctx else ap
            lr = lat_rows(lat, tok0 - S_CTX + s * 128, S_LAT)
            for wi in range(128 // ROWS):
                if load:
                    P.dma(q(), tile[wi * ROWS:(wi + 1) * ROWS, :], lr[wi], out_t=tile)
                else:
                    P.dma(q(), lr[wi], tile[wi * ROWS:(wi + 1) * ROWS, :], in_t=tile)
            return
        if load:
            P.dma(q(), s_ap, d_ap, out_t=tile)
        else:
            P.dma(q(), d_ap, s_ap, in_t=tile)

    def norm_stats(x_t):
        ssq = statr.next()
        junk = bfr.next()
        P.op("act", lambda e: e.activation(out=junk[:], in_=x_t[:], func=AF.Square, accum_out=ssq[:, 0:1]), reads=[x_t], writes=[junk, ssq])
        P.op("act", lambda e: e.activation(out=ssq[:, 1:2], in_=ssq[:, 0:1], func=AF.Sqrt, bias=K.eps_t[:, 0:1], scale=1.0 / D),
             reads=[ssq, K.eps_t], writes=[ssq])
        P.op("dve", lambda e: e.reciprocal(out=ssq[:, 2:3], in_=ssq[:, 1:2]), reads=[ssq], writes=[ssq])
        return ssq

    cur_row = None
    md = None
    mm_mark = None
    for (row, tok0, nsub) in token_tiles(S_LAT):
        if last and row == 1:
            continue
        if row != cur_row:
            if md is not None:
                P.end_phase(list(md.values()))
                P.emit()
                P.release(mm_mark)
            mm_mark = P.mark()
            md = {}
            if last:
                md["nf"] = P.sbuf("mod_nf", [128, D], F32)
                load_rep(P, "sp", md["nf"], io["norm_f"])
            md.update(load_mod_tiles(P, K, io, layer, mod_d, row, [("sh2", 3, "raw"), ("A2", 4, "A")], None, "norm2"))
            cur_row = row
        TT = nsub * 128
        load_rep(P, "sp", gcur, mod_d[row:row + 1, 2 * D:3 * D])
        for s in range(nsub):
            xdma(xt[s], xs_in, tok0, s, True)
            mb = bfr.next()
            P.dma(q(), mb[:], mix_tok[tok0 + s * 128: tok0 + (s + 1) * 128, :], out_t=mb)
            transpose_to(P, K, mb, hT, s * 128, scr)
        for nb in range(8):
            wa = wpr.next()
            P.dma(q(), wa[:], wproj[nb], out_t=wa)
            if mixer == "s5":
                wb = wpr.next()
                P.dma(q(), wb[:], wproj[8 + nb], out_t=wb)
            cs = slice(nb * 256, (nb + 1) * 256)
            for s in range(nsub):
                pa = mmr.next()
                for k in range(KD):
                    P.op("pe", lambda e, pa=pa, wa=wa, k=k, s=s: e.matmul(pa[:, 0:256], lhsT=hT[:, k, s * 128:(s + 1) * 128], rhs=wa[:, k, :],
                                                                         start=(k == 0), stop=(k == KD - 1)), reads=[hT, wa], writes=[pa])
                f1 = f1r.next()
                if mixer == "s5":
                    pb = mmr.next()
                    for k in range(KD):
                        P.op("pe", lambda e, pb=pb, wb=wb, k=k, s=s: e.matmul(pb[:, 0:256], lhsT=hT[:, k, s * 128:(s + 1) * 128], rhs=wb[:, k, :],
                                                                             start=(k == 0), stop=(k == KD - 1)), reads=[hT, wb], writes=[pb])
                    f2 = f2r.next()
                    P.op("act", lambda e, f2=f2, pb=pb: e.activation(out=f2[:, 0:256], in_=pb[:, 0:256], func=AF.Sigmoid), reads=[pb], writes=[f2])
                    P.op("dve", lambda e, f1=f1, pa=pa, f2=f2: e.tensor_tensor(out=f1[:, 0:256], in0=pa[:, 0:256], in1=f2[:, 0:256], op=ALU.mult),
                         reads=[pa, f2], writes=[f1])
                    P.op("pool", lambda e, f1=f1, cs=cs: e.tensor_tensor(out=f1[:, 0:256], in0=f1[:, 0:256], in1=gcur[:, cs], op=ALU.mult),
                         reads=[f1, gcur], writes=[f1])
                else:
                    P.op("dve", lambda e, f1=f1, pa=pa, cs=cs: e.tensor_tensor(out=f1[:, 0:256], in0=pa[:, 0:256], in1=gcur[:, cs], op=ALU.mult),
                         reads=[pa, gcur], writes=[f1])
                P.op("pool", lambda e, f1=f1, s=s, cs=cs: e.tensor_tensor(out=xt[s][:, cs], in0=xt[s][:, cs], in1=f1[:, 0:256], op=ALU.add),
                     reads=[f1, xt[s]], writes=[xt[s]])
        load_rep(P, "sp", gcur, mod_d[row:row + 1, 5 * D:6 * D])
        for s in range(nsub):
            ssq = norm_stats(xt[s])
            P.op("dve", lambda e, s=s, ssq=ssq: e.scalar_tensor_tensor(out=t1[:], in0=xt[s][:], scalar=ssq[:, 2:3], in1=md["A2"][:],
                                                                        op0=ALU.mult, op1=ALU.mult), reads=[xt[s], ssq, md["A2"]], writes=[t1])
            ub = bfr.next()
            P.op("pool", lambda e, ub=ub: e.tensor_tensor(out=ub[:], in0=t1[:], in1=md["sh2"][:], op=ALU.add), reads=[t1, md["sh2"]], writes=[ub])
            transpose_to(P, K, ub, u2T, s * 128, scr)
        for c in range(NHC):
            wgu = wgr.next()
            P.dma(q(), wgu[:], wgu_t[c], out_t=wgu)
            pg = mmr.next(); pu = mmr.next()
            for k in range(KD):
                P.op("pe", lambda e, pg=pg, wgu=wgu, k=k: e.matmul(pg[:, :TT], lhsT=wgu[:, k, 0:128], rhs=u2T[:, k, :TT],
                                                                  start=(k == 0), stop=(k == KD - 1)), reads=[wgu, u2T], writes=[pg])
            for k in range(KD):
                P.op("pe", lambda e, pu=pu, wgu=wgu, k=k: e.matmul(pu[:, :TT], lhsT=wgu[:, k, 128:256], rhs=u2T[:, k, :TT],
                                                                  start=(k == 0), stop=(k == KD - 1)), reads=[wgu, u2T], writes=[pu])
            sg = sgr.next()
            P.op("act", lambda e, sg=sg, pg=pg: e.activation(out=sg[:, :TT], in_=pg[:, :TT], func=AF.Silu), reads=[pg], writes=[sg])
            P.op("dve", lambda e, sg=sg, pu=pu, c=c: e.tensor_tensor(out=hT[:, c, :TT], in0=pu[:, :TT], in1=sg[:, :TT], op=ALU.mult),
                 reads=[pu, sg], writes=[hT])
        for nt in range(4):
            cs = slice(nt * 512, (nt + 1) * 512)
            pf = [mmr.next() for _ in range(nsub)]
            for w in range(11):
                wo = wor.next()
                P.dma(q(), wo[:], wo_t[nt * 11 + w], out_t=wo)
                for s in range(nsub):
                    for cc in range(4):
                        c = w * 4 + cc
                        P.op("pe", lambda e, p_=pf[s], wo=wo, cc=cc, c=c, s=s: e.matmul(p_[:], lhsT=hT[:, c, s * 128:(s + 1) * 128], rhs=wo[:, cc, :],
                                                                                      start=(c == 0), stop=(c == NHC - 1)), reads=[hT, wo], writes=[pf[s]])
            for s in range(nsub):
                f1 = f1r.next()
                P.op("dve", lambda e, f1=f1, p_=pf[s], cs=cs: e.tensor_tensor(out=f1[:], in0=p_[:], in1=gcur[:, cs], op=ALU.mult), reads=[pf[s], gcur], writes=[f1])
                P.op("pool", lambda e, f1=f1, s=s, cs=cs: e.tensor_tensor(out=xt[s][:, cs], in0=xt[s][:, cs], in1=f1[:], op=ALU.add), reads=[f1, xt[s]], writes=[xt[s]])
        for s in range(nsub):
            if not last:
                xdma(xt[s], xs_out, tok0, s, False)
            else:
                ssq = norm_stats(xt[s])
                P.op("dve", lambda e, s=s, ssq=ssq: e.scalar_tensor_tensor(out=t1[:], in0=xt[s][:], scalar=ssq[:, 2:3], in1=md["nf"][:],
                                                                            op0=ALU.mult, op1=ALU.mult), reads=[xt[s], ssq, md["nf"]], writes=[t1])
                xdma(t1, out_final, tok0, s, False)
    if md is not None:
        P.end_phase(list(md.values()))
        P.emit()
        P.release(mm_mark)
    P.end_phase(tiles)
    P.emit()
    P.release(m)


def bc(ap, axis, n):
    a = ap.unsqueeze(axis)
    shp = list(a.shape)
    shp[axis] = n
    return a.broadcast_to(shp)


def phase_c_s5(P, K, io, jl, u_tok, gy_tok, S_LAT, dve2="pool"):
    NTOK3 = 2 * S_CTX + S_LAT
    NCH = NTOK3 // T0
    NF = (S_CTX + S_LAT) // T0
    CTXC = S_CTX // T0
    NBK = 32
    BL = NF // NBK
    assert NF == NBK * BL
    NBLK = (NCH + 127) // 128
    NFB = (NF + 127) // 128
    PI = float(np.pi)
    m = P.mark()
    V = lambda e: e

    def ew(eng, fn, reads, writes):
        P.op(eng, fn, reads=reads, writes=writes)

    Pw = P.sbuf("c_Pw", [128, 2, 2, 64, T0 + 1], F32)
    PWB = P.sbuf("c_PWB", [128, 2, 2, 64, BL + 1], F32)
    Bn = P.sbuf("c_Bn", [128, 2, 2, 64, 16], F32)
    Bb = P.sbuf("c_Bb", [128, 2, 2, 64, 16], F32)
    Dp = P.sbuf("c_Dp", [128, 128], F32)
    cz = [P.sbuf(f"c_cz{i}", [128, 16, 128], F32) for i in range(2)]
    m_tmp = P.mark()
    lam = P.sbuf("c_lam", [128, 2, 2, 64], F32)
    dtt = P.sbuf("c_dt", [128, 2, 64], F32)
    for d in range(2):
        for jj in range(2):
            ps_ = slice(jj * 64, (jj + 1) * 64)
            P.dma("sp", lam[ps_, 0, d, :], io["s5_lam_re"][jl, d].rearrange("(i j) p -> j p i", j=2)[jj], out_t=lam, allow_slow_non_contiguous=True)
            P.dma("act", lam[ps_, 1, d, :], io["s5_lam_im"][jl, d].rearrange("(i j) p -> j p i", j=2)[jj], out_t=lam, allow_slow_non_contiguous=True)
            P.dma("sp", dtt[ps_, d, :], io["s5_log_dt"][jl, d:d + 1, :].rearrange("o (i j) -> o j i", j=2)[:, jj, :].partition_broadcast(64),
                  out_t=dtt, allow_slow_non_contiguous=True)
    w = [P.sbuf(f"c_w{i}", [128, 2, 64], F32) for i in range(10)]
    A1 = P.sbuf("c_A1", [128, 2, 2, 64], F32)
    Fc = P.sbuf("c_F", [128, 2, 2, 64], F32)
    A8 = P.sbuf("c_A8", [128, 2, 2, 64], F32)
    ptc = Ring([P.psum(f"c_ptc{i}", [128, 4, 128], F32) for i in range(2)])
    lre, lim = lam[:, 0], lam[:, 1]
    ew("act", lambda e: e.activation(out=dtt[:], in_=dtt[:], func=AF.Exp), [dtt], [dtt])
    ew("dve", lambda e: e.tensor_tensor(out=w[0][:], in0=lre, in1=dtt[:], op=ALU.mult), [lam, dtt], [w[0]])
    ew("act", lambda e: e.activation(out=w[0][:], in_=w[0][:], func=AF.Exp), [w[0]], [w[0]])
    ew("dve", lambda e: e.tensor_tensor(out=w[1][:], in0=lim, in1=dtt[:], op=ALU.mult), [lam, dtt], [w[1]])
    for (dst, shift) in ((w[2], 0.0), (w[3], PI / 2)):
        ew("dve", lambda e, dst=dst, shift=shift: e.tensor_scalar(out=dst[:], in0=w[1][:], scalar1=float(shift), scalar2=None, op0=ALU.add), [w[1]], [dst])
        ew("dve", lambda e, dst=dst: e.tensor_copy(out=w[4][:], in_=dst[:]), [dst], [w[4]])
        for k in range(1, 7):
            ew("act", lambda e, k=k: e.activation(out=w[5][:], in_=w[4][:], func=AF.Sign, bias=K.cbias[:, k:k + 1], scale=1.0), [w[4], K.cbias], [w[5]])
            ew("dve", lambda e, dst=dst: e.scalar_tensor_tensor(out=dst[:], in0=w[5][:], scalar=-PI, in1=dst[:], op0=ALU.mult, op1=ALU.add), [w[5], dst], [dst])
        ew("dve", lambda e, dst=dst: e.tensor_scalar(out=dst[:], in0=dst[:], scalar1=-6.0 * PI, scalar2=None, op0=ALU.add), [dst], [dst])
        ew("act", lambda e, dst=dst: e.activation(out=dst[:], in_=dst[:], func=AF.Sin), [dst], [dst])
    ew("dve", lambda e: e.tensor_tensor(out=A1[:, 0], in0=w[0][:], in1=w[3][:], op=ALU.mult), [w[0], w[3]], [A1])
    ew("dve", lambda e: e.tensor_tensor(out=A1[:, 1], in0=w[0][:], in1=w[2][:], op=ALU.mult), [w[0], w[2]], [A1])
    ew("dve", lambda e: e.tensor_tensor(out=w[4][:], in0=lre, in1=lre, op=ALU.mult), [lam], [w[4]])
    ew("dve", lambda e: e.tensor_tensor(out=w[5][:], in0=lim, in1=lim, op=ALU.mult), [lam], [w[5]])
    ew("dve", lambda e: e.tensor_tensor(out=w[4][:], in0=w[4][:], in1=w[5][:], op=ALU.add), [w[4], w[5]], [w[4]])
    ew("dve", lambda e: e.reciprocal(out=w[4][:], in_=w[4][:]), [w[4]], [w[4]])
    ew("dve", lambda e: e.tensor_scalar(out=w[5][:], in0=A1[:, 0], scalar1=-1.0, scalar2=None, op0=ALU.add), [A1], [w[5]])
    ew("dve", lambda e: e.tensor_tensor(out=w[6][:], in0=w[5][:], in1=lre, op=ALU.mult), [w[5], lam], [w[6]])
    ew("dve", lambda e: e.tensor_tensor(out=w[7][:], in0=A1[:, 1], in1=lim, op=ALU.mult), [A1, lam], [w[7]])
    ew("dve", lambda e: e.tensor_tensor(out=w[6][:], in0=w[6][:], in1=w[7][:], op=ALU.add), [w[6], w[7]], [w[6]])
    ew("dve", lambda e: e.tensor_tensor(out=Fc[:, 0], in0=w[6][:], in1=w[4][:], op=ALU.mult), [w[6], w[4]], [Fc])
    ew("dve", lambda e: e.tensor_tensor(out=w[6][:], in0=A1[:, 1], in1=lre, op=ALU.mult), [A1, lam], [w[6]])
    ew("dve", lambda e: e.tensor_tensor(out=w[7][:], in0=w[5][:], in1=lim, op=ALU.mult), [w[5], lam], [w[7]])
    ew("dve", lambda e: e.tensor_tensor(out=w[6][:], in0=w[6][:], in1=w[7][:], op=ALU.subtract), [w[6], w[7]], [w[6]])
    ew("dve", lambda e: e.tensor_tensor(out=Fc[:, 1], in0=w[6][:], in1=w[4][:], op=ALU.mult), [w[6], w[4]], [Fc])

    def cmul_pow(dst, n, base_re, base_im, tag):
        ew("dve", lambda e: e.memset(dst[:, 0, :, :, 0:1], 1.0), [], [dst])
        ew("dve", lambda e: e.memset(dst[:, 1, :, :, 0:1], 0.0), [], [dst])
        for k in range(1, n):
            pr, pi_ = dst[:, 0, :, :, k - 1], dst[:, 1, :, :, k - 1]
            ew("dve", lambda e, pr=pr: e.tensor_tensor(out=w[6][:], in0=pr, in1=base_re, op=ALU.mult), [dst, A1], [w[6]])
            ew("dve", lambda e, pi_=pi_: e.tensor_tensor(out=w[7][:], in0=pi_, in1=base_im, op=ALU.mult), [dst, A1], [w[7]])
            ew("dve", lambda e, k=k: e.tensor_tensor(out=dst[:, 0, :, :, k], in0=w[6][:], in1=w[7][:], op=ALU.subtract), [w[6], w[7]], [dst])
            ew("dve", lambda e, pr=pr: e.tensor_tensor(out=w[6][:], in0=pr, in1=base_im, op=ALU.mult), [dst, A1], [w[6]])
            ew("dve", lambda e, pi_=pi_: e.tensor_tensor(out=w[7][:], in0=pi_, in1=base_re, op=ALU.mult), [dst, A1], [w[7]])
            ew("dve", lambda e, k=k: e.tensor_tensor(out=dst[:, 1, :, :, k], in0=w[6][:], in1=w[7][:], op=ALU.add), [w[6], w[7]], [dst])

    cmul_pow(Pw, T0 + 1, A1[:, 0], A1[:, 1], "pw")
    ew("dve", lambda e: e.tensor_copy(out=A8[:], in_=Pw[:, :, :, :, T0]), [Pw], [A8])
    cmul_pow(PWB, BL + 1, A8[:, 0], A8[:, 1], "pwb")

    for d in range(2):
        for jj in range(2):
            ps_ = slice(jj * 64, (jj + 1) * 64)
            P.dma("sp", Bn[ps_, 0, d], io["s5_b_re"][jl, d].rearrange("(i j) p c -> j p i c", j=2)[jj], out_t=Bn)
            P.dma("act", Bn[ps_, 1, d], io["s5_b_im"][jl, d].rearrange("(i j) p c -> j p i c", j=2)[jj], out_t=Bn)
    tbT = cz[0]
    tbv = cz[0][:].rearrange("p (d a) (b c) -> p d (a b) c", d=2, c=16)
    fre = bc(Fc[:, 0], 3, 16)
    fim = bc(Fc[:, 1], 3, 16)
    ew("dve", lambda e: e.tensor_tensor(out=Bb[:, 0], in0=Bn[:, 0], in1=fre, op=ALU.mult), [Bn, Fc], [Bb])
    ew("dve", lambda e: e.tensor_tensor(out=tbv, in0=Bn[:, 1], in1=fim, op=ALU.mult), [Bn, Fc], [tbT])
    ew("dve", lambda e: e.tensor_tensor(out=Bb[:, 0], in0=Bb[:, 0], in1=tbv, op=ALU.subtract), [Bb, tbT], [Bb])
    ew("dve", lambda e: e.tensor_tensor(out=Bb[:, 1], in0=Bn[:, 1], in1=fre, op=ALU.mult), [Bn, Fc], [Bb])
    ew("dve", lambda e: e.tensor_tensor(out=tbv, in0=Bn[:, 0], in1=fim, op=ALU.mult), [Bn, Fc], [tbT])
    ew("dve", lambda e: e.tensor_tensor(out=Bb[:, 1], in0=Bb[:, 1], in1=tbv, op=ALU.add), [Bb, tbT], [Bb])

    Cn = Bn
    for t in cz:
        ew("pool", lambda e, t=t: e.memset(t[:], 0.0), [], [t])
    ci = 0
    for d in range(2):
        for part, key in ((0, "s5_c_re"), (1, "s5_c_im")):
            t = cz[ci % 2]; ci += 1
            src = io[key][jl, d].rearrange("(kc q j) c p -> q j c kc p", q=4, j=2)
            for q_ in range(4):
                for j_ in range(2):
                    r0 = 32 * q_ + 16 * j_
                    P.dma("sp" if (q_ + j_) % 2 == 0 else "act", t[r0:r0 + 16, :, 64 * j_:64 * j_ + 64], src[q_, j_], out_t=t)
            for k4 in range(4):
                pt = ptc.next()
                for kk in range(4):
                    kc = k4 * 4 + kk
                    ew("pe", lambda e, pt=pt, kk=kk, kc=kc, t=t: e.transpose(out=pt[:, kk, :], in_=t[:, kc, :], identity=K.ident_f[:]), [t, K.ident_f], [pt])
                for jj in range(2):
                    ps_ = slice(jj * 64, (jj + 1) * 64)
                    src_ap = pt[ps_].rearrange("p k (q j c) -> p k q j c", q=4, j=2)[:, :, :, jj, :]
                    dst_ap = Cn[ps_, part, d, 16 * k4:16 * k4 + 16, :].rearrange("p (k q) c -> p k q c", q=4)
                    if part == 0:
                        ew("dve", lambda e, dst_ap=dst_ap, src_ap=src_ap: e.tensor_copy(out=dst_ap, in_=src_ap), [pt], [Cn])
                    else:
                        ew("dve", lambda e, dst_ap=dst_ap, src_ap=src_ap: e.tensor_scalar(out=dst_ap, in0=src_ap, scalar1=-1.0, scalar2=None, op0=ALU.mult), [pt], [Cn])
    for t_ in range(T0):
        P.dma("sp", Dp[16 * t_:16 * t_ + 16, :], io["s5_d"][jl:jl + 1, :].rearrange("o (g c) -> (o c) g", c=16), out_t=Dp, allow_slow_non_contiguous=True)

    P.end_phase([lam, dtt, A1, Fc, A8] + w + ptc.tiles)
    P.emit()
    P.release(m_tmp)

    VF, VB = cz[0], cz[1]
    VFv = cz[0][:].rearrange("p (a b x) (y d) -> p a b (x y) d", a=2, b=4, d=16)
    VBv = cz[1][:].rearrange("p (a b x) (y d) -> p a b (x y) d", a=2, b=4, d=16)
    ew("pool", lambda e: e.memset(cz[0][:], 0.0), [], [VF])
    ew("pool", lambda e: e.memset(cz[1][:], 0.0), [], [VB])
    vt = P.sbuf("c_vt", [128, 4, 8, 16], F32)
    vu = P.sbuf("c_vu", [128, 4, 8, 16], F32)
    Ro = P.sbuf("c_Ro", [128, 2, 2, 4, 8, 16], BF16)
    TS = P.sbuf("c_TS", [128, 2, 2, 4, 128], BF16)
    IT = P.sbuf("c_IT", [128, 2, 8, 128], BF16)
    U = P.sbuf("c_U", [128, 8, NBLK * 128], BF16)
    Zr = Ring([P.sbuf(f"c_Z{i}", [128, 8, 128], BF16) for i in range(2)])
    Zpr = Ring([P.sbuf(f"c_Zp{i}", [128, 8, 8, 16], BF16) for i in range(2)])
    Hs = P.sbuf("c_Hs", [128, 2, 8, NBK, BL], F32)
    Hr = P.sbuf("c_Hr", [128, 2, 2, 4, NCH], BF16)
    ew("pool", lambda e: e.memset(Hr[:], 0.0), [], [Hr])
    A8l = P.sbuf("c_A8l", [128, 2, 8], F32)
    ABl = P.sbuf("c_ABl", [128, 2, 8], F32)
    PWl = P.sbuf("c_PWl", [128, 2, 8, BL], F32)
    s1 = [P.sbuf(f"c_s1{i}", [128, 2, 8, NBK], F32) for i in range(2)]
    s3 = P.sbuf("c_s3", [128, 2, NBK - 1, max(BL - 1, 1)], F32)
    A8s = P.sbuf("c_A8s", [128, 2, 8], F32)
    Pcs = P.sbuf("c_Pcs", [128, 2, 8], F32)
    Pcur = P.sbuf("c_Pcur", [128, 2, 8], F32)
    sq = P.sbuf("c_sq", [128, 2, 8], F32)
    PWls = P.sbuf("c_PWls", [128, 2, 8, BL], F32)
    Ysb = Ring([P.sbuf(f"c_Y{i}", [128, NFB * 128], F32) for i in range(2)])
    Zo = P.sbuf("c_Zo", [128, NFB, 8, 128], BF16)
    gl_ = [P.sbuf(f"c_g{i}", [128, NFB * 128], F32) for i in range(2)]
    Ybr = Ring([P.sbuf(f"c_Yb{i}", [128, NFB * 128], BF16) for i in range(2)])
    ptb = Ring([P.psum(f"c_ptb{i}", [128, 8, 128], BF16) for i in range(2)])
    psS = Ring([P.psum(f"c_psS{i}", [128, 1024], F32) for i in range(1)])
    psY = Ring([P.psum(f"c_psY{i}", [128, 1024], F32) for i in range(1)])
    psT = Ring([P.psum(f"c_psT{i}", [128, 4, 128], F32) for i in range(1)])
    alt = ["dve", dve2]

    for b in range(16):
        i0 = 4 * b
        f0 = 128 * b
        for d in range(2):
            for part in range(2):
                pre = bc(Pw[:, 0, d, i0:i0 + 4, 0:T0], 3, 16)
                pim = bc(Pw[:, 1, d, i0:i0 + 4, 0:T0], 3, 16)
                bre = bc(Bb[:, 0, d, i0:i0 + 4, :], 2, T0)
                bim = bc(Bb[:, 1, d, i0:i0 + 4, :], 2, T0)
                dstv = VFv[:, part, :, 7::-1, :] if d == 0 else VBv[:, part, :, 8:16, :]
                dt_ = VF if d == 0 else VB
                if part == 0:
                    ew("dve", lambda e, pre=pre, bre=bre: e.tensor_tensor(out=vt[:], in0=pre, in1=bre, op=ALU.mult), [Pw, Bb], [vt])
                    ew("dve", lambda e, pim=pim, bim=bim, dstv=dstv: e.tensor_tensor(out=dstv, in0=pim, in1=bim, op=ALU.mult), [Pw, Bb], [dt_])
                    ew("dve", lambda e, dstv=dstv: e.tensor_tensor(out=dstv, in0=vt[:], in1=dstv, op=ALU.subtract), [vt, dt_], [dt_])
                else:
                    ew("dve", lambda e, pre=pre, bim=bim: e.tensor_tensor(out=vt[:], in0=pre, in1=bim, op=ALU.mult), [Pw, Bb], [vt])
                    ew("dve", lambda e, pim=pim, bre=bre, dstv=dstv: e.tensor_tensor(out=dstv, in0=pim, in1=bre, op=ALU.mult), [Pw, Bb], [dt_])
                    ew("dve", lambda e, dstv=dstv: e.tensor_tensor(out=dstv, in0=vt[:], in1=dstv, op=ALU.add), [vt, dt_], [dt_])
            ks = slice(1, T0 + 1) if d == 0 else slice(T0, 0, -1)
            pre = bc(Pw[:, 0, d, i0:i0 + 4, ks], 3, 16)
            pim = bc(Pw[:, 1, d, i0:i0 + 4, ks], 3, 16)
            cre = bc(Cn[:, 0, d, i0:i0 + 4, :], 2, T0)
            cimn = bc(Cn[:, 1, d, i0:i0 + 4, :], 2, T0)
            ew("dve", lambda e, pre=pre, cre=cre: e.tensor_tensor(out=vt[:], in0=pre, in1=cre, op=ALU.mult), [Pw, Cn], [vt])
            ew(dve2, lambda e, pim=pim, cimn=cimn: e.tensor_tensor(out=vu[:], in0=pim, in1=cimn, op=ALU.mult), [Pw, Cn], [vu])
            ew("dve", lambda e, d=d: e.tensor_tensor(out=Ro[:, d, 0], in0=vt[:], in1=vu[:], op=ALU.add), [vt, vu], [Ro])
            ew("dve", lambda e, pim=pim, cre=cre: e.tensor_tensor(out=vt[:], in0=pim, in1=cre, op=ALU.mult), [Pw, Cn], [vt])
            ew(dve2, lambda e, pre=pre, cimn=cimn: e.tensor_tensor(out=vu[:], in0=pre, in1=cimn, op=ALU.mult), [Pw, Cn], [vu])
            ew("dve", lambda e, d=d: e.tensor_tensor(out=Ro[:, d, 1], in0=vu[:], in1=vt[:], op=ALU.subtract), [vt, vu], [Ro])
        for d in range(2):
            for part in range(2):
                pt = psT.next()
                for il in range(4):
                    src = (VFv[:, part, il, 0:8, :] if d == 0 else VBv[:, part, il, 8:16, :]).rearrange("p b c -> p (b c)")
                    ew("pe", lambda e, pt=pt, il=il, src=src: e.transpose(out=pt[:, il, :], in_=src, identity=K.ident_f[:]), [VF, VB, K.ident_f], [pt])
                ew("act", lambda e, pt=pt, d=d, part=part: e.activation(out=TS[:, d, part], in_=pt[:], func=AF.Copy), [pt], [TS])
        for d in range(2):
            for g4 in range(2):
                pt = psT.next()
                for gg in range(4):
                    g_ = g4 * 4 + gg
                    il, jj = g_ // 2, g_ % 2
                    ps_ = slice(jj * 64, (jj + 1) * 64)
                    for t_ in range(T0):
                        for part in range(2):
                            VX = VFv if d == 0 else VBv
                            w0 = (7 - t_) if d == 0 else (8 - t_)
                            lhsT = VX[ps_, part, il, w0:w0 + 8, :].rearrange("p b c -> p (b c)")
                            rhs = Cn[ps_, part, d, i0 + il, :]
                            ew("pe", lambda e, pt=pt, gg=gg, t_=t_, part=part, lhsT=lhsT, rhs=rhs: e.matmul(
                                pt[:, gg, 16 * t_:16 * t_ + 16], lhsT=lhsT, rhs=rhs, start=(part == 0), stop=(part == 1), skip_group_check=True),
                               [VF, VB, Cn], [pt])
                ew("act", lambda e, pt=pt, d=d, g4=g4: e.activation(out=IT[:, d, g4 * 4:(g4 + 1) * 4], in_=pt[:], func=AF.Copy), [pt], [IT])
        for part in range(2):
            for d in range(2):
                ew("dve", lambda e, part=part, d=d: e.tensor_copy(out=A8l[:, part, d * 4:(d + 1) * 4], in_=PWB[:, part, d, i0:i0 + 4, 1]), [PWB], [A8l])
                ew("dve", lambda e, part=part, d=d: e.tensor_copy(out=ABl[:, part, d * 4:(d + 1) * 4], in_=PWB[:, part, d, i0:i0 + 4, BL]), [PWB], [ABl])
                ew("dve", lambda e, part=part, d=d: e.tensor_copy(out=PWl[:, part, d * 4:(d + 1) * 4, :], in_=PWB[:, part, d, i0:i0 + 4, 1:BL + 1]), [PWB], [PWl])
        for blk in range(NBLK):
            nn = min(128, NCH - blk * 128)
            Z = Zr.next()
            P.dma("sp" if blk % 2 == 0 else "act", Z[0:nn],
                  u_tok[blk * 1024: blk * 1024 + nn * 8, f0:f0 + 128].rearrange("(n s) f -> n s f", s=8), out_t=Z)
            pt = ptb.next()
            Zp = Zpr.next()
            ew("pool", lambda e, Z=Z, Zp=Zp, nn=nn: e.tensor_copy(out=Zp[0:nn], in_=Z[0:nn].rearrange("n s (g c) -> n g s c", c=16)), [Z], [Zp])
            for g_ in range(8):
                ew("pe", lambda e, pt=pt, g_=g_, Zp=Zp, nn=nn: e.transpose(out=pt[:, g_, 0:nn], in_=Zp[0:nn, g_].rearrange("n s c -> n (s c)"), identity=K.ident_b[0:nn, 0:nn]),
                   [Zp, K.ident_b], [pt])
            ew("act" if blk % 2 == 0 else "dve",
               (lambda e, pt=pt, blk=blk, nn=nn: e.activation(out=U[:, :, blk * 128: blk * 128 + nn], in_=pt[:, :, 0:nn], func=AF.Copy)) if blk % 2 == 0 else
               (lambda e, pt=pt, blk=blk, nn=nn: e.tensor_copy(out=U[:, :, blk * 128: blk * 128 + nn], in_=pt[:, :, 0:nn])), [pt], [U])
        for il in range(4):
            for d in range(2):
                lane = d * 4 + il
                lo, hi = (0, NF) if d == 0 else (CTXC, NCH)
                for part in range(2):
                    ps = psS.next()
                    for (a, b_) in col_blocks(0, NF):
                        for jj in range(2):
                            ps_ = slice(jj * 64, (jj + 1) * 64)
                            ew("pe", lambda e, ps=ps, ps_=ps_, a=a, b_=b_, d=d, part=part, il=il, jj=jj, lo=lo: e.matmul(
                                ps[ps_, a:b_], lhsT=TS[:, d, part, il, ps_], rhs=U[:, 2 * il + jj, lo + a: lo + b_], start=True, stop=True, skip_group_check=True),
                               [TS, U], [ps])
                    dst = Hs[:, part, lane].rearrange("p b l -> p (b l)")
                    if d == 0:
                        ew("act", lambda e, dst=dst, ps=ps: e.activation(out=dst, in_=ps[:, 0:NF], func=AF.Copy), [ps], [Hs])
                    else:
                        ew("dve", lambda e, dst=dst, ps=ps: e.tensor_copy(out=dst[:, ::-1], in_=ps[:, 0:NF]), [ps], [Hs])
        for (src_, dst_) in ((A8l, A8s), (ABl, Pcs)):
            ew("dve", lambda e, src_=src_, dst_=dst_: e.tensor_copy(out=dst_[:, 1], in_=src_[:, 1]), [src_], [dst_])
            ew("dve", lambda e, src_=src_, dst_=dst_: e.tensor_scalar(out=dst_[:, 0], in0=src_[:, 1], scalar1=-1.0, scalar2=None, op0=ALU.mult), [src_], [dst_])
        ew("dve", lambda e: e.tensor_copy(out=PWls[:, 1], in_=PWl[:, 1]), [PWl], [PWls])
        ew("dve", lambda e: e.tensor_scalar(out=PWls[:, 0], in0=PWl[:, 1], scalar1=-1.0, scalar2=None, op0=ALU.mult), [PWl], [PWls])
        ew("dve", lambda e: e.tensor_copy(out=Pcur[:], in_=ABl[:]), [ABl], [Pcur])
        are_b = bc(bc(A8l[:, 0, :], 1, 2), 3, NBK)
        aims_b = bc(A8s[:], 3, NBK)
        for l in range(1, BL):
            Y = Hs[:, :, :, :, l - 1]
            Ysw = Hs[:, ::-1, :, :, l - 1]
            X = Hs[:, :, :, :, l]
            ew("dve", lambda e, Y=Y: e.tensor_tensor(out=s1[0][:], in0=Y, in1=are_b, op=ALU.mult), [Hs, A8l], [s1[0]])
            ew("dve", lambda e, Ysw=Ysw: e.tensor_tensor(out=s1[1][:], in0=Ysw, in1=aims_b, op=ALU.mult), [Hs, A8s], [s1[1]])
            ew("dve", lambda e, X=X: e.tensor_tensor(out=X, in0=X, in1=s1[0][:], op=ALU.add), [Hs, s1[0]], [Hs])
            ew("dve", lambda e, X=X: e.tensor_tensor(out=X, in0=X, in1=s1[1][:], op=ALU.add), [Hs, s1[1]], [Hs])
        sft = 1
        while sft < NBK:
            n_ = NBK - sft
            Y = Hs[:, :, :, 0:n_, BL - 1]
            Ysw = Hs[:, ::-1, :, 0:n_, BL - 1]
            X = Hs[:, :, :, sft:NBK, BL - 1]
            pre_b = bc(bc(Pcur[:, 0, :], 1, 2), 3, n_)
            pims_b = bc(Pcs[:], 3, n_)
            ew("dve", lambda e, Y=Y, pre_b=pre_b, n_=n_: e.tensor_tensor(out=s1[0][:, :, :, 0:n_], in0=Y, in1=pre_b, op=ALU.mult), [Hs, Pcur], [s1[0]])
            ew("dve", lambda e, Ysw=Ysw, pims_b=pims_b, n_=n_: e.tensor_tensor(out=s1[1][:, :, :, 0:n_], in0=Ysw, in1=pims_b, op=ALU.mult), [Hs, Pcs], [s1[1]])
            ew("dve", lambda e, X=X, n_=n_: e.tensor_tensor(out=X, in0=X, in1=s1[0][:, :, :, 0:n_], op=ALU.add), [Hs, s1[0]], [Hs])
            ew("dve", lambda e, X=X, n_=n_: e.tensor_tensor(out=X, in0=X, in1=s1[1][:, :, :, 0:n_], op=ALU.add), [Hs, s1[1]], [Hs])
            sft *= 2
            if sft < NBK:
                ew("dve", lambda e: e.tensor_tensor(out=sq[:, 0], in0=Pcur[:, 0], in1=Pcur[:, 0], op=ALU.mult), [Pcur], [sq])
                ew("dve", lambda e: e.tensor_tensor(out=sq[:, 1], in0=Pcur[:, 1], in1=Pcur[:, 1], op=ALU.mult), [Pcur], [sq])
                ew("dve", lambda e: e.scalar_tensor_tensor(out=Pcur[:, 1], in0=Pcur[:, 0], scalar=2.0, in1=Pcur[:, 1], op0=ALU.mult, op1=ALU.mult), [Pcur], [Pcur])
                ew("dve", lambda e: e.tensor_tensor(out=Pcur[:, 0], in0=sq[:, 0], in1=sq[:, 1], op=ALU.subtract), [sq], [Pcur])
                ew("dve", lambda e: e.tensor_copy(out=Pcs[:, 1], in_=Pcur[:, 1]), [Pcur], [Pcs])
                ew("dve", lambda e: e.tensor_scalar(out=Pcs[:, 0], in0=Pcur[:, 1], scalar1=-1.0, scalar2=None, op0=ALU.mult), [Pcur], [Pcs])
        if BL > 1:
            for ls in range(8):
                X = Hs[:, :, ls, 1:NBK, 0:BL - 1]
                Y = bc(Hs[:, :, ls, 0:NBK - 1, BL - 1], 3, BL - 1)
                Ysw = bc(Hs[:, ::-1, ls, 0:NBK - 1, BL - 1], 3, BL - 1)
                pre3 = bc(bc(PWl[:, 0, ls, 0:BL - 1], 1, 2), 2, NBK - 1)
                pims3 = bc(PWls[:, :, ls, 0:BL - 1], 2, NBK - 1)
                ew("dve", lambda e, Y=Y, pre3=pre3: e.tensor_tensor(out=s3[:], in0=Y, in1=pre3, op=ALU.mult), [Hs, PWl], [s3])
                ew("dve", lambda e, X=X: e.tensor_tensor(out=X, in0=X, in1=s3[:], op=ALU.add), [Hs, s3], [Hs])
                ew("dve", lambda e, Ysw=Ysw, pims3=pims3: e.tensor_tensor(out=s3[:], in0=Ysw, in1=pims3, op=ALU.mult), [Hs, PWls], [s3])
                ew("dve", lambda e, X=X: e.tensor_tensor(out=X, in0=X, in1=s3[:], op=ALU.add), [Hs, s3], [Hs])
        for part in range(2):
            hf = Hs[:, part, 0:4].rearrange("p a b l -> p a (b l)")
            hb = Hs[:, part, 4:8].rearrange("p a b l -> p a (b l)")
            ew("act", lambda e, part=part, hf=hf: e.activation(out=Hr[:, 0, part, :, 1:NF], in_=hf[:, :, 0:NF - 1], func=AF.Copy), [Hs], [Hr])
            ew(dve2, lambda e, part=part, hb=hb: e.tensor_copy(out=Hr[:, 1, part, :, CTXC:NCH - 1], in_=hb[:, :, NF - 2::-1]), [Hs], [Hr])
        for g_ in range(8):
            il, jj = g_ // 2, g_ % 2
            ps_ = slice(jj * 64, (jj + 1) * 64)
            py = psY.next()
            first = {}
            mm = []
            for d in range(2):
                lo, hi = (0, NF) if d == 0 else (CTXC, NCH)
                for (a, b_) in col_blocks(lo, hi):
                    mm.append((a, b_, IT[:, d, g_, :], U[:, g_, a:b_]))
                    mm.append((a, b_, Ro[ps_, d, 0, il].rearrange("p t c -> p (t c)"), Hr[ps_, d, 0, il, a:b_]))
                    mm.append((a, b_, Ro[ps_, d, 1, il].rearrange("p t c -> p (t c)"), Hr[ps_, d, 1, il, a:b_]))
            nlast = {}
            for idx, (a, b_, _, _) in enumerate(mm):
                nlast[a // 512] = idx
            for idx, (a, b_, lhsT, rhs) in enumerate(mm):
                bank = a // 512
                st = bank not in first
                first[bank] = True
                ew("pe", lambda e, py=py, a=a, b_=b_, lhsT=lhsT, rhs=rhs, st=st, sp=(nlast[bank] == idx): e.matmul(
                    py[:, a:b_], lhsT=lhsT, rhs=rhs, start=st, stop=sp, skip_group_check=True), [IT, U, Ro, Hr], [py])
            Y = Ysb.next()
            ew("dve", lambda e, Y=Y, py=py, g_=g_: e.scalar_tensor_tensor(out=Y[:, 0:NF], in0=U[:, g_, 0:NF], scalar=Dp[:, 8 * b + g_: 8 * b + g_ + 1],
                                                                          in1=py[:, 0:NF], op0=ALU.mult, op1=ALU.add), [U, Dp, py], [Y])
            ew("dve", lambda e, Y=Y, py=py: e.tensor_tensor(out=Y[:, 0:CTXC], in0=Y[:, 0:CTXC], in1=py[:, NF:NCH], op=ALU.add), [Y, py], [Y])
            ga, gb = gl_[0], gl_[1]
            ew(dve2, lambda e, Y=Y: e.tensor_tensor(out=ga[:, 0:NF], in0=Y[:, 0:NF], in1=Y[:, 0:NF], op=ALU.mult), [Y], [ga])
            ew("dve", lambda e: e.tensor_scalar(out=ga[:, 0:NF], in0=ga[:, 0:NF], scalar1=0.044715, scalar2=1.0, op0=ALU.mult, op1=ALU.add), [ga], [ga])
            ew(dve2, lambda e, Y=Y: e.tensor_tensor(out=ga[:, 0:NF], in0=ga[:, 0:NF], in1=Y[:, 0:NF], op=ALU.mult), [ga, Y], [ga])
            ew("act", lambda e: e.activation(out=gb[:, 0:NF], in_=ga[:, 0:NF], func=AF.Sigmoid, scale=1.5957691216057308), [ga], [gb])
            Yb = Ybr.next()
            ew("dve", lambda e, Y=Y, Yb=Yb: e.tensor_tensor(out=Yb[:, 0:NF], in0=gb[:, 0:NF], in1=Y[:, 0:NF], op=ALU.mult), [gb, Y], [Yb])
            pt = ptb.next()
            for blk in range(NFB):
                nn = min(128, NF - blk * 128)
                ew("pe", lambda e, pt=pt, blk=blk, nn=nn, Yb=Yb: e.transpose(out=pt[0:nn, blk, :], in_=Yb[:, blk * 128: blk * 128 + nn], identity=K.ident_b[:]),
                   [Yb, K.ident_b], [pt])
            nfull = NF // 128
            if nfull > 0:
                ew("act", lambda e, pt=pt, g_=g_: e.activation(out=Zo[:, 0:nfull, :, 16 * g_:16 * g_ + 16],
                                                               in_=pt[:, 0:nfull, :].rearrange("p k (t c) -> p k t c", c=16), func=AF.Copy), [pt], [Zo])
            if NF % 128:
                nn = NF % 128
                ew("act", lambda e, pt=pt, g_=g_, nn=nn: e.activation(out=Zo[0:nn, nfull, :, 16 * g_:16 * g_ + 16],
                                                                      in_=pt[0:nn, nfull, :].rearrange("p (t c) -> p t c", c=16), func=AF.Copy), [pt], [Zo])
        for blk in range(NFB):
            nn = min(128, NF - blk * 128)
            P.dma("sp" if blk % 2 == 0 else "act",
                  gy_tok[blk * 1024: blk * 1024 + nn * 8, f0:f0 + 128].rearrange("(n s) f -> n s f", s=8), Zo[0:nn, blk], in_t=Zo)
        P.emit()
    allt = ([Pw, PWB, Bn, Bb, Dp, vt, vu, Ro, TS, IT, U, Hs, Hr, A8l, ABl, PWl, s3, Zo, A8s, Pcs, Pcur, sq, PWls] + cz + Zr.tiles + Zpr.tiles + s1
            + Ysb.tiles + gl_ + Ybr.tiles + ptb.tiles + psS.tiles + psY.tiles + psT.tiles)
    P.end_phase(allt)
    P.emit()
    P.release(m)


W_SHAPES = {
    "ada_w": [4, D, 6 * D], "ada_b": [4, 6 * D], "norm1": [4, D], "norm2": [4, D], "norm_f": [1, D],
    "s5_lam_re": [2, 2, G, PST], "s5_lam_im": [2, 2, G, PST], "s5_log_dt": [2, 2, G],
    "s5_b_re": [2, 2, G, PST, 16], "s5_b_im": [2, 2, G, PST, 16], "s5_c_re": [2, 2, G, 16, PST], "s5_c_im": [2, 2, G, 16, PST],
    "s5_d": [2, D], "s5_w_glu": [2, D, 2 * D], "ml_w_in": [2, D, ML_IN], "ml_b_gates": [2, 32], "ml_norm": [2, D],
    "ml_w_out": [2, D, D], "ffn_w_in": [4, D, 2 * FH], "ffn_w_out": [4, FH, D],
}


def build_program(S_LAT, layers=(0, 1, 2, 3), debug_x=False, depth_total=4):
    nc = bass.Bass("TRN2", target_bir_lowering=False)
    io = {}
    io["x"] = nc.dram_tensor("x", [S_LAT, D], F32, kind="ExternalInput").ap()
    io["ctx"] = nc.dram_tensor("ctx", [S_CTX, D], F32, kind="ExternalInput").ap()
    io["c"] = nc.dram_tensor("c", [1, D], F32, kind="ExternalInput").ap()
    io["c_ctx"] = nc.dram_tensor("c_ctx", [1, D], F32, kind="ExternalInput").ap()
    for k, shp in W_SHAPES.items():
        io[k] = nc.dram_tensor(k, shp, F32, kind="ExternalInput").ap()
    out = nc.dram_tensor("out", [S_LAT, D], F32, kind="ExternalOutput").ap()
    NTOK = S_CTX + S_LAT

    def scratch(name, shape, dt):
        return nc.dram_tensor(name, shape, dt, kind="Internal").ap()

    P = Prog(nc)
    K = Ctx()
    make_consts(P, K)
    phase_cond(P, K, io)
    xs = [scratch(f"xs{i}", [NTOK, D], F32) if not (debug_x and i == len(layers)) else
          nc.dram_tensor("xdbg", [NTOK, D], F32, kind="ExternalOutput").ap() for i in range(len(layers) + 1)]
    m0 = P.mark()
    cp = Ring([P.sbuf(f"cp{i}", [128, D], F32) for i in range(3)])
    for t in range(NTOK // 128):
        tl = cp.next()
        src = io["ctx"][t * 128:(t + 1) * 128, :] if t < S_CTX // 128 else io["x"][t * 128 - S_CTX:(t + 1) * 128 - S_CTX, :]
        P.dma("sp", tl[:], src, out_t=tl)
        P.dma("act", xs[0][t * 128:(t + 1) * 128, :], tl[:], in_t=tl)
    P.end_phase(cp.tiles)
    P.emit()
    P.release(m0)
    mods = [scratch(f"mod{i}", [2, 6 * D], F32) for i in layers]
    u_tok = scratch("u_tok", [2 * S_CTX + S_LAT, D], BF16)
    mix_tok = scratch("mix_tok", [NTOK, D], BF16)
    wgu_t = scratch("wgu_t", [NHC, 128, KD, 256], BF16)
    wo_t = scratch("wo_t", [NHC, 128, 4, 512], BF16)
    wglu_t = scratch("wglu_t", [16, 128, KD, 256], BF16)
    NT3_ = 2 * S_CTX + S_LAT
    wq_t = scratch("wq_t", [ML_H, 128, KD, 128], BF16)
    wk_t = scratch("wk_t", [ML_H, 128, KD, 128], BF16)
    wtok_t = scratch("wtok_t", [8, 128, KD, 512], BF16)
    wgate_t = scratch("wgate_t", [4, 128, KD, 8], BF16)
    wout_t = scratch("wout_t", [8, 128, KD, 256], BF16)
    qT_d = scratch("qT_d", [1024, NT3_], BF16)
    kT_d = scratch("kT_d", [1024, NT3_], BF16)
    v_d = scratch("v_d", [NT3_, D], BF16)
    o_d = scratch("o_d", [NT3_, D], BF16)
    gT_d = scratch("gT_d", [4, 8, NT3_], F32)
    hf_d = scratch("hf_d", [NTOK, D], F32)
    for li, layer in enumerate(layers):
        last = layer == depth_total - 1
        jl = layer // 2
        phase_ada(P, K, io, layer, mods[li])
        wi = io["ffn_w_in"][layer].rearrange("(k p) n -> p k n", p=128)
        jobs = []
        for c in range(NHC):
            jobs.append(([(lambda t: t[:, :, 0:128], wi[:, :, c * 128:(c + 1) * 128]),
                          (lambda t: t[:, :, 128:256], wi[:, :, FH + c * 128: FH + (c + 1) * 128])], wgu_t[c]))
        phase_precast(P, K, jobs, [128, KD, 256], "a")
        wo_src = io["ffn_w_out"][layer].rearrange("(w cc p) (nt n) -> nt w p cc n", cc=4, p=128, n=512)
        jobs = [([(lambda t: t[:], wo_src[nt, w])], wo_t[nt * 11 + w]) for nt in range(4) for w in range(11)]
        phase_precast(P, K, jobs, [128, 4, 512], "b")
        if layer % 2 == 0:
            wsrc = io["s5_w_glu"][jl].rearrange("(k p) (n c) -> n p k c", p=128, c=256)
            jobs = [([(lambda t: t[:], wsrc[n])], wglu_t[n]) for n in range(16)]
            phase_precast(P, K, jobs, [128, KD, 256], "c")
            phase_b_s5(P, K, io, layer, mods[li], xs[li], u_tok, S_LAT)
            phase_c_s5(P, K, io, jl, u_tok, mix_tok, S_LAT)
            phase_d(P, K, io, layer, mods[li], xs[li], xs[li + 1], mix_tok, [wglu_t[n] for n in range(16)],
                    [wgu_t[c] for c in range(NHC)], [wo_t[c] for c in range(NHC)], S_LAT, "s5", last, out)
        else:
            NT3 = 2 * S_CTX + S_LAT
            win = io["ml_w_in"][jl].rearrange("(k p) n -> p k n", p=128)
            jobs = [([(lambda t: t[:], win[:, :, h * 128:(h + 1) * 128])], wq_t[h]) for h in range(ML_H)]
            jobs += [([(lambda t: t[:], win[:, :, 1024 + h * 128:1024 + (h + 1) * 128])], wk_t[h]) for h in range(ML_H)]
            phase_precast(P, K, jobs, [128, KD, 128], "d")
            jobs = [([(lambda t: t[:], win[:, :, 2048 + j * 512:2048 + (j + 1) * 512])], wtok_t[j]) for j in range(8)]
            phase_precast(P, K, jobs, [128, KD, 512], "e")
            jobs = [([(lambda t: t[:], win[:, :, 6144 + ty * 8:6144 + (ty + 1) * 8])], wgate_t[ty]) for ty in range(4)]
            phase_precast(P, K, jobs, [128, KD, 8], "f")
            wsrc = io["ml_w_out"][jl].rearrange("(k p) (n c) -> n p k c", p=128, c=256)
            jobs = [([(lambda t: t[:], wsrc[n])], wout_t[n]) for n in range(8)]
            phase_precast(P, K, jobs, [128, KD, 256], "g")
            phase_b_ml(P, K, io, layer, mods[li], xs[li], S_LAT, [wq_t[h] for h in range(ML_H)], [wk_t[h] for h in range(ML_H)],
                       [wtok_t[j] for j in range(8)], [wgate_t[ty] for ty in range(4)], qT_d, kT_d, v_d, o_d, gT_d)
            phase_e_ml(P, K, io, jl, S_LAT, qT_d, kT_d, v_d, o_d, gT_d, hf_d, mix_tok)
            phase_d(P, K, io, layer, mods[li], xs[li], xs[li + 1], mix_tok, [wout_t[n] for n in range(8)],
                    [wgu_t[c] for c in range(NHC)], [wo_t[c] for c in range(NHC)], S_LAT, "ml", last, out, tok_rows=True)
    P.barrier_all()
    P.emit()
    print("instructions:", P.n_instr)
    return nc


def phase_b_ml(P, K, io, layer, mod_d, xs, S_LAT, wq_t, wk_t, wtok_t, wgate_t, qT_d, kT_d, v_d, o_d, gT_d):
    m = P.mark()
    ROWS = S_LAT // 64
    S = S_CTX + S_LAT
    xr = Ring([P.sbuf(f"e_x{i}", [128, D], F32) for i in range(2)])
    bfr = Ring([P.sbuf(f"e_bf{i}", [128, D], BF16) for i in range(2)])
    t1 = P.sbuf("e_t1", [128, D], F32)
    uT = P.sbuf("e_uT", [128, KD, 512], BF16)
    wqr = Ring([P.sbuf(f"e_wq{i}", [128, KD, 128], BF16) for i in range(3)])
    wtr = Ring([P.sbuf(f"e_wt{i}", [128, KD, 512], BF16) for i in range(2)])
    wg = P.sbuf("e_wg", [128, 4, KD, 8], BF16)
    obr = Ring([P.sbuf(f"e_ob{i}", [128, 512], BF16) for i in range(4)])
    ogr = Ring([P.sbuf(f"e_og{i}", [8, 512], F32) for i in range(2)])
    statr = Ring([P.sbuf(f"e_st{i}", [128, 4], F32) for i in range(4)])
    ptr = Ring([P.psum(f"e_pt{i}", [128, 8, 128], BF16) for i in range(2)])
    mmr = Ring([P.psum(f"e_mm{i}", [128, 512], F32) for i in range(5)])
    pg = P.psum("e_pg", [8, 512], F32)
    scr = dict(pt=ptr)
    tiles = xr.tiles + bfr.tiles + [t1, uT, wg, pg] + wqr.tiles + wtr.tiles + obr.tiles + ogr.tiles + statr.tiles + ptr.tiles + mmr.tiles
    for ty in range(4):
        P.dma("sp", wg[:, ty], wgate_t[ty], out_t=wg)
    qi = [0]

    def q():
        qi[0] += 1
        return "sp" if qi[0] % 2 == 0 else "act"

    xs_lat = xs[S_CTX:S_CTX + S_LAT, :]
    for row in (1, 0):
        mm_mark = P.mark()
        md = load_mod_tiles(P, K, io, layer, mod_d, row, [("sh", 0, "raw"), ("A", 1, "A")], None, "norm1")
        ntok = S_CTX if row == 1 else S_LAT
        for t0 in range(0, ntok, 512):
            nsub = min(4, (ntok - t0) // 128)
            TT = nsub * 128
            dsts = [t0, S + t0] if row == 1 else [S_CTX + t0]
            for s in range(nsub):
                xt = xr.next()
                if row == 1:
                    P.dma(q(), xt[:], xs[t0 + s * 128: t0 + (s + 1) * 128, :], out_t=xt)
                else:
                    lr = lat_rows(xs_lat, t0 + s * 128, S_LAT)
                    for wi in range(128 // ROWS):
                        P.dma(q(), xt[wi * ROWS:(wi + 1) * ROWS, :], lr[wi], out_t=xt)
                ssq = statr.next()
                junk = bfr.next()
                P.op("act", lambda e, xt=xt, ssq=ssq, junk=junk: e.activation(out=junk[:], in_=xt[:], func=AF.Square, accum_out=ssq[:, 0:1]), reads=[xt], writes=[junk, ssq])
                P.op("act", lambda e, ssq=ssq: e.activation(out=ssq[:, 1:2], in_=ssq[:, 0:1], func=AF.Sqrt, bias=K.eps_t[:, 0:1], scale=1.0 / D), reads=[ssq, K.eps_t], writes=[ssq])
                P.op("dve", lambda e, ssq=ssq: e.reciprocal(out=ssq[:, 2:3], in_=ssq[:, 1:2]), reads=[ssq], writes=[ssq])
                P.op("dve", lambda e, xt=xt, ssq=ssq: e.scalar_tensor_tensor(out=t1[:], in0=xt[:], scalar=ssq[:, 2:3], in1=md["A"][:], op0=ALU.mult, op1=ALU.mult),
                     reads=[xt, ssq, md["A"]], writes=[t1])
                ub = bfr.next()
                P.op("pool", lambda e, ub=ub: e.tensor_tensor(out=ub[:], in0=t1[:], in1=md["sh"][:], op=ALU.add), reads=[t1, md["sh"]], writes=[ub])
                transpose_to(P, K, ub, uT, s * 128, scr)
            for (wt_, dd) in ((wq_t, qT_d), (wk_t, kT_d)):
                for h in range(ML_H):
                    wq = wqr.next()
                    P.dma(q(), wq[:], wt_[h], out_t=wq)
                    ps = mmr.next()
                    for k in range(KD):
                        P.op("pe", lambda e, ps=ps, wq=wq, k=k: e.matmul(ps[:, :TT], lhsT=wq[:, k, :], rhs=uT[:, k, :TT], start=(k == 0), stop=(k == KD - 1)),
                             reads=[wq, uT], writes=[ps])
                    ob = obr.next()
                    P.op("act", lambda e, ob=ob, ps=ps: e.activation(out=ob[:, :TT], in_=ps[:, :TT], func=AF.Copy), reads=[ps], writes=[ob])
                    for p0 in dsts:
                        P.dma(q(), dd[h * 128:(h + 1) * 128, p0:p0 + TT], ob[:, :TT], in_t=ob)
            for j in range(8):
                wt = wtr.next()
                P.dma(q(), wt[:], wtok_t[j], out_t=wt)
                dd, c0 = (v_d, j * 512) if j < 4 else (o_d, (j - 4) * 512)
                for s in range(nsub):
                    ps = mmr.next()
                    for k in range(KD):
                        P.op("pe", lambda e, ps=ps, wt=wt, k=k, s=s: e.matmul(ps[:], lhsT=uT[:, k, s * 128:(s + 1) * 128], rhs=wt[:, k, :], start=(k == 0), stop=(k == KD - 1)),
                             reads=[wt, uT], writes=[ps])
                    ob = obr.next()
                    if (j + s) % 2 == 0:
                        P.op("act", lambda e, ob=ob, ps=ps: e.activation(out=ob[:], in_=ps[:], func=AF.Copy), reads=[ps], writes=[ob])
                    else:
                        P.op("dve", lambda e, ob=ob, ps=ps: e.tensor_copy(out=ob[:], in_=ps[:]), reads=[ps], writes=[ob])
                    for p0 in dsts:
                        P.dma(q(), dd[p0 + s * 128: p0 + (s + 1) * 128, c0:c0 + 512], ob[:], in_t=ob)
            for ty in range(4):
                for k in range(KD):
                    P.op("pe", lambda e, k=k, ty=ty: e.matmul(pg[:, :TT], lhsT=wg[:, ty, k, :], rhs=uT[:, k, :TT], start=(k == 0), stop=(k == KD - 1)),
                         reads=[wg, uT], writes=[pg])
                og = ogr.next()
                P.op("dve", lambda e, og=og: e.tensor_copy(out=og[:, :TT], in_=pg[:, :TT]), reads=[pg], writes=[og])
                for p0 in dsts:
                    P.dma(q(), gT_d[ty, :, p0:p0 + TT], og[:, :TT], in_t=og)
        P.end_phase(list(md.values()))
        P.emit()
        P.release(mm_mark)
    P.end_phase(tiles)
    P.emit()
    P.release(m)


def phase_e_ml(P, K, io, jl, S_LAT, qT_d, kT_d, v_d, o_d, gT_d, hf_d, mix_tok):
    S = S_CTX + S_LAT
    NTOK3 = S + S_CTX
    NC = S // 64
    NC3 = NTOK3 // 64
    CC = S_CTX // 64
    SCALE = float(128 ** -0.5)
    m = P.mark()
    omT = P.sbuf("f_omT", [64, NC, 40], F32)
    clT = P.sbuf("f_clT", [64, NC, 40], F32)
    lamR = P.sbuf("f_lamR", [128, 16, NC], F32)
    lamSR = P.sbuf("f_lamSR", [128, 16, NC], F32)
    m1 = P.mark()
    X = [P.sbuf(f"f_X{i}", [40, S], F32) for i in range(5)]
    Mr = P.sbuf("f_Mr", [40, NC], F32)
    lam = P.sbuf("f_lam", [40, NC], F32)
    bia = P.sbuf("f_bias", [40, 4], F32)
    one = P.sbuf("f_one", [40, 2], F32)
    sel = P.sbuf("f_sel", [40, 16, 128], F32)
    pto = Ring([P.psum(f"f_pto{i}", [64, 12, 40], F32) for i in range(2)])
    pl = P.psum("f_pl", [128, NC], F32)
    ew = lambda eng, fn, r, w: P.op(eng, fn, reads=r, writes=w)
    for t in X:
        ew("pool", lambda e, t=t: e.memset(t[:], 0.0), [], [t])
    ew("pool", lambda e: e.memset(bia[:], 0.0), [], [bia])
    ew("pool", lambda e: e.memset(one[:, 0:1], 1.0), [], [one])
    ew("pool", lambda e: e.memset(one[:, 1:2], 0.0), [], [one])
    bg = io["ml_b_gates"][jl:jl + 1, :]
    for (col, lo, p0) in ((0, 0, 0), (1, 8, 0), (0, 16, 32), (1, 24, 32)):
        P.dma("sp", bia[p0:p0 + 8, col:col + 1], bg[:, lo:lo + 8].rearrange("o h -> h o"), out_t=bia, allow_slow_non_contiguous=True)
    P.dma("sp", X[3][0:8, :], gT_d[0, :, 0:S], out_t=X[3])
    P.dma("act", X[3][32:40, :], gT_d[2, :, S_CTX:NTOK3], out_t=X[3])
    P.dma("sp", X[4][0:8, :], gT_d[1, :, 0:S], out_t=X[4])
    P.dma("act", X[4][32:40, :], gT_d[3, :, S_CTX:NTOK3], out_t=X[4])
    ew("dve", lambda e: e.tensor_scalar(out=bia[:, 2:4], in0=bia[:, 0:2], scalar1=1.0 / GATE_CAP, scalar2=None, op0=ALU.mult), [bia], [bia])
    for (src, dst) in ((X[3], X[0]), (X[4], X[1])):
        ew("dve", lambda e, src=src, dst=dst: e.tensor_copy(out=dst[0:8, :], in_=src[0:8, :]), [src], [dst])
        ew("pool", lambda e, src=src, dst=dst: e.tensor_copy(out=dst[32:40, :], in_=src[32:40, ::-1]), [src], [dst])
    for (t, c) in ((X[0], 2), (X[1], 3)):
        ew("act", lambda e, t=t, c=c: e.activation(out=t[:], in_=t[:], func=AF.Tanh, bias=bia[:, c:c + 1], scale=1.0 / GATE_CAP), [t, bia], [t])
        ew("dve", lambda e, t=t: e.tensor_scalar(out=t[:], in0=t[:], scalar1=GATE_CAP, scalar2=None, op0=ALU.mult), [t], [t])
    ew("act", lambda e: e.activation(out=X[3][:], in_=X[1][:], func=AF.Exp, scale=-1.0), [X[1]], [X[3]])
    ew("dve", lambda e: e.tensor_scalar(out=X[4][:], in0=X[3][:], scalar1=2.0, scalar2=None, op0=ALU.add), [X[3]], [X[4]])
    ew("dve", lambda e: e.reciprocal(out=X[4][:], in_=X[4][:]), [X[4]], [X[4]])
    ew("dve", lambda e: e.tensor_tensor(out=X[4][:], in0=X[4][:], in1=X[3][:], op=ALU.mult), [X[4], X[3]], [X[4]])
    ew("pool", lambda e: e.tensor_tensor(out=X[2][:], in0=X[4][:], in1=X[4][:], op=ALU.mult), [X[4]], [X[2]])
    ew("dve", lambda e: e.tensor_scalar(out=X[3][:], in0=X[2][:], scalar1=1.0 / 15, scalar2=1.0 / 13, op0=ALU.mult, op1=ALU.add), [X[2]], [X[3]])
    for cst in (1.0 / 11, 1.0 / 9, 1.0 / 7, 1.0 / 5, 1.0 / 3, 1.0):
        ew("dve", lambda e: e.tensor_tensor(out=X[3][:], in0=X[3][:], in1=X[2][:], op=ALU.mult), [X[3], X[2]], [X[3]])
        ew("dve", lambda e, cst=cst: e.tensor_scalar(out=X[3][:], in0=X[3][:], scalar1=float(cst), scalar2=None, op0=ALU.add), [X[3]], [X[3]])
    ew("dve", lambda e: e.scalar_tensor_tensor(out=X[1][:], in0=X[4][:], scalar=-2.0, in1=X[3][:], op0=ALU.mult, op1=ALU.mult), [X[4], X[3]], [X[1]])
    ew("dve", lambda e: e.tensor_tensor_scan(out=X[2][:], data0=one[:, 0:1].broadcast_to([40, S]), data1=X[1][:], initial=0.0, op0=ALU.mult, op1=ALU.add),
       [one, X[1]], [X[2]])
    ew("dve", lambda e: e.tensor_tensor(out=X[0][:], in0=X[0][:], in1=X[2][:], op=ALU.subtract), [X[0], X[2]], [X[0]])
    ew("dve", lambda e: e.tensor_tensor_scan(out=X[3][:], data0=one[:, 1:2].broadcast_to([40, S]), data1=X[0][:], initial=0.0, op0=ALU.add, op1=ALU.max),
       [one, X[0]], [X[3]])
    ew("dve", lambda e: e.tensor_copy(out=Mr[:], in_=X[3][:, 63::64]), [X[3]], [Mr])
    mrb = bc(Mr[:], 2, 64)
    ew("dve", lambda e: e.tensor_tensor(out=X[0][:].rearrange("p (n t) -> p n t", t=64), in0=X[0][:].rearrange("p (n t) -> p n t", t=64), in1=mrb, op=ALU.subtract), [X[0], Mr], [X[0]])
    ew("act", lambda e: e.activation(out=X[0][:], in_=X[0][:], func=AF.Exp), [X[0]], [X[0]])
    ew("dve", lambda e: e.tensor_tensor(out=X[2][:].rearrange("p (n t) -> p n t", t=64), in0=X[2][:].rearrange("p (n t) -> p n t", t=64), in1=mrb, op=ALU.add), [X[2], Mr], [X[2]])
    ew("act", lambda e: e.activation(out=X[2][:], in_=X[2][:], func=AF.Exp, scale=-1.0), [X[2]], [X[2]])
    ew("dve", lambda e: e.tensor_scalar(out=lam[:, 0:1], in0=Mr[:, 0:1], scalar1=-1.0, scalar2=None, op0=ALU.mult), [Mr], [lam])
    ew("dve", lambda e: e.tensor_tensor(out=lam[:, 1:NC], in0=Mr[:, 0:NC - 1], in1=Mr[:, 1:NC], op=ALU.subtract), [Mr], [lam])
    ew("act", lambda e: e.activation(out=lam[:], in_=lam[:], func=AF.Exp), [lam], [lam])
    for (src, dst) in ((X[0], X[3]), (X[2], X[4])):
        ew("dve", lambda e, src=src, dst=dst: e.tensor_copy(out=dst[0:8, :], in_=src[0:8, :]), [src], [dst])
        ew("pool", lambda e, src=src, dst=dst: e.tensor_copy(out=dst[32:40, :], in_=src[32:40, ::-1]), [src], [dst])
    for (src, dstT) in ((X[3], omT), (X[4], clT)):
        for k0 in range(0, NC, 12):
            nk = min(12, NC - k0)
            pt = pto.next()
            for kk in range(nk):
                k = k0 + kk
                ew("pe", lambda e, pt=pt, kk=kk, k=k, src=src: e.transpose(out=pt[:, kk, :], in_=src[:, 64 * k:64 * k + 64], identity=K.ident_f[0:40, 0:40]),
                   [src, K.ident_f], [pt])
            ew("act", lambda e, pt=pt, k0=k0, nk=nk, dstT=dstT: e.activation(out=dstT[:, k0:k0 + nk, :], in_=pt[:, 0:nk, :], func=AF.Copy), [pt], [dstT])
    for r in range(16):
        row = (r // 8) * 32 + (r % 8)
        ew("dve", lambda e, r=r, row=row: e.tensor_copy(out=sel[:, r, :], in_=K.ident_f[0:40, row:row + 1].broadcast_to([40, 128])), [K.ident_f], [sel])
    for r in range(16):
        ew("pe", lambda e, r=r: e.matmul(pl[:], lhsT=sel[:, r, :], rhs=lam[:], start=True, stop=True), [sel, lam], [pl])
        ew("act", lambda e, r=r: e.activation(out=lamR[:, r, :], in_=pl[:], func=AF.Copy), [pl], [lamR])
    ew("dve", lambda e: e.tensor_scalar(out=lamSR[:], in0=lamR[:], scalar1=SCALE, scalar2=None, op0=ALU.mult), [lamR], [lamSR])
    P.end_phase(X + [Mr, lam, bia, one, sel, pl] + pto.tiles)
    P.emit()
    P.release(m1)

    qT = P.sbuf("f_qT", [128, NTOK3], BF16)
    kT = P.sbuf("f_kT", [128, NTOK3], BF16)
    vv = P.sbuf("f_vv", [64, NC3, 257], BF16)
    Cf = P.sbuf("f_Cf", [128, 257], F32)
    Csr = Ring([P.sbuf(f"f_Cs{i}", [128, 257], BF16) for i in range(2)])
    Spr = Ring([P.sbuf(f"f_Sp{i}", [64, 64], BF16) for i in range(3)])
    kwr = Ring([P.sbuf(f"f_kw{i}", [64, 128], BF16) for i in range(3)])
    mask = [P.sbuf(f"f_mask{i}", [64, 64], F32) for i in range(2)]
    dnr = Ring([P.sbuf(f"f_dn{i}", [64, 6], F32) for i in range(4)])
    hfr = Ring([P.sbuf(f"f_hf{i}", [64, 256], F32) for i in range(3)])
    hsr = Ring([P.sbuf(f"f_hs{i}", [64, 256], F32) for i in range(3)])
    oor = Ring([P.sbuf(f"f_oo{i}", [64, 256], BF16) for i in range(3)])
    sgr = Ring([P.sbuf(f"f_sg{i}", [64, 256], F32) for i in range(2)])
    hor = Ring([P.sbuf(f"f_ho{i}", [64, 256], BF16) for i in range(3)])
    junk = P.sbuf("f_junk", [64, 256], BF16)
    nw = P.sbuf("f_nw", [64, D], F32)
    eps64 = P.sbuf("f_eps", [64, 1], F32)
    pS = Ring([P.psum(f"f_pS{i}", [64, 64], F32) for i in range(2)])
    pK = Ring([P.psum(f"f_pK{i}", [64, 128], BF16) for i in range(2)])
    pH = Ring([P.psum(f"f_pH{i}", [64, 257], F32) for i in range(2)])
    pC = Ring([P.psum(f"f_pC{i}", [128, 257], F32) for i in range(2)])
    tiles = ([qT, kT, vv, Cf, junk, nw, eps64, omT, clT, lamR, lamSR] + Csr.tiles + Spr.tiles + kwr.tiles + mask + dnr.tiles + hfr.tiles + hsr.tiles + oor.tiles
             + sgr.tiles + hor.tiles + pS.tiles + pK.tiles + pH.tiles + pC.tiles)
    ew("pool", lambda e: e.memset(eps64[:], EPS), [], [eps64])
    for i, (cm, pat) in enumerate(((-1, 1), (1, -1))):
        ew("pool", lambda e, i=i: e.memset(mask[i][:], SCALE), [], [mask[i]])
        ew("pool", lambda e, i=i, cm=cm, pat=pat: e.affine_select(out=mask[i][:], in_=mask[i][:], compare_op=ALU.is_ge, fill=0.0, base=0,
                                                                  pattern=[[pat, 64]], channel_multiplier=cm), [mask[i]], [mask[i]])
    ew("pool", lambda e: e.memset(vv[:, :, 256:257], 1.0), [], [vv])
    load_rep_n = lambda: P.dma("sp", nw[:], io["ml_norm"][jl:jl + 1, :].partition_broadcast(64), out_t=nw)
    load_rep_n()

    for h in range(ML_H):
        P.dma("sp", qT[:], qT_d[h * 128:(h + 1) * 128, :], out_t=qT)
        P.dma("act", kT[:], kT_d[h * 128:(h + 1) * 128, :], out_t=kT)
        half = NC3 // 2
        P.dma("sp", vv[:, 0:half, 0:256], v_d[0:half * 64, h * 256:(h + 1) * 256].rearrange("(n t) e -> t n e", t=64), out_t=vv)
        P.dma("act", vv[:, half:NC3, 0:256], v_d[half * 64:NTOK3, h * 256:(h + 1) * 256].rearrange("(n t) e -> t n e", t=64), out_t=vv)
        for d in range(2):
            r = d * 8 + h
            ew("dve", lambda e: e.memset(Cf[:], 0.0), [], [Cf])
            Cs = Csr.next()
            ew("pool", lambda e, Cs=Cs: e.memset(Cs[:], 0.0), [], [Cs])

            def front(mi):
                c = mi if d == 0 else NC3 - 1 - mi
                k = c if d == 0 else c - CC
                cols = slice(64 * c, 64 * c + 64)
                ps = pS.next()
                ew("pe", lambda e, ps=ps, cols=cols: e.matmul(ps[:], lhsT=kT[:, cols], rhs=qT[:, cols], start=True, stop=True), [kT, qT], [ps])
                pk = pK.next()
                ew("pe", lambda e, pk=pk, cols=cols: e.transpose(out=pk[:], in_=kT[:, cols], identity=K.ident_b[:]), [kT, K.ident_b], [pk])
                Sp = Spr.next()
                om = omT[:, k, 32 * d + h: 32 * d + h + 1]
                ew("dve", lambda e, Sp=Sp, ps=ps, om=om: e.scalar_tensor_tensor(out=Sp[:], in0=ps[:], scalar=om, in1=mask[d][:], op0=ALU.mult, op1=ALU.mult),
                   [ps, omT, mask[d]], [Sp])
                kw = kwr.next()
                ew("act", lambda e, kw=kw, pk=pk, om=om: e.activation(out=kw[:], in_=pk[:], func=AF.Copy, scale=om), [pk, omT], [kw])
                return (c, k, cols, Sp, kw)

            nxt = front(0)
            for mi in range(NC):
                c, k, cols, Sp, kw = nxt
                if mi + 1 < NC:
                    nxt = front(mi + 1)
                tc = c if c < NC else c - NC
                ph = pH.next()
                ew("pe", lambda e, ph=ph, Sp=Sp, c=c: e.matmul(ph[:], lhsT=Sp[:], rhs=vv[:, c, :], start=True, stop=False), [Sp, vv], [ph])
                ew("pe", lambda e, ph=ph, Cs=Cs, cols=cols: e.matmul(ph[:], lhsT=qT[:, cols], rhs=Cs[:], start=False, stop=True), [qT, Cs], [ph])
                pc = pC.next()
                ew("pe", lambda e, pc=pc, kw=kw, c=c: e.matmul(pc[:], lhsT=kw[:], rhs=vv[:, c, :], start=True, stop=True), [kw, vv], [pc])
                ew("dve", lambda e, pc=pc, mi=mi, r=r: e.scalar_tensor_tensor(out=Cf[:], in0=Cf[:], scalar=lamR[:, r, mi:mi + 1], in1=pc[:], op0=ALU.mult, op1=ALU.add),
                   [Cf, lamR, pc], [Cf])
                if mi + 1 < NC:
                    Cs = Csr.next()
                    ew("act", lambda e, Cs=Cs, mi=mi, r=r: e.activation(out=Cs[:], in_=Cf[:], func=AF.Copy, scale=lamSR[:, r, mi + 1:mi + 2]), [Cf, lamSR], [Cs])
                dn = dnr.next()
                ew("act", lambda e, dn=dn, ph=ph: e.activation(out=dn[:, 4:5], in_=ph[:, 256:257], func=AF.Copy), [ph], [dn])
                ew("dve", lambda e, dn=dn: e.scalar_tensor_tensor(out=dn[:, 0:1], in0=dn[:, 4:5], scalar=-1.0, in1=dn[:, 4:5], op0=ALU.mult, op1=ALU.max),
                   [dn], [dn])
                ew("dve", lambda e, dn=dn, k=k: e.tensor_tensor(out=dn[:, 1:2], in0=dn[:, 0:1], in1=clT[:, k, 32 * d + h: 32 * d + h + 1], op=ALU.max), [dn, clT], [dn])
                ew("dve", lambda e, dn=dn: e.reciprocal(out=dn[:, 2:3], in_=dn[:, 1:2]), [dn], [dn])
                rows = slice(64 * tc, 64 * tc + 64)
                hcols = slice(h * 256, (h + 1) * 256)
                if d == 0:
                    hf = hfr.next()
                    ew("act", lambda e, hf=hf, ph=ph, dn=dn: e.activation(out=hf[:], in_=ph[:, 0:256], func=AF.Copy, scale=dn[:, 2:3]), [ph, dn], [hf])
                    P.dma("sp", hf_d[rows, hcols], hf[:], in_t=hf)
                else:
                    hf = hfr.next()
                    P.dma("sp", hf[:], hf_d[rows, hcols], out_t=hf)
                    oo = oor.next()
                    P.dma("act", oo[:], o_d[rows, hcols], out_t=oo)
                    hs = hsr.next()
                    ew("dve", lambda e, hs=hs, ph=ph, dn=dn, hf=hf: e.scalar_tensor_tensor(out=hs[:], in0=ph[:, 0:256], scalar=dn[:, 2:3], in1=hf[:], op0=ALU.mult, op1=ALU.add),
                       [ph, dn, hf], [hs])
                    ew("act", lambda e, hs=hs, dn=dn: e.activation(out=junk[:], in_=hs[:], func=AF.Square, accum_out=dn[:, 3:4]), [hs], [junk, dn])
                    ew("act", lambda e, dn=dn: e.activation(out=dn[:, 3:4], in_=dn[:, 3:4], func=AF.Sqrt, bias=eps64[:, 0:1], scale=1.0 / 256), [dn, eps64], [dn])
                    ew("dve", lambda e, dn=dn: e.reciprocal(out=dn[:, 3:4], in_=dn[:, 3:4]), [dn], [dn])
                    sg = sgr.next()
                    ew("act", lambda e, sg=sg, oo=oo: e.activation(out=sg[:], in_=oo[:], func=AF.Sigmoid), [oo], [sg])
                    ew("dve", lambda e, hs=hs, dn=dn, hcols=hcols: e.scalar_tensor_tensor(out=hs[:], in0=hs[:], scalar=dn[:, 3:4], in1=nw[:, hcols], op0=ALU.mult, op1=ALU.mult),
                       [hs, dn, nw], [hs])
                    ho = hor.next()
                    ew("pool", lambda e, ho=ho, hs=hs, sg=sg: e.tensor_tensor(out=ho[:], in0=hs[:], in1=sg[:], op=ALU.mult), [hs, sg], [ho])
                    P.dma("act", mix_tok[rows, hcols], ho[:], in_t=ho)
            if d == 0:
                P.barrier_all()
            P.emit()
    P.end_phase(tiles)
    P.emit()
    P.release(m)


_NC_CACHE = {}


def kernel(**inputs):
    S_LAT = inputs["x"].shape[1]
    B = inputs["x"].shape[0]
    if S_LAT not in _NC_CACHE:
        _NC_CACHE[S_LAT] = build_program(S_LAT)
    nc = _NC_CACHE[S_LAT]
    shared = {}
    for k in W_SHAPES:
        a = np.ascontiguousarray(np.asarray(inputs[k], dtype=np.float32))
        if k == "norm_f":
            a = a.reshape(1, D)
        shared[k] = a
    shared["c_ctx"] = np.ascontiguousarray(np.asarray(inputs["c_ctx"], dtype=np.float32)).reshape(1, D)
    n_cores = 8
    in_maps = []
    for core in range(n_cores):
        b = core % B
        mp = dict(shared)
        mp["x"] = np.ascontiguousarray(np.asarray(inputs["x"][b], dtype=np.float32))
        mp["ctx"] = np.ascontiguousarray(np.asarray(inputs["ctx"][b], dtype=np.float32))
        mp["c"] = np.ascontiguousarray(np.asarray(inputs["c"][b:b + 1], dtype=np.float32))
        in_maps.append(mp)
    res = run_bass_kernel_spmd(nc, in_maps, core_ids=list(range(n_cores)))
    out = np.stack([np.asarray(res.results[b]["out"]) for b in range(B)], axis=0)
    return out.astype(np.float32)
```

```python
import numpy as np
import concourse.bass as bass
import concourse.mybir as mybir
from concourse.bass_utils import run_bass_kernel_spmd

F32 = mybir.dt.float32
BF16 = mybir.dt.bfloat16
AF = mybir.ActivationFunctionType
ALU = mybir.AluOpType
AX = mybir.AxisListType

ENGS = ("pe", "act", "dve", "pool", "sp")

D = 2048
KD = D // 128
FH = 5632
NHC = FH // 128
S_CTX = 256
EPS = 1e-6
T0 = 8
G = 128
PST = 64
ML_H = 8
ML_IN = 6176
GATE_CAP = 15.0


class T:
    def __init__(self, h, name):
        self.h = h
        self.name = name
        self.last_w = None
        self.readers = []
        self.ld_sem = None
        self.ld_cnt = 0
        self.st_sem = None
        self.st_cnt = 0

    def __getitem__(self, k):
        return self.h[k]


class Prog:
    def __init__(self, nc, same_engine_sync=True):
        self.nc = nc
        self.ops = {e: [] for e in ENGS}
        self.cnt = {e: 0 for e in ENGS}
        self.waited = {}
        self.esem = {}
        self.same_engine_sync = same_engine_sync
        self.all_st = []
        self._stack = []
        self.dma_sems = []
        self.dma_sem_i = 0
        self.n_instr = 0
        for e in ("pe", "act", "dve", "pool"):
            self.esem[e] = nc.alloc_semaphore("prog_" + e)
        self.free_sems = [nc.alloc_semaphore(f"dsem{i}") for i in range(90)]
        self.sem_users = {}

    def sbuf(self, name, shape, dt):
        self.uid = getattr(self, "uid", 0) + 1
        name = f"{name}_{self.uid}"
        cm = self.nc.sbuf_tensor(name, list(shape), dt)
        h = cm.__enter__()
        self._stack.append(cm)
        return T(h, name)

    def psum(self, name, shape, dt=F32):
        self.uid = getattr(self, "uid", 0) + 1
        name = f"{name}_{self.uid}"
        cm = self.nc.psum_tensor(name, list(shape), dt)
        h = cm.__enter__()
        self._stack.append(cm)
        return T(h, name)

    def mark(self):
        return len(self._stack)

    def release(self, mark):
        while len(self._stack) > mark:
            cm = self._stack.pop()
            cm.__exit__(None, None, None)

    def _get_sem(self, t, kind):
        if not self.free_sems:
            raise RuntimeError("out of DMA semaphores")
        return self.free_sems.pop()

    def _need(self, eng, dep, waits):
        if dep is None:
            return
        if dep[0] == "eng":
            _, e2, seq = dep
            if e2 == eng and (eng == "pe" or not self.same_engine_sync):
                return
            key = (eng, "eng", e2)
            if self.waited.get(key, 0) >= seq:
                return
            self.waited[key] = seq
            waits.append((self.esem[e2], seq))
        else:
            _, sem, val = dep
            key = (eng, "sem", sem.num)
            if self.waited.get(key, 0) >= val:
                return
            self.waited[key] = val
            waits.append((sem, val))

    def op(self, eng, fn, reads=(), writes=()):
        waits = []
        for t in reads:
            self._need(eng, t.last_w, waits)
        for t in writes:
            self._need(eng, t.last_w, waits)
            for r in t.readers:
                self._need(eng, r, waits)
        self.cnt[eng] += 1
        seq = self.cnt[eng]
        me = ("eng", eng, seq)
        for t in writes:
            t.last_w = me
            t.readers = []
        for t in reads:
            if t.last_w is not me:
                t.readers.append(me)
                if len(t.readers) > 48:
                    best = {}
                    for r in t.readers:
                        k = (r[0], r[1] if r[0] == "eng" else r[1].num)
                        if k not in best or best[k][2] < r[2]:
                            best[k] = r
                    t.readers = list(best.values())
        self.ops[eng].append((waits, fn, (self.esem[eng], 1)))
        self.n_instr += 1

    def dma(self, q, out, in_, out_t=None, in_t=None, **kw):
        waits = []
        if in_t is not None:
            self._need(q, in_t.last_w, waits)
        if out_t is not None:
            self._need(q, out_t.last_w, waits)
            for r in out_t.readers:
                self._need(q, r, waits)
        if out_t is not None:
            if out_t.ld_sem is None:
                out_t.ld_sem = self._get_sem(out_t, "ld")
                out_t.ld_cnt = self.sem_users.get(out_t.ld_sem.num, 0)
            out_t.ld_cnt += 16
            self.sem_users[out_t.ld_sem.num] = out_t.ld_cnt
            sem, val = out_t.ld_sem, out_t.ld_cnt
            out_t.last_w = ("dma", sem, val)
            out_t.readers = []
            if in_t is not None:
                in_t.readers.append(("dma", sem, val))
        else:
            if in_t.st_sem is None:
                in_t.st_sem = self._get_sem(in_t, "st")
                in_t.st_cnt = self.sem_users.get(in_t.st_sem.num, 0)
                self.all_st.append(in_t)
            in_t.st_cnt += 16
            self.sem_users[in_t.st_sem.num] = in_t.st_cnt
            sem, val = in_t.st_sem, in_t.st_cnt
            in_t.readers.append(("dma", sem, val))

        def fn(e, out=out, in_=in_, kw=kw):
            return e.dma_start(out=out, in_=in_, **kw)

        self.ops[q].append((waits, fn, (sem, 16)))
        self.n_instr += 1

    def barrier_all(self):
        for e in ENGS:
            waits = []
            for e2 in ("pe", "act", "dve", "pool"):
                if self.cnt[e2] > 0:
                    self._need(e, ("eng", e2, self.cnt[e2]), waits)
            for t in self.all_st:
                self._need(e, ("dma", t.st_sem, t.st_cnt), waits)
            if waits:
                self.ops[e].append((waits, None, None))

    def end_phase(self, tiles):
        for t in tiles:
            if t.ld_sem is not None and t.last_w is not None and t.last_w[0] == "dma":
                w = []
                self._need("sp", t.last_w, w)
                if w:
                    self.ops["sp"].append((w, None, None))
        self.barrier_all()
        for t in tiles:
            for s in (t.ld_sem, t.st_sem):
                if s is not None:
                    self.free_sems.append(s)
            if t in self.all_st:
                self.all_st.remove(t)
            t.ld_sem = None
            t.st_sem = None

    def emit(self):
        nc = self.nc
        ops = self.ops

        def run(eng_obj, lst):
            for waits, fn, inc in lst:
                for sem, val in waits:
                    eng_obj.wait_ge(sem, val)
                if fn is not None:
                    ins = fn(eng_obj)
                    ins.then_inc(inc[0], inc[1])

        with nc.Block() as block:
            @block.tensor
            def _(e):
                run(e, ops["pe"])

            @block.scalar
            def _(e):
                run(e, ops["act"])

            @block.vector
            def _(e):
                run(e, ops["dve"])

            @block.gpsimd
            def _(e):
                run(e, ops["pool"])

            @block.sync
            def _(e):
                run(e, ops["sp"])
        self.ops = {e: [] for e in ENGS}


class Ring:
    def __init__(self, tiles):
        self.tiles = tiles
        self.i = 0

    def next(self):
        t = self.tiles[self.i % len(self.tiles)]
        self.i += 1
        return t


def col_blocks(lo, hi, step=512):
    out = []
    a = lo
    while a < hi:
        b = min(hi, (a // step + 1) * step)
        out.append((a, b))
        a = b
    return out


class Ctx:
    pass


def make_consts(P, K):
    K.ident_b = P.sbuf("ident_b", [128, 128], BF16)
    K.ident_f = P.sbuf("ident_f", [128, 128], F32)
    for t in (K.ident_b, K.ident_f):
        P.op("pool", lambda e, t=t: e.memset(t[:], 0.0), writes=[t])
        P.op("pool", lambda e, t=t: e.affine_select(out=t[:], in_=t[:], compare_op=ALU.not_equal, fill=1.0,
                                                    base=0, pattern=[[-1, 128]], channel_multiplier=1),
             reads=[t], writes=[t])
    K.eps_t = P.sbuf("eps_t", [128, 1], F32)
    P.op("pool", lambda e: e.memset(K.eps_t[:], EPS), writes=[K.eps_t])
    K.cbias = P.sbuf("cbias", [128, 8], F32)
    for k in range(8):
        P.op("pool", lambda e, k=k: e.memset(K.cbias[:, k:k + 1], -(2 * k - 1) * float(np.pi)), writes=[K.cbias])


def phase_precast(P, K, jobs, shape, tag):
    m = P.mark()
    f32r = Ring([P.sbuf(f"pc_f{i}_{tag}", shape, F32) for i in range(4)])
    b16r = Ring([P.sbuf(f"pc_b{i}_{tag}", shape, BF16) for i in range(4)])
    tiles = f32r.tiles + b16r.tiles
    for i, (parts, d) in enumerate(jobs):
        tf = f32r.next()
        tb = b16r.next()
        for pi_, (sl, s_) in enumerate(parts):
            P.dma("sp" if (i + pi_) % 2 == 0 else "act", sl(tf), s_, out_t=tf)
        if i % 2 == 0:
            P.op("dve", lambda e, tb=tb, tf=tf: e.tensor_copy(out=tb[:], in_=tf[:]), reads=[tf], writes=[tb])
        else:
            P.op("act", lambda e, tb=tb, tf=tf: e.activation(out=tb[:], in_=tf[:], func=AF.Copy), reads=[tf], writes=[tb])
        P.dma("sp" if i % 2 == 1 else "act", d, tb[:], in_t=tb)
    P.end_phase(tiles)
    P.emit()
    P.release(m)


def load_rep(P, q, tile, dram_row_ap):
    P.dma(q, tile[:], dram_row_ap.partition_broadcast(128), out_t=tile)


def rmsnorm_mod_T(P, K, xt, A_rep, sh_rep, uT, col0, scr):
    junk = scr["junk"]
    ssq = scr["stat"].next()
    P.op("act", lambda e: e.activation(out=junk[:], in_=xt[:], func=AF.Square, accum_out=ssq[:, 0:1]),
         reads=[xt], writes=[junk, ssq])
    P.op("act", lambda e: e.activation(out=ssq[:, 1:2], in_=ssq[:, 0:1], func=AF.Sqrt, bias=K.eps_t[:, 0:1], scale=1.0 / D),
         reads=[ssq, K.eps_t], writes=[ssq])
    P.op("dve", lambda e: e.reciprocal(out=ssq[:, 2:3], in_=ssq[:, 1:2]), reads=[ssq], writes=[ssq])
    t1 = scr["t1"]
    P.op("dve", lambda e: e.scalar_tensor_tensor(out=t1[:], in0=xt[:], scalar=ssq[:, 2:3], in1=A_rep[:],
                                                  op0=ALU.mult, op1=ALU.mult), reads=[xt, ssq, A_rep], writes=[t1])
    ub = scr["ub"].next()
    P.op("pool", lambda e: e.tensor_tensor(out=ub[:], in0=t1[:], in1=sh_rep[:], op=ALU.add), reads=[t1, sh_rep], writes=[ub])
    transpose_to(P, K, ub, uT, col0, scr)
    return ssq


def transpose_to(P, K, ub, uT, col0, scr):
    for g in range(KD // 8):
        pt = scr["pt"].next()
        for j in range(8):
            k = g * 8 + j
            P.op("pe", lambda e, pt=pt, j=j, k=k: e.transpose(out=pt[:, j, :], in_=ub[:, k * 128:(k + 1) * 128], identity=K.ident_b[:]),
                 reads=[ub, K.ident_b], writes=[pt])
        eng = "act" if g % 2 == 0 else "dve"
        if eng == "act":
            P.op("act", lambda e, pt=pt, g=g: e.activation(out=uT[:, g * 8:(g + 1) * 8, col0:col0 + 128], in_=pt[:], func=AF.Copy),
                 reads=[pt], writes=[uT])
        else:
            P.op("dve", lambda e, pt=pt, g=g: e.tensor_copy(out=uT[:, g * 8:(g + 1) * 8, col0:col0 + 128], in_=pt[:]),
                 reads=[pt], writes=[uT])


def phase_cond(P, K, io):
    K.condT = P.sbuf("condT", [128, KD, 2], F32)
    m = P.mark()
    craw = P.sbuf("craw", [KD, 2, 128], F32)
    csil = P.sbuf("csil", [KD, 2, 128], F32)
    pt = P.psum("cond_pt", [128, 2, KD], F32)
    P.dma("sp", craw[:, 0, :], io["c"].rearrange("o (k p) -> (o k) p", p=128), out_t=craw)
    P.dma("sp", craw[:, 1, :], io["c_ctx"].rearrange("o (k p) -> (o k) p", p=128), out_t=craw)
    P.op("act", lambda e: e.activation(out=csil[:], in_=craw[:], func=AF.Silu), reads=[craw], writes=[csil])
    for r in range(2):
        P.op("pe", lambda e, r=r: e.transpose(out=pt[:, r, :], in_=csil[:, r, :], identity=K.ident_f[0:KD, 0:KD]),
             reads=[csil, K.ident_f], writes=[pt])
    P.op("dve", lambda e: e.tensor_copy(out=K.condT[:].rearrange("p k r -> p r k"), in_=pt[:]), reads=[pt], writes=[K.condT])
    P.end_phase([craw, csil, pt])
    P.emit()
    P.release(m)


def phase_ada(P, K, io, layer, mod_out):
    m = P.mark()
    NB = 6 * D // 512
    wr = Ring([P.sbuf(f"ada_w{i}", [128, KD, 512], F32) for i in range(4)])
    br = Ring([P.sbuf(f"ada_b{i}", [2, 512], F32) for i in range(2)])
    orr = Ring([P.sbuf(f"ada_o{i}", [2, 512], F32) for i in range(2)])
    pr = Ring([P.psum(f"ada_p{i}", [2, 512], F32) for i in range(2)])
    aw = io["ada_w"][layer].rearrange("(k p) n -> p k n", p=128)
    ab = io["ada_b"][layer:layer + 1, :]
    for nb in range(NB):
        wt = wr.next(); bt = br.next(); ot = orr.next(); ps = pr.next()
        cs = slice(nb * 512, (nb + 1) * 512)
        P.dma("sp" if nb % 2 == 0 else "act", wt[:], aw[:, :, cs], out_t=wt)
        P.dma("sp", bt[:], ab[:, cs].partition_broadcast(2), out_t=bt)
        for k in range(KD):
            P.op("pe", lambda e, ps=ps, wt=wt, k=k: e.matmul(ps[:], lhsT=K.condT[:, k, :], rhs=wt[:, k, :], start=(k == 0), stop=(k == KD - 1)),
                 reads=[K.condT, wt], writes=[ps])
        P.op("dve", lambda e, ot=ot, ps=ps, bt=bt: e.tensor_tensor(out=ot[:], in0=ps[:], in1=bt[:], op=ALU.add), reads=[ps, bt], writes=[ot])
        P.dma("sp", mod_out[:, cs], ot[:], in_t=ot)
    P.end_phase(wr.tiles + br.tiles + orr.tiles + pr.tiles)
    P.emit()
    P.release(m)


def load_mod_tiles(P, K, io, layer, mod_d, row, which, names, normkey):
    out = {}
    gt = None
    for name, ci, kind in which:
        t = P.sbuf(f"mod_{name}", [128, D], F32)
        load_rep(P, "sp", t, mod_d[row:row + 1, ci * D:(ci + 1) * D])
        if kind == "A":
            if gt is None:
                gt = P.sbuf("mod_g", [128, D], F32)
                load_rep(P, "act", gt, io[normkey][layer:layer + 1, :])
            P.op("dve", lambda e, t=t, gt=gt: e.scalar_tensor_tensor(out=t[:], in0=t[:], scalar=1.0, in1=gt[:], op0=ALU.add, op1=ALU.mult),
                 reads=[t, gt], writes=[t])
        out[name] = t
    if gt is not None:
        out["_g"] = gt
    return out


def lat_rows(ap_lat, p0, S_LAT, n=128):
    ROWS = S_LAT // 64
    w0, nw = p0 // ROWS, n // ROWS
    return ap_lat.rearrange("(r w) d -> w r d", w=64)[w0:w0 + nw]


def token_tiles(S_LAT):
    tl = [(1, 0, S_CTX // 128)]
    for t0 in range(0, S_LAT, 512):
        tl.append((0, S_CTX + t0, min(4, (S_LAT - t0) // 128)))
    return tl


def phase_b_s5(P, K, io, layer, mod_d, xs, u_tok, S_LAT):
    m = P.mark()
    xr = Ring([P.sbuf(f"b_x{i}", [128, D], F32) for i in range(3)])
    scr = dict(junk=P.sbuf("b_junk", [128, D], BF16), t1=P.sbuf("b_t1", [128, D], F32),
               stat=Ring([P.sbuf(f"b_st{i}", [128, 4], F32) for i in range(4)]),
               ub=Ring([P.sbuf(f"b_ub{i}", [128, D], BF16) for i in range(3)]))
    tiles = xr.tiles + [scr["junk"], scr["t1"]] + scr["stat"].tiles + scr["ub"].tiles
    for row in (1, 0):
        mm = P.mark()
        md = load_mod_tiles(P, K, io, layer, mod_d, row, [("sh", 0, "raw"), ("A", 1, "A")], None, "norm1")
        ntok = S_CTX if row == 1 else S_LAT
        base = 0 if row == 1 else S_CTX
        for t in range(ntok // 128):
            xt = xr.next()
            P.dma("sp", xt[:], xs[base + t * 128: base + (t + 1) * 128, :], out_t=xt)
            ssq = scr["stat"].next()
            junk = scr["junk"]
            P.op("act", lambda e, xt=xt, ssq=ssq: e.activation(out=junk[:], in_=xt[:], func=AF.Square, accum_out=ssq[:, 0:1]),
                 reads=[xt], writes=[junk, ssq])
            P.op("act", lambda e, ssq=ssq: e.activation(out=ssq[:, 1:2], in_=ssq[:, 0:1], func=AF.Sqrt, bias=K.eps_t[:, 0:1], scale=1.0 / D),
                 reads=[ssq, K.eps_t], writes=[ssq])
            P.op("dve", lambda e, ssq=ssq: e.reciprocal(out=ssq[:, 2:3], in_=ssq[:, 1:2]), reads=[ssq], writes=[ssq])
            t1 = scr["t1"]
            P.op("dve", lambda e, xt=xt, ssq=ssq: e.scalar_tensor_tensor(out=t1[:], in0=xt[:], scalar=ssq[:, 2:3], in1=md["A"][:],
                                                                          op0=ALU.mult, op1=ALU.mult), reads=[xt, ssq, md["A"]], writes=[t1])
            ub = scr["ub"].next()
            P.op("pool", lambda e, ub=ub: e.tensor_tensor(out=ub[:], in0=t1[:], in1=md["sh"][:], op=ALU.add), reads=[t1, md["sh"]], writes=[ub])
            P.dma("act", u_tok[base + t * 128: base + (t + 1) * 128, :], ub[:], in_t=ub)
            if row == 1:
                P.dma("act", u_tok[S_CTX + S_LAT + t * 128: S_CTX + S_LAT + (t + 1) * 128, :], ub[:], in_t=ub)
        P.end_phase(list(md.values()))
        P.emit()
        P.release(mm)
    P.end_phase(tiles)
    P.emit()
    P.release(m)


def phase_d(P, K, io, layer, mod_d, xs_in, xs_out, mix_tok, wproj, wgu_t, wo_t, S_LAT, mixer, last, out_final, tok_rows=None):
    m = P.mark()
    xt = [P.sbuf(f"d_x{i}", [128, D], F32) for i in range(4)]
    bfr = Ring([P.sbuf(f"d_bf{i}", [128, D], BF16) for i in range(2)])
    u2T = P.sbuf("d_u2T", [128, KD, 512], BF16)
    hT = P.sbuf("d_hT", [128, NHC, 512], BF16)
    wpr = Ring([P.sbuf(f"d_wp{i}", [128, KD, 256], BF16) for i in range(2)])
    wgr = Ring([P.sbuf(f"d_wgu{i}", [128, KD, 256], BF16) for i in range(2)])
    wor = Ring([P.sbuf(f"d_wo{i}", [128, 4, 512], BF16) for i in range(3)])
    sgr = Ring([P.sbuf(f"d_sg{i}", [128, 512], BF16) for i in range(2)])
    f1r = Ring([P.sbuf(f"d_f1{i}", [128, 512], F32) for i in range(2)])
    f2r = Ring([P.sbuf(f"d_f2{i}", [128, 512], F32) for i in range(2)])
    t1 = P.sbuf("d_t1", [128, D], F32)
    gcur = P.sbuf("d_gcur", [128, D], F32)
    statr = Ring([P.sbuf(f"d_st{i}", [128, 4], F32) for i in range(4)])
    ptr = Ring([P.psum(f"d_pt{i}", [128, 8, 128], BF16) for i in range(2)])
    mmr = Ring([P.psum(f"d_mm{i}", [128, 512], F32) for i in range(6)])
    tiles = (xt + bfr.tiles + [u2T, hT, t1, gcur] + wpr.tiles + wgr.tiles + wor.tiles + sgr.tiles + f1r.tiles + f2r.tiles
             + statr.tiles + ptr.tiles + mmr.tiles)
    scr = dict(pt=ptr)
    qi = [0]

    def q():
        qi[0] += 1
        return "sp" if qi[0] % 2 == 0 else "act"

    ROWS = S_LAT // 64

    def xdma(tile, ap, tok0, s, load):
        has_ctx = ap.shape[0] == S_CTX + S_LAT
        if tok_rows is None or tok0 < S_CTX:
            off = 0 if has_ctx else -S_CTX
            d_ap = ap[tok0 + off + s * 128: tok0 + off + (s + 1) * 128, :]
            s_ap = tile[:]
        else:
            lat = ap[S_CTX:S_CTX + S_LAT, :] if has_ctx else ap
            lr = lat_rows(lat, tok0 - S_CTX + s * 128, S_LAT)
            for wi in range(128 // ROWS):
                if load:
                    P.dma(q(), tile[wi * ROWS:(wi + 1) * ROWS, :], lr[wi], out_t=tile)
                else:
                    P.dma(q(), lr[wi], tile[wi * ROWS:(wi + 1) * ROWS, :], in_t=tile)
            return
        if load:
            P.dma(q(), s_ap, d_ap, out_t=tile)
        else:
            P.dma(q(), d_ap, s_ap, in_t=tile)

    def norm_stats(x_t):
        ssq = statr.next()
        junk = bfr.next()
        P.op("act", lambda e: e.activation(out=junk[:], in_=x_t[:], func=AF.Square, accum_out=ssq[:, 0:1]), reads=[x_t], writes=[junk, ssq])
        P.op("act", lambda e: e.activation(out=ssq[:, 1:2], in_=ssq[:, 0:1], func=AF.Sqrt, bias=K.eps_t[:, 0:1], scale=1.0 / D),
             reads=[ssq, K.eps_t], writes=[ssq])
        P.op("dve", lambda e: e.reciprocal(out=ssq[:, 2:3], in_=ssq[:, 1:2]), reads=[ssq], writes=[ssq])
        return ssq

    cur_row = None
    md = None
    mm_mark = None
    for (row, tok0, nsub) in token_tiles(S_LAT):
        if last and row == 1:
            continue
        if row != cur_row:
            if md is not None:
                P.end_phase(list(md.values()))
                P.emit()
                P.release(mm_mark)
            mm_mark = P.mark()
            md = {}
            if last:
                md["nf"] = P.sbuf("mod_nf", [128, D], F32)
                load_rep(P, "sp", md["nf"], io["norm_f"])
            md.update(load_mod_tiles(P, K, io, layer, mod_d, row, [("sh2", 3, "raw"), ("A2", 4, "A")], None, "norm2"))
            cur_row = row
        TT = nsub * 128
        load_rep(P, "sp", gcur, mod_d[row:row + 1, 2 * D:3 * D])
        for s in range(nsub):
            xdma(xt[s], xs_in, tok0, s, True)
            mb = bfr.next()
            P.dma(q(), mb[:], mix_tok[tok0 + s * 128: tok0 + (s + 1) * 128, :], out_t=mb)
            transpose_to(P, K, mb, hT, s * 128, scr)
        for nb in range(8):
            wa = wpr.next()
            P.dma(q(), wa[:], wproj[nb], out_t=wa)
            if mixer == "s5":
                wb = wpr.next()
                P.dma(q(), wb[:], wproj[8 + nb], out_t=wb)
            cs = slice(nb * 256, (nb + 1) * 256)
            for s in range(nsub):
                pa = mmr.next()
                for k in range(KD):
                    P.op("pe", lambda e, pa=pa, wa=wa, k=k, s=s: e.matmul(pa[:, 0:256], lhsT=hT[:, k, s * 128:(s + 1) * 128], rhs=wa[:, k, :],
                                                                         start=(k == 0), stop=(k == KD - 1)), reads=[hT, wa], writes=[pa])
                f1 = f1r.next()
                if mixer == "s5":
                    pb = mmr.next()
                    for k in range(KD):
                        P.op("pe", lambda e, pb=pb, wb=wb, k=k, s=s: e.matmul(pb[:, 0:256], lhsT=hT[:, k, s * 128:(s + 1) * 128], rhs=wb[:, k, :],
                                                                             start=(k == 0), stop=(k == KD - 1)), reads=[hT, wb], writes=[pb])
                    f2 = f2r.next()
                    P.op("act", lambda e, f2=f2, pb=pb: e.activation(out=f2[:, 0:256], in_=pb[:, 0:256], func=AF.Sigmoid), reads=[pb], writes=[f2])
                    P.op("dve", lambda e, f1=f1, pa=pa, f2=f2: e.tensor_tensor(out=f1[:, 0:256], in0=pa[:, 0:256], in1=f2[:, 0:256], op=ALU.mult),
                         reads=[pa, f2], writes=[f1])
                    P.op("pool", lambda e, f1=f1, cs=cs: e.tensor_tensor(out=f1[:, 0:256], in0=f1[:, 0:256], in1=gcur[:, cs], op=ALU.mult),
                         reads=[f1, gcur], writes=[f1])
                else:
                    P.op("dve", lambda e, f1=f1, pa=pa, cs=cs: e.tensor_tensor(out=f1[:, 0:256], in0=pa[:, 0:256], in1=gcur[:, cs], op=ALU.mult),
                         reads=[pa, gcur], writes=[f1])
                P.op("pool", lambda e, f1=f1, s=s, cs=cs: e.tensor_tensor(out=xt[s][:, cs], in0=xt[s][:, cs], in1=f1[:, 0:256], op=ALU.add),
                     reads=[f1, xt[s]], writes=[xt[s]])
        load_rep(P, "sp", gcur, mod_d[row:row + 1, 5 * D:6 * D])
        for s in range(nsub):
            ssq = norm_stats(xt[s])
            P.op("dve", lambda e, s=s, ssq=ssq: e.scalar_tensor_tensor(out=t1[:], in0=xt[s][:], scalar=ssq[:, 2:3], in1=md["A2"][:],
                                                                        op0=ALU.mult, op1=ALU.mult), reads=[xt[s], ssq, md["A2"]], writes=[t1])
            ub = bfr.next()
            P.op("pool", lambda e, ub=ub: e.tensor_tensor(out=ub[:], in0=t1[:], in1=md["sh2"][:], op=ALU.add), reads=[t1, md["sh2"]], writes=[ub])
            transpose_to(P, K, ub, u2T, s * 128, scr)
        for c in range(NHC):
            wgu = wgr.next()
            P.dma(q(), wgu[:], wgu_t[c], out_t=wgu)
            pg = mmr.next(); pu = mmr.next()
            for k in range(KD):
                P.op("pe", lambda e, pg=pg, wgu=wgu, k=k: e.matmul(pg[:, :TT], lhsT=wgu[:, k, 0:128], rhs=u2T[:, k, :TT],
                                                                  start=(k == 0), stop=(k == KD - 1)), reads=[wgu, u2T], writes=[pg])
            for k in range(KD):
                P.op("pe", lambda e, pu=pu, wgu=wgu, k=k: e.matmul(pu[:, :TT], lhsT=wgu[:, k, 128:256], rhs=u2T[:, k, :TT],
                                                                  start=(k == 0), stop=(k == KD - 1)), reads=[wgu, u2T], writes=[pu])
            sg = sgr.next()
            P.op("act", lambda e, sg=sg, pg=pg: e.activation(out=sg[:, :TT], in_=pg[:, :TT], func=AF.Silu), reads=[pg], writes=[sg])
            P.op("dve", lambda e, sg=sg, pu=pu, c=c: e.tensor_tensor(out=hT[:, c, :TT], in0=pu[:, :TT], in1=sg[:, :TT], op=ALU.mult),
                 reads=[pu, sg], writes=[hT])
        for nt in range(4):
            cs = slice(nt * 512, (nt + 1) * 512)
            pf = [mmr.next() for _ in range(nsub)]
            for w in range(11):
                wo = wor.next()
                P.dma(q(), wo[:], wo_t[nt * 11 + w], out_t=wo)
                for s in range(nsub):
                    for cc in range(4):
                        c = w * 4 + cc
                        P.op("pe", lambda e, p_=pf[s], wo=wo, cc=cc, c=c, s=s: e.matmul(p_[:], lhsT=hT[:, c, s * 128:(s + 1) * 128], rhs=wo[:, cc, :],
                                                                                      start=(c == 0), stop=(c == NHC - 1)), reads=[hT, wo], writes=[pf[s]])
            for s in range(nsub):
                f1 = f1r.next()
                P.op("dve", lambda e, f1=f1, p_=pf[s], cs=cs: e.tensor_tensor(out=f1[:], in0=p_[:], in1=gcur[:, cs], op=ALU.mult), reads=[pf[s], gcur], writes=[f1])
                P.op("pool", lambda e, f1=f1, s=s, cs=cs: e.tensor_tensor(out=xt[s][:, cs], in0=xt[s][:, cs], in1=f1[:], op=ALU.add), reads=[f1, xt[s]], writes=[xt[s]])
        for s in range(nsub):
            if not last:
                xdma(xt[s], xs_out, tok0, s, False)
            else:
                ssq = norm_stats(xt[s])
                P.op("dve", lambda e, s=s, ssq=ssq: e.scalar_tensor_tensor(out=t1[:], in0=xt[s][:], scalar=ssq[:, 2:3], in1=md["nf"][:],
                                                                            op0=ALU.mult, op1=ALU.mult), reads=[xt[s], ssq, md["nf"]], writes=[t1])
                xdma(t1, out_final, tok0, s, False)
    if md is not None:
        P.end_phase(list(md.values()))
        P.emit()
        P.release(mm_mark)
    P.end_phase(tiles)
    P.emit()
    P.release(m)


def bc(ap, axis, n):
    a = ap.unsqueeze(axis)
    shp = list(a.shape)
    shp[axis] = n
    return a.broadcast_to(shp)


def phase_c_s5(P, K, io, jl, u_tok, gy_tok, S_LAT, dve2="pool"):
    NTOK3 = 2 * S_CTX + S_LAT
    NCH = NTOK3 // T0
    NF = (S_CTX + S_LAT) // T0
    CTXC = S_CTX // T0
    NBK = 32
    BL = NF // NBK
    assert NF == NBK * BL
    NBLK = (NCH + 127) // 128
    NFB = (NF + 127) // 128
    PI = float(np.pi)
    m = P.mark()
    V = lambda e: e

    def ew(eng, fn, reads, writes):
        P.op(eng, fn, reads=reads, writes=writes)

    Pw = P.sbuf("c_Pw", [128, 2, 2, 64, T0 + 1], F32)
    PWB = P.sbuf("c_PWB", [128, 2, 2, 64, BL + 1], F32)
    Bn = P.sbuf("c_Bn", [128, 2, 2, 64, 16], F32)
    Bb = P.sbuf("c_Bb", [128, 2, 2, 64, 16], F32)
    Dp = P.sbuf("c_Dp", [128, 128], F32)
    cz = [P.sbuf(f"c_cz{i}", [128, 16, 128], F32) for i in range(2)]
    m_tmp = P.mark()
    lam = P.sbuf("c_lam", [128, 2, 2, 64], F32)
    dtt = P.sbuf("c_dt", [128, 2, 64], F32)
    for d in range(2):
        for jj in range(2):
            ps_ = slice(jj * 64, (jj + 1) * 64)
            P.dma("sp", lam[ps_, 0, d, :], io["s5_lam_re"][jl, d].rearrange("(i j) p -> j p i", j=2)[jj], out_t=lam, allow_slow_non_contiguous=True)
            P.dma("act", lam[ps_, 1, d, :], io["s5_lam_im"][jl, d].rearrange("(i j) p -> j p i", j=2)[jj], out_t=lam, allow_slow_non_contiguous=True)
            P.dma("sp", dtt[ps_, d, :], io["s5_log_dt"][jl, d:d + 1, :].rearrange("o (i j) -> o j i", j=2)[:, jj, :].partition_broadcast(64),
                  out_t=dtt, allow_slow_non_contiguous=True)
    w = [P.sbuf(f"c_w{i}", [128, 2, 64], F32) for i in range(10)]
    A1 = P.sbuf("c_A1", [128, 2, 2, 64], F32)
    Fc = P.sbuf("c_F", [128, 2, 2, 64], F32)
    A8 = P.sbuf("c_A8", [128, 2, 2, 64], F32)
    ptc = Ring([P.psum(f"c_ptc{i}", [128, 4, 128], F32) for i in range(2)])
    lre, lim = lam[:, 0], lam[:, 1]
    ew("act", lambda e: e.activation(out=dtt[:], in_=dtt[:], func=AF.Exp), [dtt], [dtt])
    ew("dve", lambda e: e.tensor_tensor(out=w[0][:], in0=lre, in1=dtt[:], op=ALU.mult), [lam, dtt], [w[0]])
    ew("act", lambda e: e.activation(out=w[0][:], in_=w[0][:], func=AF.Exp), [w[0]], [w[0]])
    ew("dve", lambda e: e.tensor_tensor(out=w[1][:], in0=lim, in1=dtt[:], op=ALU.mult), [lam, dtt], [w[1]])
    for (dst, shift) in ((w[2], 0.0), (w[3], PI / 2)):
        ew("dve", lambda e, dst=dst, shift=shift: e.tensor_scalar(out=dst[:], in0=w[1][:], scalar1=float(shift), scalar2=None, op0=ALU.add), [w[1]], [dst])
        ew("dve", lambda e, dst=dst: e.tensor_copy(out=w[4][:], in_=dst[:]), [dst], [w[4]])
        for k in range(1, 7):
            ew("act", lambda e, k=k: e.activation(out=w[5][:], in_=w[4][:], func=AF.Sign, bias=K.cbias[:, k:k + 1], scale=1.0), [w[4], K.cbias], [w[5]])
            ew("dve", lambda e, dst=dst: e.scalar_tensor_tensor(out=dst[:], in0=w[5][:], scalar=-PI, in1=dst[:], op0=ALU.mult, op1=ALU.add), [w[5], dst], [dst])
        ew("dve", lambda e, dst=dst: e.tensor_scalar(out=dst[:], in0=dst[:], scalar1=-6.0 * PI, scalar2=None, op0=ALU.add), [dst], [dst])
        ew("act", lambda e, dst=dst: e.activation(out=dst[:], in_=dst[:], func=AF.Sin), [dst], [dst])
    ew("dve", lambda e: e.tensor_tensor(out=A1[:, 0], in0=w[0][:], in1=w[3][:], op=ALU.mult), [w[0], w[3]], [A1])
    ew("dve", lambda e: e.tensor_tensor(out=A1[:, 1], in0=w[0][:], in1=w[2][:], op=ALU.mult), [w[0], w[2]], [A1])
    ew("dve", lambda e: e.tensor_tensor(out=w[4][:], in0=lre, in1=lre, op=ALU.mult), [lam], [w[4]])
    ew("dve", lambda e: e.tensor_tensor(out=w[5][:], in0=lim, in1=lim, op=ALU.mult), [lam], [w[5]])
    ew("dve", lambda e: e.tensor_tensor(out=w[4][:], in0=w[4][:], in1=w[5][:], op=ALU.add), [w[4], w[5]], [w[4]])
    ew("dve", lambda e: e.reciprocal(out=w[4][:], in_=w[4][:]), [w[4]], [w[4]])
    ew("dve", lambda e: e.tensor_scalar(out=w[5][:], in0=A1[:, 0], scalar1=-1.0, scalar2=None, op0=ALU.add), [A1], [w[5]])
    ew("dve", lambda e: e.tensor_tensor(out=w[6][:], in0=w[5][:], in1=lre, op=ALU.mult), [w[5], lam], [w[6]])
    ew("dve", lambda e: e.tensor_tensor(out=w[7][:], in0=A1[:, 1], in1=lim, op=ALU.mult), [A1, lam], [w[7]])
    ew("dve", lambda e: e.tensor_tensor(out=w[6][:], in0=w[6][:], in1=w[7][:], op=ALU.add), [w[6], w[7]], [w[6]])
    ew("dve", lambda e: e.tensor_tensor(out=Fc[:, 0], in0=w[6][:], in1=w[4][:], op=ALU.mult), [w[6], w[4]], [Fc])
    ew("dve", lambda e: e.tensor_tensor(out=w[6][:], in0=A1[:, 1], in1=lre, op=ALU.mult), [A1, lam], [w[6]])
    ew("dve", lambda e: e.tensor_tensor(out=w[7][:], in0=w[5][:], in1=lim, op=ALU.mult), [w[5], lam], [w[7]])
    ew("dve", lambda e: e.tensor_tensor(out=w[6][:], in0=w[6][:], in1=w[7][:], op=ALU.subtract), [w[6], w[7]], [w[6]])
    ew("dve", lambda e: e.tensor_tensor(out=Fc[:, 1], in0=w[6][:], in1=w[4][:], op=ALU.mult), [w[6], w[4]], [Fc])

    def cmul_pow(dst, n, base_re, base_im, tag):
        ew("dve", lambda e: e.memset(dst[:, 0, :, :, 0:1], 1.0), [], [dst])
        ew("dve", lambda e: e.memset(dst[:, 1, :, :, 0:1], 0.0), [], [dst])
        for k in range(1, n):
            pr, pi_ = dst[:, 0, :, :, k - 1], dst[:, 1, :, :, k - 1]
            ew("dve", lambda e, pr=pr: e.tensor_tensor(out=w[6][:], in0=pr, in1=base_re, op=ALU.mult), [dst, A1], [w[6]])
            ew("dve", lambda e, pi_=pi_: e.tensor_tensor(out=w[7][:], in0=pi_, in1=base_im, op=ALU.mult), [dst, A1], [w[7]])
            ew("dve", lambda e, k=k: e.tensor_tensor(out=dst[:, 0, :, :, k], in0=w[6][:], in1=w[7][:], op=ALU.subtract), [w[6], w[7]], [dst])
            ew("dve", lambda e, pr=pr: e.tensor_tensor(out=w[6][:], in0=pr, in1=base_im, op=ALU.mult), [dst, A1], [w[6]])
            ew("dve", lambda e, pi_=pi_: e.tensor_tensor(out=w[7][:], in0=pi_, in1=base_re, op=ALU.mult), [dst, A1], [w[7]])
            ew("dve", lambda e, k=k: e.tensor_tensor(out=dst[:, 1, :, :, k], in0=w[6][:], in1=w[7][:], op=ALU.add), [w[6], w[7]], [dst])

    cmul_pow(Pw, T0 + 1, A1[:, 0], A1[:, 1], "pw")
    ew("dve", lambda e: e.tensor_copy(out=A8[:], in_=Pw[:, :, :, :, T0]), [Pw], [A8])
    cmul_pow(PWB, BL + 1, A8[:, 0], A8[:, 1], "pwb")

    for d in range(2):
        for jj in range(2):
            ps_ = slice(jj * 64, (jj + 1) * 64)
            P.dma("sp", Bn[ps_, 0, d], io["s5_b_re"][jl, d].rearrange("(i j) p c -> j p i c", j=2)[jj], out_t=Bn)
            P.dma("act", Bn[ps_, 1, d], io["s5_b_im"][jl, d].rearrange("(i j) p c -> j p i c", j=2)[jj], out_t=Bn)
    tbT = cz[0]
    tbv = cz[0][:].rearrange("p (d a) (b c) -> p d (a b) c", d=2, c=16)
    fre = bc(Fc[:, 0], 3, 16)
    fim = bc(Fc[:, 1], 3, 16)
    ew("dve", lambda e: e.tensor_tensor(out=Bb[:, 0], in0=Bn[:, 0], in1=fre, op=ALU.mult), [Bn, Fc], [Bb])
    ew("dve", lambda e: e.tensor_tensor(out=tbv, in0=Bn[:, 1], in1=fim, op=ALU.mult), [Bn, Fc], [tbT])
    ew("dve", lambda e: e.tensor_tensor(out=Bb[:, 0], in0=Bb[:, 0], in1=tbv, op=ALU.subtract), [Bb, tbT], [Bb])
    ew("dve", lambda e: e.tensor_tensor(out=Bb[:, 1], in0=Bn[:, 1], in1=fre, op=ALU.mult), [Bn, Fc], [Bb])
    ew("dve", lambda e: e.tensor_tensor(out=tbv, in0=Bn[:, 0], in1=fim, op=ALU.mult), [Bn, Fc], [tbT])
    ew("dve", lambda e: e.tensor_tensor(out=Bb[:, 1], in0=Bb[:, 1], in1=tbv, op=ALU.add), [Bb, tbT], [Bb])

    Cn = Bn
    for t in cz:
        ew("pool", lambda e, t=t: e.memset(t[:], 0.0), [], [t])
    ci = 0
    for d in range(2):
        for part, key in ((0, "s5_c_re"), (1, "s5_c_im")):
            t = cz[ci % 2]; ci += 1
            src = io[key][jl, d].rearrange("(kc q j) c p -> q j c kc p", q=4, j=2)
            for q_ in range(4):
                for j_ in range(2):
                    r0 = 32 * q_ + 16 * j_
                    P.dma("sp" if (q_ + j_) % 2 == 0 else "act", t[r0:r0 + 16, :, 64 * j_:64 * j_ + 64], src[q_, j_], out_t=t)
            for k4 in range(4):
                pt = ptc.next()
                for kk in range(4):
                    kc = k4 * 4 + kk
                    ew("pe", lambda e, pt=pt, kk=kk, kc=kc, t=t: e.transpose(out=pt[:, kk, :], in_=t[:, kc, :], identity=K.ident_f[:]), [t, K.ident_f], [pt])
                for jj in range(2):
                    ps_ = slice(jj * 64, (jj + 1) * 64)
                    src_ap = pt[ps_].rearrange("p k (q j c) -> p k q j c", q=4, j=2)[:, :, :, jj, :]
                    dst_ap = Cn[ps_, part, d, 16 * k4:16 * k4 + 16, :].rearrange("p (k q) c -> p k q c", q=4)
                    if part == 0:
                        ew("dve", lambda e, dst_ap=dst_ap, src_ap=src_ap: e.tensor_copy(out=dst_ap, in_=src_ap), [pt], [Cn])
                    else:
                        ew("dve", lambda e, dst_ap=dst_ap, src_ap=src_ap: e.tensor_scalar(out=dst_ap, in0=src_ap, scalar1=-1.0, scalar2=None, op0=ALU.mult), [pt], [Cn])
    for t_ in range(T0):
        P.dma("sp", Dp[16 * t_:16 * t_ + 16, :], io["s5_d"][jl:jl + 1, :].rearrange("o (g c) -> (o c) g", c=16), out_t=Dp, allow_slow_non_contiguous=True)

    P.end_phase([lam, dtt, A1, Fc, A8] + w + ptc.tiles)
    P.emit()
    P.release(m_tmp)

    VF, VB = cz[0], cz[1]
    VFv = cz[0][:].rearrange("p (a b x) (y d) -> p a b (x y) d", a=2, b=4, d=16)
    VBv = cz[1][:].rearrange("p (a b x) (y d) -> p a b (x y) d", a=2, b=4, d=16)
    ew("pool", lambda e: e.memset(cz[0][:], 0.0), [], [VF])
    ew("pool", lambda e: e.memset(cz[1][:], 0.0), [], [VB])
    vt = P.sbuf("c_vt", [128, 4, 8, 16], F32)
    vu = P.sbuf("c_vu", [128, 4, 8, 16], F32)
    Ro = P.sbuf("c_Ro", [128, 2, 2, 4, 8, 16], BF16)
    TS = P.sbuf("c_TS", [128, 2, 2, 4, 128], BF16)
    IT = P.sbuf("c_IT", [128, 2, 8, 128], BF16)
    U = P.sbuf("c_U", [128, 8, NBLK * 128], BF16)
    Zr = Ring([P.sbuf(f"c_Z{i}", [128, 8, 128], BF16) for i in range(2)])
    Zpr = Ring([P.sbuf(f"c_Zp{i}", [128, 8, 8, 16], BF16) for i in range(2)])
    Hs = P.sbuf("c_Hs", [128, 2, 8, NBK, BL], F32)
    Hr = P.sbuf("c_Hr", [128, 2, 2, 4, NCH], BF16)
    ew("pool", lambda e: e.memset(Hr[:], 0.0), [], [Hr])
    A8l = P.sbuf("c_A8l", [128, 2, 8], F32)
    ABl = P.sbuf("c_ABl", [128, 2, 8], F32)
    PWl = P.sbuf("c_PWl", [128, 2, 8, BL], F32)
    s1 = [P.sbuf(f"c_s1{i}", [128, 2, 8, NBK], F32) for i in range(2)]
    s3 = P.sbuf("c_s3", [128, 2, NBK - 1, max(BL - 1, 1)], F32)
    A8s = P.sbuf("c_A8s", [128, 2, 8], F32)
    Pcs = P.sbuf("c_Pcs", [128, 2, 8], F32)
    Pcur = P.sbuf("c_Pcur", [128, 2, 8], F32)
    sq = P.sbuf("c_sq", [128, 2, 8], F32)
    PWls = P.sbuf("c_PWls", [128, 2, 8, BL], F32)
    Ysb = Ring([P.sbuf(f"c_Y{i}", [128, NFB * 128], F32) for i in range(2)])
    Zo = P.sbuf("c_Zo", [128, NFB, 8, 128], BF16)
    gl_ = [P.sbuf(f"c_g{i}", [128, NFB * 128], F32) for i in range(2)]
    Ybr = Ring([P.sbuf(f"c_Yb{i}", [128, NFB * 128], BF16) for i in range(2)])
    ptb = Ring([P.psum(f"c_ptb{i}", [128, 8, 128], BF16) for i in range(2)])
    psS = Ring([P.psum(f"c_psS{i}", [128, 1024], F32) for i in range(1)])
    psY = Ring([P.psum(f"c_psY{i}", [128, 1024], F32) for i in range(1)])
    psT = Ring([P.psum(f"c_psT{i}", [128, 4, 128], F32) for i in range(1)])
    alt = ["dve", dve2]

    for b in range(16):
        i0 = 4 * b
        f0 = 128 * b
        for d in range(2):
            for part in range(2):
                pre = bc(Pw[:, 0, d, i0:i0 + 4, 0:T0], 3, 16)
                pim = bc(Pw[:, 1, d, i0:i0 + 4, 0:T0], 3, 16)
                bre = bc(Bb[:, 0, d, i0:i0 + 4, :], 2, T0)
                bim = bc(Bb[:, 1, d, i0:i0 + 4, :], 2, T0)
                dstv = VFv[:, part, :, 7::-1, :] if d == 0 else VBv[:, part, :, 8:16, :]
                dt_ = VF if d == 0 else VB
                if part == 0:
                    ew("dve", lambda e, pre=pre, bre=bre: e.tensor_tensor(out=vt[:], in0=pre, in1=bre, op=ALU.mult), [Pw, Bb], [vt])
                    ew("dve", lambda e, pim=pim, bim=bim, dstv=dstv: e.tensor_tensor(out=dstv, in0=pim, in1=bim, op=ALU.mult), [Pw, Bb], [dt_])
                    ew("dve", lambda e, dstv=dstv: e.tensor_tensor(out=dstv, in0=vt[:], in1=dstv, op=ALU.subtract), [vt, dt_], [dt_])
                else:
                    ew("dve", lambda e, pre=pre, bim=bim: e.tensor_tensor(out=vt[:], in0=pre, in1=bim, op=ALU.mult), [Pw, Bb], [vt])
                    ew("dve", lambda e, pim=pim, bre=bre, dstv=dstv: e.tensor_tensor(out=dstv, in0=pim, in1=bre, op=ALU.mult), [Pw, Bb], [dt_])
                    ew("dve", lambda e, dstv=dstv: e.tensor_tensor(out=dstv, in0=vt[:], in1=dstv, op=ALU.add), [vt, dt_], [dt_])
            ks = slice(1, T0 + 1) if d == 0 else slice(T0, 0, -1)
            pre = bc(Pw[:, 0, d, i0:i0 + 4, ks], 3, 16)
            pim = bc(Pw[:, 1, d, i0:i0 + 4, ks], 3, 16)
            cre = bc(Cn[:, 0, d, i0:i0 + 4, :], 2, T0)
            cimn = bc(Cn[:, 1, d, i0:i0 + 4, :], 2, T0)
            ew("dve", lambda e, pre=pre, cre=cre: e.tensor_tensor(out=vt[:], in0=pre, in1=cre, op=ALU.mult), [Pw, Cn], [vt])
            ew(dve2, lambda e, pim=pim, cimn=cimn: e.tensor_tensor(out=vu[:], in0=pim, in1=cimn, op=ALU.mult), [Pw, Cn], [vu])
            ew("dve", lambda e, d=d: e.tensor_tensor(out=Ro[:, d, 0], in0=vt[:], in1=vu[:], op=ALU.add), [vt, vu], [Ro])
            ew("dve", lambda e, pim=pim, cre=cre: e.tensor_tensor(out=vt[:], in0=pim, in1=cre, op=ALU.mult), [Pw, Cn], [vt])
            ew(dve2, lambda e, pre=pre, cimn=cimn: e.tensor_tensor(out=vu[:], in0=pre, in1=cimn, op=ALU.mult), [Pw, Cn], [vu])
            ew("dve", lambda e, d=d: e.tensor_tensor(out=Ro[:, d, 1], in0=vu[:], in1=vt[:], op=ALU.subtract), [vt, vu], [Ro])
        for d in range(2):
            for part in range(2):
                pt = psT.next()
                for il in range(4):
                    src = (VFv[:, part, il, 0:8, :] if d == 0 else VBv[:, part, il, 8:16, :]).rearrange("p b c -> p (b c)")
                    ew("pe", lambda e, pt=pt, il=il, src=src: e.transpose(out=pt[:, il, :], in_=src, identity=K.ident_f[:]), [VF, VB, K.ident_f], [pt])
                ew("act", lambda e, pt=pt, d=d, part=part: e.activation(out=TS[:, d, part], in_=pt[:], func=AF.Copy), [pt], [TS])
        for d in range(2):
            for g4 in range(2):
                pt = psT.next()
                for gg in range(4):
                    g_ = g4 * 4 + gg
                    il, jj = g_ // 2, g_ % 2
                    ps_ = slice(jj * 64, (jj + 1) * 64)
                    for t_ in range(T0):
                        for part in range(2):
                            VX = VFv if d == 0 else VBv
                            w0 = (7 - t_) if d == 0 else (8 - t_)
                            lhsT = VX[ps_, part, il, w0:w0 + 8, :].rearrange("p b c -> p (b c)")
                            rhs = Cn[ps_, part, d, i0 + il, :]
                            ew("pe", lambda e, pt=pt, gg=gg, t_=t_, part=part, lhsT=lhsT, rhs=rhs: e.matmul(
                                pt[:, gg, 16 * t_:16 * t_ + 16], lhsT=lhsT, rhs=rhs, start=(part == 0), stop=(part == 1), skip_group_check=True),
                               [VF, VB, Cn], [pt])
                ew("act", lambda e, pt=pt, d=d, g4=g4: e.activation(out=IT[:, d, g4 * 4:(g4 + 1) * 4], in_=pt[:], func=AF.Copy), [pt], [IT])
        for part in range(2):
            for d in range(2):
                ew("dve", lambda e, part=part, d=d: e.tensor_copy(out=A8l[:, part, d * 4:(d + 1) * 4], in_=PWB[:, part, d, i0:i0 + 4, 1]), [PWB], [A8l])
                ew("dve", lambda e, part=part, d=d: e.tensor_copy(out=ABl[:, part, d * 4:(d + 1) * 4], in_=PWB[:, part, d, i0:i0 + 4, BL]), [PWB], [ABl])
                ew("dve", lambda e, part=part, d=d: e.tensor_copy(out=PWl[:, part, d * 4:(d + 1) * 4, :], in_=PWB[:, part, d, i0:i0 + 4, 1:BL + 1]), [PWB], [PWl])
        for blk in range(NBLK):
            nn = min(128, NCH - blk * 128)
            Z = Zr.next()
            P.dma("sp" if blk % 2 == 0 else "act", Z[0:nn],
                  u_tok[blk * 1024: blk * 1024 + nn * 8, f0:f0 + 128].rearrange("(n s) f -> n s f", s=8), out_t=Z)
            pt = ptb.next()
            Zp = Zpr.next()
            ew("pool", lambda e, Z=Z, Zp=Zp, nn=nn: e.tensor_copy(out=Zp[0:nn], in_=Z[0:nn].rearrange("n s (g c) -> n g s c", c=16)), [Z], [Zp])
            for g_ in range(8):
                ew("pe", lambda e, pt=pt, g_=g_, Zp=Zp, nn=nn: e.transpose(out=pt[:, g_, 0:nn], in_=Zp[0:nn, g_].rearrange("n s c -> n (s c)"), identity=K.ident_b[0:nn, 0:nn]),
                   [Zp, K.ident_b], [pt])
            ew("act" if blk % 2 == 0 else "dve",
               (lambda e, pt=pt, blk=blk, nn=nn: e.activation(out=U[:, :, blk * 128: blk * 128 + nn], in_=pt[:, :, 0:nn], func=AF.Copy)) if blk % 2 == 0 else
               (lambda e, pt=pt, blk=blk, nn=nn: e.tensor_copy(out=U[:, :, blk * 128: blk * 128 + nn], in_=pt[:, :, 0:nn])), [pt], [U])
        for il in range(4):
            for d in range(2):
                lane = d * 4 + il
                lo, hi = (0, NF) if d == 0 else (CTXC, NCH)
                for part in range(2):
                    ps = psS.next()
                    for (a, b_) in col_blocks(0, NF):
                        for jj in range(2):
                            ps_ = slice(jj * 64, (jj + 1) * 64)
                            ew("pe", lambda e, ps=ps, ps_=ps_, a=a, b_=b_, d=d, part=part, il=il, jj=jj, lo=lo: e.matmul(
                                ps[ps_, a:b_], lhsT=TS[:, d, part, il, ps_], rhs=U[:, 2 * il + jj, lo + a: lo + b_], start=True, stop=True, skip_group_check=True),
                               [TS, U], [ps])
                    dst = Hs[:, part, lane].rearrange("p b l -> p (b l)")
                    if d == 0:
                        ew("act", lambda e, dst=dst, ps=ps: e.activation(out=dst, in_=ps[:, 0:NF], func=AF.Copy), [ps], [Hs])
                    else:
                        ew("dve", lambda e, dst=dst, ps=ps: e.tensor_copy(out=dst[:, ::-1], in_=ps[:, 0:NF]), [ps], [Hs])
        for (src_, dst_) in ((A8l, A8s), (ABl, Pcs)):
            ew("dve", lambda e, src_=src_, dst_=dst_: e.tensor_copy(out=dst_[:, 1], in_=src_[:, 1]), [src_], [dst_])
            ew("dve", lambda e, src_=src_, dst_=dst_: e.tensor_scalar(out=dst_[:, 0], in0=src_[:, 1], scalar1=-1.0, scalar2=None, op0=ALU.mult), [src_], [dst_])
        ew("dve", lambda e: e.tensor_copy(out=PWls[:, 1], in_=PWl[:, 1]), [PWl], [PWls])
        ew("dve", lambda e: e.tensor_scalar(out=PWls[:, 0], in0=PWl[:, 1], scalar1=-1.0, scalar2=None, op0=ALU.mult), [PWl], [PWls])
        ew("dve", lambda e: e.tensor_copy(out=Pcur[:], in_=ABl[:]), [ABl], [Pcur])
        are_b = bc(bc(A8l[:, 0, :], 1, 2), 3, NBK)
        aims_b = bc(A8s[:], 3, NBK)
        for l in range(1, BL):
            Y = Hs[:, :, :, :, l - 1]
            Ysw = Hs[:, ::-1, :, :, l - 1]
            X = Hs[:, :, :, :, l]
            ew("dve", lambda e, Y=Y: e.tensor_tensor(out=s1[0][:], in0=Y, in1=are_b, op=ALU.mult), [Hs, A8l], [s1[0]])
            ew("dve", lambda e, Ysw=Ysw: e.tensor_tensor(out=s1[1][:], in0=Ysw, in1=aims_b, op=ALU.mult), [Hs, A8s], [s1[1]])
            ew("dve", lambda e, X=X: e.tensor_tensor(out=X, in0=X, in1=s1[0][:], op=ALU.add), [Hs, s1[0]], [Hs])
            ew("dve", lambda e, X=X: e.tensor_tensor(out=X, in0=X, in1=s1[1][:], op=ALU.add), [Hs, s1[1]], [Hs])
        sft = 1
        while sft < NBK:
            n_ = NBK - sft
            Y = Hs[:, :, :, 0:n_, BL - 1]
            Ysw = Hs[:, ::-1, :, 0:n_, BL - 1]
            X = Hs[:, :, :, sft:NBK, BL - 1]
            pre_b = bc(bc(Pcur[:, 0, :], 1, 2), 3, n_)
            pims_b = bc(Pcs[:], 3, n_)
            ew("dve", lambda e, Y=Y, pre_b=pre_b, n_=n_: e.tensor_tensor(out=s1[0][:, :, :, 0:n_], in0=Y, in1=pre_b, op=ALU.mult), [Hs, Pcur], [s1[0]])
            ew("dve", lambda e, Ysw=Ysw, pims_b=pims_b, n_=n_: e.tensor_tensor(out=s1[1][:, :, :, 0:n_], in0=Ysw, in1=pims_b, op=ALU.mult), [Hs, Pcs], [s1[1]])
            ew("dve", lambda e, X=X, n_=n_: e.tensor_tensor(out=X, in0=X, in1=s1[0][:, :, :, 0:n_], op=ALU.add), [Hs, s1[0]], [Hs])
            ew("dve", lambda e, X=X, n_=n_: e.tensor_tensor(out=X, in0=X, in1=s1[1][:, :, :, 0:n_], op=ALU.add), [Hs, s1[1]], [Hs])
            sft *= 2
            if sft < NBK:
                ew("dve", lambda e: e.tensor_tensor(out=sq[:, 0], in0=Pcur[:, 0], in1=Pcur[:, 0], op=ALU.mult), [Pcur], [sq])
                ew("dve", lambda e: e.tensor_tensor(out=sq[:, 1], in0=Pcur[:, 1], in1=Pcur[:, 1], op=ALU.mult), [Pcur], [sq])
                ew("dve", lambda e: e.scalar_tensor_tensor(out=Pcur[:, 1], in0=Pcur[:, 0], scalar=2.0, in1=Pcur[:, 1], op0=ALU.mult, op1=ALU.mult), [Pcur], [Pcur])
                ew("dve", lambda e: e.tensor_tensor(out=Pcur[:, 0], in0=sq[:, 0], in1=sq[:, 1], op=ALU.subtract), [sq], [Pcur])
                ew("dve", lambda e: e.tensor_copy(out=Pcs[:, 1], in_=Pcur[:, 1]), [Pcur], [Pcs])
                ew("dve", lambda e: e.tensor_scalar(out=Pcs[:, 0], in0=Pcur[:, 1], scalar1=-1.0, scalar2=None, op0=ALU.mult), [Pcur], [Pcs])
        if BL > 1:
            for ls in range(8):
                X = Hs[:, :, ls, 1:NBK, 0:BL - 1]
                Y = bc(Hs[:, :, ls, 0:NBK - 1, BL - 1], 3, BL - 1)
                Ysw = bc(Hs[:, ::-1, ls, 0:NBK - 1, BL - 1], 3, BL - 1)
                pre3 = bc(bc(PWl[:, 0, ls, 0:BL - 1], 1, 2), 2, NBK - 1)
                pims3 = bc(PWls[:, :, ls, 0:BL - 1], 2, NBK - 1)
                ew("dve", lambda e, Y=Y, pre3=pre3: e.tensor_tensor(out=s3[:], in0=Y, in1=pre3, op=ALU.mult), [Hs, PWl], [s3])
                ew("dve", lambda e, X=X: e.tensor_tensor(out=X, in0=X, in1=s3[:], op=ALU.add), [Hs, s3], [Hs])
                ew("dve", lambda e, Ysw=Ysw, pims3=pims3: e.tensor_tensor(out=s3[:], in0=Ysw, in1=pims3, op=ALU.mult), [Hs, PWls], [s3])
                ew("dve", lambda e, X=X: e.tensor_tensor(out=X, in0=X, in1=s3[:], op=ALU.add), [Hs, s3], [Hs])
        for part in range(2):
            hf = Hs[:, part, 0:4].rearrange("p a b l -> p a (b l)")
            hb = Hs[:, part, 4:8].rearrange("p a b l -> p a (b l)")
            ew("act", lambda e, part=part, hf=hf: e.activation(out=Hr[:, 0, part, :, 1:NF], in_=hf[:, :, 0:NF - 1], func=AF.Copy), [Hs], [Hr])
            ew(dve2, lambda e, part=part, hb=hb: e.tensor_copy(out=Hr[:, 1, part, :, CTXC:NCH - 1], in_=hb[:, :, NF - 2::-1]), [Hs], [Hr])
        for g_ in range(8):
            il, jj = g_ // 2, g_ % 2
            ps_ = slice(jj * 64, (jj + 1) * 64)
            py = psY.next()
            first = {}
            mm = []
            for d in range(2):
                lo, hi = (0, NF) if d == 0 else (CTXC, NCH)
                for (a, b_) in col_blocks(lo, hi):
                    mm.append((a, b_, IT[:, d, g_, :], U[:, g_, a:b_]))
                    mm.append((a, b_, Ro[ps_, d, 0, il].rearrange("p t c -> p (t c)"), Hr[ps_, d, 0, il, a:b_]))
                    mm.append((a, b_, Ro[ps_, d, 1, il].rearrange("p t c -> p (t c)"), Hr[ps_, d, 1, il, a:b_]))
            nlast = {}
            for idx, (a, b_, _, _) in enumerate(mm):
                nlast[a // 512] = idx
            for idx, (a, b_, lhsT, rhs) in enumerate(mm):
                bank = a // 512
                st = bank not in first
                first[bank] = True
                ew("pe", lambda e, py=py, a=a, b_=b_, lhsT=lhsT, rhs=rhs, st=st, sp=(nlast[bank] == idx): e.matmul(
                    py[:, a:b_], lhsT=lhsT, rhs=rhs, start=st, stop=sp, skip_group_check=True), [IT, U, Ro, Hr], [py])
            Y = Ysb.next()
            ew("dve", lambda e, Y=Y, py=py, g_=g_: e.scalar_tensor_tensor(out=Y[:, 0:NF], in0=U[:, g_, 0:NF], scalar=Dp[:, 8 * b + g_: 8 * b + g_ + 1],
                                                                          in1=py[:, 0:NF], op0=ALU.mult, op1=ALU.add), [U, Dp, py], [Y])
            ew("dve", lambda e, Y=Y, py=py: e.tensor_tensor(out=Y[:, 0:CTXC], in0=Y[:, 0:CTXC], in1=py[:, NF:NCH], op=ALU.add), [Y, py], [Y])
            ga, gb = gl_[0], gl_[1]
            ew(dve2, lambda e, Y=Y: e.tensor_tensor(out=ga[:, 0:NF], in0=Y[:, 0:NF], in1=Y[:, 0:NF], op=ALU.mult), [Y], [ga])
            ew("dve", lambda e: e.tensor_scalar(out=ga[:, 0:NF], in0=ga[:, 0:NF], scalar1=0.044715, scalar2=1.0, op0=ALU.mult, op1=ALU.add), [ga], [ga])
            ew(dve2, lambda e, Y=Y: e.tensor_tensor(out=ga[:, 0:NF], in0=ga[:, 0:NF], in1=Y[:, 0:NF], op=ALU.mult), [ga, Y], [ga])
            ew("act", lambda e: e.activation(out=gb[:, 0:NF], in_=ga[:, 0:NF], func=AF.Sigmoid, scale=1.5957691216057308), [ga], [gb])
            Yb = Ybr.next()
            ew("dve", lambda e, Y=Y, Yb=Yb: e.tensor_tensor(out=Yb[:, 0:NF], in0=gb[:, 0:NF], in1=Y[:, 0:NF], op=ALU.mult), [gb, Y], [Yb])
            pt = ptb.next()
            for blk in range(NFB):
                nn = min(128, NF - blk * 128)
                ew("pe", lambda e, pt=pt, blk=blk, nn=nn, Yb=Yb: e.transpose(out=pt[0:nn, blk, :], in_=Yb[:, blk * 128: blk * 128 + nn], identity=K.ident_b[:]),
                   [Yb, K.ident_b], [pt])
            nfull = NF // 128
            if nfull > 0:
                ew("act", lambda e, pt=pt, g_=g_: e.activation(out=Zo[:, 0:nfull, :, 16 * g_:16 * g_ + 16],
                                                               in_=pt[:, 0:nfull, :].rearrange("p k (t c) -> p k t c", c=16), func=AF.Copy), [pt], [Zo])
            if NF % 128:
                nn = NF % 128
                ew("act", lambda e, pt=pt, g_=g_, nn=nn: e.activation(out=Zo[0:nn, nfull, :, 16 * g_:16 * g_ + 16],
                                                                      in_=pt[0:nn, nfull, :].rearrange("p (t c) -> p t c", c=16), func=AF.Copy), [pt], [Zo])
        for blk in range(NFB):
            nn = min(128, NF - blk * 128)
            P.dma("sp" if blk % 2 == 0 else "act",
                  gy_tok[blk * 1024: blk * 1024 + nn * 8, f0:f0 + 128].rearrange("(n s) f -> n s f", s=8), Zo[0:nn, blk], in_t=Zo)
        P.emit()
    allt = ([Pw, PWB, Bn, Bb, Dp, vt, vu, Ro, TS, IT, U, Hs, Hr, A8l, ABl, PWl, s3, Zo, A8s, Pcs, Pcur, sq, PWls] + cz + Zr.tiles + Zpr.tiles + s1
            + Ysb.tiles + gl_ + Ybr.tiles + ptb.tiles + psS.tiles + psY.tiles + psT.tiles)
    P.end_phase(allt)
    P.emit()
    P.release(m)


W_SHAPES = {
    "ada_w": [4, D, 6 * D], "ada_b": [4, 6 * D], "norm1": [4, D], "norm2": [4, D], "norm_f": [1, D],
    "s5_lam_re": [2, 2, G, PST], "s5_lam_im": [2, 2, G, PST], "s5_log_dt": [2, 2, G],
    "s5_b_re": [2, 2, G, PST, 16], "s5_b_im": [2, 2, G, PST, 16], "s5_c_re": [2, 2, G, 16, PST], "s5_c_im": [2, 2, G, 16, PST],
    "s5_d": [2, D], "s5_w_glu": [2, D, 2 * D], "ml_w_in": [2, D, ML_IN], "ml_b_gates": [2, 32], "ml_norm": [2, D],
    "ml_w_out": [2, D, D], "ffn_w_in": [4, D, 2 * FH], "ffn_w_out": [4, FH, D],
}


def build_program(S_LAT, layers=(0, 1, 2, 3), debug_x=False, depth_total=4):
    nc = bass.Bass("TRN2", target_bir_lowering=False)
    io = {}
    io["x"] = nc.dram_tensor("x", [S_LAT, D], F32, kind="ExternalInput").ap()
    io["ctx"] = nc.dram_tensor("ctx", [S_CTX, D], F32, kind="ExternalInput").ap()
    io["c"] = nc.dram_tensor("c", [1, D], F32, kind="ExternalInput").ap()
    io["c_ctx"] = nc.dram_tensor("c_ctx", [1, D], F32, kind="ExternalInput").ap()
    for k, shp in W_SHAPES.items():
        io[k] = nc.dram_tensor(k, shp, F32, kind="ExternalInput").ap()
    out = nc.dram_tensor("out", [S_LAT, D], F32, kind="ExternalOutput").ap()
    NTOK = S_CTX + S_LAT

    def scratch(name, shape, dt):
        return nc.dram_tensor(name, shape, dt, kind="Internal").ap()

    P = Prog(nc)
    K = Ctx()
    make_consts(P, K)
    phase_cond(P, K, io)
    xs = [scratch(f"xs{i}", [NTOK, D], F32) if not (debug_x and i == len(layers)) else
          nc.dram_tensor("xdbg", [NTOK, D], F32, kind="ExternalOutput").ap() for i in range(len(layers) + 1)]
    m0 = P.mark()
    cp = Ring([P.sbuf(f"cp{i}", [128, D], F32) for i in range(3)])
    for t in range(NTOK // 128):
        tl = cp.next()
        src = io["ctx"][t * 128:(t + 1) * 128, :] if t < S_CTX // 128 else io["x"][t * 128 - S_CTX:(t + 1) * 128 - S_CTX, :]
        P.dma("sp", tl[:], src, out_t=tl)
        P.dma("act", xs[0][t * 128:(t + 1) * 128, :], tl[:], in_t=tl)
    P.end_phase(cp.tiles)
    P.emit()
    P.release(m0)
    mods = [scratch(f"mod{i}", [2, 6 * D], F32) for i in layers]
    u_tok = scratch("u_tok", [2 * S_CTX + S_LAT, D], BF16)
    mix_tok = scratch("mix_tok", [NTOK, D], BF16)
    wgu_t = scratch("wgu_t", [NHC, 128, KD, 256], BF16)
    wo_t = scratch("wo_t", [NHC, 128, 4, 512], BF16)
    wglu_t = scratch("wglu_t", [16, 128, KD, 256], BF16)
    NT3_ = 2 * S_CTX + S_LAT
    wq_t = scratch("wq_t", [ML_H, 128, KD, 128], BF16)
    wk_t = scratch("wk_t", [ML_H, 128, KD, 128], BF16)
    wtok_t = scratch("wtok_t", [8, 128, KD, 512], BF16)
    wgate_t = scratch("wgate_t", [4, 128, KD, 8], BF16)
    wout_t = scratch("wout_t", [8, 128, KD, 256], BF16)
    qT_d = scratch("qT_d", [1024, NT3_], BF16)
    kT_d = scratch("kT_d", [1024, NT3_], BF16)
    v_d = scratch("v_d", [NT3_, D], BF16)
    o_d = scratch("o_d", [NT3_, D], BF16)
    gT_d = scratch("gT_d", [4, 8, NT3_], F32)
    hf_d = scratch("hf_d", [NTOK, D], F32)
    for li, layer in enumerate(layers):
        last = layer == depth_total - 1
        jl = layer // 2
        phase_ada(P, K, io, layer, mods[li])
        wi = io["ffn_w_in"][layer].rearrange("(k p) n -> p k n", p=128)
        jobs = []
        for c in range(NHC):
            jobs.append(([(lambda t: t[:, :, 0:128], wi[:, :, c * 128:(c + 1) * 128]),
                          (lambda t: t[:, :, 128:256], wi[:, :, FH + c * 128: FH + (c + 1) * 128])], wgu_t[c]))
        phase_precast(P, K, jobs, [128, KD, 256], "a")
        wo_src = io["ffn_w_out"][layer].rearrange("(w cc p) (nt n) -> nt w p cc n", cc=4, p=128, n=512)
        jobs = [([(lambda t: t[:], wo_src[nt, w])], wo_t[nt * 11 + w]) for nt in range(4) for w in range(11)]
        phase_precast(P, K, jobs, [128, 4, 512], "b")
        if layer % 2 == 0:
            wsrc = io["s5_w_glu"][jl].rearrange("(k p) (n c) -> n p k c", p=128, c=256)
            jobs = [([(lambda t: t[:], wsrc[n])], wglu_t[n]) for n in range(16)]
            phase_precast(P, K, jobs, [128, KD, 256], "c")
            phase_b_s5(P, K, io, layer, mods[li], xs[li], u_tok, S_LAT)
            phase_c_s5(P, K, io, jl, u_tok, mix_tok, S_LAT)
            phase_d(P, K, io, layer, mods[li], xs[li], xs[li + 1], mix_tok, [wglu_t[n] for n in range(16)],
                    [wgu_t[c] for c in range(NHC)], [wo_t[c] for c in range(NHC)], S_LAT, "s5", last, out)
        else:
            NT3 = 2 * S_CTX + S_LAT
            win = io["ml_w_in"][jl].rearrange("(k p) n -> p k n", p=128)
            jobs = [([(lambda t: t[:], win[:, :, h * 128:(h + 1) * 128])], wq_t[h]) for h in range(ML_H)]
            jobs += [([(lambda t: t[:], win[:, :, 1024 + h * 128:1024 + (h + 1) * 128])], wk_t[h]) for h in range(ML_H)]
            phase_precast(P, K, jobs, [128, KD, 128], "d")
            jobs = [([(lambda t: t[:], win[:, :, 2048 + j * 512:2048 + (j + 1) * 512])], wtok_t[j]) for j in range(8)]
            phase_precast(P, K, jobs, [128, KD, 512], "e")
            jobs = [([(lambda t: t[:], win[:, :, 6144 + ty * 8:6144 + (ty + 1) * 8])], wgate_t[ty]) for ty in range(4)]
            phase_precast(P, K, jobs, [128, KD, 8], "f")
            wsrc = io["ml_w_out"][jl].rearrange("(k p) (n c) -> n p k c", p=128, c=256)
            jobs = [([(lambda t: t[:], wsrc[n])], wout_t[n]) for n in range(8)]
            phase_precast(P, K, jobs, [128, KD, 256], "g")
            phase_b_ml(P, K, io, layer, mods[li], xs[li], S_LAT, [wq_t[h] for h in range(ML_H)], [wk_t[h] for h in range(ML_H)],
                       [wtok_t[j] for j in range(8)], [wgate_t[ty] for ty in range(4)], qT_d, kT_d, v_d, o_d, gT_d)
            phase_e_ml(P, K, io, jl, S_LAT, qT_d, kT_d, v_d, o_d, gT_d, hf_d, mix_tok)
            phase_d(P, K, io, layer, mods[li], xs[li], xs[li + 1], mix_tok, [wout_t[n] for n in range(8)],
                    [wgu_t[c] for c in range(NHC)], [wo_t[c] for c in range(NHC)], S_LAT, "ml", last, out, tok_rows=True)
    P.barrier_all()
    P.emit()
    print("instructions:", P.n_instr)
    return nc


def phase_b_ml(P, K, io, layer, mod_d, xs, S_LAT, wq_t, wk_t, wtok_t, wgate_t, qT_d, kT_d, v_d, o_d, gT_d):
    m = P.mark()
    ROWS = S_LAT // 64
    S = S_CTX + S_LAT
    xr = Ring([P.sbuf(f"e_x{i}", [128, D], F32) for i in range(2)])
    bfr = Ring([P.sbuf(f"e_bf{i}", [128, D], BF16) for i in range(2)])
    t1 = P.sbuf("e_t1", [128, D], F32)
    uT = P.sbuf("e_uT", [128, KD, 512], BF16)
    wqr = Ring([P.sbuf(f"e_wq{i}", [128, KD, 128], BF16) for i in range(3)])
    wtr = Ring([P.sbuf(f"e_wt{i}", [128, KD, 512], BF16) for i in range(2)])
    wg = P.sbuf("e_wg", [128, 4, KD, 8], BF16)
    obr = Ring([P.sbuf(f"e_ob{i}", [128, 512], BF16) for i in range(4)])
    ogr = Ring([P.sbuf(f"e_og{i}", [8, 512], F32) for i in range(2)])
    statr = Ring([P.sbuf(f"e_st{i}", [128, 4], F32) for i in range(4)])
    ptr = Ring([P.psum(f"e_pt{i}", [128, 8, 128], BF16) for i in range(2)])
    mmr = Ring([P.psum(f"e_mm{i}", [128, 512], F32) for i in range(5)])
    pg = P.psum("e_pg", [8, 512], F32)
    scr = dict(pt=ptr)
    tiles = xr.tiles + bfr.tiles + [t1, uT, wg, pg] + wqr.tiles + wtr.tiles + obr.tiles + ogr.tiles + statr.tiles + ptr.tiles + mmr.tiles
    for ty in range(4):
        P.dma("sp", wg[:, ty], wgate_t[ty], out_t=wg)
    qi = [0]

    def q():
        qi[0] += 1
        return "sp" if qi[0] % 2 == 0 else "act"

    xs_lat = xs[S_CTX:S_CTX + S_LAT, :]
    for row in (1, 0):
        mm_mark = P.mark()
        md = load_mod_tiles(P, K, io, layer, mod_d, row, [("sh", 0, "raw"), ("A", 1, "A")], None, "norm1")
        ntok = S_CTX if row == 1 else S_LAT
        for t0 in range(0, ntok, 512):
            nsub = min(4, (ntok - t0) // 128)
            TT = nsub * 128
            dsts = [t0, S + t0] if row == 1 else [S_CTX + t0]
            for s in range(nsub):
                xt = xr.next()
                if row == 1:
                    P.dma(q(), xt[:], xs[t0 + s * 128: t0 + (s + 1) * 128, :], out_t=xt)
                else:
                    lr = lat_rows(xs_lat, t0 + s * 128, S_LAT)
                    for wi in range(128 // ROWS):
                        P.dma(q(), xt[wi * ROWS:(wi + 1) * ROWS, :], lr[wi], out_t=xt)
                ssq = statr.next()
                junk = bfr.next()
                P.op("act", lambda e, xt=xt, ssq=ssq, junk=junk: e.activation(out=junk[:], in_=xt[:], func=AF.Square, accum_out=ssq[:, 0:1]), reads=[xt], writes=[junk, ssq])
                P.op("act", lambda e, ssq=ssq: e.activation(out=ssq[:, 1:2], in_=ssq[:, 0:1], func=AF.Sqrt, bias=K.eps_t[:, 0:1], scale=1.0 / D), reads=[ssq, K.eps_t], writes=[ssq])
                P.op("dve", lambda e, ssq=ssq: e.reciprocal(out=ssq[:, 2:3], in_=ssq[:, 1:2]), reads=[ssq], writes=[ssq])
                P.op("dve", lambda e, xt=xt, ssq=ssq: e.scalar_tensor_tensor(out=t1[:], in0=xt[:], scalar=ssq[:, 2:3], in1=md["A"][:], op0=ALU.mult, op1=ALU.mult),
                     reads=[xt, ssq, md["A"]], writes=[t1])
                ub = bfr.next()
                P.op("pool", lambda e, ub=ub: e.tensor_tensor(out=ub[:], in0=t1[:], in1=md["sh"][:], op=ALU.add), reads=[t1, md["sh"]], writes=[ub])
                transpose_to(P, K, ub, uT, s * 128, scr)
            for (wt_, dd) in ((wq_t, qT_d), (wk_t, kT_d)):
                for h in range(ML_H):
                    wq = wqr.next()
                    P.dma(q(), wq[:], wt_[h], out_t=wq)
                    ps = mmr.next()
                    for k in range(KD):
                        P.op("pe", lambda e, ps=ps, wq=wq, k=k: e.matmul(ps[:, :TT], lhsT=wq[:, k, :], rhs=uT[:, k, :TT], start=(k == 0), stop=(k == KD - 1)),
                             reads=[wq, uT], writes=[ps])
                    ob = obr.next()
                    P.op("act", lambda e, ob=ob, ps=ps: e.activation(out=ob[:, :TT], in_=ps[:, :TT], func=AF.Copy), reads=[ps], writes=[ob])
                    for p0 in dsts:
                        P.dma(q(), dd[h * 128:(h + 1) * 128, p0:p0 + TT], ob[:, :TT], in_t=ob)
            for j in range(8):
                wt = wtr.next()
                P.dma(q(), wt[:], wtok_t[j], out_t=wt)
                dd, c0 = (v_d, j * 512) if j < 4 else (o_d, (j - 4) * 512)
                for s in range(nsub):
                    ps = mmr.next()
                    for k in range(KD):
                        P.op("pe", lambda e, ps=ps, wt=wt, k=k, s=s: e.matmul(ps[:], lhsT=uT[:, k, s * 128:(s + 1) * 128], rhs=wt[:, k, :], start=(k == 0), stop=(k == KD - 1)),
                             reads=[wt, uT], writes=[ps])
                    ob = obr.next()
                    if (j + s) % 2 == 0:
                        P.op("act", lambda e, ob=ob, ps=ps: e.activation(out=ob[:], in_=ps[:], func=AF.Copy), reads=[ps], writes=[ob])
                    else:
                        P.op("dve", lambda e, ob=ob, ps=ps: e.tensor_copy(out=ob[:], in_=ps[:]), reads=[ps], writes=[ob])
                    for p0 in dsts:
                        P.dma(q(), dd[p0 + s * 128: p0 + (s + 1) * 128, c0:c0 + 512], ob[:], in_t=ob)
            for ty in range(4):
                for k in range(KD):
                    P.op("pe", lambda e, k=k, ty=ty: e.matmul(pg[:, :TT], lhsT=wg[:, ty, k, :], rhs=uT[:, k, :TT], start=(k == 0), stop=(k == KD - 1)),
                         reads=[wg, uT], writes=[pg])
                og = ogr.next()
                P.op("dve", lambda e, og=og: e.tensor_copy(out=og[:, :TT], in_=pg[:, :TT]), reads=[pg], writes=[og])
                for p0 in dsts:
                    P.dma(q(), gT_d[ty, :, p0:p0 + TT], og[:, :TT], in_t=og)
        P.end_phase(list(md.values()))
        P.emit()
        P.release(mm_mark)
    P.end_phase(tiles)
    P.emit()
    P.release(m)


def phase_e_ml(P, K, io, jl, S_LAT, qT_d, kT_d, v_d, o_d, gT_d, hf_d, mix_tok):
    S = S_CTX + S_LAT
    NTOK3 = S + S_CTX
    NC = S // 64
    NC3 = NTOK3 // 64
    CC = S_CTX // 64
    SCALE = float(128 ** -0.5)
    m = P.mark()
    omT = P.sbuf("f_omT", [64, NC, 40], F32)
    clT = P.sbuf("f_clT", [64, NC, 40], F32)
    lamR = P.sbuf("f_lamR", [128, 16, NC], F32)
    lamSR = P.sbuf("f_lamSR", [128, 16, NC], F32)
    m1 = P.mark()
    X = [P.sbuf(f"f_X{i}", [40, S], F32) for i in range(5)]
    Mr = P.sbuf("f_Mr", [40, NC], F32)
    lam = P.sbuf("f_lam", [40, NC], F32)
    bia = P.sbuf("f_bias", [40, 4], F32)
    one = P.sbuf("f_one", [40, 2], F32)
    sel = P.sbuf("f_sel", [40, 16, 128], F32)
    pto = Ring([P.psum(f"f_pto{i}", [64, 12, 40], F32) for i in range(2)])
    pl = P.psum("f_pl", [128, NC], F32)
    ew = lambda eng, fn, r, w: P.op(eng, fn, reads=r, writes=w)
    for t in X:
        ew("pool", lambda e, t=t: e.memset(t[:], 0.0), [], [t])
    ew("pool", lambda e: e.memset(bia[:], 0.0), [], [bia])
    ew("pool", lambda e: e.memset(one[:, 0:1], 1.0), [], [one])
    ew("pool", lambda e: e.memset(one[:, 1:2], 0.0), [], [one])
    bg = io["ml_b_gates"][jl:jl + 1, :]
    for (col, lo, p0) in ((0, 0, 0), (1, 8, 0), (0, 16, 32), (1, 24, 32)):
        P.dma("sp", bia[p0:p0 + 8, col:col + 1], bg[:, lo:lo + 8].rearrange("o h -> h o"), out_t=bia, allow_slow_non_contiguous=True)
    P.dma("sp", X[3][0:8, :], gT_d[0, :, 0:S], out_t=X[3])
    P.dma("act", X[3][32:40, :], gT_d[2, :, S_CTX:NTOK3], out_t=X[3])
    P.dma("sp", X[4][0:8, :], gT_d[1, :, 0:S], out_t=X[4])
    P.dma("act", X[4][32:40, :], gT_d[3, :, S_CTX:NTOK3], out_t=X[4])
    ew("dve", lambda e: e.tensor_scalar(out=bia[:, 2:4], in0=bia[:, 0:2], scalar1=1.0 / GATE_CAP, scalar2=None, op0=ALU.mult), [bia], [bia])
    for (src, dst) in ((X[3], X[0]), (X[4], X[1])):
        ew("dve", lambda e, src=src, dst=dst: e.tensor_copy(out=dst[0:8, :], in_=src[0:8, :]), [src], [dst])
        ew("pool", lambda e, src=src, dst=dst: e.tensor_copy(out=dst[32:40, :], in_=src[32:40, ::-1]), [src], [dst])
    for (t, c) in ((X[0], 2), (X[1], 3)):
        ew("act", lambda e, t=t, c=c: e.activation(out=t[:], in_=t[:], func=AF.Tanh, bias=bia[:, c:c + 1], scale=1.0 / GATE_CAP), [t, bia], [t])
        ew("dve", lambda e, t=t: e.tensor_scalar(out=t[:], in0=t[:], scalar1=GATE_CAP, scalar2=None, op0=ALU.mult), [t], [t])
    ew("act", lambda e: e.activation(out=X[3][:], in_=X[1][:], func=AF.Exp, scale=-1.0), [X[1]], [X[3]])
    ew("dve", lambda e: e.tensor_scalar(out=X[4][:], in0=X[3][:], scalar1=2.0, scalar2=None, op0=ALU.add), [X[3]], [X[4]])
    ew("dve", lambda e: e.reciprocal(out=X[4][:], in_=X[4][:]), [X[4]], [X[4]])
    ew("dve", lambda e: e.tensor_tensor(out=X[4][:], in0=X[4][:], in1=X[3][:], op=ALU.mult), [X[4], X[3]], [X[4]])
    ew("pool", lambda e: e.tensor_tensor(out=X[2][:], in0=X[4][:], in1=X[4][:], op=ALU.mult), [X[4]], [X[2]])
    ew("dve", lambda e: e.tensor_scalar(out=X[3][:], in0=X[2][:], scalar1=1.0 / 15, scalar2=1.0 / 13, op0=ALU.mult, op1=ALU.add), [X[2]], [X[3]])
    for cst in (1.0 / 11, 1.0 / 9, 1.0 / 7, 1.0 / 5, 1.0 / 3, 1.0):
        ew("dve", lambda e: e.tensor_tensor(out=X[3][:], in0=X[3][:], in1=X[2][:], op=ALU.mult), [X[3], X[2]], [X[3]])
        ew("dve", lambda e, cst=cst: e.tensor_scalar(out=X[3][:], in0=X[3][:], scalar1=float(cst), scalar2=None, op0=ALU.add), [X[3]], [X[3]])
    ew("dve", lambda e: e.scalar_tensor_tensor(out=X[1][:], in0=X[4][:], scalar=-2.0, in1=X[3][:], op0=ALU.mult, op1=ALU.mult), [X[4], X[3]], [X[1]])
    ew("dve", lambda e: e.tensor_tensor_scan(out=X[2][:], data0=one[:, 0:1].broadcast_to([40, S]), data1=X[1][:], initial=0.0, op0=ALU.mult, op1=ALU.add),
       [one, X[1]], [X[2]])
    ew("dve", lambda e: e.tensor_tensor(out=X[0][:], in0=X[0][:], in1=X[2][:], op=ALU.subtract), [X[0], X[2]], [X[0]])
    ew("dve", lambda e: e.tensor_tensor_scan(out=X[3][:], data0=one[:, 1:2].broadcast_to([40, S]), data1=X[0][:], initial=0.0, op0=ALU.add, op1=ALU.max),
       [one, X[0]], [X[3]])
    ew("dve", lambda e: e.tensor_copy(out=Mr[:], in_=X[3][:, 63::64]), [X[3]], [Mr])
    mrb = bc(Mr[:], 2, 64)
    ew("dve", lambda e: e.tensor_tensor(out=X[0][:].rearrange("p (n t) -> p n t", t=64), in0=X[0][:].rearrange("p (n t) -> p n t", t=64), in1=mrb, op=ALU.subtract), [X[0], Mr], [X[0]])
    ew("act", lambda e: e.activation(out=X[0][:], in_=X[0][:], func=AF.Exp), [X[0]], [X[0]])
    ew("dve", lambda e: e.tensor_tensor(out=X[2][:].rearrange("p (n t) -> p n t", t=64), in0=X[2][:].rearrange("p (n t) -> p n t", t=64), in1=mrb, op=ALU.add), [X[2], Mr], [X[2]])
    ew("act", lambda e: e.activation(out=X[2][:], in_=X[2][:], func=AF.Exp, scale=-1.0), [X[2]], [X[2]])
    ew("dve", lambda e: e.tensor_scalar(out=lam[:, 0:1], in0=Mr[:, 0:1], scalar1=-1.0, scalar2=None, op0=ALU.mult), [Mr], [lam])
    ew("dve", lambda e: e.tensor_tensor(out=lam[:, 1:NC], in0=Mr[:, 0:NC - 1], in1=Mr[:, 1:NC], op=ALU.subtract), [Mr], [lam])
    ew("act", lambda e: e.activation(out=lam[:], in_=lam[:], func=AF.Exp), [lam], [lam])
    for (src, dst) in ((X[0], X[3]), (X[2], X[4])):
        ew("dve", lambda e, src=src, dst=dst: e.tensor_copy(out=dst[0:8, :], in_=src[0:8, :]), [src], [dst])
        ew("pool", lambda e, src=src, dst=dst: e.tensor_copy(out=dst[32:40, :], in_=src[32:40, ::-1]), [src], [dst])
    for (src, dstT) in ((X[3], omT), (X[4], clT)):
        for k0 in range(0, NC, 12):
            nk = min(12, NC - k0)
            pt = pto.next()
            for kk in range(nk):
                k = k0 + kk
                ew("pe", lambda e, pt=pt, kk=kk, k=k, src=src: e.transpose(out=pt[:, kk, :], in_=src[:, 64 * k:64 * k + 64], identity=K.ident_f[0:40, 0:40]),
                   [src, K.ident_f], [pt])
            ew("act", lambda e, pt=pt, k0=k0, nk=nk, dstT=dstT: e.activation(out=dstT[:, k0:k0 + nk, :], in_=pt[:, 0:nk, :], func=AF.Copy), [pt], [dstT])
    for r in range(16):
        row = (r // 8) * 32 + (r % 8)
        ew("dve", lambda e, r=r, row=row: e.tensor_copy(out=sel[:, r, :], in_=K.ident_f[0:40, row:row + 1].broadcast_to([40, 128])), [K.ident_f], [sel])
    for r in range(16):
        ew("pe", lambda e, r=r: e.matmul(pl[:], lhsT=sel[:, r, :], rhs=lam[:], start=True, stop=True), [sel, lam], [pl])
        ew("act", lambda e, r=r: e.activation(out=lamR[:, r, :], in_=pl[:], func=AF.Copy), [pl], [lamR])
    ew("dve", lambda e: e.tensor_scalar(out=lamSR[:], in0=lamR[:], scalar1=SCALE, scalar2=None, op0=ALU.mult), [lamR], [lamSR])
    P.end_phase(X + [Mr, lam, bia, one, sel, pl] + pto.tiles)
    P.emit()
    P.release(m1)

    qT = P.sbuf("f_qT", [128, NTOK3], BF16)
    kT = P.sbuf("f_kT", [128, NTOK3], BF16)
    vv = P.sbuf("f_vv", [64, NC3, 257], BF16)
    Cf = P.sbuf("f_Cf", [128, 257], F32)
    Csr = Ring([P.sbuf(f"f_Cs{i}", [128, 257], BF16) for i in range(2)])
    Spr = Ring([P.sbuf(f"f_Sp{i}", [64, 64], BF16) for i in range(3)])
    kwr = Ring([P.sbuf(f"f_kw{i}", [64, 128], BF16) for i in range(3)])
    mask = [P.sbuf(f"f_mask{i}", [64, 64], F32) for i in range(2)]
    dnr = Ring([P.sbuf(f"f_dn{i}", [64, 6], F32) for i in range(4)])
    hfr = Ring([P.sbuf(f"f_hf{i}", [64, 256], F32) for i in range(3)])
    hsr = Ring([P.sbuf(f"f_hs{i}", [64, 256], F32) for i in range(3)])
    oor = Ring([P.sbuf(f"f_oo{i}", [64, 256], BF16) for i in range(3)])
    sgr = Ring([P.sbuf(f"f_sg{i}", [64, 256], F32) for i in range(2)])
    hor = Ring([P.sbuf(f"f_ho{i}", [64, 256], BF16) for i in range(3)])
    junk = P.sbuf("f_junk", [64, 256], BF16)
    nw = P.sbuf("f_nw", [64, D], F32)
    eps64 = P.sbuf("f_eps", [64, 1], F32)
    pS = Ring([P.psum(f"f_pS{i}", [64, 64], F32) for i in range(2)])
    pK = Ring([P.psum(f"f_pK{i}", [64, 128], BF16) for i in range(2)])
    pH = Ring([P.psum(f"f_pH{i}", [64, 257], F32) for i in range(2)])
    pC = Ring([P.psum(f"f_pC{i}", [128, 257], F32) for i in range(2)])
    tiles = ([qT, kT, vv, Cf, junk, nw, eps64, omT, clT, lamR, lamSR] + Csr.tiles + Spr.tiles + kwr.tiles + mask + dnr.tiles + hfr.tiles + hsr.tiles + oor.tiles
             + sgr.tiles + hor.tiles + pS.tiles + pK.tiles + pH.tiles + pC.tiles)
    ew("pool", lambda e: e.memset(eps64[:], EPS), [], [eps64])
    for i, (cm, pat) in enumerate(((-1, 1), (1, -1))):
        ew("pool", lambda e, i=i: e.memset(mask[i][:], SCALE), [], [mask[i]])
        ew("pool", lambda e, i=i, cm=cm, pat=pat: e.affine_select(out=mask[i][:], in_=mask[i][:], compare_op=ALU.is_ge, fill=0.0, base=0,
                                                                  pattern=[[pat, 64]], channel_multiplier=cm), [mask[i]], [mask[i]])
    ew("pool", lambda e: e.memset(vv[:, :, 256:257], 1.0), [], [vv])
    load_rep_n = lambda: P.dma("sp", nw[:], io["ml_norm"][jl:jl + 1, :].partition_broadcast(64), out_t=nw)
    load_rep_n()

    for h in range(ML_H):
        P.dma("sp", qT[:], qT_d[h * 128:(h + 1) * 128, :], out_t=qT)
        P.dma("act", kT[:], kT_d[h * 128:(h + 1) * 128, :], out_t=kT)
        half = NC3 // 2
        P.dma("sp", vv[:, 0:half, 0:256], v_d[0:half * 64, h * 256:(h + 1) * 256].rearrange("(n t) e -> t n e", t=64), out_t=vv)
        P.dma("act", vv[:, half:NC3, 0:256], v_d[half * 64:NTOK3, h * 256:(h + 1) * 256].rearrange("(n t) e -> t n e", t=64), out_t=vv)
        for d in range(2):
            r = d * 8 + h
            ew("dve", lambda e: e.memset(Cf[:], 0.0), [], [Cf])
            Cs = Csr.next()
            ew("pool", lambda e, Cs=Cs: e.memset(Cs[:], 0.0), [], [Cs])

            def front(mi):
                c = mi if d == 0 else NC3 - 1 - mi
                k = c if d == 0 else c - CC
                cols = slice(64 * c, 64 * c + 64)
                ps = pS.next()
                ew("pe", lambda e, ps=ps, cols=cols: e.matmul(ps[:], lhsT=kT[:, cols], rhs=qT[:, cols], start=True, stop=True), [kT, qT], [ps])
                pk = pK.next()
                ew("pe", lambda e, pk=pk, cols=cols: e.transpose(out=pk[:], in_=kT[:, cols], identity=K.ident_b[:]), [kT, K.ident_b], [pk])
                Sp = Spr.next()
                om = omT[:, k, 32 * d + h: 32 * d + h + 1]
                ew("dve", lambda e, Sp=Sp, ps=ps, om=om: e.scalar_tensor_tensor(out=Sp[:], in0=ps[:], scalar=om, in1=mask[d][:], op0=ALU.mult, op1=ALU.mult),
                   [ps, omT, mask[d]], [Sp])
                kw = kwr.next()
                ew("act", lambda e, kw=kw, pk=pk, om=om: e.activation(out=kw[:], in_=pk[:], func=AF.Copy, scale=om), [pk, omT], [kw])
                return (c, k, cols, Sp, kw)

            nxt = front(0)
            for mi in range(NC):
                c, k, cols, Sp, kw = nxt
                if mi + 1 < NC:
                    nxt = front(mi + 1)
                tc = c if c < NC else c - NC
                ph = pH.next()
                ew("pe", lambda e, ph=ph, Sp=Sp, c=c: e.matmul(ph[:], lhsT=Sp[:], rhs=vv[:, c, :], start=True, stop=False), [Sp, vv], [ph])
                ew("pe", lambda e, ph=ph, Cs=Cs, cols=cols: e.matmul(ph[:], lhsT=qT[:, cols], rhs=Cs[:], start=False, stop=True), [qT, Cs], [ph])
                pc = pC.next()
                ew("pe", lambda e, pc=pc, kw=kw, c=c: e.matmul(pc[:], lhsT=kw[:], rhs=vv[:, c, :], start=True, stop=True), [kw, vv], [pc])
                ew("dve", lambda e, pc=pc, mi=mi, r=r: e.scalar_tensor_tensor(out=Cf[:], in0=Cf[:], scalar=lamR[:, r, mi:mi + 1], in1=pc[:], op0=ALU.mult, op1=ALU.add),
                   [Cf, lamR, pc], [Cf])
                if mi + 1 < NC:
                    Cs = Csr.next()
                    ew("act", lambda e, Cs=Cs, mi=mi, r=r: e.activation(out=Cs[:], in_=Cf[:], func=AF.Copy, scale=lamSR[:, r, mi + 1:mi + 2]), [Cf, lamSR], [Cs])
                dn = dnr.next()
                ew("act", lambda e, dn=dn, ph=ph: e.activation(out=dn[:, 4:5], in_=ph[:, 256:257], func=AF.Copy), [ph], [dn])
                ew("dve", lambda e, dn=dn: e.scalar_tensor_tensor(out=dn[:, 0:1], in0=dn[:, 4:5], scalar=-1.0, in1=dn[:, 4:5], op0=ALU.mult, op1=ALU.max),
                   [dn], [dn])
                ew("dve", lambda e, dn=dn, k=k: e.tensor_tensor(out=dn[:, 1:2], in0=dn[:, 0:1], in1=clT[:, k, 32 * d + h: 32 * d + h + 1], op=ALU.max), [dn, clT], [dn])
                ew("dve", lambda e, dn=dn: e.reciprocal(out=dn[:, 2:3], in_=dn[:, 1:2]), [dn], [dn])
                rows = slice(64 * tc, 64 * tc + 64)
                hcols = slice(h * 256, (h + 1) * 256)
                if d == 0:
                    hf = hfr.next()
                    ew("act", lambda e, hf=hf, ph=ph, dn=dn: e.activation(out=hf[:], in_=ph[:, 0:256], func=AF.Copy, scale=dn[:, 2:3]), [ph, dn], [hf])
                    P.dma("sp", hf_d[rows, hcols], hf[:], in_t=hf)
                else:
                    hf = hfr.next()
                    P.dma("sp", hf[:], hf_d[rows, hcols], out_t=hf)
                    oo = oor.next()
                    P.dma("act", oo[:], o_d[rows, hcols], out_t=oo)
                    hs = hsr.next()
                    ew("dve", lambda e, hs=hs, ph=ph, dn=dn, hf=hf: e.scalar_tensor_tensor(out=hs[:], in0=ph[:, 0:256], scalar=dn[:, 2:3], in1=hf[:], op0=ALU.mult, op1=ALU.add),
                       [ph, dn, hf], [hs])
                    ew("act", lambda e, hs=hs, dn=dn: e.activation(out=junk[:], in_=hs[:], func=AF.Square, accum_out=dn[:, 3:4]), [hs], [junk, dn])
                    ew("act", lambda e, dn=dn: e.activation(out=dn[:, 3:4], in_=dn[:, 3:4], func=AF.Sqrt, bias=eps64[:, 0:1], scale=1.0 / 256), [dn, eps64], [dn])
                    ew("dve", lambda e, dn=dn: e.reciprocal(out=dn[:, 3:4], in_=dn[:, 3:4]), [dn], [dn])
                    sg = sgr.next()
                    ew("act", lambda e, sg=sg, oo=oo: e.activation(out=sg[:], in_=oo[:], func=AF.Sigmoid), [oo], [sg])
                    ew("dve", lambda e, hs=hs, dn=dn, hcols=hcols: e.scalar_tensor_tensor(out=hs[:], in0=hs[:], scalar=dn[:, 3:4], in1=nw[:, hcols], op0=ALU.mult, op1=ALU.mult),
                       [hs, dn, nw], [hs])
                    ho = hor.next()
                    ew("pool", lambda e, ho=ho, hs=hs, sg=sg: e.tensor_tensor(out=ho[:], in0=hs[:], in1=sg[:], op=ALU.mult), [hs, sg], [ho])
                    P.dma("act", mix_tok[rows, hcols], ho[:], in_t=ho)
            if d == 0:
                P.barrier_all()
            P.emit()
    P.end_phase(tiles)
    P.emit()
    P.release(m)


_NC_CACHE = {}


def kernel(**inputs):
    S_LAT = inputs["x"].shape[1]
    B = inputs["x"].shape[0]
    if S_LAT not in _NC_CACHE:
        _NC_CACHE[S_LAT] = build_program(S_LAT)
    nc = _NC_CACHE[S_LAT]
    shared = {}
    for k in W_SHAPES:
        a = np.ascontiguousarray(np.asarray(inputs[k], dtype=np.float32))
        if k == "norm_f":
            a = a.reshape(1, D)
        shared[k] = a
    shared["c_ctx"] = np.ascontiguousarray(np.asarray(inputs["c_ctx"], dtype=np.float32)).reshape(1, D)
    n_cores = 8
    in_maps = []
    for core in range(n_cores):
        b = core % B
        mp = dict(shared)
        mp["x"] = np.ascontiguousarray(np.asarray(inputs["x"][b], dtype=np.float32))
        mp["ctx"] = np.ascontiguousarray(np.asarray(inputs["ctx"][b], dtype=np.float32))
        mp["c"] = np.ascontiguousarray(np.asarray(inputs["c"][b:b + 1], dtype=np.float32))
        in_maps.append(mp)
    res = run_bass_kernel_spmd(nc, in_maps, core_ids=list(range(n_cores)))
    out = np.stack([np.asarray(res.results[b]["out"]) for b in range(B)], axis=0)
    return out.astype(np.float32)
```
